# Optimizing a Trainium2 kernel written in Bass

```python
import math
import jax, jax.numpy as jnp
from jax import lax
import numpy as np

D_MODEL = 1024
BATCH = 16
SEQ = 2048
DEPTH = 1

HEAD_DIM = 64
ROPE_THETA = 500000.0
ROPE_FRACTION = 4
A_HEADS = 8
A_KV_HEADS = 2
A_REP = A_HEADS // A_KV_HEADS
A_WIDTH = A_HEADS * HEAD_DIM
A_KV_WIDTH = A_KV_HEADS * HEAD_DIM
IDX_HEADS = 8
IDX_DIM = 32
DSA_TOPK_MAX = 256
A_QBLOCK = 64
B_HEADS = 8
B_KV_HEADS = 2
B_REP = B_HEADS // B_KV_HEADS
B_WIDTH = B_HEADS * HEAD_DIM
B_KV_WIDTH = B_KV_HEADS * HEAD_DIM
CMP_BLOCK = 32
CMP_STRIDE = 16
CMP_HIDDEN = 256
SEL_BLOCK = 64
SEL_COUNT = 16
SEL_LOCAL = 2
WINDOW = 512
SEL_QBLOCK = 16
WIN_QBLOCK = 128
N_EXPERTS = 256
TOP_K = 8
N_GROUPS = 8
TOPK_GROUPS = 4
EXPERT_DIM = 256
SHARED_DIM = 256
ROUTED_SCALE = 2.5
MOE_BLOCK = 128
DN_ALPHA = (2 * DEPTH) ** 0.25
DN_BETA = (8 * DEPTH) ** -0.25
LN_EPS = 1e-5
NEG = -1e30
FORCE = 1e9

IN_WIDTHS = (A_WIDTH, A_KV_WIDTH, A_KV_WIDTH, IDX_HEADS * IDX_DIM, IDX_DIM, IDX_HEADS,
             B_WIDTH, B_KV_WIDTH, B_KV_WIDTH, B_KV_WIDTH, B_KV_WIDTH, B_KV_WIDTH, B_KV_WIDTH, 3 * B_HEADS,
             D_MODEL, D_MODEL)
VALUE_SLOTS = (2, 8, 10, 12)
N_IN = sum(IN_WIDTHS)

kernel_name = 'hybrid_dsa_nsa_moe_block'


def _layer_norm(x, g, b):
    xf = x.astype(jnp.float32)
    mu = xf.mean(-1, keepdims=True)
    var = jnp.square(xf - mu).mean(-1, keepdims=True)
    return ((xf - mu) * lax.rsqrt(var + LN_EPS) * g.astype(jnp.float32) + b.astype(jnp.float32)).astype(x.dtype)


def _masked_softmax(s, mask):
    s = jnp.where(mask, s.astype(jnp.float32), NEG)
    e = jnp.exp(s - s.max(-1, keepdims=True)) * mask
    return e / jnp.maximum(e.sum(-1, keepdims=True), 1e-30)


def _rope_partial(x, positions):
    d = x.shape[-1]
    rot = d // ROPE_FRACTION
    half = rot // 2
    inv = ROPE_THETA ** (-(jnp.arange(half, dtype=jnp.float32) * 2.0) / rot)
    ang = positions.astype(jnp.float32)[..., None] * inv
    ang = ang.reshape(ang.shape[:2] + (1,) * (x.ndim - 3) + (half,))
    cos, sin = jnp.cos(ang), jnp.sin(ang)
    x1 = x[..., :half].astype(jnp.float32)
    x2 = x[..., half:rot].astype(jnp.float32)
    return jnp.concatenate([(x1 * cos - x2 * sin).astype(x.dtype), (x2 * cos + x1 * sin).astype(x.dtype), x[..., rot:]], axis=-1)


def _blocks_to_seq(out):
    out = jnp.swapaxes(out, 0, 1)
    return out.reshape((out.shape[0], out.shape[1] * out.shape[2]) + out.shape[3:])


def _dsa_attention(q, k, v, q_idx, k_idx, w_idx, positions):
    bsz, seq, _ = q.shape
    q = _rope_partial(q.reshape(bsz, seq, A_HEADS, HEAD_DIM), positions).reshape(bsz, seq, A_KV_HEADS, A_REP, HEAD_DIM)
    k = _rope_partial(k.reshape(bsz, seq, A_KV_HEADS, HEAD_DIM), positions)
    v = v.reshape(bsz, seq, A_KV_HEADS, HEAD_DIM)
    qi = _rope_partial(q_idx.reshape(bsz, seq, IDX_HEADS, IDX_DIM), positions)
    ki = _rope_partial(k_idx, positions)
    wi = w_idx * IDX_HEADS ** -0.5
    n_keep = min(DSA_TOPK_MAX, seq // 4)
    key_pos = jnp.arange(seq)
    take = jax.vmap(lambda arr, idx: arr[idx])

    def block(i):
        start = i * A_QBLOCK
        qb = lax.dynamic_slice_in_dim(q, start, A_QBLOCK, axis=1)
        qib = lax.dynamic_slice_in_dim(qi, start, A_QBLOCK, axis=1)
        wib = lax.dynamic_slice_in_dim(wi, start, A_QBLOCK, axis=1)
        t_q = start + jnp.arange(A_QBLOCK)
        rel = jax.nn.relu(jnp.einsum('bqhd,bsd->bqhs', qib, ki) * IDX_DIM ** -0.5)
        score = jnp.einsum('bqhs,bqh->bqs', rel, wib).astype(jnp.float32)
        score = jnp.where(key_pos[None, None, :] <= t_q[None, :, None], score, -jnp.inf)
        _, idx = lax.top_k(score, n_keep)
        ks = take(k, idx)
        vs = take(v, idx)
        s = jnp.einsum('bqgrd,bqkgd->bqgrk', qb, ks) * HEAD_DIM ** -0.5
        valid = (idx <= t_q[None, :, None])[:, :, None, None, :]
        p = _masked_softmax(s, valid).astype(v.dtype)
        o = jnp.einsum('bqgrk,bqkgd->bqgrd', p, vs)
        return o.reshape(bsz, A_QBLOCK, A_WIDTH)

    return _blocks_to_seq(lax.map(block, jnp.arange(seq // A_QBLOCK)))


def _nsa_compress(a, pos_emb, w1, w2, tok):
    bsz = a.shape[0]
    blocks = a[:, tok] + pos_emb[None, None, :, None, :]
    flat = jnp.swapaxes(blocks, 2, 3).reshape(bsz, tok.shape[0], B_KV_HEADS, CMP_BLOCK * HEAD_DIM)
    return jax.nn.gelu(flat @ w1) @ w2


def _nsa_attention(q, k_cmp, v_cmp, k_sel, v_sel, k_win, v_win, g, positions,
                   cmp_pos_k, cmp_pos_v, cmp_k_w1, cmp_k_w2, cmp_v_w1, cmp_v_w2):
    bsz, seq, _ = q.shape
    dt = q.dtype
    scale = HEAD_DIM ** -0.5
    kv = lambda a: a.reshape(bsz, seq, B_KV_HEADS, HEAD_DIM)
    q = q.reshape(bsz, seq, B_HEADS, HEAD_DIM)
    q_raw = q.reshape(bsz, seq, B_KV_HEADS, B_REP, HEAD_DIM)
    q_rot = _rope_partial(q, positions).reshape(bsz, seq, B_KV_HEADS, B_REP, HEAD_DIM)
    t = np.arange(seq)

    n_c = (seq - CMP_BLOCK) // CMP_STRIDE + 1
    c_start = np.arange(n_c) * CMP_STRIDE
    tok = c_start[:, None] + np.arange(CMP_BLOCK)
    kc = _nsa_compress(kv(k_cmp), cmp_pos_k, cmp_k_w1, cmp_k_w2, tok)
    vc = _nsa_compress(kv(v_cmp), cmp_pos_v, cmp_v_w1, cmp_v_w2, tok)
    valid_c = (c_start[None, :] + CMP_BLOCK - 1) <= t[:, None]
    p_c = _masked_softmax(jnp.einsum('bsgrd,bcgd->bsgrc', q_raw, kc) * scale, valid_c[None, :, None, None, :])
    o_cmp = jnp.einsum('bsgrc,bcgd->bsgrd', p_c.astype(dt), vc)

    n_s = seq // SEL_BLOCK
    s_start = np.arange(n_s) * SEL_BLOCK
    overlap = ((c_start[:, None] <= s_start[None, :] + SEL_BLOCK - 1)
               & (c_start[:, None] + CMP_BLOCK - 1 >= s_start[None, :])).astype(np.float32)
    imp = jnp.einsum('bsgrc,cj->bsgj', p_c, overlap)
    blk = np.arange(n_s)
    cur = t // SEL_BLOCK
    forced = (blk[None, :] == 0) | ((cur[:, None] - blk[None, :] >= 0) & (cur[:, None] - blk[None, :] < SEL_LOCAL))
    causal = blk[None, :] * SEL_BLOCK <= t[:, None]
    imp = jnp.where(forced[None, :, None, :], FORCE, jnp.where(causal[None, :, None, :], imp, NEG))
    n_pick = min(SEL_COUNT, n_s)
    _, sel = lax.top_k(imp, n_pick)

    to_blocks = lambda a: a.reshape(bsz, n_s, SEL_BLOCK, B_KV_HEADS, HEAD_DIM).transpose(0, 3, 1, 2, 4)
    ksb = to_blocks(_rope_partial(kv(k_sel), positions))
    vsb = to_blocks(kv(v_sel))
    take2 = jax.vmap(jax.vmap(lambda arr, idx: arr[idx]))

    def sel_block(i):
        start = i * SEL_QBLOCK
        qb = lax.dynamic_slice_in_dim(q_rot, start, SEL_QBLOCK, axis=1)
        sb = lax.dynamic_slice_in_dim(sel, start, SEL_QBLOCK, axis=1)
        t_q = start + jnp.arange(SEL_QBLOCK)
        idx = sb.transpose(0, 2, 1, 3)
        kg = take2(ksb, idx)
        vg = take2(vsb, idx)
        s = jnp.einsum('bqgrd,bgqnld->bqgrnl', qb, kg) * scale
        tok_pos = sb[..., None] * SEL_BLOCK + jnp.arange(SEL_BLOCK)
        valid = (tok_pos <= t_q[None, :, None, None, None])[:, :, :, None]
        sh = s.shape
        p = _masked_softmax(s.reshape(sh[:4] + (-1,)), valid.reshape(valid.shape[:4] + (-1,))).reshape(sh)
        return jnp.einsum('bqgrnl,bgqnld->bqgrd', p.astype(dt), vg)

    o_sel = _blocks_to_seq(lax.map(sel_block, jnp.arange(seq // SEL_QBLOCK)))

    span = WINDOW + WIN_QBLOCK
    pad = ((0, 0), (WINDOW, 0), (0, 0), (0, 0))
    kwp = jnp.pad(_rope_partial(kv(k_win), positions), pad)
    vwp = jnp.pad(kv(v_win), pad)

    def win_block(i):
        start = i * WIN_QBLOCK
        qb = lax.dynamic_slice_in_dim(q_rot, start, WIN_QBLOCK, axis=1)
        kb = lax.dynamic_slice_in_dim(kwp, start, span, axis=1)
        vb = lax.dynamic_slice_in_dim(vwp, start, span, axis=1)
        t_q = start + jnp.arange(WIN_QBLOCK)
        s_pos = start - WINDOW + jnp.arange(span)
        diff = t_q[:, None] - s_pos[None, :]
        valid = (s_pos[None, :] >= 0) & (diff >= 0) & (diff < WINDOW)
        s = jnp.einsum('bqgrd,bkgd->bqgrk', qb, kb) * scale
        p = _masked_softmax(s, valid[None, :, None, None, :])
        return jnp.einsum('bqgrk,bkgd->bqgrd', p.astype(dt), vb)

    o_win = _blocks_to_seq(lax.map(win_block, jnp.arange(seq // WIN_QBLOCK)))

    gates = jax.nn.sigmoid(g.reshape(bsz, seq, B_KV_HEADS, B_REP, 3))
    o = gates[..., 0:1] * o_cmp + gates[..., 1:2] * o_sel + gates[..., 2:3] * o_win
    return o.reshape(bsz, seq, B_WIDTH)


def _token_mixer(u, positions, w_in, w_br_a, w_br_b, w_out,
                 cmp_pos_k, cmp_pos_v, cmp_k_w1, cmp_k_w2, cmp_v_w1, cmp_v_w2):
    z = u @ w_in
    offsets = np.cumsum(IN_WIDTHS)[:-1].tolist()
    (q_a, k_a, v_a, q_idx, k_idx, w_idx, q_b, k_cmp, v_cmp, k_sel, v_sel, k_win, v_win,
     g_nsa, gate_a, gate_b) = jnp.split(z, offsets, axis=-1)
    o_a = _dsa_attention(q_a, k_a, v_a, q_idx, k_idx, w_idx, positions)
    o_b = _nsa_attention(q_b, k_cmp, v_cmp, k_sel, v_sel, k_win, v_win, g_nsa, positions,
                         cmp_pos_k, cmp_pos_v, cmp_k_w1, cmp_k_w2, cmp_v_w1, cmp_v_w2)
    merged = jax.nn.sigmoid(gate_a) * (o_a @ w_br_a) + jax.nn.sigmoid(gate_b) * (o_b @ w_br_b)
    return merged @ w_out


def _routed_experts(h, idx, wts, w_gate, w_up, w_down):
    n_tok, _ = h.shape
    n_asg = n_tok * TOP_K
    e_flat = idx.reshape(-1)
    order = jnp.argsort(e_flat)
    e_sorted = e_flat[order]
    t_sorted = (order // TOP_K).astype(jnp.int32)
    w_sorted = wts.reshape(-1)[order].astype(h.dtype)
    counts = jnp.bincount(e_flat, length=N_EXPERTS)
    starts = jnp.cumsum(counts) - counts
    padded = (counts + MOE_BLOCK - 1) // MOE_BLOCK * MOE_BLOCK
    p_ends = jnp.cumsum(padded)
    dest = (p_ends - padded)[e_sorted] + jnp.arange(n_asg) - starts[e_sorted]
    n_blocks = -(-n_asg // MOE_BLOCK) + N_EXPERTS
    cap = n_blocks * MOE_BLOCK
    tok_buf = jnp.zeros((cap,), jnp.int32).at[dest].set(t_sorted)
    w_buf = jnp.zeros((cap,), h.dtype).at[dest].set(w_sorted)
    blk_e = jnp.minimum(jnp.searchsorted(p_ends, jnp.arange(n_blocks) * MOE_BLOCK, side='right'), N_EXPERTS - 1)

    def step(acc, xs):
        tb, wb, e = xs
        hb = h[tb]
        y = (jax.nn.silu(hb @ w_gate[e]) * (hb @ w_up[e])) @ w_down[e]
        return acc.at[tb].add(y * wb[:, None]), None

    out, _ = lax.scan(step, jnp.zeros_like(h),
                      (tok_buf.reshape(n_blocks, MOE_BLOCK), w_buf.reshape(n_blocks, MOE_BLOCK), blk_e))
    return out


def _moe_ffn(u, w_router, router_bias, w_exp_gate, w_exp_up, w_exp_down, w_sh_gate, w_sh_up, w_sh_down):
    bsz, seq, d = u.shape
    h = u.reshape(bsz * seq, d)
    n_tok = h.shape[0]
    scores = jax.nn.sigmoid((h @ w_router).astype(jnp.float32))
    choice = scores + router_bias.astype(jnp.float32)
    grp = choice.reshape(n_tok, N_GROUPS, N_EXPERTS // N_GROUPS)
    grp_score = lax.top_k(grp, 2)[0].sum(-1)
    _, top_g = lax.top_k(grp_score, TOPK_GROUPS)
    keep = jax.nn.one_hot(top_g, N_GROUPS, dtype=jnp.float32).sum(1) > 0
    masked = jnp.where(keep[:, :, None], grp, NEG).reshape(n_tok, N_EXPERTS)
    _, idx = lax.top_k(masked, TOP_K)
    w = jnp.take_along_axis(scores, idx, axis=-1)
    w = w / w.sum(-1, keepdims=True) * ROUTED_SCALE
    routed = _routed_experts(h, idx, w, w_exp_gate, w_exp_up, w_exp_down)
    shared = (jax.nn.silu(h @ w_sh_gate) * (h @ w_sh_up)) @ w_sh_down
    return (routed + shared).reshape(bsz, seq, d)


def setup_inputs(seed: int = 0) -> dict:
    key = jax.random.key(seed)
    ks = jax.random.split(key, 27)
    nrm = lambda k, shape, s: jax.random.normal(k, shape, jnp.float32) * s
    D = D_MODEL
    col_scale = np.concatenate([np.full((w,), DN_BETA if i in VALUE_SLOTS else 1.0, np.float32)
                                for i, w in enumerate(IN_WIDTHS)])
    offset = jax.random.randint(ks[2], (BATCH, 1), 0, 4096, dtype=jnp.int32)
    positions = offset + jnp.arange(SEQ, dtype=jnp.int32)[None, :]
    return {
        'x': nrm(ks[0], (BATCH, SEQ, D), 1.0),
        'c': nrm(ks[1], (BATCH, D), 1.0),
        'positions': positions,
        'w_ada': nrm(ks[3], (DEPTH, D, 6 * D), 0.5 * D ** -0.5),
        'b_ada': nrm(ks[4], (DEPTH, 6 * D), 0.02),
        'w_in': nrm(ks[5], (DEPTH, D, N_IN), D ** -0.5) * jnp.asarray(col_scale),
        'w_br_a': nrm(ks[6], (DEPTH, A_WIDTH, D), A_WIDTH ** -0.5),
        'w_br_b': nrm(ks[7], (DEPTH, B_WIDTH, D), B_WIDTH ** -0.5),
        'w_out': nrm(ks[8], (DEPTH, D, D), DN_BETA * D ** -0.5),
        'cmp_pos_k': nrm(ks[9], (DEPTH, CMP_BLOCK, HEAD_DIM), 0.1),
        'cmp_pos_v': nrm(ks[10], (DEPTH, CMP_BLOCK, HEAD_DIM), 0.1),
        'cmp_k_w1': nrm(ks[11], (DEPTH, CMP_BLOCK * HEAD_DIM, CMP_HIDDEN), (CMP_BLOCK * HEAD_DIM) ** -0.5),
        'cmp_k_w2': nrm(ks[12], (DEPTH, CMP_HIDDEN, HEAD_DIM), CMP_HIDDEN ** -0.5),
        'cmp_v_w1': nrm(ks[13], (DEPTH, CMP_BLOCK * HEAD_DIM, CMP_HIDDEN), (CMP_BLOCK * HEAD_DIM) ** -0.5),
        'cmp_v_w2': nrm(ks[14], (DEPTH, CMP_HIDDEN, HEAD_DIM), CMP_HIDDEN ** -0.5),
        'ln1_g': 1.0 + nrm(ks[15], (DEPTH, D), 0.02),
        'ln1_b': nrm(ks[16], (DEPTH, D), 0.02),
        'w_router': nrm(ks[17], (DEPTH, D, N_EXPERTS), D ** -0.5),
        'router_bias': nrm(ks[18], (DEPTH, N_EXPERTS), 0.01),
        'w_exp_gate': nrm(ks[19], (DEPTH, N_EXPERTS, D, EXPERT_DIM), D ** -0.5),
        'w_exp_up': nrm(ks[20], (DEPTH, N_EXPERTS, D, EXPERT_DIM), D ** -0.5),
        'w_exp_down': nrm(ks[21], (DEPTH, N_EXPERTS, EXPERT_DIM, D), DN_BETA * EXPERT_DIM ** -0.5),
        'w_sh_gate': nrm(ks[22], (DEPTH, D, SHARED_DIM), D ** -0.5),
        'w_sh_up': nrm(ks[23], (DEPTH, D, SHARED_DIM), D ** -0.5),
        'w_sh_down': nrm(ks[24], (DEPTH, SHARED_DIM, D), DN_BETA * SHARED_DIM ** -0.5),
        'ln2_g': 1.0 + nrm(ks[25], (DEPTH, D), 0.02),
        'ln2_b': nrm(ks[26], (DEPTH, D), 0.02),
    }


def reference(x, c, positions, w_ada, b_ada, w_in, w_br_a, w_br_b, w_out,
              cmp_pos_k, cmp_pos_v, cmp_k_w1, cmp_k_w2, cmp_v_w1, cmp_v_w2, ln1_g, ln1_b,
              w_router, router_bias, w_exp_gate, w_exp_up, w_exp_down,
              w_sh_gate, w_sh_up, w_sh_down, ln2_g, ln2_b):
    cond = jax.nn.silu(c)
    for l in range(DEPTH):
        mod = cond @ w_ada[l] + b_ada[l]
        shift1, scale1, gate1, shift2, scale2, gate2 = jnp.split(mod[:, None, :], 6, axis=-1)
        u = x * (1.0 + scale1) + shift1
        mix = _token_mixer(u, positions, w_in[l], w_br_a[l], w_br_b[l], w_out[l],
                           cmp_pos_k[l], cmp_pos_v[l], cmp_k_w1[l], cmp_k_w2[l], cmp_v_w1[l], cmp_v_w2[l])
        x = _layer_norm(DN_ALPHA * x + gate1 * mix, ln1_g[l], ln1_b[l])
        u = x * (1.0 + scale2) + shift2
        ffn = _moe_ffn(u, w_router[l], router_bias[l], w_exp_gate[l], w_exp_up[l], w_exp_down[l],
                       w_sh_gate[l], w_sh_up[l], w_sh_down[l])
        x = _layer_norm(DN_ALPHA * x + gate2 * ffn, ln2_g[l], ln2_b[l])
    return x
```

```python
import numpy as np
import concourse.bass as bass
import concourse.mybir as mybir
from concourse.bass_utils import run_bass_kernel_spmd

F32 = mybir.dt.float32
BF16 = mybir.dt.bfloat16
I32 = mybir.dt.int32
ALU = mybir.AluOpType
AF = mybir.ActivationFunctionType
AX = mybir.AxisListType
IOA = bass.IndirectOffsetOnAxis

N_CORES = 8
D = 1024
SEQ = 2048
BPC = 2
TOK = BPC * SEQ
NT = TOK // 128
NE = 256
BLK = 128
NBLK = TOK * 8 // BLK + NE
NSLOT = NBLK * BLK
CAP = BLK
DN_ALPHA = 2.0 ** 0.25
LN_EPS = 1e-5
ROUTED_SCALE = 2.5
EPOCH = 16000


class _Op:
    __slots__ = ("eng", "fn", "deps", "is_dma", "need_sig", "sig", "waits", "idx", "slot_prev", "r", "w", "is_barrier")


class Sched:
    ENGS = ("pe", "dve", "act", "pool", "sp")
    NSLOTS = {"sp": 12, "act": 4, "pool": 16}

    def __init__(self, nc):
        self.nc = nc
        self.cur = []
        self._atom = None
        self.ops = {e: [] for e in self.ENGS}
        self.allops = []
        self.lastw = {}
        self.readers = {}

    def op(self, eng, fn, r=(), w=(), dma=False):
        o = _Op()
        o.eng, o.fn, o.is_dma, o.deps, o.need_sig, o.sig, o.waits = eng, fn, dma, [], dma, None, []
        o.r, o.w, o.is_barrier, o.slot_prev = list(r), list(w), False, None
        if self._atom is not None:
            self._atom.append(o)
        else:
            self.cur.append([o])
        return o

    def barrier(self):
        o = _Op()
        o.eng, o.fn, o.is_dma, o.deps, o.need_sig, o.sig, o.waits = None, None, False, [], False, None, []
        o.r, o.w, o.is_barrier, o.slot_prev = [], [], True, None
        assert self._atom is None
        self.cur.append([o])

    class _Atomic:
        def __init__(self, s):
            self.s = s

        def __enter__(self):
            self.nested = self.s._atom is not None
            if not self.nested:
                self.s._atom = []

        def __exit__(self, *a):
            if not self.nested:
                seg, self.s._atom = self.s._atom, None
                if seg:
                    self.s.cur.append(seg)

    def atomic(self):
        return Sched._Atomic(self)

    def record_parts(self, body):
        old = self.cur
        self._parts = []
        self.cur = []
        body()
        self._parts.append(self.cur)
        parts, self._parts = self._parts, None
        self.cur = old
        return parts

    def cut(self):
        if getattr(self, "_parts", None) is not None:
            self._parts.append(self.cur)
            self.cur = []

    def merge_rr(self, streams):
        streams = [list(s) for s in streams]
        pos = [0] * len(streams)
        alive = True
        while alive:
            alive = False
            for i, s in enumerate(streams):
                if pos[i] < len(s):
                    self.cur.append(s[pos[i]])
                    pos[i] += 1
                    alive = True

    def merge_seq(self, streams):
        for s in streams:
            self.cur.extend(s)

    def _track(self, o):
        o.idx = len(self.allops)
        seen = set()

        def add(d):
            if d is not None and d.idx not in seen:
                seen.add(d.idx)
                o.deps.append(d)

        for k in o.r:
            add(self.lastw.get(k))
        for k in o.w:
            add(self.lastw.get(k))
            rd = self.readers.get(k)
            if rd:
                for d in rd[0].values():
                    add(d)
                for d in rd[1]:
                    add(d)
        for k in o.r:
            rd = self.readers.setdefault(k, ({}, []))
            if o.is_dma:
                rd[1].append(o)
            else:
                rd[0][o.eng] = o
        for k in o.w:
            self.lastw[k] = o
            self.readers[k] = ({}, [])
        self.ops[o.eng].append(o)
        self.allops.append(o)

    def _track_barrier(self):
        deps = []
        for e in self.ENGS:
            got_c = False
            nd = 0
            for o in reversed(self.ops[e]):
                if o.fn is None:
                    continue
                if o.is_dma:
                    if nd < self.NSLOTS.get(e, 0):
                        deps.append(o)
                        nd += 1
                elif not got_c:
                    deps.append(o)
                    got_c = True
                if got_c and nd >= self.NSLOTS.get(e, 0):
                    break
        for e in self.ENGS:
            o = _Op()
            o.eng, o.fn, o.is_dma, o.need_sig, o.sig, o.waits = e, None, False, False, None, []
            o.deps = list(deps)
            o.idx = len(self.allops)
            o.slot_prev = None
            o.r, o.w, o.is_barrier = [], [], False
            self.ops[e].append(o)
            self.allops.append(o)

    @staticmethod
    def _needs_sync(d, o):
        if d.is_dma:
            return True
        if d.eng == o.eng and d.eng == "pe" and not o.is_dma:
            return False
        if d.fn is None:
            return False
        return True

    def finalize_and_emit(self):
        nc = self.nc
        for seg in self.cur:
            for o in seg:
                if o.is_barrier:
                    self._track_barrier()
                else:
                    self._track(o)
        for o in self.allops:
            for d in o.deps:
                if self._needs_sync(d, o):
                    d.need_sig = True
        sems = {}

        def get_sem(name):
            if name not in sems:
                sems[name] = nc.alloc_semaphore(name)
            return sems[name]

        for e in self.ENGS:
            n = 0
            slot_cnt = {}
            nd = 0
            for o in self.ops[e]:
                if o.fn is None:
                    continue
                if o.is_dma:
                    ns = self.NSLOTS[e]
                    s = nd % ns
                    nd += 1
                    c = slot_cnt.get(s, 0)
                    sem = get_sem(f"d_{e}_{s}")
                    o.slot_prev = (sem, 16 * c) if c > 0 else None
                    slot_cnt[s] = c + 1
                    o.sig = (sem, 16 * (c + 1), 16)
                elif o.need_sig:
                    ep, v = divmod(n, EPOCH)
                    n += 1
                    o.sig = (get_sem(f"c_{e}_{ep}"), v + 1, 1)
            self._final_dma = getattr(self, "_final_dma", {})
            self._final_dma[e] = {s: c for s, c in slot_cnt.items()}
        for e in self.ENGS:
            waited = {}
            for o in self.ops[e]:
                need = {}
                for d in o.deps:
                    if d.sig is None:
                        continue
                    if not self._needs_sync(d, o):
                        continue
                    sem, val, _ = d.sig
                    if need.get(sem, (None, 0))[1] < val:
                        need[sem] = (sem, val)
                if o.is_dma and o.slot_prev is not None:
                    sem, val = o.slot_prev
                    if need.get(sem, (None, 0))[1] < val:
                        need[sem] = (sem, val)
                for sem, val in need.values():
                    if waited.get(sem, 0) < val:
                        waited[sem] = val
                        o.waits.append((sem, val))
        self.sems = sems
        engmap = {"pe": "tensor", "dve": "vector", "act": "scalar", "pool": "gpsimd", "sp": "sync"}
        with nc.Block() as block:
            for e in self.ENGS:
                def body(eng, e=e):
                    for o in self.ops[e]:
                        for sem, val in o.waits:
                            eng.wait_ge(sem, val)
                        if o.fn is None:
                            continue
                        ins = o.fn(eng)
                        if o.sig is not None:
                            ins.then_inc(o.sig[0], o.sig[2])
                    if e in self.NSLOTS:
                        for s, c in self._final_dma[e].items():
                            eng.wait_ge(sems[f"d_{e}_{s}"], 16 * c)
                getattr(block, engmap[e])(body)


class Buf:
    def __init__(self, t, name, nreg=1):
        self.t, self.name, self.nreg = t, name, nreg

    def k(self, i=0):
        return (self.name, i)

    def all(self):
        return [(self.name, i) for i in range(self.nreg)]

    def __getitem__(self, idx):
        return self.t[idx]


class Prog:
    def __init__(self, cfg=None):
        self.cfg = cfg or {}
        self.nc = bass.Bass("TRN2", target_bir_lowering=False)
        self.S = Sched(self.nc)
        self._uid = 0
        self._scopes = [[]]

    def sb(self, name, shape, dtype, nreg=1):
        self._uid += 1
        g = self.nc.sbuf_tensor(f"s{self._uid}_{name}", list(shape), dtype)
        t = g.__enter__()
        self._scopes[-1].append(g)
        return Buf(t, name, nreg)

    def push(self):
        self._scopes.append([])

    def pop(self):
        self.S.barrier()
        for g in reversed(self._scopes.pop()):
            g.__exit__(None, None, None)

    def dram(self, name, shape, dtype, kind):
        return self.nc.dram_tensor(name, list(shape), dtype, kind=kind).ap()

    def op(self, eng, fn, r=(), w=(), dma=False):
        return self.S.op(eng, fn, r, w, dma)

    def PE(self, out, lhsT, rhs, start, stop, r, w, **kw):
        return self.S.op("pe", lambda e: e.matmul(out, lhsT, rhs, start=start, stop=stop, **kw), r, w)

    def TR(self, out, in_, ident, r, w):
        return self.S.op("pe", lambda e: e.transpose(out, in_, ident), r, w)

    def ACT(self, out, in_, func, r, w, **kw):
        return self.S.op("act", lambda e: e.activation(out, in_, func, **kw), r, w)

    def TT(self, eng, out, a, b, alu, r, w):
        return self.S.op(eng, lambda e: e.tensor_tensor(out, a, b, alu), r, w)

    def TS(self, eng, out, a, s1, s2, op0, op1, r, w, accum=None):
        if accum is None:
            return self.S.op(eng, lambda e: e.tensor_scalar(out, a, s1, s2, op0, op1), r, w)
        return self.S.op(eng, lambda e: e.tensor_scalar(out, a, s1, s2, op0, op1, accum_out=accum), r, w)

    def STT(self, out, in0, scalar, in1, op0, op1, r, w, accum=None):
        if accum is None:
            return self.S.op("dve", lambda e: e.scalar_tensor_tensor(out, in0, scalar, in1, op0, op1), r, w)
        return self.S.op("dve", lambda e: e.scalar_tensor_tensor(out, in0, scalar, in1, op0, op1, accum_out=accum), r, w)

    def CP(self, eng, out, in_, r, w):
        if eng == "act":
            return self.S.op("act", lambda e: e.activation(out, in_, AF.Copy), r, w)
        return self.S.op(eng, lambda e: e.tensor_copy(out, in_), r, w)

    def DMA(self, q, out, in_, r, w):
        return self.S.op(q, lambda e: e.dma_start(out=out, in_=in_), r, w, dma=True)

    def build(self):
        nc, S = self.nc, self.S
        cfg = self.cfg
        op = self.op
        PE, TR, ACT, TT, TS, STT, CP, DMA = self.PE, self.TR, self.ACT, self.TT, self.TS, self.STT, self.CP, self.DMA
        do_mixer = cfg.get("mixer", True)
        x_d = self.dram("x", [TOK, D], F32, "ExternalInput")
        cT_d = self.dram("cT", [128, 8, BPC], F32, "ExternalInput")
        pos_d = self.dram("positions", [BPC, SEQ], I32, "ExternalInput")
        w_ada_d = self.dram("w_ada", [D, 6 * D], F32, "ExternalInput")
        b_ada_d = self.dram("b_ada", [1, 6 * D], F32, "ExternalInput")
        ln_d = self.dram("ln", [4, D], F32, "ExternalInput")
        w_router_d = self.dram("w_router", [D, NE], F32, "ExternalInput")
        rbias_d = self.dram("router_bias", [1, NE], F32, "ExternalInput")
        wg_d = self.dram("w_exp_gate", [NE, D, 256], F32, "ExternalInput")
        wu_d = self.dram("w_exp_up", [NE, D, 256], F32, "ExternalInput")
        wd_d = self.dram("w_exp_down", [NE, 256, D], F32, "ExternalInput")
        wsg_d = self.dram("w_sh_gate", [D, 256], F32, "ExternalInput")
        wsu_d = self.dram("w_sh_up", [D, 256], F32, "ExternalInput")
        wsd_d = self.dram("w_sh_down", [256, D], F32, "ExternalInput")
        wmA_d = self.dram("wmA", [N_CH_A, 128, 8, 128], F32, "ExternalInput")
        wmB_d = self.dram("wmB", [N_CH_B, 128, 8, 128], F32, "ExternalInput")
        wmG_d = self.dram("wmG", [16, 128, 8, 128], F32, "ExternalInput")
        wtA_d = self.dram("wtA", [128, 8, 136], F32, "ExternalInput")
        wtB_d = self.dram("wtB", [128, 8, 280], F32, "ExternalInput")
        wbrA_d = self.dram("w_br_a", [512, D], F32, "ExternalInput")
        wbrB_d = self.dram("w_br_b", [512, D], F32, "ExternalInput")
        wout_d = self.dram("w_out", [D, D], F32, "ExternalInput")
        cw1k_d = self.dram("cmp_k_w1", [2048, 256], F32, "ExternalInput")
        cw1v_d = self.dram("cmp_v_w1", [2048, 256], F32, "ExternalInput")
        cw2k_d = self.dram("cmp_k_w2", [256, 64], F32, "ExternalInput")
        cw2v_d = self.dram("cmp_v_w2", [256, 64], F32, "ExternalInput")
        cposT_d = self.dram("cmp_posT", [128, 2, 32], F32, "ExternalInput")
        cst_d = self.dram("cst", [128, 8], F32, "ExternalInput")
        selAB_d = self.dram("selAB", [128, 2, 16, 32], F32, "ExternalInput")
        expall_d = self.dram("expall", [32, 16, 128], F32, "ExternalInput")
        ov_d = self.dram("ovl", [128, 32], F32, "ExternalInput")
        out_d = self.dram("out", [TOK, D], F32, "ExternalOutput")
        x1_d = self.dram("x1_scr", [TOK, D], F32, "Internal")
        u2_d = self.dram("u2_scr", [TOK, D], BF16, "Internal")
        xs_d = self.dram("xs_scr", [NSLOT, D], BF16, "Internal")
        ys_d = self.dram("ys_scr", [NSLOT, D], BF16, "Internal")
        mod_d = self.dram("mod_scr", [BPC * 6, D], F32, "Internal")
        dbg = {}
        for name, shape in cfg.get("dbg", {}).items():
            dbg[name] = self.dram("dbg_" + name, shape, F32, "ExternalOutput")
        self.dbg = dbg

        _regs = {}

        def bc_reg(e):
            if "bc" not in _regs:
                _regs["bc"] = e.to_reg(NSLOT - 1)
            return _regs["bc"]

        banks = [Buf(nc.alloc_psum_tensor(f"pb{i}", [128, 512], F32), f"pb{i}") for i in range(8)]

        def bfv(bk):
            return bk.t[:, :].bitcast(BF16)

        ones_f = self.sb("ones_f", [128, 128], F32)
        ones_b = self.sb("ones_b", [128, 128], BF16)
        ident_b = self.sb("ident_b", [128, 128], BF16)
        tri_b = self.sb("tri_b", [128, 128], BF16)
        trib = self.sb("trib", [128, 128], BF16)
        edgeb = self.sb("edgeb", [128, 128], BF16)
        negb = self.sb("negb", [128, 128], F32)
        iotaC = self.sb("iotaC", [128, NE], F32)
        iotaE1 = self.sb("iotaE1", [128, NE], F32)
        jcol = self.sb("jcol", [128, NBLK // 128], F32)
        pcol = self.sb("pcol", [128, 1], F32)
        Utri = self.sb("Utri", [128, 2, NE], BF16)
        cst = self.sb("cst", [128, 8], F32)
        eps_t = self.sb("eps_t", [128, 1], F32)
        op("pool", lambda e: e.memset(ones_f[:, :], 1.0), w=ones_f.all())
        op("pool", lambda e: e.memset(ones_b[:, :], 1.0), w=ones_b.all())
        op("pool", lambda e: e.memset(negb[:, :], -30000.0), w=negb.all())
        op("pool", lambda e: e.memset(eps_t[:, :], LN_EPS), w=eps_t.all())
        op("pool", lambda e: e.affine_select(ident_b[:, :], ones_f[:, :], [[-1, 128]], ALU.is_equal, 0.0,
                                            base=0, channel_multiplier=1), r=ones_f.all(), w=ident_b.all())
        op("pool", lambda e: e.affine_select(tri_b[:, :], ones_f[:, :], [[1, 128]], ALU.is_gt, 0.0,
                                            base=0, channel_multiplier=-1), r=ones_f.all(), w=tri_b.all())
        op("pool", lambda e: e.affine_select(trib[:, :], negb[:, :], [[-1, 128]], ALU.is_gt, 0.0,
                                            base=0, channel_multiplier=1), r=negb.all(), w=trib.all())
        op("pool", lambda e: e.affine_select(edgeb[:, :], negb[:, :], [[1, 128]], ALU.is_ge, 0.0,
                                            base=0, channel_multiplier=-1), r=negb.all(), w=edgeb.all())
        op("pool", lambda e: e.iota(iotaC[:, :], [[4096, NE]], base=0, channel_multiplier=0,
                                    allow_small_or_imprecise_dtypes=True), w=iotaC.all())
        op("pool", lambda e: e.iota(jcol[:, :], [[128, NBLK // 128]], base=0, channel_multiplier=1,
                                    allow_small_or_imprecise_dtypes=True), w=jcol.all())
        for a_ in range(2):
            op("pool", lambda e, a_=a_: e.iota(iotaE1[:, :], [[1, NE]], base=-128 * a_, channel_multiplier=-1,
                                              allow_small_or_imprecise_dtypes=True), r=Utri.all(), w=iotaE1.all())
            TS("dve", Utri[:, a_, :], iotaE1[:, :], 0.0, None, ALU.is_gt, ALU.bypass, iotaE1.all(), Utri.all())
        op("pool", lambda e: e.iota(iotaE1[:, :], [[1, NE]], base=0, channel_multiplier=0,
                                    allow_small_or_imprecise_dtypes=True), r=Utri.all(), w=iotaE1.all())
        op("pool", lambda e: e.iota(pcol[:, :], [[0, 1]], base=0, channel_multiplier=1,
                                    allow_small_or_imprecise_dtypes=True), w=pcol.all())
        DMA("sp", cst[:, :], cst_d[:, :], [], cst.all())

        ln1g = self.sb("ln1g", [128, D], F32)
        ln1b = self.sb("ln1b", [128, D], F32)
        rb_bc = self.sb("rb_bc", [128, NE], F32)
        wr_b = self.sb("wr_b", [128, 8, NE], BF16)
        carry = self.sb("carry", [128, NE], F32)
        dest_f = self.sb("dest_f", [128, NT, 8], F32, nreg=NT)
        dest_i = self.sb("dest_i", [128, NT, 8], I32, nreg=NT)
        wk = self.sb("wk", [128, NT, 8], F32, nreg=NT)
        op("pool", lambda e: e.memset(carry[:, :], 0.0), w=carry.all())
        DMA("sp", ln1g[:, :], ln_d[0:1, :].to_broadcast([128, D]), [], ln1g.all())
        DMA("sp", ln1b[:, :], ln_d[1:2, :].to_broadcast([128, D]), [], ln1b.all())
        DMA("sp", rb_bc[:, :], rbias_d[0:1, :].to_broadcast([128, NE]), [], rb_bc.all())

        self.push()
        condT = self.sb("condT", [128, 8, BPC], F32)
        condS = self.sb("condS", [128, 8, BPC], F32)
        DMA("sp", condT[:, :, :], cT_d[:, :, :], [], condT.all())
        ACT(condS[:, :, :], condT[:, :, :], AF.Silu, condT.all(), condS.all())
        wst = [self.sb(f"wada_st{i}", [128, 8, 512], F32) for i in range(2)]
        mrow = [self.sb(f"mrow{i}", [1, D], F32) for i in range(4)]
        brow = [self.sb(f"brow{i}", [1, D], F32) for i in range(2)]
        w_ada_v = w_ada_d.rearrange("(k p) n -> p k n", p=128)
        nmm = 0
        for j in range(6):
            br = brow[j % 2]
            DMA("sp", br[:, :], b_ada_d[:, j * D:(j + 1) * D], [], br.all())
            rows_j = [mrow[(j * BPC + b) % 4] for b in range(BPC)]
            for half in range(2):
                cc = j * 2 + half
                st = wst[cc % 2]
                DMA("sp", st[:, :, :], w_ada_v[:, :, cc * 512:(cc + 1) * 512], [], st.all())
                for b in range(BPC):
                    bk = banks[nmm % 2]
                    nmm += 1
                    for k in range(8):
                        PE(bk[0:1, :], condS[:, k, b:b + 1], st[:, k, :], k == 0, k == 7, condS.all() + st.all(), bk.all())
                    add = 1.0 if j in (1, 4) else 0.0
                    STT(rows_j[b][0:1, half * 512:(half + 1) * 512], bk[0:1, :], add, br[0:1, half * 512:(half + 1) * 512],
                        ALU.add, ALU.add, bk.all() + br.all(), rows_j[b].all())
            for b in range(BPC):
                DMA("sp", mod_d[b * 6 + j:b * 6 + j + 1, :], rows_j[b][0:1, :], rows_j[b].all(), [("mod_d", b, j)])
        wr_st = self.sb("wr_st", [128, 8, NE], F32)
        DMA("sp", wr_st[:, :, :], w_router_d.rearrange("(k p) n -> p k n", p=128), [], wr_st.all())
        CP("pool", wr_b[:, :, :], wr_st[:, :, :], wr_st.all(), wr_b.all())
        self.pop()

        def load_mod(dst, b, j):
            DMA("sp", dst[:, :], mod_d[b * 6 + j:b * 6 + j + 1, :].to_broadcast([128, D]), [("mod_d", b, j)], dst.all())

        def layernorm(z, dst, g_bc, b_bc, tagbufs):
            stats, mv, rstd = tagbufs
            for h in range(2):
                op("dve", lambda e, h=h: e.bn_stats(stats[:, h, :], z[:, h * 512:(h + 1) * 512]), r=z.all(), w=stats.all())
            op("dve", lambda e: e.bn_aggr(mv[:, :], stats[:, :, :]), r=stats.all(), w=mv.all())
            ACT(rstd[:, :], mv[:, 1:2], AF.Sqrt, mv.all() + eps_t.all(), rstd.all(), bias=eps_t[:, 0:1], scale=1.0)
            op("dve", lambda e: e.reciprocal(rstd[:, :], rstd[:, :]), r=rstd.all(), w=rstd.all())
            TS("dve", z[:, :], z[:, :], mv[:, 0:1], rstd[:, 0:1], ALU.subtract, ALU.mult, z.all() + mv.all() + rstd.all(), z.all())
            TT("pool", z[:, :], z[:, :], g_bc[:, :], ALU.mult, z.all() + g_bc.all(), z.all())
            TT("pool", dst[:, :], z[:, :], b_bc[:, :], ALU.add, z.all() + b_bc.all(), dst.all())

        n_tiles = cfg.get("n_tiles", NT)
        TPS = SEQ // 128
        PI = float(np.pi)
        C1 = 6.28125
        C2 = float(2.0 * np.pi - 6.28125)
        NBIS = 18

        def build_uT(b, uT):
            self.push()
            s1 = self.sb("m0_s1", [128, D], F32)
            h1 = self.sb("m0_h1", [128, D], F32)
            load_mod(h1, b, 0)
            load_mod(s1, b, 1)
            xs = [self.sb(f"m0_x{i}", [128, D], F32) for i in range(2)]
            us = [self.sb(f"m0_u{i}", [128, D], BF16) for i in range(2)]
            for tt in range(TPS):
                X, U = xs[tt % 2], us[tt % 2]
                r0 = b * SEQ + tt * 128
                DMA("sp", X[:, :], x_d[r0:r0 + 128, :], [], X.all())
                TT("dve", X[:, :], X[:, :], s1[:, :], ALU.mult, X.all() + s1.all(), X.all())
                TT("pool", U[:, :], X[:, :], h1[:, :], ALU.add, X.all() + h1.all(), U.all())
                bk = banks[tt % 2]
                pT = bfv(bk)
                for k in range(8):
                    TR(pT[:, k * 128:(k + 1) * 128], U[:, k * 128:(k + 1) * 128], ident_b[:, :], U.all() + ident_b.all(), bk.all())
                CP("act", uT[:, :, tt * 128:(tt + 1) * 128], pT.rearrange("p (k t) -> p k t", k=8), bk.all(), [uT.k(tt // 4)])
            self.pop()

        def rope_tables(b, CS, col_inv, col_sgn, nrow):
            self.push()
            posi = self.sb("rp_posi", [1, SEQ], I32)
            posf = self.sb("rp_posf", [1, SEQ], F32)
            ang = self.sb("rp_ang", [128, SEQ], F32)
            a2 = self.sb("rp_a2", [128, SEQ], F32)
            qf = self.sb("rp_qf", [128, SEQ], F32)
            qi = self.sb("rp_qi", [128, SEQ], I32)
            DMA("sp", posi[:, :], pos_d[b:b + 1, :], [], posi.all())
            CP("dve", posf[:, :], posi[:, :], posi.all(), posf.all())
            for c4 in range(4):
                bk = banks[2 + c4 % 2]
                PE(bk[:, :], ones_f[0:1, :], posf[0:1, c4 * 512:(c4 + 1) * 512], True, True, ones_f.all() + posf.all(), bk.all())
                TS("dve", ang[0:nrow, c4 * 512:(c4 + 1) * 512], bk[0:nrow, :], cst[0:nrow, col_inv:col_inv + 1], None, ALU.mult, ALU.bypass,
                   bk.all() + cst.all(), ang.all())
            for which in range(2):
                src = ang
                if which == 0:
                    TS("dve", a2[0:nrow, :], ang[0:nrow, :], PI / 2.0, None, ALU.add, ALU.bypass, ang.all(), a2.all())
                    src = a2
                TS("dve", qf[0:nrow, :], src[0:nrow, :], 1.0 / (2.0 * PI), None, ALU.mult, ALU.bypass, src.all(), qf.all())
                CP("dve", qi[0:nrow, :], qf[0:nrow, :], qf.all(), qi.all())
                CP("dve", qf[0:nrow, :], qi[0:nrow, :], qi.all(), qf.all())
                STT(a2[0:nrow, :], qf[0:nrow, :], -C1, src[0:nrow, :], ALU.mult, ALU.add, qf.all() + src.all(), a2.all())
                STT(a2[0:nrow, :], qf[0:nrow, :], -C2, a2[0:nrow, :], ALU.mult, ALU.add, qf.all() + a2.all(), a2.all())
                TS("dve", qf[0:nrow, :], a2[0:nrow, :], PI, -2.0 * PI, ALU.is_gt, ALU.mult, a2.all(), qf.all())
                TT("dve", a2[0:nrow, :], a2[0:nrow, :], qf[0:nrow, :], ALU.add, a2.all() + qf.all(), a2.all())
                TS("dve", a2[0:nrow, :], a2[0:nrow, :], 3.14159, -3.14159, ALU.min, ALU.max, a2.all(), a2.all())
                if which == 0:
                    ACT(CS[0:nrow, 0, :], a2[0:nrow, :], AF.Sin, a2.all(), [CS.k(0)])
                else:
                    ACT(qf[0:nrow, :], a2[0:nrow, :], AF.Sin, a2.all(), qf.all())
                    TS("dve", CS[0:nrow, 1, :], qf[0:nrow, :], cst[0:nrow, col_sgn:col_sgn + 1], None, ALU.mult, ALU.bypass,
                       qf.all() + cst.all(), [CS.k(1)])
            self.pop()

        def project(wm_d, jobs, uT, wbufs):
            st, wb, t1, t2 = wbufs
            cnt = [0]

            def load(ch):
                i = cnt[0] % 3
                cnt[0] += 1
                DMA("sp", st[i][:, :, :], wm_d[ch], [], st[i].all())
                CP("pool", wb[i][:, :, :], st[i][:, :, :], st[i].all(), wb[i].all())
                return wb[i]

            nb = 0
            for (ch, chsw, nrow, dst_fn, CS) in jobs:
                wA = load(ch)
                wB = load(chsw) if chsw is not None else None
                for tc in range(4):
                    ts_ = slice(tc * 512, (tc + 1) * 512)
                    bA = banks[2 + nb % 4]
                    nb += 1
                    for k in range(8):
                        PE(bA[:, :], wA[:, k, :], uT[:, k, ts_], k == 0, k == 7, wA.all() + [uT.k(tc)], bA.all())
                    dst, dkeys = dst_fn(tc)
                    if wB is None:
                        CP("act", dst, bA[0:nrow, :], bA.all(), dkeys)
                    else:
                        bB = banks[2 + nb % 4]
                        nb += 1
                        for k in range(8):
                            PE(bB[:, :], wB[:, k, :], uT[:, k, ts_], k == 0, k == 7, wB.all() + [uT.k(tc)], bB.all())
                        T1, T2 = t1[tc % 2], t2[tc % 2]
                        TT("dve", T1[0:nrow, :], bA[0:nrow, :], CS[0:nrow, 0, ts_], ALU.mult, bA.all() + [CS.k(0)], T1.all())
                        TT("dve", T2[0:nrow, :], bB[0:nrow, :], CS[0:nrow, 1, ts_], ALU.mult, bB.all() + [CS.k(1)], T2.all())
                        TT("pool", dst, T1[0:nrow, :], T2[0:nrow, :], ALU.add, T1.all() + T2.all(), dkeys)

        def proj_bufs():
            st = [self.sb(f"pj_st{i}", [128, 8, 128], F32) for i in range(3)]
            wb = [self.sb(f"pj_wb{i}", [128, 8, 128], BF16) for i in range(3)]
            t1 = [self.sb(f"pj_t1{i}", [128, 512], F32) for i in range(2)]
            t2 = [self.sb(f"pj_t2{i}", [128, 512], F32) for i in range(2)]
            return st, wb, t1, t2

        def attn_out(accs, O, coef=None, first=True, tmpb=None):
            Ov = O[:, :].rearrange("p (g e q d) -> p g q e d", g=2, e=2, q=2)
            for g in range(2):
                a = accs[g]
                W = a.W
                av = a.buf[:, 0:4 * W].rearrange("p (q e w) -> p q e w", q=2, e=2)[:, :, :, 0:64]
                cf = coef[:, g * 4:(g + 1) * 4].rearrange("p (q e) -> p q e", q=2).unsqueeze(3).to_broadcast([128, 2, 2, 64])
                if first:
                    TT("dve", Ov[:, g], av, cf, ALU.mult, a.buf.all() + coef.all(), O.all())
                else:
                    tv = tmpb[:, :].rearrange("p (q e d) -> p q e d", q=2, e=2)
                    TT("dve", tv, av, cf, ALU.mult, a.buf.all() + coef.all(), tmpb.all())
                    TT("pool", Ov[:, g], Ov[:, g], tv, ALU.add, O.all() + tmpb.all(), O.all())

        class Acc:
            def __init__(self, buf, W):
                self.buf, self.W = buf, W

        def o_to_T(O, Ob, oT, i, bank):
            CP("act", Ob[:, :], O[:, :], O.all(), Ob.all())
            pT = bfv(bank)
            for c in range(4):
                TR(pT[:, c * 128:(c + 1) * 128], Ob[:, c * 128:(c + 1) * 128], ident_b[:, :], Ob.all() + ident_b.all(), bank.all())
            CP("dve", oT[:, :, i * 128:(i + 1) * 128], pT[:, 0:512].rearrange("p (c t) -> p c t", c=4), bank.all(), [oT.k(i // 4)])

        def dsa(b, oaT):
            self.push()
            QA = self.sb("QA", [128, 4, SEQ], BF16, nreg=4)
            KA = self.sb("KA", [128, 2, SEQ], BF16, nreg=4)
            QI = self.sb("QI", [96, 3, SEQ], BF16, nreg=4)
            KI = self.sb("KI", [96, SEQ], BF16, nreg=4)
            VA = self.sb("VA", [128, TPS, 2, 65], BF16)
            widx = self.sb("widx", [128, TPS, 8], F32)
            wabs = self.sb("wabs", [128, TPS, 8], F32)
            wsgn = self.sb("wsgn", [128, TPS, 8], F32)
            CS64 = self.sb("CS64", [128, 2, SEQ], BF16, nreg=2)
            CS32 = self.sb("CS32", [128, 2, SEQ], BF16, nreg=2)
            rope_tables(b, CS64, 0, 1, 128)
            rope_tables(b, CS32, 2, 3, 96)
            self.push()
            uT = self.sb("uT", [128, 8, SEQ], BF16, nreg=4)
            build_uT(b, uT)
            wbufs = proj_bufs()
            jobs = []
            for c in range(4):
                jobs.append((c, 4 + c, 128, (lambda tc, c=c: (QA[:, c, tc * 512:(tc + 1) * 512], [QA.k(tc)])), CS64))
            for g in range(2):
                jobs.append((8 + g, 10 + g, 128, (lambda tc, g=g: (KA[:, g, tc * 512:(tc + 1) * 512], [KA.k(tc)])), CS64))
            for c in range(3):
                jobs.append((12 + c, 15 + c, 96, (lambda tc, c=c: (QI[:, c, tc * 512:(tc + 1) * 512], [QI.k(tc)])), CS32))
            jobs.append((18, 19, 96, (lambda tc: (KI[:, tc * 512:(tc + 1) * 512], [KI.k(tc)])), CS32))
            project(wmA_d, jobs, uT, wbufs)
            wt_st = self.sb("wtA_st", [128, 8, 136], F32)
            wt_b = self.sb("wtA_b", [128, 8, 136], BF16)
            DMA("sp", wt_st[:, :, :], wtA_d[:, :, :], [], wt_st.all())
            CP("pool", wt_b[:, :, :], wt_st[:, :, :], wt_st.all(), wt_b.all())
            op("pool", lambda e: e.memset(VA[:, :, :, 64:65], 1.0), w=VA.all())
            for tt in range(TPS):
                bk = banks[6 + tt % 2]
                for k in range(8):
                    PE(bk[:, 0:136], uT[:, k, tt * 128:(tt + 1) * 128], wt_b[:, k, :], k == 0, k == 7, [uT.k(tt // 4)] + wt_b.all(), bk.all())
                CP("act", VA[:, tt, :, 0:64], bk[:, 0:128].rearrange("p (g d) -> p g d", g=2), bk.all(), VA.all())
                CP("dve", widx[:, tt, :], bk[:, 128:136], bk.all(), widx.all())
            c0 = float((32.0 ** -0.5) * (8.0 ** -0.5))
            ACT(wabs[:, :, :], widx[:, :, :], AF.Abs, widx.all(), wabs.all(), scale=c0)
            TS("dve", wsgn[:, :, :], widx[:, :, :], 0.0, 2.0, ALU.is_ge, ALU.mult, widx.all(), wsgn.all())
            TS("dve", wsgn[:, :, :], wsgn[:, :, :], -1.0, None, ALU.add, ALU.bypass, wsgn.all(), wsgn.all())
            self.pop()
            self.push()
            GQ = 4
            accs = [self.sb(f"ix_acc{i}", [128, SEQ], F32, nreg=4) for i in range(GQ)]
            junks = [self.sb(f"ix_junk{i}", [128, SEQ], BF16) for i in range(GQ)]
            bss = [self.sb(f"ix_bs{i}", [128, 8 + NBIS], F32) for i in range(GQ)]
            css = [self.sb(f"ix_cs{i}", [128, 1], F32) for i in range(GQ)]
            Mb = self.sb("ix_Mb", [128, SEQ], BF16)
            BT = self.sb("ix_BT", [128, TPS, 128], BF16, nreg=2)
            Rt = [self.sb(f"ix_R{i}", [128, 512], F32) for i in range(3)]
            pw2 = self.sb("ix_pw2", [128, NBIS], F32)
            thrneg = self.sb("ix_thrneg", [128, 1], F32)
            Et = [self.sb(f"at_E{i}", [128, 512], BF16) for i in range(4)]
            O = self.sb("at_O", [128, 512], F32)
            Ob = self.sb("at_Ob", [128, 512], BF16)
            RD = self.sb("at_RD", [128, 8], F32)
            for k in range(NBIS):
                op("pool", lambda e, k=k: e.memset(pw2[:, k:k + 1], 2.0 ** -(k + 1)), w=pw2.all())
            op("pool", lambda e: e.memset(thrneg[:, :], -1e30), w=thrneg.all())
            cnts = {"R": 0, "E": 0}
            nq = cfg.get("n_qtiles", TPS)

            def indexer(i):
                acc, bs = accs[i % GQ], bss[i % GQ]
                rmax, rmin = bs[:, 1:2], bs[:, 2:3]
                n = 128 * (i + 1)
                it = slice(i * 128, (i + 1) * 128)
                nch = (n + 511) // 512
                for h in range(8):
                    pb = 32 * (h % 3)
                    for kc in range(nch):
                        kw = min(512, n - kc * 512)
                        bk = banks[(h * nch + kc) % 2]
                        R = Rt[cnts["R"] % 3]
                        cnts["R"] += 1
                        with S.atomic():
                            PE(bk[:, 0:kw], QI[pb:pb + 32, h // 3, it], KI[pb:pb + 32, kc * 512:kc * 512 + kw], True, True,
                               [QI.k(i // 4), KI.k(kc)], bk.all())
                            ACT(R[:, 0:kw], bk[:, 0:kw], AF.Relu, bk.all() + wabs.all(), R.all(), scale=wabs[:, i, h:h + 1])
                        if h == 0:
                            TS("dve", acc[:, kc * 512:kc * 512 + kw], R[:, 0:kw], wsgn[:, i, 0:1], None, ALU.mult, ALU.bypass,
                               R.all() + wsgn.all(), [acc.k(kc)])
                        else:
                            STT(acc[:, kc * 512:kc * 512 + kw], R[:, 0:kw], wsgn[:, i, h:h + 1], acc[:, kc * 512:kc * 512 + kw],
                                ALU.mult, ALU.add, R.all() + wsgn.all() + [acc.k(kc)], [acc.k(kc)])
                akeys = [acc.k(kc) for kc in range(nch)]
                if i >= 2:
                    op("dve", lambda e: e.tensor_reduce(rmin, acc[:, 0:n], AX.X, ALU.min), r=akeys, w=bs.all())
                    op("dve", lambda e: e.tensor_reduce(rmax, acc[:, 0:n], AX.X, ALU.max), r=akeys, w=bs.all())
                op("pool", lambda e: e.affine_select(acc[:, it], acc[:, it], [[-1, 128]], ALU.is_ge, -3.0e38,
                                                    base=0, channel_multiplier=1), r=[acc.k(i // 4)], w=[acc.k(i // 4)])

            def bisect(i):
                acc, bs, junk = accs[i % GQ], bss[i % GQ], junks[i % GQ]
                lo, rmax, rmin, w0, mid, cnt, stp = (bs[:, 0:1], bs[:, 1:2], bs[:, 2:3], bs[:, 3:4], bs[:, 4:5], bs[:, 5:6], bs[:, 6:7])
                Wk = bs[:, 8:8 + NBIS]
                n = 128 * (i + 1)
                akeys = [acc.k(kc) for kc in range((n + 511) // 512)]
                TS("dve", lo, rmin, -1.0, None, ALU.add, ALU.bypass, bs.all(), bs.all())
                TT("dve", w0, rmax, lo, ALU.subtract, bs.all(), bs.all())
                TS("dve", Wk, pw2[:, :], w0, None, ALU.mult, ALU.bypass, bs.all() + pw2.all(), bs.all())
                cs_ = css[i % GQ]
                for k in range(NBIS):
                    if i % 2 == 0:
                        TS("dve", mid, lo, Wk[:, k:k + 1], -1.0, ALU.add, ALU.mult, bs.all(), bs.all())
                        ACT(junk[:, 0:n], acc[:, 0:n], AF.Sign, akeys + bs.all(), junk.all() + cs_.all(), bias=mid, scale=1.0, accum_out=cs_[:, 0:1])
                        TS("dve", stp, cs_[:, 0:1], float(511 - n), Wk[:, k:k + 1], ALU.is_ge, ALU.mult, bs.all() + cs_.all(), bs.all())
                    else:
                        TT("dve", mid, lo, Wk[:, k:k + 1], ALU.add, bs.all(), bs.all())
                        TS("dve", junk[:, 0:n], acc[:, 0:n], mid, None, ALU.is_ge, ALU.add, akeys + bs.all(), junk.all() + bs.all(), accum=cnt)
                        TS("dve", stp, cnt, 255.5, Wk[:, k:k + 1], ALU.is_ge, ALU.mult, bs.all(), bs.all())
                    TT("dve", lo, lo, stp, ALU.add, bs.all(), bs.all())

            def attend(i):
                acc, bs = accs[i % GQ], bss[i % GQ]
                n = 128 * (i + 1)
                it = slice(i * 128, (i + 1) * 128)
                akeys = [acc.k(kc) for kc in range((n + 511) // 512)]
                if i >= 2:
                    thr, thr_keys = bs[:, 0:1], bs.all()
                else:
                    thr, thr_keys = thrneg[:, 0:1], thrneg.all()
                TS("dve", Mb[:, 0:n], acc[:, 0:n], thr, -30000.0, ALU.is_lt, ALU.mult, akeys + thr_keys, Mb.all())
                for j0 in range(0, i + 1, 8):
                    j1 = min(i + 1, j0 + 8)
                    bk = banks[2]
                    pT = bfv(bk)
                    for j in range(j0, j1):
                        TR(pT[:, (j - j0) * 128:(j - j0 + 1) * 128], Mb[:, j * 128:(j + 1) * 128], ident_b[:, :], Mb.all() + ident_b.all(), bk.all())
                    CP("act", BT[:, j0:j1, :], pT[:, 0:(j1 - j0) * 128].rearrange("p (j t) -> p j t", t=128), bk.all(), [BT.k(j0 // 8)])
                accb = [banks[5], banks[6]]
                def scores(j):
                    jt = slice(j * 128, (j + 1) * 128)
                    sA, sB = (banks[3], banks[4]) if j % 2 == 0 else (banks[7], banks[2])
                    for par, sb_ in ((0, sA), (1, sB)):
                        ps = slice(par * 64, par * 64 + 64)
                        for g in range(2):
                            PE(sb_[:, g * 256:(g + 1) * 256], KA[ps, g, jt], QA[ps, 2 * g:2 * g + 2, it], g == 0, False,
                               [KA.k(j // 4), QA.k(i // 4)], sb_.all(), skip_group_check=True)
                        PE(sb_[:, :], ident_b[:, :], BT[:, j, :].unsqueeze(1).to_broadcast([128, 4, 128]), False, True,
                           ident_b.all() + [BT.k(j // 8)], sb_.all(), skip_group_check=True)

                def exp_pv(j):
                    sA, sB = (banks[3], banks[4]) if j % 2 == 0 else (banks[7], banks[2])
                    EA, EB = Et[cnts["E"] % 4], Et[(cnts["E"] + 1) % 4]
                    cnts["E"] += 2
                    ACT(EA[:, :], sA[:, :], AF.Exp, sA.all(), EA.all(), scale=0.125)
                    ACT(EB[:, :], sB[:, :], AF.Exp, sB.all(), EB.all(), scale=0.125)
                    for g in range(2):
                        for par, E_ in ((0, EA), (1, EB)):
                            for e_ in range(2):
                                r_ = par * 2 + e_
                                blk = (g * 2 + e_) * 128
                                PE(accb[g][:, r_ * 65:(r_ + 1) * 65], E_[:, blk:blk + 128], VA[:, j, g, :],
                                   (j == 0 and r_ == 0), (j == i and r_ == 3), E_.all() + VA.all(), accb[g].all(), skip_group_check=True)

                scores(0)
                for j in range(i + 1):
                    if j + 1 <= i:
                        scores(j + 1)
                    exp_pv(j)
                for g in range(2):
                    op("dve", lambda e, g=g: e.reciprocal(RD[:, g * 4:(g + 1) * 4], accb[g][:, 0:260].rearrange("p (r w) -> p r w", w=65)[:, :, 64]),
                       r=accb[g].all(), w=RD.all())
                attn_out([Acc(accb[0], 65), Acc(accb[1], 65)], O, coef=RD, first=True)
                o_to_T(O, Ob, oaT, i, banks[7])

            for i0 in range(0, nq, GQ):
                grp = list(range(i0, min(i0 + GQ, nq)))
                for i in grp:
                    indexer(i)
                parts = [S.record_parts(lambda i=i: bisect(i)) for i in grp if i >= 2]
                if parts:
                    S.merge_rr([p[0] for p in parts])
                for i in grp:
                    attend(i)
            self.pop()
            self.pop()
        def nsa(b, obT):
            self.push()
            QBr = self.sb("QBr", [128, 4, SEQ], BF16, nreg=4)
            QB = self.sb("QB", [128, 4, SEQ], BF16, nreg=4)
            KCf = self.sb("KCf", [128, SEQ], BF16, nreg=4)
            VCf = self.sb("VCf", [128, SEQ], BF16, nreg=4)
            KS = self.sb("KS", [128, 2, SEQ], BF16, nreg=4)
            KW = self.sb("KW", [128, 2, SEQ], BF16, nreg=4)
            VS = self.sb("VS", [128, TPS, 2, 65], BF16)
            VW = self.sb("VW", [128, TPS, 2, 65], BF16)
            gns = self.sb("gns", [128, TPS, 24], F32)
            CS64 = self.sb("CS64n", [128, 2, SEQ], BF16, nreg=2)
            rope_tables(b, CS64, 0, 1, 128)
            self.push()
            uT = self.sb("uTn", [128, 8, SEQ], BF16, nreg=4)
            build_uT(b, uT)
            wbufs = proj_bufs()
            jobs = []
            for c in range(4):
                jobs.append((c, None, 128, (lambda tc, c=c: (QBr[:, c, tc * 512:(tc + 1) * 512], [QBr.k(tc)])), None))
            for c in range(4):
                jobs.append((c, 4 + c, 128, (lambda tc, c=c: (QB[:, c, tc * 512:(tc + 1) * 512], [QB.k(tc)])), CS64))
            jobs.append((8, None, 128, (lambda tc: (KCf[:, tc * 512:(tc + 1) * 512], [KCf.k(tc)])), None))
            jobs.append((9, None, 128, (lambda tc: (VCf[:, tc * 512:(tc + 1) * 512], [VCf.k(tc)])), None))
            for g in range(2):
                jobs.append((10 + g, 12 + g, 128, (lambda tc, g=g: (KS[:, g, tc * 512:(tc + 1) * 512], [KS.k(tc)])), CS64))
            for g in range(2):
                jobs.append((14 + g, 16 + g, 128, (lambda tc, g=g: (KW[:, g, tc * 512:(tc + 1) * 512], [KW.k(tc)])), CS64))
            project(wmB_d, jobs, uT, wbufs)
            wt_st = self.sb("wtB_st", [128, 8, 280], F32)
            wt_b = self.sb("wtB_b", [128, 8, 280], BF16)
            DMA("sp", wt_st[:, :, :], wtB_d[:, :, :], [], wt_st.all())
            CP("pool", wt_b[:, :, :], wt_st[:, :, :], wt_st.all(), wt_b.all())
            op("pool", lambda e: e.memset(VS[:, :, :, 64:65], 1.0), w=VS.all())
            op("pool", lambda e: e.memset(VW[:, :, :, 64:65], 1.0), w=VW.all())
            for tt in range(TPS):
                bk = banks[6 + tt % 2]
                for k in range(8):
                    PE(bk[:, 0:280], uT[:, k, tt * 128:(tt + 1) * 128], wt_b[:, k, :], k == 0, k == 7, [uT.k(tt // 4)] + wt_b.all(), bk.all())
                CP("act", VS[:, tt, :, 0:64], bk[:, 0:128].rearrange("p (g d) -> p g d", g=2), bk.all(), VS.all())
                CP("act", VW[:, tt, :, 0:64], bk[:, 128:256].rearrange("p (g d) -> p g d", g=2), bk.all(), VW.all())
                ACT(gns[:, tt, :], bk[:, 256:280], AF.Sigmoid, bk.all(), gns.all())
            self.pop()
            self.push()
            KCc = self.sb("KCc", [128, 2, 128], BF16)
            VX = self.sb("VX", [128, 2, 97], BF16)
            ovst = self.sb("ovst", [128, 32], F32)
            DMA("sp", ovst[:, :], ov_d[:, :], [], ovst.all())
            op("pool", lambda e: e.memset(VX[:, :, 64:65], 1.0), w=VX.all())
            for g in range(2):
                CP("pool", VX[:, g, 65:97], ovst[:, :], ovst.all(), VX.all())
            self.push()
            W1s = self.sb("W1s", [128, 8, 256], F32)
            W1 = self.sb("W1", [128, 32, 256], BF16, nreg=4)
            w2s = self.sb("w2s", [128, 2, 64], F32)
            w2 = self.sb("w2", [128, 2, 128], BF16)
            posT = self.sb("posT", [128, 2, 32], F32)
            posTb = self.sb("posTb", [128, 2, 32], BF16)
            pbias = self.sb("pbias", [128, 2], F32)
            hx = self.sb("hx", [128, 128], F32)
            hx2 = self.sb("hx2", [128, 128], F32)
            hT = self.sb("hT", [128, 2, 2, 128], BF16)
            DMA("sp", posT[:, :, :], cposT_d[:, :, :], [], posT.all())
            CP("pool", posTb[:, :, :], posT[:, :, :], posT.all(), posTb.all())
            for kind, (w1_d, w2_d, SRC) in enumerate(((cw1k_d, cw2k_d, KCf), (cw1v_d, cw2v_d, VCf))):
                w1v = w1_d.rearrange("(l d) n -> d l n", d=64)
                for l4 in range(4):
                    for hh in range(2):
                        DMA("sp", W1s[hh * 64:(hh + 1) * 64, :, :], w1v[:, l4 * 8:(l4 + 1) * 8, :], [], W1s.all())
                    CP("pool", W1[:, l4 * 8:(l4 + 1) * 8, :], W1s[:, :, :], W1s.all(), [W1.k(l4)])
                DMA("sp", w2s[:, :, :], w2_d.rearrange("(h p) d -> p h d", p=128), [], w2s.all())
                for dd in range(2):
                    CP("pool", w2[:, :, dd * 64:(dd + 1) * 64], w2s[:, :, :], w2s.all(), w2.all())
                for hc in range(2):
                    bk = banks[0]
                    for l in range(32):
                        PE(bk[:, hc:hc + 1], W1[0:64, l, hc * 128:(hc + 1) * 128], posTb[0:64, kind, l:l + 1], (l == 0 and hc == 0), (l == 31 and hc == 1),
                           W1.all() + posTb.all(), bk.all(), skip_group_check=True)
                CP("dve", pbias[:, :], banks[0][:, 0:2], banks[0].all(), pbias.all())
                for g in range(2):
                    ps = slice(g * 64, (g + 1) * 64)
                    for hc in range(2):
                        bk = banks[1 + g]
                        for l in range(32):
                            PE(bk[:, hc * 128:hc * 128 + 127], W1[ps, l, hc * 128:(hc + 1) * 128], SRC[ps, l:l + 16 * 126 + 1:16], (l == 0 and hc == 0), (l == 31 and hc == 1),
                               W1.all() + SRC.all(), bk.all(), skip_group_check=True)
                    for hc in range(2):
                        bk = banks[1 + g]
                        ACT(hx[:, 0:127], bk[:, hc * 128:hc * 128 + 127], AF.Identity, bk.all() + pbias.all(), hx.all(), bias=pbias[:, hc:hc + 1], scale=1.0)
                        TT("dve", hx2[:, 0:127], hx[:, 0:127], hx[:, 0:127], ALU.mult, hx.all(), hx2.all())
                        TS("dve", hx2[:, 0:127], hx2[:, 0:127], 0.044715, 1.0, ALU.mult, ALU.add, hx2.all(), hx2.all())
                        TT("dve", hx2[:, 0:127], hx2[:, 0:127], hx[:, 0:127], ALU.mult, hx.all() + hx2.all(), hx2.all())
                        ACT(hx2[:, 0:127], hx2[:, 0:127], AF.Sigmoid, hx2.all(), hx2.all(), scale=1.5957691216057308)
                        TT("dve", hT[:, g, hc, 0:127], hx2[:, 0:127], hx[:, 0:127], ALU.mult, hx.all() + hx2.all(), hT.all())
                    bk = banks[3 + g]
                    if kind == 0:
                        for hc in range(2):
                            PE(bk[:, 0:127], w2[:, hc, :], hT[:, g, hc, 0:127], hc == 0, hc == 1, w2.all() + hT.all(), bk.all())
                        CP("act", KCc[:, g, 0:127], bk[:, 0:127], bk.all(), KCc.all())
                    else:
                        for hc in range(2):
                            PE(bk[0:127, 0:64], hT[:, g, hc, 0:127], w2[:, hc, 0:64], hc == 0, hc == 1, w2.all() + hT.all(), bk.all())
                        CP("act", VX[0:127, g, 0:64], bk[0:127, 0:64], bk.all(), VX.all())
            self.pop()
            CM = self.sb("CM", [128, SEQ], BF16)
            onesb2 = self.sb("onesb2", [128, SEQ], BF16)
            op("pool", lambda e: e.memset(onesb2[:, :], 1.0), w=onesb2.all())
            op("pool", lambda e: e.affine_select(CM[:, :], onesb2[:, :], [[1, SEQ]], ALU.is_ge, 0.0, base=-31, channel_multiplier=-16),
               r=onesb2.all(), w=CM.all())
            selAB = self.sb("selAB", [128, 2, 16, 32], F32)
            DMA("sp", selAB[:, :, :, :], selAB_d[:, :, :, :], [], selAB.all())
            expst = self.sb("expst", [32, 16, 128], F32)
            expall = self.sb("expall", [32, 16, 128], BF16)
            DMA("sp", expst[:, :, :], expall_d[:, :, :], [], expst.all())
            CP("pool", expall[:, :, :], expst[:, :, :], expst.all(), expall.all())
            Et = [self.sb(f"nt_E{i}", [128, 512], BF16) for i in range(4)]
            O = self.sb("nt_O", [128, 512], F32)
            Ob = self.sb("nt_Ob", [128, 512], BF16)
            otmp = self.sb("nt_otmp", [128, 256], F32)
            RD = self.sb("nt_RD", [128, 8], F32)
            coef = self.sb("nt_coef", [128, 8], F32)
            imp = self.sb("nt_imp", [128, 2, 32], F32)
            imt = self.sb("nt_imt", [128, 4, 32], F32)
            top8 = self.sb("nt_top8", [128, 2, 8], F32)
            imw = self.sb("nt_imw", [128, 32], F32)
            selb = self.sb("nt_selb", [128, 2, 32], BF16)
            selT = self.sb("nt_selT", [32, 2, 128], BF16)
            nE = 0
            cntE = [0]
            nq = cfg.get("n_qtiles", TPS)
            for i in range(nq):
                it = slice(i * 128, (i + 1) * 128)
                gv = gns[:, i, :].rearrange("p (g e q k) -> p g q e k", g=2, e=2, q=2)

                def mk_coef(kbr, accs, W):
                    for g in range(2):
                        dcol = accs[g][:, 0:4 * W].rearrange("p (r w) -> p r w", w=W)[:, :, 64]
                        TS("dve", RD[:, g * 4:(g + 1) * 4], dcol, 1e-30, None, ALU.max, ALU.bypass, accs[g].all(), RD.all())
                    op("dve", lambda e: e.reciprocal(RD[:, :], RD[:, :]), r=RD.all(), w=RD.all())
                    TT("dve", coef[:, :].rearrange("p (g q e) -> p g q e", g=2, q=2), RD[:, :].rearrange("p (g q e) -> p g q e", g=2, q=2),
                       gv[:, :, :, :, kbr], ALU.mult, RD.all() + gns.all(), coef.all())

                ncv = min(127, 8 * i + 7)
                sA, sB = banks[3], banks[4]
                for par, sb_ in ((0, sA), (1, sB)):
                    ps = slice(par * 64, par * 64 + 64)
                    for g in range(2):
                        PE(sb_[0:ncv, g * 256:(g + 1) * 256], KCc[ps, g, 0:ncv], QBr[ps, 2 * g:2 * g + 2, it], g == 0, g == 1,
                           KCc.all() + [QBr.k(i // 4)], sb_.all(), skip_group_check=True)
                EA, EB = Et[nE % 4], Et[(nE + 1) % 4]
                nE += 2
                for sb_, E_ in ((sA, EA), (sB, EB)):
                    ACT(E_[0:ncv, :], sb_[0:ncv, :], AF.Exp, sb_.all(), E_.all(), scale=0.125)
                    TT("pool", E_[0:ncv, :].rearrange("p (r t) -> p r t", r=4), E_[0:ncv, :].rearrange("p (r t) -> p r t", r=4),
                       CM[0:ncv, it].unsqueeze(1).to_broadcast([ncv, 4, 128]), ALU.mult, E_.all() + CM.all(), E_.all())
                accc = [banks[5], banks[6]]
                for g in range(2):
                    for par, E_ in ((0, EA), (1, EB)):
                        for e_ in range(2):
                            r_ = par * 2 + e_
                            blk = (g * 2 + e_) * 128
                            PE(accc[g][:, r_ * 97:(r_ + 1) * 97], E_[0:ncv, blk:blk + 128], VX[0:ncv, g, :], r_ == 0, r_ == 3,
                               E_.all() + VX.all(), accc[g].all(), skip_group_check=True)
                mk_coef(0, accc, 97)
                attn_out([Acc(accc[0], 97), Acc(accc[1], 97)], O, coef=coef, first=True)
                need_sel = i >= 8
                if need_sel:
                    for g in range(2):
                        TT("dve", imt[:, :, :], accc[g][:, 0:388].rearrange("p (r w) -> p r w", w=97)[:, :, 65:97],
                           RD[:, g * 4:(g + 1) * 4].unsqueeze(2).to_broadcast([128, 4, 32]), ALU.mult, accc[g].all() + RD.all(), imt.all())
                        op("dve", lambda e, g=g: e.tensor_reduce(imp[:, g, :], imt[:, :, :].rearrange("p r j -> p j r"), AX.X, ALU.add),
                           r=imt.all(), w=imp.all())
                        TT("dve", imp[:, g, :], imp[:, g, :], selAB[:, 0, i, :], ALU.mult, imp.all() + selAB.all(), imp.all())
                        TT("dve", imp[:, g, :], imp[:, g, :], selAB[:, 1, i, :], ALU.add, imp.all() + selAB.all(), imp.all())
                        op("dve", lambda e, g=g: e.max(top8[:, g, :], imp[:, g, :]), r=imp.all(), w=top8.all())
                        op("dve", lambda e, g=g: e.match_replace(imw[:, :], top8[:, g, :], imp[:, g, :], -3.0e38), r=imp.all() + top8.all(), w=imw.all())
                        op("dve", lambda e, g=g: e.max(top8[:, g, :], imw[:, :]), r=imw.all(), w=top8.all())
                        TS("dve", selb[:, g, :], imp[:, g, :], top8[:, g, 7:8], -30000.0, ALU.is_lt, ALU.mult, imp.all() + top8.all(), selb.all())
                    bk = banks[2]
                    pT = bfv(bk)
                    for g in range(2):
                        TR(pT[0:32, g * 128:(g + 1) * 128], selb[:, g, :], ident_b[:, :], selb.all() + ident_b.all(), bk.all())
                    CP("act", selT[:, :, :], pT[0:32, 0:256].rearrange("p (g t) -> p g t", g=2), bk.all(), selT.all())

                for kbr, (KK, VV) in ((2, (KW, VW)), (1, (KS, VS))):
                    j_lo = 0 if kbr == 1 else max(0, i - 4)
                    accb = [banks[7], banks[0]] if kbr == 2 else [banks[5], banks[6]]
                    def scores(j, kbr=kbr, KK=KK):
                        jt = slice(j * 128, (j + 1) * 128)
                        sA, sB = (banks[3], banks[4]) if (j % 2 == 0) else (banks[1], banks[2])
                        for par, sb_ in ((0, sA), (1, sB)):
                            ps = slice(par * 64, par * 64 + 64)
                            bias = None
                            if j == i:
                                bias = trib
                            elif kbr == 2 and j == i - 4:
                                bias = edgeb
                            selbias = (kbr == 1 and need_sel and j < i)
                            last_plain = (bias is None and not selbias)
                            for g in range(2):
                                PE(sb_[:, g * 256:(g + 1) * 256], KK[ps, g, jt], QB[ps, 2 * g:2 * g + 2, it], g == 0, (g == 1 and last_plain),
                                   [KK.k(j // 4), QB.k(i // 4)], sb_.all(), skip_group_check=True)
                            if bias is not None:
                                PE(sb_[:, :], ident_b[:, :], bias[:, :].unsqueeze(1).to_broadcast([128, 4, 128]), False, True,
                                   ident_b.all() + bias.all(), sb_.all(), skip_group_check=True)
                            elif selbias:
                                for g in range(2):
                                    PE(sb_[:, g * 256:(g + 1) * 256], expall[:, j, :], selT[:, g, :].unsqueeze(1).to_broadcast([32, 2, 128]), False, g == 1,
                                       expall.all() + selT.all(), sb_.all(), skip_group_check=True)

                    def exp_pv(j, VV=VV, accb=accb, j_lo=j_lo):
                        sA, sB = (banks[3], banks[4]) if (j % 2 == 0) else (banks[1], banks[2])
                        EA, EB = Et[cntE[0] % 4], Et[(cntE[0] + 1) % 4]
                        cntE[0] += 2
                        ACT(EA[:, :], sA[:, :], AF.Exp, sA.all(), EA.all(), scale=0.125)
                        ACT(EB[:, :], sB[:, :], AF.Exp, sB.all(), EB.all(), scale=0.125)
                        for g in range(2):
                            for par, E_ in ((0, EA), (1, EB)):
                                for e_ in range(2):
                                    r_ = par * 2 + e_
                                    blk = (g * 2 + e_) * 128
                                    PE(accb[g][:, r_ * 65:(r_ + 1) * 65], E_[:, blk:blk + 128], VV[:, j, g, :],
                                       (j == j_lo and r_ == 0), (j == i and r_ == 3), E_.all() + VV.all(), accb[g].all(), skip_group_check=True)

                    scores(j_lo)
                    for j in range(j_lo, i + 1):
                        if j + 1 <= i:
                            scores(j + 1)
                        exp_pv(j)
                    mk_coef(kbr, accb, 65)
                    attn_out([Acc(accb[0], 65), Acc(accb[1], 65)], O, coef=coef, first=False, tmpb=otmp)
                o_to_T(O, Ob, obT, i, banks[2])
            self.pop()
            self.pop()

        NB = 4

        def m3(b, oaT, obT):
            self.push()
            mergedT = self.sb("mergedT", [128, 8, SEQ], BF16, nreg=4)
            wout = self.sb("wout", [128, 8, D], BF16, nreg=2)
            g1t = self.sb("g1t", [128, D], F32)
            s2t = self.sb("s2t", [128, D], F32)
            h2t = self.sb("h2t", [128, D], F32)
            load_mod(g1t, b, 2)
            load_mod(h2t, b, 3)
            load_mod(s2t, b, 4)
            if do_mixer:
                self.push()
                uT = self.sb("uTm", [128, 8, SEQ], BF16, nreg=4)
                wbra = self.sb("wbra", [128, 4, D], BF16)
                wbrb = self.sb("wbrb", [128, 4, D], BF16)
                build_uT(b, uT)
                wst_ = self.sb("m3_wst", [128, 4, D], F32)
                DMA("sp", wst_[:, :, :], wbrA_d.rearrange("(k p) n -> p k n", p=128), [], wst_.all())
                CP("pool", wbra[:, :, :], wst_[:, :, :], wst_.all(), wbra.all())
                DMA("sp", wst_[:, :, :], wbrB_d.rearrange("(k p) n -> p k n", p=128), [], wst_.all())
                CP("pool", wbrb[:, :, :], wst_[:, :, :], wst_.all(), wbrb.all())
                wov = wout_d.rearrange("(k p) n -> p k n", p=128)
                for hh in range(2):
                    DMA("sp", wst_[:, :, :], wov[:, hh * 4:(hh + 1) * 4, :], [], wst_.all())
                    CP("pool", wout[:, hh * 4:(hh + 1) * 4, :], wst_[:, :, :], wst_.all(), [wout.k(hh)])
                gst = [self.sb(f"m3_gst{i}", [128, 8, 128], F32) for i in range(2)]
                gwb = [self.sb(f"m3_gwb{i}", [128, 8, 128], BF16) for i in range(4)]
                sga = [self.sb(f"m3_sga{i}", [128, 512], F32) for i in range(2)]
                sgb = [self.sb(f"m3_sgb{i}", [128, 512], F32) for i in range(2)]
                nl = 0
                for fc in range(8):
                    gws = []
                    for which in range(2):
                        st_, wb_ = gst[nl % 2], gwb[nl % 4]
                        nl += 1
                        DMA("sp", st_[:, :, :], wmG_d[which * 8 + fc], [], st_.all())
                        CP("pool", wb_[:, :, :], st_[:, :, :], st_.all(), wb_.all())
                        gws.append(wb_)
                    fs = slice(fc * 128, (fc + 1) * 128)
                    for tc in range(4):
                        ts_ = slice(tc * 512, (tc + 1) * 512)
                        bya, byb, bga, bgb = banks[0], banks[1], banks[2], banks[3]
                        if tc % 2 == 1:
                            bya, byb, bga, bgb = banks[4], banks[5], banks[6], banks[7]
                        for k in range(4):
                            PE(bya[:, :], wbra[:, k, fs], oaT[:, k, ts_], k == 0, k == 3, wbra.all() + [oaT.k(tc)], bya.all())
                        for k in range(4):
                            PE(byb[:, :], wbrb[:, k, fs], obT[:, k, ts_], k == 0, k == 3, wbrb.all() + [obT.k(tc)], byb.all())
                        for k in range(8):
                            PE(bga[:, :], gws[0][:, k, :], uT[:, k, ts_], k == 0, k == 7, gws[0].all() + [uT.k(tc)], bga.all())
                        for k in range(8):
                            PE(bgb[:, :], gws[1][:, k, :], uT[:, k, ts_], k == 0, k == 7, gws[1].all() + [uT.k(tc)], bgb.all())
                        SA, SB = sga[tc % 2], sgb[tc % 2]
                        ACT(SA[:, :], bga[:, :], AF.Sigmoid, bga.all(), SA.all())
                        ACT(SB[:, :], bgb[:, :], AF.Sigmoid, bgb.all(), SB.all())
                        TT("dve", SA[:, :], SA[:, :], bya[:, :], ALU.mult, SA.all() + bya.all(), SA.all())
                        TT("dve", SB[:, :], SB[:, :], byb[:, :], ALU.mult, SB.all() + byb.all(), SB.all())
                        TT("pool", mergedT[:, fc, ts_], SA[:, :], SB[:, :], ALU.add, SA.all() + SB.all(), [mergedT.k(tc)])
                self.pop()
            if "mergedT" in dbg and b == 0:
                self.push()
                stg = [self.sb(f"dbg_stg{i}", [128, SEQ], F32) for i in range(2)]
                nd = 0
                for nm, srcb, nk in (("mergedT", mergedT, 8), ("oaT", oaT, 4), ("obT", obT, 4)):
                    for k in range(nk):
                        sg_ = stg[nd % 2]
                        nd += 1
                        CP("dve", sg_[:, :], srcb[:, k, :], srcb.all(), sg_.all())
                        DMA("sp", dbg[nm][:, k, :], sg_[:, :], sg_.all(), [("dbg", nm, k)])
                self.pop()
            s2_bc = {b: s2t}
            h2_bc = {b: h2t}
            xt = [self.sb(f"xt{i}", [128, D], F32) for i in range(NB)]
            x1t = [self.sb(f"x1t{i}", [128, D], F32) for i in range(NB)]
            u2r = [self.sb(f"u2r{i}", [128, D], BF16) for i in range(NB)]
            u2T = [self.sb(f"u2T{i}", [128, 8, 128], BF16) for i in range(NB)]
            lnb = [(self.sb(f"ln_st{i}", [128, 2, 6], F32), self.sb(f"ln_mv{i}", [128, 2], F32),
                    self.sb(f"ln_rs{i}", [128, 1], F32)) for i in range(NB)]
            sc = [self.sb(f"r_sc{i}", [128, NE], F32) for i in range(NB)]
            ch = [self.sb(f"r_ch{i}", [128, NE], F32) for i in range(NB)]
            tmp = [self.sb(f"r_tmp{i}", [128, NE], F32) for i in range(NB)]
            selb = [self.sb(f"r_selb{i}", [128, NE], BF16) for i in range(NB)]
            Gm = [self.sb(f"r_G{i}", [128, NE], F32) for i in range(NB)]
            Dm = [self.sb(f"r_D{i}", [128, NE], F32) for i in range(NB)]
            sm = [self.sb(f"r_sm{i}", [128, 64], F32) for i in range(NB)]
            mixs = [self.sb(f"mixs{i}", [128, D], F32) for i in range(NB)]

            def phase1_tile(t):
                i = t % NB
                tt = t % TPS
                X, X1, U2, U2T, LB = xt[i], x1t[i], u2r[i], u2T[i], lnb[i]
                SC, CH, TMP, SELB, G, DD, SM = sc[i], ch[i], tmp[i], selb[i], Gm[i], Dm[i], sm[i]
                MX = mixs[i]
                rows = slice(t * 128, (t + 1) * 128)
                DMA("sp", X[:, :], x_d[rows, :], [], X.all())
                if do_mixer:
                    for hf in range(2):
                        bk = banks[hf]
                        with S.atomic():
                            for k in range(8):
                                PE(bk[:, :], mergedT[:, k, tt * 128:(tt + 1) * 128], wout[:, k, hf * 512:(hf + 1) * 512], k == 0, k == 7,
                                   [mergedT.k(tt // 4)] + wout.all(), bk.all())
                            TT("dve", MX[:, hf * 512:(hf + 1) * 512], bk[:, :], g1t[:, hf * 512:(hf + 1) * 512], ALU.mult, bk.all() + g1t.all(), MX.all())
                    if "mix" in dbg and b == 0:
                        DMA("sp", dbg["mix"][tt * 128:(tt + 1) * 128, :], MX[:, :], MX.all(), [("dbg", "mix", tt)])
                    STT(X[:, :], X[:, :], float(DN_ALPHA), MX[:, :], ALU.mult, ALU.add, X.all() + MX.all(), X.all())
                else:
                    ACT(X[:, :], X[:, :], AF.Copy, X.all(), X.all(), scale=float(DN_ALPHA))
                layernorm(X, X1, ln1g, ln1b, LB)
                DMA("sp", x1_d[rows, :], X1[:, :], X1.all(), [("x1_d", t)])
                op("dve", lambda e, X=X, X1=X1, b=b: e.tensor_tensor(X[:, :], X1[:, :], s2_bc[b][:, :], ALU.mult),
                   r=X1.all() + s2_bc[b].all(), w=X.all())
                op("dve", lambda e, X=X, U2=U2, b=b: e.tensor_tensor(U2[:, :], X[:, :], h2_bc[b][:, :], ALU.add),
                   r=X.all() + h2_bc[b].all(), w=U2.all())
                bkT = banks[4 + (t % 2)]
                pT = bkT.t[:, :].bitcast(BF16)
                with S.atomic():
                    for k in range(8):
                        op("pe", lambda e, k=k, U2=U2, pT=pT: e.transpose(pT[:, k * 128:(k + 1) * 128], U2[:, k * 128:(k + 1) * 128], ident_b[:, :]),
                           r=U2.all() + ident_b.all(), w=bkT.all())
                    op("act", lambda e, U2T=U2T, pT=pT: e.activation(U2T[:, :, :], pT.rearrange("p (k t) -> p k t", k=8), AF.Copy),
                       r=bkT.all(), w=U2T.all())
                bkR = banks[6 + (t % 2)]
                with S.atomic():
                    for k in range(8):
                        op("pe", lambda e, k=k, U2T=U2T, bkR=bkR: e.matmul(bkR[:, 0:NE], U2T[:, k, :], wr_b[:, k, :], start=(k == 0), stop=(k == 7)),
                           r=U2T.all() + wr_b.all(), w=bkR.all())
                    op("act", lambda e, SC=SC, bkR=bkR: e.activation(SC[:, :], bkR[:, 0:NE], AF.Sigmoid), r=bkR.all(), w=SC.all())
                op("dve", lambda e, SC=SC, CH=CH: e.tensor_tensor(CH[:, :], SC[:, :], rb_bc[:, :], ALU.add), r=SC.all() + rb_bc.all(), w=CH.all())
                ch3 = CH[:, :].rearrange("p (g j) -> p g j", g=8)
                tmp3 = TMP[:, :].rearrange("p (g j) -> p g j", g=8)
                m1, m2, gs, top4, keep, pen, top8, wsum, rw = (SM[:, 0:8], SM[:, 8:16], SM[:, 16:24], SM[:, 24:32], SM[:, 32:40],
                                                               SM[:, 40:48], SM[:, 48:56], SM[:, 56:57], SM[:, 57:58])
                op("dve", lambda e: e.tensor_reduce(m1, ch3, AX.X, ALU.max), r=CH.all(), w=SM.all())
                op("dve", lambda e: e.tensor_tensor(tmp3, ch3, m1.unsqueeze(2).to_broadcast([128, 8, 32]), ALU.is_equal),
                   r=CH.all() + SM.all(), w=TMP.all())
                op("dve", lambda e, TMP=TMP, CH=CH: e.scalar_tensor_tensor(TMP[:, :], TMP[:, :], -1e9, CH[:, :], ALU.mult, ALU.add),
                   r=TMP.all() + CH.all(), w=TMP.all())
                op("dve", lambda e: e.tensor_reduce(m2, tmp3, AX.X, ALU.max), r=TMP.all(), w=SM.all())
                op("dve", lambda e: e.tensor_tensor(gs, m1, m2, ALU.add), r=SM.all(), w=SM.all())
                op("dve", lambda e: e.max(top4, gs), r=SM.all(), w=SM.all())
                op("dve", lambda e: e.tensor_scalar(keep, gs, top4[:, 3:4], None, ALU.is_ge), r=SM.all(), w=SM.all())
                op("dve", lambda e: e.tensor_scalar(pen, keep, -1.0, 1e9, ALU.add, ALU.mult), r=SM.all(), w=SM.all())
                op("dve", lambda e: e.tensor_tensor(ch3, ch3, pen.unsqueeze(2).to_broadcast([128, 8, 32]), ALU.add),
                   r=CH.all() + SM.all(), w=CH.all())
                op("dve", lambda e, CH=CH: e.max(top8, CH[:, :]), r=CH.all(), w=SM.all())
                op("dve", lambda e, TMP=TMP, CH=CH: e.tensor_scalar(TMP[:, :], CH[:, :], top8[:, 7:8], None, ALU.is_ge),
                   r=CH.all() + SM.all(), w=TMP.all())
                op("pool", lambda e, TMP=TMP, SELB=SELB: e.tensor_copy(SELB[:, :], TMP[:, :]), r=TMP.all(), w=SELB.all())
                op("dve", lambda e, G=G, TMP=TMP, SC=SC: e.scalar_tensor_tensor(G[:, :], TMP[:, :], 1.0, SC[:, :], ALU.mult, ALU.mult, accum_out=wsum),
                   r=TMP.all() + SC.all(), w=G.all() + SM.all())
                op("dve", lambda e: e.reciprocal(rw, wsum), r=SM.all(), w=SM.all())
                op("dve", lambda e, G=G: e.tensor_scalar(G[:, :], G[:, :], rw, float(ROUTED_SCALE), ALU.mult, ALU.mult),
                   r=G.all() + SM.all(), w=G.all())
                S.cut()
                bkP = banks[2 + (t % 2)]
                op("pe", lambda e, bkP=bkP, SELB=SELB: e.matmul(bkP[:, 0:NE], tri_b[:, :], SELB[:, :], start=True, stop=True),
                   r=tri_b.all() + SELB.all(), w=bkP.all())
                op("pe", lambda e, bkP=bkP, SELB=SELB: e.matmul(bkP[:, NE:2 * NE], ones_b[:, :], SELB[:, :], start=False, stop=True),
                   r=ones_b.all() + SELB.all(), w=bkP.all())
                op("dve", lambda e, DD=DD, bkP=bkP: e.tensor_tensor(DD[:, :], bkP[:, 0:NE], carry[:, :], ALU.add),
                   r=bkP.all() + carry.all(), w=DD.all())
                op("dve", lambda e, bkP=bkP: e.tensor_tensor(carry[:, :], carry[:, :], bkP[:, NE:2 * NE], ALU.add),
                   r=bkP.all() + carry.all(), w=carry.all())
                S.cut()
                op("dve", lambda e, DD=DD: e.tensor_tensor(DD[:, :], DD[:, :], iotaC[:, :], ALU.add), r=DD.all() + iotaC.all(), w=DD.all())
                for k in range(8):
                    op("dve", lambda e, k=k, TMP=TMP, CH=CH, DD=DD, t=t: e.scalar_tensor_tensor(
                        TMP[:, :], CH[:, :], top8[:, k:k + 1], DD[:, :], ALU.is_equal, ALU.mult, accum_out=dest_f[:, t, k:k + 1]),
                       r=CH.all() + SM.all() + DD.all(), w=TMP.all() + [dest_f.k(t)])
                    op("dve", lambda e, k=k, TMP=TMP, CH=CH, G=G, t=t: e.scalar_tensor_tensor(
                        TMP[:, :], CH[:, :], top8[:, k:k + 1], G[:, :], ALU.is_equal, ALU.mult, accum_out=wk[:, t, k:k + 1]),
                       r=CH.all() + SM.all() + G.all(), w=TMP.all() + [wk.k(t)])
                DMA("sp", u2_d[rows, :], U2[:, :], U2.all(), [("u2_d", t)])

            for t0 in range(b * TPS, min((b + 1) * TPS, n_tiles), NB):
                grp = [t for t in range(t0, min(t0 + NB, n_tiles, (b + 1) * TPS))]
                parts = [S.record_parts(lambda t=t: phase1_tile(t)) for t in grp]
                S.merge_rr([p[0] for p in parts])
                S.merge_seq([p[1] for p in parts])
                S.merge_rr([p[2] for p in parts])
            self.pop()

        for b in range(BPC):
            if b * TPS >= n_tiles:
                break
            self.push()
            oaT = self.sb("oaT", [128, 4, SEQ], BF16, nreg=4)
            obT = self.sb("obT", [128, 4, SEQ], BF16, nreg=4)
            if do_mixer:
                dsa(b, oaT)
                nsa(b, obT)
            m3(b, oaT, obT)
            self.pop()

        if "dest" in dbg:
            DMA("sp", dbg["dest"], dest_f[:, :, :], dest_f.all(), [("dbg", 0)])
        if "wk" in dbg:
            DMA("sp", dbg["wk"], wk[:, :, :], wk.all(), [("dbg", 1)])

        self.push()
        nb_f = self.sb("nb_f", [128, NE], F32)
        nb_t = self.sb("nb_t", [128, NE], F32)
        nb_i = self.sb("nb_i", [128, NE], I32)
        nb_b = self.sb("nb_b", [128, NE], BF16)
        nbT = self.sb("nbT", [128, 2, 128], BF16)
        startr = self.sb("startr", [128, NE], F32)
        endb = self.sb("endb", [128, NE], F32)
        blkE = self.sb("blkE", [128, 4], F32)
        Rm = self.sb("Rm", [128, NBLK // 128, 128], BF16)
        widx_f = self.sb("widx_f", [128, NBLK], F32)
        widx_i = self.sb("widx_i", [128, NBLK], I32)
        TS("dve", nb_f[:, :], carry[:, :], float(BLK - 1), 1.0 / BLK, ALU.add, ALU.mult, carry.all(), nb_f.all())
        CP("dve", nb_i[:, :], nb_f[:, :], nb_f.all(), nb_i.all())
        CP("dve", nb_t[:, :], nb_i[:, :], nb_i.all(), nb_t.all())
        TT("dve", nb_f[:, :], nb_t[:, :], nb_f[:, :], ALU.is_gt, nb_t.all() + nb_f.all(), nb_f.all())
        TT("dve", nb_f[:, :], nb_t[:, :], nb_f[:, :], ALU.subtract, nb_t.all() + nb_f.all(), nb_f.all())
        CP("dve", nb_b[:, :], nb_f[:, :], nb_f.all(), nb_b.all())
        bk = banks[0]
        pT = bfv(bk)
        for a_ in range(2):
            TR(pT[:, a_ * 128:(a_ + 1) * 128], nb_b[:, a_ * 128:(a_ + 1) * 128], ident_b[:, :], nb_b.all() + ident_b.all(), bk.all())
        CP("act", nbT[:, :, :], pT[:, 0:256].rearrange("p (a t) -> p a t", a=2), bk.all(), nbT.all())
        bk = banks[1]
        for a_ in range(2):
            PE(bk[:, 0:NE], nbT[:, a_, :], Utri[:, a_, :], a_ == 0, a_ == 1, nbT.all() + Utri.all(), bk.all())
        TS("dve", startr[:, :], bk[:, 0:NE], float(BLK), None, ALU.mult, ALU.bypass, bk.all(), startr.all())
        TT("dve", endb[:, :], bk[:, 0:NE], nb_f[:, :], ALU.add, bk.all() + nb_f.all(), endb.all())
        for a_ in range(NBLK // 128):
            TS("dve", nb_t[:, :], endb[:, :], jcol[:, a_:a_ + 1], None, ALU.is_le, ALU.add, endb.all() + jcol.all(), nb_t.all() + blkE.all(),
               accum=blkE[:, a_:a_ + 1])
        for a_ in range(NBLK // 128):
            TS("dve", Rm[:, a_, :], ident_b[:, :], blkE[:, a_:a_ + 1], None, ALU.mult, ALU.bypass, ident_b.all() + blkE.all(), Rm.all())
        bk = banks[2]
        PE(bk[:, 0:NBLK], ones_b[:, :], Rm[:, :, :].rearrange("p a t -> p (a t)"), True, True, ones_b.all() + Rm.all(), bk.all())
        TS("dve", widx_f[:, :], bk[:, 0:NBLK], 128.0, pcol[:, 0:1], ALU.mult, ALU.add, bk.all() + pcol.all(), widx_f.all())
        CP("dve", widx_i[:, :], widx_f[:, :], widx_f.all(), widx_i.all())
        if "blkE" in dbg:
            DMA("sp", dbg["blkE"], blkE[:, :], blkE.all(), [("dbg", "blkE")])
            DMA("sp", dbg["startr"], startr[:, :], startr.all(), [("dbg", "startr")])

        self.push()
        U2s = [self.sb(f"p2_u{i}", [128, D], BF16) for i in range(4)]
        p2 = [self.sb(f"p2_s{i}", [128, 40], F32) for i in range(4)]
        p2i = [self.sb(f"p2_i{i}", [128, 8], I32) for i in range(4)]
        p2j = [self.sb(f"p2_j{i}", [128, NE], F32) for i in range(4)]
        def pass2_tile(t):
            U2 = U2s[t % 4]
            P2, P2I, P2J = p2[t % 4], p2i[t % 4], p2j[t % 4]
            ef, et, ps_, sl = P2[:, 0:8], P2[:, 8:16], P2[:, 16:24], P2[:, 24:32]
            rows = slice(t * 128, (t + 1) * 128)
            DMA("sp", U2[:, :], u2_d[rows, :], [("u2_d", t)], U2.all())
            TS("dve", ef, dest_f[:, t, :], 1.0 / 4096.0, None, ALU.mult, ALU.bypass, [dest_f.k(t)], P2.all())
            CP("dve", P2I[:, :], ef, P2.all(), P2I.all())
            CP("dve", et, P2I[:, :], P2I.all(), P2.all())
            TT("dve", ps_, et, ef, ALU.is_gt, P2.all(), P2.all())
            TT("dve", et, et, ps_, ALU.subtract, P2.all(), P2.all())
            STT(ps_, et, -4096.0, dest_f[:, t, :], ALU.mult, ALU.add, P2.all() + [dest_f.k(t)], P2.all())
            for k in range(8):
                STT(P2J[:, :], iotaE1[:, :], et[:, k:k + 1], startr[:, :], ALU.is_equal, ALU.mult, iotaE1.all() + P2.all() + startr.all(),
                    P2J.all() + P2.all(), accum=sl[:, k:k + 1])
            TT("dve", sl, sl, ps_, ALU.add, P2.all(), P2.all())
            CP("dve", dest_i[:, t, :], sl, P2.all(), [dest_i.k(t)])
            for k in range(8):
                op("pool", lambda e, k=k, t=t, U2=U2: e.indirect_dma_start(
                    out=xs_d[:, :], out_offset=IOA(ap=dest_i[:, t, k:k + 1], axis=0), in_=U2[:, :], in_offset=None,
                    bounds_check=bc_reg(e), oob_is_err=False),
                   r=U2.all() + [dest_i.k(t)], w=[("xs_d", "all")], dma=True)
        for t0 in range(0, n_tiles, 4):
            grp = list(range(t0, min(t0 + 4, n_tiles)))
            parts = [S.record_parts(lambda t=t: pass2_tile(t)) for t in grp]
            S.merge_rr([p[0] for p in parts])
        self.pop()
        if "desti" in dbg:
            self.push()
            dtmp = self.sb("dtmp", [128, NT, 8], F32)
            CP("dve", dtmp[:, :, :], dest_i[:, :, :], dest_i.all(), dtmp.all())
            DMA("sp", dbg["desti"], dtmp[:, :, :], dtmp.all(), [("dbg", "desti")])
            self.pop()

        n_blk = cfg.get("n_blk", NBLK)

        wgv = wg_d.rearrange("e (p k) n -> (e p) (k n)", k=8)
        wuv = wu_d.rearrange("e (p k) n -> (e p) (k n)", k=8)
        wdv = wd_d.rearrange("e (p h) n -> (e p) (h n)", h=2)

        def wreg(e):
            if "wb" not in _regs:
                _regs["wb"] = e.to_reg(NE * 128 - 1)
            return _regs["wb"]

        NWB = 3
        w_st = [self.sb(f"w_st{i}", [128, 6144], F32, nreg=3) for i in range(NWB)]
        w_bf = [self.sb(f"w_bf{i}", [128, 6144], BF16, nreg=3) for i in range(NWB)]
        hsel = [self.sb(f"hsel{i}", [128, D], BF16) for i in range(3)]
        hselT = [self.sb(f"hselT{i}", [128, 8, 128], BF16) for i in range(2)]
        sg = [self.sb(f"sg{i}", [128, 256], F32) for i in range(2)]
        actk = [self.sb(f"actk{i}", [128, 256], BF16) for i in range(2)]
        actT = [self.sb(f"actT{i}", [128, 2, 128], BF16) for i in range(2)]
        ysb = [self.sb(f"ysb{i}", [128, D], BF16, nreg=2) for i in range(3)]

        hselT3 = [self.sb(f"hselT3_{i}", [128, 8, 128], BF16) for i in range(3)]
        w_bf4 = w_bf + [self.sb("w_bf3", [128, 6144], BF16, nreg=3)]
        NWF = len(w_bf4)

        def stageA0(j):
            WS, HS = w_st[j % NWB], hsel[j % 3]
            for q, wv in enumerate((wgv, wuv, wdv)):
                op("pool", lambda e, q=q, wv=wv: e.indirect_dma_start(
                    out=WS[:, q * 2048:(q + 1) * 2048], out_offset=None, in_=wv, in_offset=IOA(ap=widx_i[:, j:j + 1], axis=0),
                    bounds_check=wreg(e), oob_is_err=False),
                   r=widx_i.all(), w=[WS.k(q)], dma=True)
            DMA("sp", HS[:, :], xs_d[j * BLK:(j + 1) * BLK, :], [("xs_d", "all")], HS.all())

        def stageA(j):
            WS, WB, HS, HT = w_st[j % NWB], w_bf4[j % NWF], hsel[j % 3], hselT3[j % 3]
            WBgu = WB[:, 0:4096].rearrange("p (k two n) -> p k two n", k=8, two=2)
            CP("act", WBgu[:, :, 0, :], WS[:, 0:2048].rearrange("p (k n) -> p k n", k=8), [WS.k(0)], [WB.k(0)])
            CP("dve", WBgu[:, :, 1, :], WS[:, 2048:4096].rearrange("p (k n) -> p k n", k=8), [WS.k(1)], [WB.k(1)])
            CP("dve", WB[:, 4096:6144], WS[:, 4096:6144], [WS.k(2)], [WB.k(2)])
            bkT = banks[j % 2]
            pT_ = bfv(bkT)
            with S.atomic():
                for k in range(8):
                    TR(pT_[:, k * 128:(k + 1) * 128], HS[:, k:D:8], ident_b[:, :], HS.all() + ident_b.all(), bkT.all())
                CP("act", HT[:, :, :], pT_.rearrange("p (k t) -> p k t", k=8), bkT.all(), HT.all())

        def stageB1(j):
            i = j % 2
            WB, HT, SG, AK = w_bf4[j % NWF], hselT3[j % 3], sg[i], actk[i]
            bk_ = banks[2 + j % 2]
            with S.atomic():
                for k in range(8):
                    PE(bk_[:, :], HT[:, k, :], WB[:, k * 512:(k + 1) * 512], k == 0, k == 7, HT.all() + [WB.k(0), WB.k(1)], bk_.all())
                ACT(SG[:, :], bk_[:, 0:256], AF.Silu, bk_.all(), SG.all())
                TT("dve", AK[:, :], SG[:, :], bk_[:, 256:512], ALU.mult, SG.all() + bk_.all(), AK.all())

        def stageB2(j):
            i = j % 2
            AK, AT = actk[i], actT[i]
            bk2 = banks[4 + j % 2]
            pT2 = bfv(bk2)
            with S.atomic():
                for h in range(2):
                    TR(pT2[:, h * 128:(h + 1) * 128], AK[:, h:256:2], ident_b[:, :], AK.all() + ident_b.all(), bk2.all())
                CP("act", AT[:, :, :], pT2[:, 0:256].rearrange("p (h t) -> p h t", h=2), bk2.all(), AT.all())

        def stageB3(j):
            i = j % 2
            WB, AT, YS = w_bf4[j % NWF], actT[i], ysb[j % 3]
            for hf in range(2):
                bkd = banks[6 + hf]
                with S.atomic():
                    for h in range(2):
                        col = 4096 + h * 1024 + hf * 512
                        PE(bkd[:, :], AT[:, h, :], WB[:, col:col + 512], h == 0, h == 1, AT.all() + [WB.k(2)], bkd.all())
                    CP("act" if hf == 0 else "dve", YS[:, hf * 512:(hf + 1) * 512], bkd[:, :], bkd.all(), [YS.k(hf)])
            DMA("sp", ys_d[j * BLK:(j + 1) * BLK, :], YS[:, :], YS.all(), [("ys_d", j)])

        stages = [(0, stageA0), (2, stageA), (3, stageB1), (4, stageB2), (5, stageB3)]
        for s_ in range(n_blk + 5):
            streams = []
            for off, st_fn in stages:
                j = s_ - off
                if 0 <= j < n_blk:
                    streams.append(S.record_parts(lambda j=j, st_fn=st_fn: st_fn(j))[0])
            S.merge_rr(list(reversed(streams)))
        ys_keys = [("ys_d", j) for j in range(n_blk)]
        self.pop()
        self.push()
        ln2g = self.sb("ln2g", [128, D], F32)
        ln2b = self.sb("ln2b", [128, D], F32)
        DMA("sp", ln2g[:, :], ln_d[2:3, :].to_broadcast([128, D]), [], ln2g.all())
        DMA("sp", ln2b[:, :], ln_d[3:4, :].to_broadcast([128, D]), [], ln2b.all())
        s2_bc, h2_bc, g2_bc = {}, {}, {}
        for b in range(BPC):
            g2_bc[b] = self.sb(f"f_g2{b}", [128, D], F32)
            load_mod(g2_bc[b], b, 5)
        GF = 4
        wsh = self.sb("wsh", [128, 6144], BF16, nreg=3)
        self.push()
        wsh_st = self.sb("wsh_st", [128, 6144], F32, nreg=3)
        DMA("sp", wsh_st[:, 0:2048].rearrange("p (k n) -> p k n", k=8), wsg_d.rearrange("(k p) n -> p k n", p=128), [], [wsh_st.k(0)])
        DMA("sp", wsh_st[:, 2048:4096].rearrange("p (k n) -> p k n", k=8), wsu_d.rearrange("(k p) n -> p k n", p=128), [], [wsh_st.k(1)])
        DMA("sp", wsh_st[:, 4096:6144].rearrange("p (k n) -> p k n", k=2), wsd_d.rearrange("(k p) n -> p k n", p=128), [], [wsh_st.k(2)])
        CP("act", wsh[:, 0:2048], wsh_st[:, 0:2048], [wsh_st.k(0)], [wsh.k(0)])
        CP("dve", wsh[:, 2048:4096], wsh_st[:, 2048:4096], [wsh_st.k(1)], [wsh.k(1)])
        CP("pool", wsh[:, 4096:6144], wsh_st[:, 4096:6144], [wsh_st.k(2)], [wsh.k(2)])
        self.pop()
        xt = [self.sb(f"fxt{i}", [128, D], F32) for i in range(GF)]
        x1t = [self.sb(f"fx1t{i}", [128, D], F32) for i in range(GF)]
        u2r = [self.sb(f"fu2r{i}", [128, D], BF16) for i in range(GF)]
        u2T = [self.sb(f"fu2T{i}", [128, 8, 128], BF16) for i in range(GF)]
        lnb = [(self.sb(f"fln_st{i}", [128, 2, 6], F32), self.sb(f"fln_mv{i}", [128, 2], F32),
                self.sb(f"fln_rs{i}", [128, 1], F32)) for i in range(GF)]
        yg = [self.sb(f"yg{i}", [128, 8, D], BF16, nreg=8) for i in range(GF)]
        acc = [self.sb(f"acc{i}", [128, D], F32) for i in range(GF)]
        sgs = [self.sb(f"sgs{i}", [128, 256], F32) for i in range(GF)]
        atk = [self.sb(f"atk{i}", [128, 256], BF16) for i in range(GF)]
        ats = [self.sb(f"ats{i}", [128, 2, 128], BF16) for i in range(GF)]

        def final_tile(t):
            b = t // TPS
            i = t % GF
            X, X1, U2, U2T, LB = xt[i], x1t[i], u2r[i], u2T[i], lnb[i]
            YG, ACC, SGS, ATK, ATS = yg[i], acc[i], sgs[i], atk[i], ats[i]
            rows = slice(t * 128, (t + 1) * 128)
            DMA("sp", X1[:, :], x1_d[rows, :], [("x1_d", t)], X1.all())
            DMA("sp", U2[:, :], u2_d[rows, :], [("u2_d", t)], U2.all())
            for k in range(8):
                op("pool", lambda e, k=k: e.indirect_dma_start(
                    out=YG[:, k, :], out_offset=None, in_=ys_d[:, :], in_offset=IOA(ap=dest_i[:, t, k:k + 1], axis=0),
                    bounds_check=bc_reg(e), oob_is_err=False),
                   r=ys_keys + [dest_i.k(t)], w=[YG.k(k)], dma=True)
            bkT = banks[t % 2]
            pT = bfv(bkT)
            with S.atomic():
                for k in range(8):
                    TR(pT[:, k * 128:(k + 1) * 128], U2[:, k * 128:(k + 1) * 128], ident_b[:, :], U2.all() + ident_b.all(), bkT.all())
                CP("act", U2T[:, :, :], pT.rearrange("p (k t) -> p k t", k=8), bkT.all(), U2T.all())
            bk = banks[2 + (t % 2)]
            with S.atomic():
                for k in range(8):
                    PE(bk[:, 0:256], U2T[:, k, :], wsh[:, k * 256:(k + 1) * 256], k == 0, False, U2T.all() + [wsh.k(0)], bk.all(), skip_group_check=True)
                for k in range(8):
                    PE(bk[:, 256:512], U2T[:, k, :], wsh[:, 2048 + k * 256:2048 + (k + 1) * 256], False, k == 7, U2T.all() + [wsh.k(1)], bk.all(), skip_group_check=True)
                ACT(SGS[:, :], bk[:, 0:256], AF.Silu, bk.all(), SGS.all())
                TT("dve", ATK[:, :], SGS[:, :], bk[:, 256:512], ALU.mult, SGS.all() + bk.all(), ATK.all())
            bk2 = banks[4 + (t % 2)]
            pT2 = bfv(bk2)
            with S.atomic():
                for h in range(2):
                    TR(pT2[:, h * 128:(h + 1) * 128], ATK[:, h * 128:(h + 1) * 128], ident_b[:, :], ATK.all() + ident_b.all(), bk2.all())
                CP("act", ATS[:, :, :], pT2[:, 0:256].rearrange("p (h t) -> p h t", h=2), bk2.all(), ATS.all())
            for hf in range(2):
                bkd = banks[6 + hf]
                with S.atomic():
                    for h in range(2):
                        col = 4096 + h * 1024 + hf * 512
                        PE(bkd[:, :], ATS[:, h, :], wsh[:, col:col + 512], h == 0, h == 1, ATS.all() + [wsh.k(2)], bkd.all())
                    CP("act", ACC[:, hf * 512:(hf + 1) * 512], bkd[:, :], bkd.all(), ACC.all())
            for k in range(8):
                STT(ACC[:, :], YG[:, k, :], wk[:, t, k:k + 1], ACC[:, :], ALU.mult, ALU.add, [YG.k(k), wk.k(t)] + ACC.all(), ACC.all())
            TT("pool", ACC[:, :], ACC[:, :], g2_bc[b][:, :], ALU.mult, ACC.all() + g2_bc[b].all(), ACC.all())
            STT(ACC[:, :], X1[:, :], float(DN_ALPHA), ACC[:, :], ALU.mult, ALU.add, ACC.all() + X1.all(), ACC.all())
            layernorm(ACC, X, ln2g, ln2b, LB)
            DMA("sp", out_d[rows, :], X[:, :], X.all(), [("out_d", t)])

        for t0 in range(0, n_tiles, GF):
            grp = list(range(t0, min(t0 + GF, n_tiles)))
            parts = [S.record_parts(lambda t=t: final_tile(t)) for t in grp]
            S.merge_rr([p[0] for p in parts])
        S.finalize_and_emit()
        return nc


_OFF = dict(q_a=0, k_a=512, v_a=640, q_idx=768, k_idx=1024, w_idx=1056, q_b=1064, k_cmp=1576, v_cmp=1704,
            k_sel=1832, v_sel=1960, k_win=2088, v_win=2216, g_nsa=2344, gate_a=2368, gate_b=3392)


def _sw64(d):
    return d + 8 if d < 8 else (d - 8 if d < 16 else d)


def _sw32(d):
    return d + 4 if d < 4 else (d - 4 if d < 8 else d)


def _chunks_A():
    O = _OFF
    ch = []
    for c in range(4):
        ch.append([O["q_a"] + c * 128 + p for p in range(128)])
    for c in range(4):
        ch.append([O["q_a"] + c * 128 + (p // 64) * 64 + _sw64(p % 64) for p in range(128)])
    for g in range(2):
        ch.append([O["k_a"] + g * 64 + (p % 64) for p in range(128)])
    for g in range(2):
        ch.append([O["k_a"] + g * 64 + _sw64(p % 64) for p in range(128)])
    for sw in (False, True):
        for c in range(3):
            cols = []
            for p in range(128):
                h = min(7, 3 * c + min(p, 95) // 32)
                d = p % 32
                cols.append(O["q_idx"] + h * 32 + (_sw32(d) if sw else d))
            ch.append(cols)
    ch.append([O["k_idx"] + (p % 32) for p in range(128)])
    ch.append([O["k_idx"] + _sw32(p % 32) for p in range(128)])
    return ch


def _chunks_B():
    O = _OFF
    ch = []
    for c in range(4):
        ch.append([O["q_b"] + c * 128 + p for p in range(128)])
    for c in range(4):
        ch.append([O["q_b"] + c * 128 + (p // 64) * 64 + _sw64(p % 64) for p in range(128)])
    ch.append([O["k_cmp"] + p for p in range(128)])
    ch.append([O["v_cmp"] + p for p in range(128)])
    for nm in ("k_sel", "k_win"):
        for g in range(2):
            ch.append([O[nm] + g * 64 + (p % 64) for p in range(128)])
        for g in range(2):
            ch.append([O[nm] + g * 64 + _sw64(p % 64) for p in range(128)])
    return ch


N_CH_A = 20
N_CH_B = 18


def _const_tables():
    theta = 500000.0
    cst = np.zeros((128, 8), np.float32)
    inv64 = (np.float32(theta) ** (-(np.arange(8, dtype=np.float32) * np.float32(2.0)) / np.float32(16))).astype(np.float32)
    inv32 = (np.float32(theta) ** (-(np.arange(4, dtype=np.float32) * np.float32(2.0)) / np.float32(8))).astype(np.float32)
    for p in range(128):
        d = p % 64
        if d < 16:
            cst[p, 0] = inv64[d % 8]
            cst[p, 1] = -1.0 if d < 8 else 1.0
        d2 = p % 32
        if p < 96 and d2 < 8:
            cst[p, 2] = inv32[d2 % 4]
            cst[p, 3] = -1.0 if d2 < 4 else 1.0
    selAB = np.zeros((128, 2, 16, 32), np.float32)
    for i in range(16):
        for tl in range(128):
            t = i * 128 + tl
            cur = t // 64
            for j in range(32):
                forced = (j == 0) or (0 <= cur - j < 2)
                causal = j * 64 <= t
                if forced:
                    selAB[tl, 0, i, j] = 0.0
                    selAB[tl, 1, i, j] = 1e9
                elif causal:
                    selAB[tl, 0, i, j] = 1.0
                else:
                    selAB[tl, 1, i, j] = -1e30
    expall = np.zeros((32, 16, 128), np.float32)
    for jj in range(16):
        for s in range(128):
            expall[2 * jj + s // 64, jj, s] = 1.0
    ov = np.zeros((128, 32), np.float32)
    for c in range(127):
        cs = 16 * c
        for j in range(32):
            ss = 64 * j
            if cs <= ss + 63 and cs + 31 >= ss:
                ov[c, j] = 1.0
    return cst, selAB, expall, ov


def _prep_inputs(inputs):
    f32 = lambda a: np.ascontiguousarray(np.asarray(a, dtype=np.float32))
    x = f32(inputs["x"])
    c = f32(inputs["c"])
    pos = np.ascontiguousarray(np.asarray(inputs["positions"], dtype=np.int32))
    w_in = f32(inputs["w_in"][0])
    w3 = w_in.reshape(8, 128, w_in.shape[1])

    def gather(chunks):
        return np.ascontiguousarray(np.stack([w3[:, :, cols].transpose(1, 0, 2) for cols in chunks]))

    O = _OFF
    tokA = list(range(O["v_a"], O["v_a"] + 128)) + list(range(O["w_idx"], O["w_idx"] + 8))
    tokB = list(range(O["v_sel"], O["v_sel"] + 128)) + list(range(O["v_win"], O["v_win"] + 128)) + list(range(O["g_nsa"], O["g_nsa"] + 24))
    gch = [[O["gate_a"] + cc * 128 + p for p in range(128)] for cc in range(8)] + \
          [[O["gate_b"] + cc * 128 + p for p in range(128)] for cc in range(8)]
    cst, selAB, expall, ov = _const_tables()
    cpos = np.stack([f32(inputs["cmp_pos_k"][0]).T, f32(inputs["cmp_pos_v"][0]).T], axis=1)
    cposT = np.ascontiguousarray(np.concatenate([cpos, cpos], axis=0))
    shared = {
        "w_ada": f32(inputs["w_ada"][0]),
        "b_ada": f32(inputs["b_ada"][0]).reshape(1, -1),
        "ln": f32(np.stack([inputs["ln1_g"][0], inputs["ln1_b"][0], inputs["ln2_g"][0], inputs["ln2_b"][0]])),
        "w_router": f32(inputs["w_router"][0]),
        "router_bias": f32(inputs["router_bias"][0]).reshape(1, -1),
        "w_exp_gate": f32(inputs["w_exp_gate"][0]),
        "w_exp_up": f32(inputs["w_exp_up"][0]),
        "w_exp_down": f32(inputs["w_exp_down"][0]),
        "w_sh_gate": f32(inputs["w_sh_gate"][0]),
        "w_sh_up": f32(inputs["w_sh_up"][0]),
        "w_sh_down": f32(inputs["w_sh_down"][0]),
        "wmA": gather(_chunks_A()),
        "wmB": gather(_chunks_B()),
        "wmG": gather(gch),
        "wtA": np.ascontiguousarray(w3[:, :, tokA].transpose(1, 0, 2)),
        "wtB": np.ascontiguousarray(w3[:, :, tokB].transpose(1, 0, 2)),
        "w_br_a": f32(inputs["w_br_a"][0]),
        "w_br_b": f32(inputs["w_br_b"][0]),
        "w_out": f32(inputs["w_out"][0]),
        "cmp_k_w1": f32(inputs["cmp_k_w1"][0]),
        "cmp_v_w1": f32(inputs["cmp_v_w1"][0]),
        "cmp_k_w2": f32(inputs["cmp_k_w2"][0]),
        "cmp_v_w2": f32(inputs["cmp_v_w2"][0]),
        "cmp_posT": cposT,
        "cst": cst,
        "selAB": selAB,
        "expall": expall,
        "ovl": ov,
    }
    in_maps = []
    for i in range(N_CORES):
        m = dict(shared)
        m["x"] = np.ascontiguousarray(x[BPC * i:BPC * (i + 1)].reshape(TOK, D))
        cc = c[BPC * i:BPC * (i + 1)]
        m["cT"] = np.ascontiguousarray(cc.reshape(BPC, 8, 128).transpose(2, 1, 0))
        m["positions"] = np.ascontiguousarray(pos[BPC * i:BPC * (i + 1)])
        in_maps.append(m)
    return in_maps


def kernel(**inputs):
    in_maps = _prep_inputs(inputs)
    prog = Prog()
    nc = prog.build()
    res = run_bass_kernel_spmd(nc, in_maps, core_ids=list(range(N_CORES)))
    outs = [np.asarray(r["out"], dtype=np.float32).reshape(BPC, SEQ, D) for r in res.results]
    return np.concatenate(outs, axis=0)
```

```python
import numpy as np
import concourse.bass as bass
import concourse.mybir as mybir
from concourse.bass_utils import run_bass_kernel_spmd

F32 = mybir.dt.float32
BF16 = mybir.dt.bfloat16
I32 = mybir.dt.int32
ALU = mybir.AluOpType
AF = mybir.ActivationFunctionType
AX = mybir.AxisListType
IOA = bass.IndirectOffsetOnAxis

N_CORES = 8
D = 1024
SEQ = 2048
BPC = 2
TOK = BPC * SEQ
NT = TOK // 128
NE = 256
BLK = 128
NBLK = TOK * 8 // BLK + NE
NSLOT = NBLK * BLK
CAP = BLK
DN_ALPHA = 2.0 ** 0.25
LN_EPS = 1e-5
ROUTED_SCALE = 2.5
EPOCH = 16000


class _Op:
    __slots__ = ("eng", "fn", "deps", "is_dma", "need_sig", "sig", "waits", "idx", "slot_prev", "r", "w", "is_barrier")


class Sched:
    ENGS = ("pe", "dve", "act", "pool", "sp")
    NSLOTS = {"sp": 12, "act": 4, "pool": 16}

    def __init__(self, nc):
        self.nc = nc
        self.cur = []
        self._atom = None
        self.ops = {e: [] for e in self.ENGS}
        self.allops = []
        self.lastw = {}
        self.readers = {}

    def op(self, eng, fn, r=(), w=(), dma=False):
        o = _Op()
        o.eng, o.fn, o.is_dma, o.deps, o.need_sig, o.sig, o.waits = eng, fn, dma, [], dma, None, []
        o.r, o.w, o.is_barrier, o.slot_prev = list(r), list(w), False, None
        if self._atom is not None:
            self._atom.append(o)
        else:
            self.cur.append([o])
        return o

    def barrier(self):
        o = _Op()
        o.eng, o.fn, o.is_dma, o.deps, o.need_sig, o.sig, o.waits = None, None, False, [], False, None, []
        o.r, o.w, o.is_barrier, o.slot_prev = [], [], True, None
        assert self._atom is None
        self.cur.append([o])

    class _Atomic:
        def __init__(self, s):
            self.s = s

        def __enter__(self):
            self.nested = self.s._atom is not None
            if not self.nested:
                self.s._atom = []

        def __exit__(self, *a):
            if not self.nested:
                seg, self.s._atom = self.s._atom, None
                if seg:
                    self.s.cur.append(seg)

    def atomic(self):
        return Sched._Atomic(self)

    def record_parts(self, body):
        old = self.cur
        self._parts = []
        self.cur = []
        body()
        self._parts.append(self.cur)
        parts, self._parts = self._parts, None
        self.cur = old
        return parts

    def cut(self):
        if getattr(self, "_parts", None) is not None:
            self._parts.append(self.cur)
            self.cur = []

    def merge_rr(self, streams):
        streams = [list(s) for s in streams]
        pos = [0] * len(streams)
        alive = True
        while alive:
            alive = False
            for i, s in enumerate(streams):
                if pos[i] < len(s):
                    self.cur.append(s[pos[i]])
                    pos[i] += 1
                    alive = True

    def merge_seq(self, streams):
        for s in streams:
            self.cur.extend(s)

    def _track(self, o):
        o.idx = len(self.allops)
        seen = set()

        def add(d):
            if d is not None and d.idx not in seen:
                seen.add(d.idx)
                o.deps.append(d)

        for k in o.r:
            add(self.lastw.get(k))
        for k in o.w:
            add(self.lastw.get(k))
            rd = self.readers.get(k)
            if rd:
                for d in rd[0].values():
                    add(d)
                for d in rd[1]:
                    add(d)
        for k in o.r:
            rd = self.readers.setdefault(k, ({}, []))
            if o.is_dma:
                rd[1].append(o)
            else:
                rd[0][o.eng] = o
        for k in o.w:
            self.lastw[k] = o
            self.readers[k] = ({}, [])
        self.ops[o.eng].append(o)
        self.allops.append(o)

    def _track_barrier(self):
        deps = []
        for e in self.ENGS:
            got_c = False
            nd = 0
            for o in reversed(self.ops[e]):
                if o.fn is None:
                    continue
                if o.is_dma:
                    if nd < self.NSLOTS.get(e, 0):
                        deps.append(o)
                        nd += 1
                elif not got_c:
                    deps.append(o)
                    got_c = True
                if got_c and nd >= self.NSLOTS.get(e, 0):
                    break
        for e in self.ENGS:
            o = _Op()
            o.eng, o.fn, o.is_dma, o.need_sig, o.sig, o.waits = e, None, False, False, None, []
            o.deps = list(deps)
            o.idx = len(self.allops)
            o.slot_prev = None
            o.r, o.w, o.is_barrier = [], [], False
            self.ops[e].append(o)
            self.allops.append(o)

    @staticmethod
    def _needs_sync(d, o):
        if d.is_dma:
            return True
        if d.eng == o.eng and d.eng == "pe" and not o.is_dma:
            return False
        if d.fn is None:
            return False
        return True

    def finalize_and_emit(self):
        nc = self.nc
        for seg in self.cur:
            for o in seg:
                if o.is_barrier:
                    self._track_barrier()
                else:
                    self._track(o)
        for o in self.allops:
            for d in o.deps:
                if self._needs_sync(d, o):
                    d.need_sig = True
        sems = {}

        def get_sem(name):
            if name not in sems:
                sems[name] = nc.alloc_semaphore(name)
            return sems[name]

        for e in self.ENGS:
            n = 0
            slot_cnt = {}
            nd = 0
            for o in self.ops[e]:
                if o.fn is None:
                    continue
                if o.is_dma:
                    ns = self.NSLOTS[e]
                    s = nd % ns
                    nd += 1
                    c = slot_cnt.get(s, 0)
                    sem = get_sem(f"d_{e}_{s}")
                    o.slot_prev = (sem, 16 * c) if c > 0 else None
                    slot_cnt[s] = c + 1
                    o.sig = (sem, 16 * (c + 1), 16)
                elif o.need_sig:
                    ep, v = divmod(n, EPOCH)
                    n += 1
                    o.sig = (get_sem(f"c_{e}_{ep}"), v + 1, 1)
            self._final_dma = getattr(self, "_final_dma", {})
            self._final_dma[e] = {s: c for s, c in slot_cnt.items()}
        for e in self.ENGS:
            waited = {}
            for o in self.ops[e]:
                need = {}
                for d in o.deps:
                    if d.sig is None:
                        continue
                    if not self._needs_sync(d, o):
                        continue
                    sem, val, _ = d.sig
                    if need.get(sem, (None, 0))[1] < val:
                        need[sem] = (sem, val)
                if o.is_dma and o.slot_prev is not None:
                    sem, val = o.slot_prev
                    if need.get(sem, (None, 0))[1] < val:
                        need[sem] = (sem, val)
                for sem, val in need.values():
                    if waited.get(sem, 0) < val:
                        waited[sem] = val
                        o.waits.append((sem, val))
        self.sems = sems
        engmap = {"pe": "tensor", "dve": "vector", "act": "scalar", "pool": "gpsimd", "sp": "sync"}
        with nc.Block() as block:
            for e in self.ENGS:
                def body(eng, e=e):
                    for o in self.ops[e]:
                        for sem, val in o.waits:
                            eng.wait_ge(sem, val)
                        if o.fn is None:
                            continue
                        ins = o.fn(eng)
                        if o.sig is not None:
                            ins.then_inc(o.sig[0], o.sig[2])
                    if e in self.NSLOTS:
                        for s, c in self._final_dma[e].items():
                            eng.wait_ge(sems[f"d_{e}_{s}"], 16 * c)
                getattr(block, engmap[e])(body)


class Buf:
    def __init__(self, t, name, nreg=1):
        self.t, self.name, self.nreg = t, name, nreg

    def k(self, i=0):
        return (self.name, i)

    def all(self):
        return [(self.name, i) for i in range(self.nreg)]

    def __getitem__(self, idx):
        return self.t[idx]


class Prog:
    def __init__(self, cfg=None):
        self.cfg = cfg or {}
        self.nc = bass.Bass("TRN2", target_bir_lowering=False)
        self.S = Sched(self.nc)
        self._uid = 0
        self._scopes = [[]]

    def sb(self, name, shape, dtype, nreg=1):
        self._uid += 1
        g = self.nc.sbuf_tensor(f"s{self._uid}_{name}", list(shape), dtype)
        t = g.__enter__()
        self._scopes[-1].append(g)
        return Buf(t, name, nreg)

    def push(self):
        self._scopes.append([])

    def pop(self):
        self.S.barrier()
        for g in reversed(self._scopes.pop()):
            g.__exit__(None, None, None)

    def dram(self, name, shape, dtype, kind):
        return self.nc.dram_tensor(name, list(shape), dtype, kind=kind).ap()

    def op(self, eng, fn, r=(), w=(), dma=False):
        return self.S.op(eng, fn, r, w, dma)

    def PE(self, out, lhsT, rhs, start, stop, r, w, **kw):
        return self.S.op("pe", lambda e: e.matmul(out, lhsT, rhs, start=start, stop=stop, **kw), r, w)

    def TR(self, out, in_, ident, r, w):
        return self.S.op("pe", lambda e: e.transpose(out, in_, ident), r, w)

    def ACT(self, out, in_, func, r, w, **kw):
        return self.S.op("act", lambda e: e.activation(out, in_, func, **kw), r, w)

    def TT(self, eng, out, a, b, alu, r, w):
        return self.S.op(eng, lambda e: e.tensor_tensor(out, a, b, alu), r, w)

    def TS(self, eng, out, a, s1, s2, op0, op1, r, w, accum=None):
        if accum is None:
            return self.S.op(eng, lambda e: e.tensor_scalar(out, a, s1, s2, op0, op1), r, w)
        return self.S.op(eng, lambda e: e.tensor_scalar(out, a, s1, s2, op0, op1, accum_out=accum), r, w)

    def STT(self, out, in0, scalar, in1, op0, op1, r, w, accum=None):
        if accum is None:
            return self.S.op("dve", lambda e: e.scalar_tensor_tensor(out, in0, scalar, in1, op0, op1), r, w)
        return self.S.op("dve", lambda e: e.scalar_tensor_tensor(out, in0, scalar, in1, op0, op1, accum_out=accum), r, w)

    def CP(self, eng, out, in_, r, w):
        if eng == "act":
            return self.S.op("act", lambda e: e.activation(out, in_, AF.Copy), r, w)
        return self.S.op(eng, lambda e: e.tensor_copy(out, in_), r, w)

    def DMA(self, q, out, in_, r, w):
        return self.S.op(q, lambda e: e.dma_start(out=out, in_=in_), r, w, dma=True)

    def build(self):
        nc, S = self.nc, self.S
        cfg = self.cfg
        op = self.op
        PE, TR, ACT, TT, TS, STT, CP, DMA = self.PE, self.TR, self.ACT, self.TT, self.TS, self.STT, self.CP, self.DMA
        do_mixer = cfg.get("mixer", True)
        x_d = self.dram("x", [TOK, D], F32, "ExternalInput")
        cT_d = self.dram("cT", [128, 8, BPC], F32, "ExternalInput")
        pos_d = self.dram("positions", [BPC, SEQ], I32, "ExternalInput")
        w_ada_d = self.dram("w_ada", [D, 6 * D], F32, "ExternalInput")
        b_ada_d = self.dram("b_ada", [1, 6 * D], F32, "ExternalInput")
        ln_d = self.dram("ln", [4, D], F32, "ExternalInput")
        w_router_d = self.dram("w_router", [D, NE], F32, "ExternalInput")
        rbias_d = self.dram("router_bias", [1, NE], F32, "ExternalInput")
        wg_d = self.dram("w_exp_gate", [NE, D, 256], F32, "ExternalInput")
        wu_d = self.dram("w_exp_up", [NE, D, 256], F32, "ExternalInput")
        wd_d = self.dram("w_exp_down", [NE, 256, D], F32, "ExternalInput")
        wsg_d = self.dram("w_sh_gate", [D, 256], F32, "ExternalInput")
        wsu_d = self.dram("w_sh_up", [D, 256], F32, "ExternalInput")
        wsd_d = self.dram("w_sh_down", [256, D], F32, "ExternalInput")
        wmA_d = self.dram("wmA", [N_CH_A, 128, 8, 128], F32, "ExternalInput")
        wmB_d = self.dram("wmB", [N_CH_B, 128, 8, 128], F32, "ExternalInput")
        wmG_d = self.dram("wmG", [16, 128, 8, 128], F32, "ExternalInput")
        wtA_d = self.dram("wtA", [128, 8, 136], F32, "ExternalInput")
        wtB_d = self.dram("wtB", [128, 8, 280], F32, "ExternalInput")
        wbrA_d = self.dram("w_br_a", [512, D], F32, "ExternalInput")
        wbrB_d = self.dram("w_br_b", [512, D], F32, "ExternalInput")
        wout_d = self.dram("w_out", [D, D], F32, "ExternalInput")
        cw1k_d = self.dram("cmp_k_w1", [2048, 256], F32, "ExternalInput")
        cw1v_d = self.dram("cmp_v_w1", [2048, 256], F32, "ExternalInput")
        cw2k_d = self.dram("cmp_k_w2", [256, 64], F32, "ExternalInput")
        cw2v_d = self.dram("cmp_v_w2", [256, 64], F32, "ExternalInput")
        cposT_d = self.dram("cmp_posT", [128, 2, 32], F32, "ExternalInput")
        cst_d = self.dram("cst", [128, 8], F32, "ExternalInput")
        selAB_d = self.dram("selAB", [128, 2, 16, 32], F32, "ExternalInput")
        expall_d = self.dram("expall", [32, 16, 128], F32, "ExternalInput")
        ov_d = self.dram("ovl", [128, 32], F32, "ExternalInput")
        out_d = self.dram("out", [TOK, D], F32, "ExternalOutput")
        x1_d = self.dram("x1_scr", [TOK, D], F32, "Internal")
        u2_d = self.dram("u2_scr", [TOK, D], BF16, "Internal")
        xs_d = self.dram("xs_scr", [NSLOT, D], BF16, "Internal")
        ys_d = self.dram("ys_scr", [NSLOT, D], BF16, "Internal")
        mod_d = self.dram("mod_scr", [BPC * 6, D], F32, "Internal")
        dbg = {}
        for name, shape in cfg.get("dbg", {}).items():
            dbg[name] = self.dram("dbg_" + name, shape, F32, "ExternalOutput")
        self.dbg = dbg

        _regs = {}

        def bc_reg(e):
            if "bc" not in _regs:
                _regs["bc"] = e.to_reg(NSLOT - 1)
            return _regs["bc"]

        banks = [Buf(nc.alloc_psum_tensor(f"pb{i}", [128, 512], F32), f"pb{i}") for i in range(8)]

        def bfv(bk):
            return bk.t[:, :].bitcast(BF16)

        ones_f = self.sb("ones_f", [128, 128], F32)
        ones_b = self.sb("ones_b", [128, 128], BF16)
        ident_b = self.sb("ident_b", [128, 128], BF16)
        tri_b = self.sb("tri_b", [128, 128], BF16)
        trib = self.sb("trib", [128, 128], BF16)
        edgeb = self.sb("edgeb", [128, 128], BF16)
        negb = self.sb("negb", [128, 128], F32)
        iotaC = self.sb("iotaC", [128, NE], F32)
        iotaE1 = self.sb("iotaE1", [128, NE], F32)
        jcol = self.sb("jcol", [128, NBLK // 128], F32)
        pcol = self.sb("pcol", [128, 1], F32)
        Utri = self.sb("Utri", [128, 2, NE], BF16)
        cst = self.sb("cst", [128, 8], F32)
        eps_t = self.sb("eps_t", [128, 1], F32)
        op("pool", lambda e: e.memset(ones_f[:, :], 1.0), w=ones_f.all())
        op("pool", lambda e: e.memset(ones_b[:, :], 1.0), w=ones_b.all())
        op("pool", lambda e: e.memset(negb[:, :], -30000.0), w=negb.all())
        op("pool", lambda e: e.memset(eps_t[:, :], LN_EPS), w=eps_t.all())
        op("pool", lambda e: e.affine_select(ident_b[:, :], ones_f[:, :], [[-1, 128]], ALU.is_equal, 0.0,
                                            base=0, channel_multiplier=1), r=ones_f.all(), w=ident_b.all())
        op("pool", lambda e: e.affine_select(tri_b[:, :], ones_f[:, :], [[1, 128]], ALU.is_gt, 0.0,
                                            base=0, channel_multiplier=-1), r=ones_f.all(), w=tri_b.all())
        op("pool", lambda e: e.affine_select(trib[:, :], negb[:, :], [[-1, 128]], ALU.is_gt, 0.0,
                                            base=0, channel_multiplier=1), r=negb.all(), w=trib.all())
        op("pool", lambda e: e.affine_select(edgeb[:, :], negb[:, :], [[1, 128]], ALU.is_ge, 0.0,
                                            base=0, channel_multiplier=-1), r=negb.all(), w=edgeb.all())
        op("pool", lambda e: e.iota(iotaC[:, :], [[4096, NE]], base=0, channel_multiplier=0,
                                    allow_small_or_imprecise_dtypes=True), w=iotaC.all())
        op("pool", lambda e: e.iota(jcol[:, :], [[128, NBLK // 128]], base=0, channel_multiplier=1,
                                    allow_small_or_imprecise_dtypes=True), w=jcol.all())
        for a_ in range(2):
            op("pool", lambda e, a_=a_: e.iota(iotaE1[:, :], [[1, NE]], base=-128 * a_, channel_multiplier=-1,
                                              allow_small_or_imprecise_dtypes=True), r=Utri.all(), w=iotaE1.all())
            TS("dve", Utri[:, a_, :], iotaE1[:, :], 0.0, None, ALU.is_gt, ALU.bypass, iotaE1.all(), Utri.all())
        op("pool", lambda e: e.iota(iotaE1[:, :], [[1, NE]], base=0, channel_multiplier=0,
                                    allow_small_or_imprecise_dtypes=True), r=Utri.all(), w=iotaE1.all())
        op("pool", lambda e: e.iota(pcol[:, :], [[0, 1]], base=0, channel_multiplier=1,
                                    allow_small_or_imprecise_dtypes=True), w=pcol.all())
        DMA("sp", cst[:, :], cst_d[:, :], [], cst.all())

        ln1g = self.sb("ln1g", [128, D], F32)
        ln1b = self.sb("ln1b", [128, D], F32)
        rb_bc = self.sb("rb_bc", [128, NE], F32)
        wr_b = self.sb("wr_b", [128, 8, NE], BF16)
        carry = self.sb("carry", [128, NE], F32)
        dest_f = self.sb("dest_f", [128, NT, 8], F32, nreg=NT)
        dest_i = self.sb("dest_i", [128, NT, 8], I32, nreg=NT)
        wk = self.sb("wk", [128, NT, 8], F32, nreg=NT)
        op("pool", lambda e: e.memset(carry[:, :], 0.0), w=carry.all())
        DMA("sp", ln1g[:, :], ln_d[0:1, :].to_broadcast([128, D]), [], ln1g.all())
        DMA("sp", ln1b[:, :], ln_d[1:2, :].to_broadcast([128, D]), [], ln1b.all())
        DMA("sp", rb_bc[:, :], rbias_d[0:1, :].to_broadcast([128, NE]), [], rb_bc.all())

        self.push()
        condT = self.sb("condT", [128, 8, BPC], F32)
        condS = self.sb("condS", [128, 8, BPC], F32)
        DMA("sp", condT[:, :, :], cT_d[:, :, :], [], condT.all())
        ACT(condS[:, :, :], condT[:, :, :], AF.Silu, condT.all(), condS.all())
        wst = [self.sb(f"wada_st{i}", [128, 8, 512], F32) for i in range(2)]
        mrow = [self.sb(f"mrow{i}", [1, D], F32) for i in range(4)]
        brow = [self.sb(f"brow{i}", [1, D], F32) for i in range(2)]
        w_ada_v = w_ada_d.rearrange("(k p) n -> p k n", p=128)
        nmm = 0
        for j in range(6):
            br = brow[j % 2]
            DMA("sp", br[:, :], b_ada_d[:, j * D:(j + 1) * D], [], br.all())
            rows_j = [mrow[(j * BPC + b) % 4] for b in range(BPC)]
            for half in range(2):
                cc = j * 2 + half
                st = wst[cc % 2]
                DMA("sp", st[:, :, :], w_ada_v[:, :, cc * 512:(cc + 1) * 512], [], st.all())
                for b in range(BPC):
                    bk = banks[nmm % 2]
                    nmm += 1
                    for k in range(8):
                        PE(bk[0:1, :], condS[:, k, b:b + 1], st[:, k, :], k == 0, k == 7, condS.all() + st.all(), bk.all())
                    add = 1.0 if j in (1, 4) else 0.0
                    STT(rows_j[b][0:1, half * 512:(half + 1) * 512], bk[0:1, :], add, br[0:1, half * 512:(half + 1) * 512],
                        ALU.add, ALU.add, bk.all() + br.all(), rows_j[b].all())
            for b in range(BPC):
                DMA("sp", mod_d[b * 6 + j:b * 6 + j + 1, :], rows_j[b][0:1, :], rows_j[b].all(), [("mod_d", b, j)])
        wr_st = self.sb("wr_st", [128, 8, NE], F32)
        DMA("sp", wr_st[:, :, :], w_router_d.rearrange("(k p) n -> p k n", p=128), [], wr_st.all())
        CP("pool", wr_b[:, :, :], wr_st[:, :, :], wr_st.all(), wr_b.all())
        self.pop()

        def load_mod(dst, b, j):
            DMA("sp", dst[:, :], mod_d[b * 6 + j:b * 6 + j + 1, :].to_broadcast([128, D]), [("mod_d", b, j)], dst.all())

        def layernorm(z, dst, g_bc, b_bc, tagbufs, eng="pool"):
            stats, mv, rstd = tagbufs
            for h in range(2):
                op("dve", lambda e, h=h: e.bn_stats(stats[:, h, :], z[:, h * 512:(h + 1) * 512]), r=z.all(), w=stats.all())
            op("dve", lambda e: e.bn_aggr(mv[:, :], stats[:, :, :]), r=stats.all(), w=mv.all())
            ACT(rstd[:, :], mv[:, 1:2], AF.Sqrt, mv.all() + eps_t.all(), rstd.all(), bias=eps_t[:, 0:1], scale=1.0)
            op("dve", lambda e: e.reciprocal(rstd[:, :], rstd[:, :]), r=rstd.all(), w=rstd.all())
            TS("dve", z[:, :], z[:, :], mv[:, 0:1], rstd[:, 0:1], ALU.subtract, ALU.mult, z.all() + mv.all() + rstd.all(), z.all())
            TT(eng, z[:, :], z[:, :], g_bc[:, :], ALU.mult, z.all() + g_bc.all(), z.all())
            TT(eng, dst[:, :], z[:, :], b_bc[:, :], ALU.add, z.all() + b_bc.all(), dst.all())

        n_tiles = cfg.get("n_tiles", NT)
        TPS = SEQ // 128
        PI = float(np.pi)
        C1 = 6.28125
        C2 = float(2.0 * np.pi - 6.28125)
        NBIS = 18

        def build_uT(b, uT):
            self.push()
            s1 = self.sb("m0_s1", [128, D], F32)
            h1 = self.sb("m0_h1", [128, D], F32)
            load_mod(h1, b, 0)
            load_mod(s1, b, 1)
            xs = [self.sb(f"m0_x{i}", [128, D], F32) for i in range(2)]
            us = [self.sb(f"m0_u{i}", [128, D], BF16) for i in range(2)]
            for tt in range(TPS):
                X, U = xs[tt % 2], us[tt % 2]
                r0 = b * SEQ + tt * 128
                DMA("sp", X[:, :], x_d[r0:r0 + 128, :], [], X.all())
                TT("dve", X[:, :], X[:, :], s1[:, :], ALU.mult, X.all() + s1.all(), X.all())
                TT("pool", U[:, :], X[:, :], h1[:, :], ALU.add, X.all() + h1.all(), U.all())
                bk = banks[tt % 2]
                pT = bfv(bk)
                for k in range(8):
                    TR(pT[:, k * 128:(k + 1) * 128], U[:, k * 128:(k + 1) * 128], ident_b[:, :], U.all() + ident_b.all(), bk.all())
                CP("act", uT[:, :, tt * 128:(tt + 1) * 128], pT.rearrange("p (k t) -> p k t", k=8), bk.all(), [uT.k(tt // 4)])
            self.pop()

        def rope_tables(b, CS, col_inv, col_sgn, nrow):
            self.push()
            posi = self.sb("rp_posi", [1, SEQ], I32)
            posf = self.sb("rp_posf", [1, SEQ], F32)
            ang = self.sb("rp_ang", [128, SEQ], F32)
            a2 = self.sb("rp_a2", [128, SEQ], F32)
            qf = self.sb("rp_qf", [128, SEQ], F32)
            qi = self.sb("rp_qi", [128, SEQ], I32)
            DMA("sp", posi[:, :], pos_d[b:b + 1, :], [], posi.all())
            CP("dve", posf[:, :], posi[:, :], posi.all(), posf.all())
            for c4 in range(4):
                bk = banks[2 + c4 % 2]
                PE(bk[:, :], ones_f[0:1, :], posf[0:1, c4 * 512:(c4 + 1) * 512], True, True, ones_f.all() + posf.all(), bk.all())
                TS("dve", ang[0:nrow, c4 * 512:(c4 + 1) * 512], bk[0:nrow, :], cst[0:nrow, col_inv:col_inv + 1], None, ALU.mult, ALU.bypass,
                   bk.all() + cst.all(), ang.all())
            for which in range(2):
                src = ang
                if which == 0:
                    TS("dve", a2[0:nrow, :], ang[0:nrow, :], PI / 2.0, None, ALU.add, ALU.bypass, ang.all(), a2.all())
                    src = a2
                TS("dve", qf[0:nrow, :], src[0:nrow, :], 1.0 / (2.0 * PI), None, ALU.mult, ALU.bypass, src.all(), qf.all())
                CP("dve", qi[0:nrow, :], qf[0:nrow, :], qf.all(), qi.all())
                CP("dve", qf[0:nrow, :], qi[0:nrow, :], qi.all(), qf.all())
                STT(a2[0:nrow, :], qf[0:nrow, :], -C1, src[0:nrow, :], ALU.mult, ALU.add, qf.all() + src.all(), a2.all())
                STT(a2[0:nrow, :], qf[0:nrow, :], -C2, a2[0:nrow, :], ALU.mult, ALU.add, qf.all() + a2.all(), a2.all())
                TS("dve", qf[0:nrow, :], a2[0:nrow, :], PI, -2.0 * PI, ALU.is_gt, ALU.mult, a2.all(), qf.all())
                TT("dve", a2[0:nrow, :], a2[0:nrow, :], qf[0:nrow, :], ALU.add, a2.all() + qf.all(), a2.all())
                TS("dve", a2[0:nrow, :], a2[0:nrow, :], 3.14159, -3.14159, ALU.min, ALU.max, a2.all(), a2.all())
                if which == 0:
                    ACT(CS[0:nrow, 0, :], a2[0:nrow, :], AF.Sin, a2.all(), [CS.k(0)])
                else:
                    ACT(qf[0:nrow, :], a2[0:nrow, :], AF.Sin, a2.all(), qf.all())
                    TS("dve", CS[0:nrow, 1, :], qf[0:nrow, :], cst[0:nrow, col_sgn:col_sgn + 1], None, ALU.mult, ALU.bypass,
                       qf.all() + cst.all(), [CS.k(1)])
            self.pop()

        def project(wm_d, jobs, uT, wbufs):
            st, wb, t1, t2 = wbufs
            cnt = [0]

            def load(ch):
                i = cnt[0] % 3
                cnt[0] += 1
                DMA("sp", st[i][:, :, :], wm_d[ch], [], st[i].all())
                CP("pool", wb[i][:, :, :], st[i][:, :, :], st[i].all(), wb[i].all())
                return wb[i]

            nb = 0
            for (ch, chsw, nrow, dst_fn, CS) in jobs:
                wA = load(ch)
                wB = load(chsw) if chsw is not None else None
                for tc in range(4):
                    ts_ = slice(tc * 512, (tc + 1) * 512)
                    bA = banks[2 + nb % 4]
                    nb += 1
                    for k in range(8):
                        PE(bA[:, :], wA[:, k, :], uT[:, k, ts_], k == 0, k == 7, wA.all() + [uT.k(tc)], bA.all())
                    dst, dkeys = dst_fn(tc)
                    if wB is None:
                        CP("act", dst, bA[0:nrow, :], bA.all(), dkeys)
                    else:
                        bB = banks[2 + nb % 4]
                        nb += 1
                        for k in range(8):
                            PE(bB[:, :], wB[:, k, :], uT[:, k, ts_], k == 0, k == 7, wB.all() + [uT.k(tc)], bB.all())
                        T1, T2 = t1[tc % 2], t2[tc % 2]
                        TT("dve", T1[0:nrow, :], bA[0:nrow, :], CS[0:nrow, 0, ts_], ALU.mult, bA.all() + [CS.k(0)], T1.all())
                        TT("dve", T2[0:nrow, :], bB[0:nrow, :], CS[0:nrow, 1, ts_], ALU.mult, bB.all() + [CS.k(1)], T2.all())
                        TT("pool", dst, T1[0:nrow, :], T2[0:nrow, :], ALU.add, T1.all() + T2.all(), dkeys)

        def proj_bufs():
            st = [self.sb(f"pj_st{i}", [128, 8, 128], F32) for i in range(3)]
            wb = [self.sb(f"pj_wb{i}", [128, 8, 128], BF16) for i in range(3)]
            t1 = [self.sb(f"pj_t1{i}", [128, 512], F32) for i in range(2)]
            t2 = [self.sb(f"pj_t2{i}", [128, 512], F32) for i in range(2)]
            return st, wb, t1, t2

        def attn_out(accs, O, coef=None, first=True, tmpb=None):
            Ov = O[:, :].rearrange("p (g e q d) -> p g q e d", g=2, e=2, q=2)
            for g in range(2):
                a = accs[g]
                W = a.W
                av = a.buf[:, 0:4 * W].rearrange("p (q e w) -> p q e w", q=2, e=2)[:, :, :, 0:64]
                cf = coef[:, g * 4:(g + 1) * 4].rearrange("p (q e) -> p q e", q=2).unsqueeze(3).to_broadcast([128, 2, 2, 64])
                if first:
                    TT("dve", Ov[:, g], av, cf, ALU.mult, a.buf.all() + coef.all(), O.all())
                else:
                    tv = tmpb[:, :].rearrange("p (q e d) -> p q e d", q=2, e=2)
                    TT("dve", tv, av, cf, ALU.mult, a.buf.all() + coef.all(), tmpb.all())
                    TT("pool", Ov[:, g], Ov[:, g], tv, ALU.add, O.all() + tmpb.all(), O.all())

        class Acc:
            def __init__(self, buf, W):
                self.buf, self.W = buf, W

        def o_to_T(O, Ob, oT, i, bank):
            CP("act", Ob[:, :], O[:, :], O.all(), Ob.all())
            pT = bfv(bank)
            for c in range(4):
                TR(pT[:, c * 128:(c + 1) * 128], Ob[:, c * 128:(c + 1) * 128], ident_b[:, :], Ob.all() + ident_b.all(), bank.all())
            CP("dve", oT[:, :, i * 128:(i + 1) * 128], pT[:, 0:512].rearrange("p (c t) -> p c t", c=4), bank.all(), [oT.k(i // 4)])

        def dsa(b, oaT):
            self.push()
            QA = self.sb("QA", [128, 4, SEQ], BF16, nreg=4)
            KA = self.sb("KA", [128, 2, SEQ], BF16, nreg=4)
            QI = self.sb("QI", [96, 3, SEQ], BF16, nreg=4)
            KI = self.sb("KI", [96, SEQ], BF16, nreg=4)
            VA = self.sb("VA", [128, TPS, 2, 65], BF16)
            widx = self.sb("widx", [128, TPS, 8], F32)
            wabs = self.sb("wabs", [128, TPS, 8], F32)
            wsgn = self.sb("wsgn", [128, TPS, 8], F32)
            CS64 = self.sb("CS64", [128, 2, SEQ], BF16, nreg=2)
            CS32 = self.sb("CS32", [128, 2, SEQ], BF16, nreg=2)
            rope_tables(b, CS64, 0, 1, 128)
            rope_tables(b, CS32, 2, 3, 96)
            self.push()
            uT = self.sb("uT", [128, 8, SEQ], BF16, nreg=4)
            build_uT(b, uT)
            wbufs = proj_bufs()
            jobs = []
            for c in range(4):
                jobs.append((c, 4 + c, 128, (lambda tc, c=c: (QA[:, c, tc * 512:(tc + 1) * 512], [QA.k(tc)])), CS64))
            for g in range(2):
                jobs.append((8 + g, 10 + g, 128, (lambda tc, g=g: (KA[:, g, tc * 512:(tc + 1) * 512], [KA.k(tc)])), CS64))
            for c in range(3):
                jobs.append((12 + c, 15 + c, 96, (lambda tc, c=c: (QI[:, c, tc * 512:(tc + 1) * 512], [QI.k(tc)])), CS32))
            jobs.append((18, 19, 96, (lambda tc: (KI[:, tc * 512:(tc + 1) * 512], [KI.k(tc)])), CS32))
            project(wmA_d, jobs, uT, wbufs)
            wt_st = self.sb("wtA_st", [128, 8, 136], F32)
            wt_b = self.sb("wtA_b", [128, 8, 136], BF16)
            DMA("sp", wt_st[:, :, :], wtA_d[:, :, :], [], wt_st.all())
            CP("pool", wt_b[:, :, :], wt_st[:, :, :], wt_st.all(), wt_b.all())
            op("pool", lambda e: e.memset(VA[:, :, :, 64:65], 1.0), w=VA.all())
            for tt in range(TPS):
                bk = banks[6 + tt % 2]
                for k in range(8):
                    PE(bk[:, 0:136], uT[:, k, tt * 128:(tt + 1) * 128], wt_b[:, k, :], k == 0, k == 7, [uT.k(tt // 4)] + wt_b.all(), bk.all())
                CP("act", VA[:, tt, :, 0:64], bk[:, 0:128].rearrange("p (g d) -> p g d", g=2), bk.all(), VA.all())
                CP("dve", widx[:, tt, :], bk[:, 128:136], bk.all(), widx.all())
            c0 = float((32.0 ** -0.5) * (8.0 ** -0.5))
            ACT(wabs[:, :, :], widx[:, :, :], AF.Abs, widx.all(), wabs.all(), scale=c0)
            TS("dve", wsgn[:, :, :], widx[:, :, :], 0.0, 2.0, ALU.is_ge, ALU.mult, widx.all(), wsgn.all())
            TS("dve", wsgn[:, :, :], wsgn[:, :, :], -1.0, None, ALU.add, ALU.bypass, wsgn.all(), wsgn.all())
            self.pop()
            self.push()
            GQ = 4
            accs = [self.sb(f"ix_acc{i}", [128, SEQ], F32, nreg=4) for i in range(GQ)]
            junks = [self.sb(f"ix_junk{i}", [128, SEQ], BF16) for i in range(GQ)]
            bss = [self.sb(f"ix_bs{i}", [128, 8 + NBIS], F32) for i in range(GQ)]
            css = [self.sb(f"ix_cs{i}", [128, 1], F32) for i in range(GQ)]
            Mb = self.sb("ix_Mb", [128, SEQ], BF16)
            BT = self.sb("ix_BT", [128, TPS, 128], BF16, nreg=2)
            Rt = [self.sb(f"ix_R{i}", [128, 512], F32) for i in range(3)]
            pw2 = self.sb("ix_pw2", [128, NBIS], F32)
            thrneg = self.sb("ix_thrneg", [128, 1], F32)
            Et = [self.sb(f"at_E{i}", [128, 512], BF16) for i in range(4)]
            O = self.sb("at_O", [128, 512], F32)
            Ob = self.sb("at_Ob", [128, 512], BF16)
            RD = self.sb("at_RD", [128, 8], F32)
            for k in range(NBIS):
                op("pool", lambda e, k=k: e.memset(pw2[:, k:k + 1], 2.0 ** -(k + 1)), w=pw2.all())
            op("pool", lambda e: e.memset(thrneg[:, :], -1e30), w=thrneg.all())
            cnts = {"R": 0, "E": 0}
            nq = cfg.get("n_qtiles", TPS)

            def indexer(i):
                acc, bs = accs[i % GQ], bss[i % GQ]
                rmax, rmin = bs[:, 1:2], bs[:, 2:3]
                n = 128 * (i + 1)
                it = slice(i * 128, (i + 1) * 128)
                nch = (n + 511) // 512
                for h in range(8):
                    pb = 32 * (h % 3)
                    for kc in range(nch):
                        kw = min(512, n - kc * 512)
                        bk = banks[(h * nch + kc) % 2]
                        R = Rt[cnts["R"] % 3]
                        cnts["R"] += 1
                        with S.atomic():
                            PE(bk[:, 0:kw], QI[pb:pb + 32, h // 3, it], KI[pb:pb + 32, kc * 512:kc * 512 + kw], True, True,
                               [QI.k(i // 4), KI.k(kc)], bk.all())
                            ACT(R[:, 0:kw], bk[:, 0:kw], AF.Relu, bk.all() + wabs.all(), R.all(), scale=wabs[:, i, h:h + 1])
                        if h == 0:
                            TS("dve", acc[:, kc * 512:kc * 512 + kw], R[:, 0:kw], wsgn[:, i, 0:1], None, ALU.mult, ALU.bypass,
                               R.all() + wsgn.all(), [acc.k(kc)])
                        else:
                            STT(acc[:, kc * 512:kc * 512 + kw], R[:, 0:kw], wsgn[:, i, h:h + 1], acc[:, kc * 512:kc * 512 + kw],
                                ALU.mult, ALU.add, R.all() + wsgn.all() + [acc.k(kc)], [acc.k(kc)])
                akeys = [acc.k(kc) for kc in range(nch)]
                if i >= 2:
                    op("dve", lambda e: e.tensor_reduce(rmin, acc[:, 0:n], AX.X, ALU.min), r=akeys, w=bs.all())
                    op("dve", lambda e: e.tensor_reduce(rmax, acc[:, 0:n], AX.X, ALU.max), r=akeys, w=bs.all())
                op("pool", lambda e: e.affine_select(acc[:, it], acc[:, it], [[-1, 128]], ALU.is_ge, -3.0e38,
                                                    base=0, channel_multiplier=1), r=[acc.k(i // 4)], w=[acc.k(i // 4)])

            def bisect(i):
                acc, bs, junk = accs[i % GQ], bss[i % GQ], junks[i % GQ]
                lo, rmax, rmin, w0, mid, cnt, stp = (bs[:, 0:1], bs[:, 1:2], bs[:, 2:3], bs[:, 3:4], bs[:, 4:5], bs[:, 5:6], bs[:, 6:7])
                Wk = bs[:, 8:8 + NBIS]
                n = 128 * (i + 1)
                akeys = [acc.k(kc) for kc in range((n + 511) // 512)]
                TS("dve", lo, rmin, -1.0, None, ALU.add, ALU.bypass, bs.all(), bs.all())
                TT("dve", w0, rmax, lo, ALU.subtract, bs.all(), bs.all())
                TS("dve", Wk, pw2[:, :], w0, None, ALU.mult, ALU.bypass, bs.all() + pw2.all(), bs.all())
                cs_ = css[i % GQ]
                for k in range(NBIS):
                    if i % 2 == 0:
                        TS("dve", mid, lo, Wk[:, k:k + 1], -1.0, ALU.add, ALU.mult, bs.all(), bs.all())
                        ACT(junk[:, 0:n], acc[:, 0:n], AF.Sign, akeys + bs.all(), junk.all() + cs_.all(), bias=mid, scale=1.0, accum_out=cs_[:, 0:1])
                        TS("dve", stp, cs_[:, 0:1], float(511 - n), Wk[:, k:k + 1], ALU.is_ge, ALU.mult, bs.all() + cs_.all(), bs.all())
                    else:
                        TT("dve", mid, lo, Wk[:, k:k + 1], ALU.add, bs.all(), bs.all())
                        TS("dve", junk[:, 0:n], acc[:, 0:n], mid, None, ALU.is_ge, ALU.add, akeys + bs.all(), junk.all() + bs.all(), accum=cnt)
                        TS("dve", stp, cnt, 255.5, Wk[:, k:k + 1], ALU.is_ge, ALU.mult, bs.all(), bs.all())
                    TT("dve", lo, lo, stp, ALU.add, bs.all(), bs.all())

            def attend(i):
                acc, bs = accs[i % GQ], bss[i % GQ]
                n = 128 * (i + 1)
                it = slice(i * 128, (i + 1) * 128)
                akeys = [acc.k(kc) for kc in range((n + 511) // 512)]
                if i >= 2:
                    thr, thr_keys = bs[:, 0:1], bs.all()
                else:
                    thr, thr_keys = thrneg[:, 0:1], thrneg.all()
                TS("dve", Mb[:, 0:n], acc[:, 0:n], thr, -30000.0, ALU.is_lt, ALU.mult, akeys + thr_keys, Mb.all())
                for j0 in range(0, i + 1, 8):
                    j1 = min(i + 1, j0 + 8)
                    bk = banks[2]
                    pT = bfv(bk)
                    for j in range(j0, j1):
                        TR(pT[:, (j - j0) * 128:(j - j0 + 1) * 128], Mb[:, j * 128:(j + 1) * 128], ident_b[:, :], Mb.all() + ident_b.all(), bk.all())
                    CP("act", BT[:, j0:j1, :], pT[:, 0:(j1 - j0) * 128].rearrange("p (j t) -> p j t", t=128), bk.all(), [BT.k(j0 // 8)])
                accb = [banks[5], banks[6]]
                def scores(j):
                    jt = slice(j * 128, (j + 1) * 128)
                    sA, sB = (banks[3], banks[4]) if j % 2 == 0 else (banks[7], banks[2])
                    for par, sb_ in ((0, sA), (1, sB)):
                        ps = slice(par * 64, par * 64 + 64)
                        for g in range(2):
                            PE(sb_[:, g * 256:(g + 1) * 256], KA[ps, g, jt], QA[ps, 2 * g:2 * g + 2, it], g == 0, False,
                               [KA.k(j // 4), QA.k(i // 4)], sb_.all(), skip_group_check=True)
                        PE(sb_[:, :], ident_b[:, :], BT[:, j, :].unsqueeze(1).to_broadcast([128, 4, 128]), False, True,
                           ident_b.all() + [BT.k(j // 8)], sb_.all(), skip_group_check=True)

                def exp_pv(j):
                    sA, sB = (banks[3], banks[4]) if j % 2 == 0 else (banks[7], banks[2])
                    EA, EB = Et[cnts["E"] % 4], Et[(cnts["E"] + 1) % 4]
                    cnts["E"] += 2
                    ACT(EA[:, :], sA[:, :], AF.Exp, sA.all(), EA.all(), scale=0.125)
                    ACT(EB[:, :], sB[:, :], AF.Exp, sB.all(), EB.all(), scale=0.125)
                    for g in range(2):
                        for par, E_ in ((0, EA), (1, EB)):
                            for e_ in range(2):
                                r_ = par * 2 + e_
                                blk = (g * 2 + e_) * 128
                                PE(accb[g][:, r_ * 65:(r_ + 1) * 65], E_[:, blk:blk + 128], VA[:, j, g, :],
                                   (j == 0 and r_ == 0), (j == i and r_ == 3), E_.all() + VA.all(), accb[g].all(), skip_group_check=True)

                scores(0)
                for j in range(i + 1):
                    if j + 1 <= i:
                        scores(j + 1)
                    exp_pv(j)
                for g in range(2):
                    op("dve", lambda e, g=g: e.reciprocal(RD[:, g * 4:(g + 1) * 4], accb[g][:, 0:260].rearrange("p (r w) -> p r w", w=65)[:, :, 64]),
                       r=accb[g].all(), w=RD.all())
                attn_out([Acc(accb[0], 65), Acc(accb[1], 65)], O, coef=RD, first=True)
                o_to_T(O, Ob, oaT, i, banks[7])

            for i0 in range(0, nq, GQ):
                grp = list(range(i0, min(i0 + GQ, nq)))
                for i in grp:
                    indexer(i)
                parts = [S.record_parts(lambda i=i: bisect(i)) for i in grp if i >= 2]
                if parts:
                    S.merge_rr([p[0] for p in parts])
                for i in grp:
                    attend(i)
            self.pop()
            self.pop()
        def nsa(b, obT):
            self.push()
            QBr = self.sb("QBr", [128, 4, SEQ], BF16, nreg=4)
            QB = self.sb("QB", [128, 4, SEQ], BF16, nreg=4)
            KCf = self.sb("KCf", [128, SEQ], BF16, nreg=4)
            VCf = self.sb("VCf", [128, SEQ], BF16, nreg=4)
            KS = self.sb("KS", [128, 2, SEQ], BF16, nreg=4)
            KW = self.sb("KW", [128, 2, SEQ], BF16, nreg=4)
            VS = self.sb("VS", [128, TPS, 2, 65], BF16)
            VW = self.sb("VW", [128, TPS, 2, 65], BF16)
            gns = self.sb("gns", [128, TPS, 24], F32)
            CS64 = self.sb("CS64n", [128, 2, SEQ], BF16, nreg=2)
            rope_tables(b, CS64, 0, 1, 128)
            self.push()
            uT = self.sb("uTn", [128, 8, SEQ], BF16, nreg=4)
            build_uT(b, uT)
            wbufs = proj_bufs()
            jobs = []
            for c in range(4):
                jobs.append((c, None, 128, (lambda tc, c=c: (QBr[:, c, tc * 512:(tc + 1) * 512], [QBr.k(tc)])), None))
            for c in range(4):
                jobs.append((c, 4 + c, 128, (lambda tc, c=c: (QB[:, c, tc * 512:(tc + 1) * 512], [QB.k(tc)])), CS64))
            jobs.append((8, None, 128, (lambda tc: (KCf[:, tc * 512:(tc + 1) * 512], [KCf.k(tc)])), None))
            jobs.append((9, None, 128, (lambda tc: (VCf[:, tc * 512:(tc + 1) * 512], [VCf.k(tc)])), None))
            for g in range(2):
                jobs.append((10 + g, 12 + g, 128, (lambda tc, g=g: (KS[:, g, tc * 512:(tc + 1) * 512], [KS.k(tc)])), CS64))
            for g in range(2):
                jobs.append((14 + g, 16 + g, 128, (lambda tc, g=g: (KW[:, g, tc * 512:(tc + 1) * 512], [KW.k(tc)])), CS64))
            project(wmB_d, jobs, uT, wbufs)
            wt_st = self.sb("wtB_st", [128, 8, 280], F32)
            wt_b = self.sb("wtB_b", [128, 8, 280], BF16)
            DMA("sp", wt_st[:, :, :], wtB_d[:, :, :], [], wt_st.all())
            CP("pool", wt_b[:, :, :], wt_st[:, :, :], wt_st.all(), wt_b.all())
            op("pool", lambda e: e.memset(VS[:, :, :, 64:65], 1.0), w=VS.all())
            op("pool", lambda e: e.memset(VW[:, :, :, 64:65], 1.0), w=VW.all())
            for tt in range(TPS):
                bk = banks[6 + tt % 2]
                for k in range(8):
                    PE(bk[:, 0:280], uT[:, k, tt * 128:(tt + 1) * 128], wt_b[:, k, :], k == 0, k == 7, [uT.k(tt // 4)] + wt_b.all(), bk.all())
                CP("act", VS[:, tt, :, 0:64], bk[:, 0:128].rearrange("p (g d) -> p g d", g=2), bk.all(), VS.all())
                CP("act", VW[:, tt, :, 0:64], bk[:, 128:256].rearrange("p (g d) -> p g d", g=2), bk.all(), VW.all())
                ACT(gns[:, tt, :], bk[:, 256:280], AF.Sigmoid, bk.all(), gns.all())
            self.pop()
            self.push()
            KCc = self.sb("KCc", [128, 2, 128], BF16)
            VX = self.sb("VX", [128, 2, 97], BF16)
            ovst = self.sb("ovst", [128, 32], F32)
            DMA("sp", ovst[:, :], ov_d[:, :], [], ovst.all())
            op("pool", lambda e: e.memset(VX[:, :, 64:65], 1.0), w=VX.all())
            for g in range(2):
                CP("pool", VX[:, g, 65:97], ovst[:, :], ovst.all(), VX.all())
            self.push()
            W1s = self.sb("W1s", [128, 8, 256], F32)
            W1 = self.sb("W1", [128, 32, 256], BF16, nreg=4)
            w2s = self.sb("w2s", [128, 2, 64], F32)
            w2 = self.sb("w2", [128, 2, 128], BF16)
            posT = self.sb("posT", [128, 2, 32], F32)
            posTb = self.sb("posTb", [128, 2, 32], BF16)
            pbias = self.sb("pbias", [128, 2], F32)
            hx = self.sb("hx", [128, 128], F32)
            hx2 = self.sb("hx2", [128, 128], F32)
            hT = self.sb("hT", [128, 2, 2, 128], BF16)
            DMA("sp", posT[:, :, :], cposT_d[:, :, :], [], posT.all())
            CP("pool", posTb[:, :, :], posT[:, :, :], posT.all(), posTb.all())
            for kind, (w1_d, w2_d, SRC) in enumerate(((cw1k_d, cw2k_d, KCf), (cw1v_d, cw2v_d, VCf))):
                w1v = w1_d.rearrange("(l d) n -> d l n", d=64)
                for l4 in range(4):
                    for hh in range(2):
                        DMA("sp", W1s[hh * 64:(hh + 1) * 64, :, :], w1v[:, l4 * 8:(l4 + 1) * 8, :], [], W1s.all())
                    CP("pool", W1[:, l4 * 8:(l4 + 1) * 8, :], W1s[:, :, :], W1s.all(), [W1.k(l4)])
                DMA("sp", w2s[:, :, :], w2_d.rearrange("(h p) d -> p h d", p=128), [], w2s.all())
                for dd in range(2):
                    CP("pool", w2[:, :, dd * 64:(dd + 1) * 64], w2s[:, :, :], w2s.all(), w2.all())
                for hc in range(2):
                    bk = banks[0]
                    for l in range(32):
                        PE(bk[:, hc:hc + 1], W1[0:64, l, hc * 128:(hc + 1) * 128], posTb[0:64, kind, l:l + 1], (l == 0 and hc == 0), (l == 31 and hc == 1),
                           W1.all() + posTb.all(), bk.all(), skip_group_check=True)
                CP("dve", pbias[:, :], banks[0][:, 0:2], banks[0].all(), pbias.all())
                for g in range(2):
                    ps = slice(g * 64, (g + 1) * 64)
                    for hc in range(2):
                        bk = banks[1 + g]
                        for l in range(32):
                            PE(bk[:, hc * 128:hc * 128 + 127], W1[ps, l, hc * 128:(hc + 1) * 128], SRC[ps, l:l + 16 * 126 + 1:16], (l == 0 and hc == 0), (l == 31 and hc == 1),
                               W1.all() + SRC.all(), bk.all(), skip_group_check=True)
                    for hc in range(2):
                        bk = banks[1 + g]
                        ACT(hx[:, 0:127], bk[:, hc * 128:hc * 128 + 127], AF.Identity, bk.all() + pbias.all(), hx.all(), bias=pbias[:, hc:hc + 1], scale=1.0)
                        TT("dve", hx2[:, 0:127], hx[:, 0:127], hx[:, 0:127], ALU.mult, hx.all(), hx2.all())
                        TS("dve", hx2[:, 0:127], hx2[:, 0:127], 0.044715, 1.0, ALU.mult, ALU.add, hx2.all(), hx2.all())
                        TT("dve", hx2[:, 0:127], hx2[:, 0:127], hx[:, 0:127], ALU.mult, hx.all() + hx2.all(), hx2.all())
                        ACT(hx2[:, 0:127], hx2[:, 0:127], AF.Sigmoid, hx2.all(), hx2.all(), scale=1.5957691216057308)
                        TT("dve", hT[:, g, hc, 0:127], hx2[:, 0:127], hx[:, 0:127], ALU.mult, hx.all() + hx2.all(), hT.all())
                    bk = banks[3 + g]
                    if kind == 0:
                        for hc in range(2):
                            PE(bk[:, 0:127], w2[:, hc, :], hT[:, g, hc, 0:127], hc == 0, hc == 1, w2.all() + hT.all(), bk.all())
                        CP("act", KCc[:, g, 0:127], bk[:, 0:127], bk.all(), KCc.all())
                    else:
                        for hc in range(2):
                            PE(bk[0:127, 0:64], hT[:, g, hc, 0:127], w2[:, hc, 0:64], hc == 0, hc == 1, w2.all() + hT.all(), bk.all())
                        CP("act", VX[0:127, g, 0:64], bk[0:127, 0:64], bk.all(), VX.all())
            self.pop()
            CM = self.sb("CM", [128, SEQ], BF16)
            onesb2 = self.sb("onesb2", [128, SEQ], BF16)
            op("pool", lambda e: e.memset(onesb2[:, :], 1.0), w=onesb2.all())
            op("pool", lambda e: e.affine_select(CM[:, :], onesb2[:, :], [[1, SEQ]], ALU.is_ge, 0.0, base=-31, channel_multiplier=-16),
               r=onesb2.all(), w=CM.all())
            selAB = self.sb("selAB", [128, 2, 16, 32], F32)
            DMA("sp", selAB[:, :, :, :], selAB_d[:, :, :, :], [], selAB.all())
            expst = self.sb("expst", [32, 16, 128], F32)
            expall = self.sb("expall", [32, 16, 128], BF16)
            DMA("sp", expst[:, :, :], expall_d[:, :, :], [], expst.all())
            CP("pool", expall[:, :, :], expst[:, :, :], expst.all(), expall.all())
            Et = [self.sb(f"nt_E{i}", [128, 512], BF16) for i in range(4)]
            O = self.sb("nt_O", [128, 512], F32)
            Ob = self.sb("nt_Ob", [128, 512], BF16)
            otmp = self.sb("nt_otmp", [128, 256], F32)
            RD = self.sb("nt_RD", [128, 8], F32)
            coef = self.sb("nt_coef", [128, 8], F32)
            imp = self.sb("nt_imp", [128, 2, 32], F32)
            imt = self.sb("nt_imt", [128, 4, 32], F32)
            top8 = self.sb("nt_top8", [128, 2, 8], F32)
            imw = self.sb("nt_imw", [128, 32], F32)
            selb = self.sb("nt_selb", [128, 2, 32], BF16)
            selT = self.sb("nt_selT", [32, 2, 128], BF16)
            nE = 0
            cntE = [0]
            nq = cfg.get("n_qtiles", TPS)
            for i in range(nq):
                it = slice(i * 128, (i + 1) * 128)
                gv = gns[:, i, :].rearrange("p (g e q k) -> p g q e k", g=2, e=2, q=2)

                def mk_coef(kbr, accs, W):
                    for g in range(2):
                        dcol = accs[g][:, 0:4 * W].rearrange("p (r w) -> p r w", w=W)[:, :, 64]
                        TS("dve", RD[:, g * 4:(g + 1) * 4], dcol, 1e-30, None, ALU.max, ALU.bypass, accs[g].all(), RD.all())
                    op("dve", lambda e: e.reciprocal(RD[:, :], RD[:, :]), r=RD.all(), w=RD.all())
                    TT("dve", coef[:, :].rearrange("p (g q e) -> p g q e", g=2, q=2), RD[:, :].rearrange("p (g q e) -> p g q e", g=2, q=2),
                       gv[:, :, :, :, kbr], ALU.mult, RD.all() + gns.all(), coef.all())

                ncv = min(127, 8 * i + 7)
                sA, sB = banks[3], banks[4]
                for par, sb_ in ((0, sA), (1, sB)):
                    ps = slice(par * 64, par * 64 + 64)
                    for g in range(2):
                        PE(sb_[0:ncv, g * 256:(g + 1) * 256], KCc[ps, g, 0:ncv], QBr[ps, 2 * g:2 * g + 2, it], g == 0, g == 1,
                           KCc.all() + [QBr.k(i // 4)], sb_.all(), skip_group_check=True)
                EA, EB = Et[nE % 4], Et[(nE + 1) % 4]
                nE += 2
                for sb_, E_ in ((sA, EA), (sB, EB)):
                    ACT(E_[0:ncv, :], sb_[0:ncv, :], AF.Exp, sb_.all(), E_.all(), scale=0.125)
                    TT("pool", E_[0:ncv, :].rearrange("p (r t) -> p r t", r=4), E_[0:ncv, :].rearrange("p (r t) -> p r t", r=4),
                       CM[0:ncv, it].unsqueeze(1).to_broadcast([ncv, 4, 128]), ALU.mult, E_.all() + CM.all(), E_.all())
                accc = [banks[5], banks[6]]
                for g in range(2):
                    for par, E_ in ((0, EA), (1, EB)):
                        for e_ in range(2):
                            r_ = par * 2 + e_
                            blk = (g * 2 + e_) * 128
                            PE(accc[g][:, r_ * 97:(r_ + 1) * 97], E_[0:ncv, blk:blk + 128], VX[0:ncv, g, :], r_ == 0, r_ == 3,
                               E_.all() + VX.all(), accc[g].all(), skip_group_check=True)
                mk_coef(0, accc, 97)
                attn_out([Acc(accc[0], 97), Acc(accc[1], 97)], O, coef=coef, first=True)
                need_sel = i >= 8
                if need_sel:
                    for g in range(2):
                        TT("dve", imt[:, :, :], accc[g][:, 0:388].rearrange("p (r w) -> p r w", w=97)[:, :, 65:97],
                           RD[:, g * 4:(g + 1) * 4].unsqueeze(2).to_broadcast([128, 4, 32]), ALU.mult, accc[g].all() + RD.all(), imt.all())
                        op("dve", lambda e, g=g: e.tensor_reduce(imp[:, g, :], imt[:, :, :].rearrange("p r j -> p j r"), AX.X, ALU.add),
                           r=imt.all(), w=imp.all())
                        TT("dve", imp[:, g, :], imp[:, g, :], selAB[:, 0, i, :], ALU.mult, imp.all() + selAB.all(), imp.all())
                        TT("dve", imp[:, g, :], imp[:, g, :], selAB[:, 1, i, :], ALU.add, imp.all() + selAB.all(), imp.all())
                        op("dve", lambda e, g=g: e.max(top8[:, g, :], imp[:, g, :]), r=imp.all(), w=top8.all())
                        op("dve", lambda e, g=g: e.match_replace(imw[:, :], top8[:, g, :], imp[:, g, :], -3.0e38), r=imp.all() + top8.all(), w=imw.all())
                        op("dve", lambda e, g=g: e.max(top8[:, g, :], imw[:, :]), r=imw.all(), w=top8.all())
                        TS("dve", selb[:, g, :], imp[:, g, :], top8[:, g, 7:8], -30000.0, ALU.is_lt, ALU.mult, imp.all() + top8.all(), selb.all())
                    bk = banks[2]
                    pT = bfv(bk)
                    for g in range(2):
                        TR(pT[0:32, g * 128:(g + 1) * 128], selb[:, g, :], ident_b[:, :], selb.all() + ident_b.all(), bk.all())
                    CP("act", selT[:, :, :], pT[0:32, 0:256].rearrange("p (g t) -> p g t", g=2), bk.all(), selT.all())

                for kbr, (KK, VV) in ((2, (KW, VW)), (1, (KS, VS))):
                    j_lo = 0 if kbr == 1 else max(0, i - 4)
                    accb = [banks[7], banks[0]] if kbr == 2 else [banks[5], banks[6]]
                    def scores(j, kbr=kbr, KK=KK):
                        jt = slice(j * 128, (j + 1) * 128)
                        sA, sB = (banks[3], banks[4]) if (j % 2 == 0) else (banks[1], banks[2])
                        for par, sb_ in ((0, sA), (1, sB)):
                            ps = slice(par * 64, par * 64 + 64)
                            bias = None
                            if j == i:
                                bias = trib
                            elif kbr == 2 and j == i - 4:
                                bias = edgeb
                            selbias = (kbr == 1 and need_sel and j < i)
                            last_plain = (bias is None and not selbias)
                            for g in range(2):
                                PE(sb_[:, g * 256:(g + 1) * 256], KK[ps, g, jt], QB[ps, 2 * g:2 * g + 2, it], g == 0, (g == 1 and last_plain),
                                   [KK.k(j // 4), QB.k(i // 4)], sb_.all(), skip_group_check=True)
                            if bias is not None:
                                PE(sb_[:, :], ident_b[:, :], bias[:, :].unsqueeze(1).to_broadcast([128, 4, 128]), False, True,
                                   ident_b.all() + bias.all(), sb_.all(), skip_group_check=True)
                            elif selbias:
                                for g in range(2):
                                    PE(sb_[:, g * 256:(g + 1) * 256], expall[:, j, :], selT[:, g, :].unsqueeze(1).to_broadcast([32, 2, 128]), False, g == 1,
                                       expall.all() + selT.all(), sb_.all(), skip_group_check=True)

                    def exp_pv(j, VV=VV, accb=accb, j_lo=j_lo):
                        sA, sB = (banks[3], banks[4]) if (j % 2 == 0) else (banks[1], banks[2])
                        EA, EB = Et[cntE[0] % 4], Et[(cntE[0] + 1) % 4]
                        cntE[0] += 2
                        ACT(EA[:, :], sA[:, :], AF.Exp, sA.all(), EA.all(), scale=0.125)
                        ACT(EB[:, :], sB[:, :], AF.Exp, sB.all(), EB.all(), scale=0.125)
                        for g in range(2):
                            for par, E_ in ((0, EA), (1, EB)):
                                for e_ in range(2):
                                    r_ = par * 2 + e_
                                    blk = (g * 2 + e_) * 128
                                    PE(accb[g][:, r_ * 65:(r_ + 1) * 65], E_[:, blk:blk + 128], VV[:, j, g, :],
                                       (j == j_lo and r_ == 0), (j == i and r_ == 3), E_.all() + VV.all(), accb[g].all(), skip_group_check=True)

                    scores(j_lo)
                    for j in range(j_lo, i + 1):
                        if j + 1 <= i:
                            scores(j + 1)
                        exp_pv(j)
                    mk_coef(kbr, accb, 65)
                    attn_out([Acc(accb[0], 65), Acc(accb[1], 65)], O, coef=coef, first=False, tmpb=otmp)
                o_to_T(O, Ob, obT, i, banks[2])
            self.pop()
            self.pop()

        NB = 4

        def m3(b, oaT, obT):
            self.push()
            mergedT = self.sb("mergedT", [128, 8, SEQ], BF16, nreg=4)
            wout = self.sb("wout", [128, 8, D], BF16, nreg=2)
            g1t = self.sb("g1t", [128, D], F32)
            s2t = self.sb("s2t", [128, D], F32)
            h2t = self.sb("h2t", [128, D], F32)
            load_mod(g1t, b, 2)
            load_mod(h2t, b, 3)
            load_mod(s2t, b, 4)
            if do_mixer:
                self.push()
                uT = self.sb("uTm", [128, 8, SEQ], BF16, nreg=4)
                wbra = self.sb("wbra", [128, 4, D], BF16)
                wbrb = self.sb("wbrb", [128, 4, D], BF16)
                build_uT(b, uT)
                wst_ = self.sb("m3_wst", [128, 4, D], F32)
                DMA("sp", wst_[:, :, :], wbrA_d.rearrange("(k p) n -> p k n", p=128), [], wst_.all())
                CP("pool", wbra[:, :, :], wst_[:, :, :], wst_.all(), wbra.all())
                DMA("sp", wst_[:, :, :], wbrB_d.rearrange("(k p) n -> p k n", p=128), [], wst_.all())
                CP("pool", wbrb[:, :, :], wst_[:, :, :], wst_.all(), wbrb.all())
                wov = wout_d.rearrange("(k p) n -> p k n", p=128)
                for hh in range(2):
                    DMA("sp", wst_[:, :, :], wov[:, hh * 4:(hh + 1) * 4, :], [], wst_.all())
                    CP("pool", wout[:, hh * 4:(hh + 1) * 4, :], wst_[:, :, :], wst_.all(), [wout.k(hh)])
                gst = [self.sb(f"m3_gst{i}", [128, 8, 128], F32) for i in range(2)]
                gwb = [self.sb(f"m3_gwb{i}", [128, 8, 128], BF16) for i in range(4)]
                sga = [self.sb(f"m3_sga{i}", [128, 512], F32) for i in range(2)]
                sgb = [self.sb(f"m3_sgb{i}", [128, 512], F32) for i in range(2)]
                nl = 0
                for fc in range(8):
                    gws = []
                    for which in range(2):
                        st_, wb_ = gst[nl % 2], gwb[nl % 4]
                        nl += 1
                        DMA("sp", st_[:, :, :], wmG_d[which * 8 + fc], [], st_.all())
                        CP("pool", wb_[:, :, :], st_[:, :, :], st_.all(), wb_.all())
                        gws.append(wb_)
                    fs = slice(fc * 128, (fc + 1) * 128)
                    for tc in range(4):
                        ts_ = slice(tc * 512, (tc + 1) * 512)
                        bya, byb, bga, bgb = banks[0], banks[1], banks[2], banks[3]
                        if tc % 2 == 1:
                            bya, byb, bga, bgb = banks[4], banks[5], banks[6], banks[7]
                        for k in range(4):
                            PE(bya[:, :], wbra[:, k, fs], oaT[:, k, ts_], k == 0, k == 3, wbra.all() + [oaT.k(tc)], bya.all())
                        for k in range(4):
                            PE(byb[:, :], wbrb[:, k, fs], obT[:, k, ts_], k == 0, k == 3, wbrb.all() + [obT.k(tc)], byb.all())
                        for k in range(8):
                            PE(bga[:, :], gws[0][:, k, :], uT[:, k, ts_], k == 0, k == 7, gws[0].all() + [uT.k(tc)], bga.all())
                        for k in range(8):
                            PE(bgb[:, :], gws[1][:, k, :], uT[:, k, ts_], k == 0, k == 7, gws[1].all() + [uT.k(tc)], bgb.all())
                        SA, SB = sga[tc % 2], sgb[tc % 2]
                        ACT(SA[:, :], bga[:, :], AF.Sigmoid, bga.all(), SA.all())
                        ACT(SB[:, :], bgb[:, :], AF.Sigmoid, bgb.all(), SB.all())
                        TT("dve", SA[:, :], SA[:, :], bya[:, :], ALU.mult, SA.all() + bya.all(), SA.all())
                        TT("dve", SB[:, :], SB[:, :], byb[:, :], ALU.mult, SB.all() + byb.all(), SB.all())
                        TT("pool", mergedT[:, fc, ts_], SA[:, :], SB[:, :], ALU.add, SA.all() + SB.all(), [mergedT.k(tc)])
                self.pop()
            if "mergedT" in dbg and b == 0:
                self.push()
                stg = [self.sb(f"dbg_stg{i}", [128, SEQ], F32) for i in range(2)]
                nd = 0
                for nm, srcb, nk in (("mergedT", mergedT, 8), ("oaT", oaT, 4), ("obT", obT, 4)):
                    for k in range(nk):
                        sg_ = stg[nd % 2]
                        nd += 1
                        CP("dve", sg_[:, :], srcb[:, k, :], srcb.all(), sg_.all())
                        DMA("sp", dbg[nm][:, k, :], sg_[:, :], sg_.all(), [("dbg", nm, k)])
                self.pop()
            s2_bc = {b: s2t}
            h2_bc = {b: h2t}
            xt = [self.sb(f"xt{i}", [128, D], F32) for i in range(NB)]
            x1t = [self.sb(f"x1t{i}", [128, D], F32) for i in range(NB)]
            u2r = [self.sb(f"u2r{i}", [128, D], BF16) for i in range(NB)]
            u2T = [self.sb(f"u2T{i}", [128, 8, 128], BF16) for i in range(NB)]
            lnb = [(self.sb(f"ln_st{i}", [128, 2, 6], F32), self.sb(f"ln_mv{i}", [128, 2], F32),
                    self.sb(f"ln_rs{i}", [128, 1], F32)) for i in range(NB)]
            sc = [self.sb(f"r_sc{i}", [128, NE], F32) for i in range(NB)]
            ch = [self.sb(f"r_ch{i}", [128, NE], F32) for i in range(NB)]
            tmp = [self.sb(f"r_tmp{i}", [128, NE], F32) for i in range(NB)]
            selb = [self.sb(f"r_selb{i}", [128, NE], BF16) for i in range(NB)]
            Gm = [self.sb(f"r_G{i}", [128, NE], F32) for i in range(NB)]
            Dm = [self.sb(f"r_D{i}", [128, NE], F32) for i in range(NB)]
            sm = [self.sb(f"r_sm{i}", [128, 64], F32) for i in range(NB)]
            mixs = [self.sb(f"mixs{i}", [128, D], F32) for i in range(NB)]

            def phase1_tile(t):
                i = t % NB
                tt = t % TPS
                X, X1, U2, U2T, LB = xt[i], x1t[i], u2r[i], u2T[i], lnb[i]
                SC, CH, TMP, SELB, G, DD, SM = sc[i], ch[i], tmp[i], selb[i], Gm[i], Dm[i], sm[i]
                MX = mixs[i]
                rows = slice(t * 128, (t + 1) * 128)
                DMA("sp", X[:, :], x_d[rows, :], [], X.all())
                if do_mixer:
                    for hf in range(2):
                        bk = banks[hf]
                        with S.atomic():
                            for k in range(8):
                                PE(bk[:, :], mergedT[:, k, tt * 128:(tt + 1) * 128], wout[:, k, hf * 512:(hf + 1) * 512], k == 0, k == 7,
                                   [mergedT.k(tt // 4)] + wout.all(), bk.all())
                            TT("dve", MX[:, hf * 512:(hf + 1) * 512], bk[:, :], g1t[:, hf * 512:(hf + 1) * 512], ALU.mult, bk.all() + g1t.all(), MX.all())
                    if "mix" in dbg and b == 0:
                        DMA("sp", dbg["mix"][tt * 128:(tt + 1) * 128, :], MX[:, :], MX.all(), [("dbg", "mix", tt)])
                    STT(X[:, :], X[:, :], float(DN_ALPHA), MX[:, :], ALU.mult, ALU.add, X.all() + MX.all(), X.all())
                else:
                    ACT(X[:, :], X[:, :], AF.Copy, X.all(), X.all(), scale=float(DN_ALPHA))
                layernorm(X, X1, ln1g, ln1b, LB)
                DMA("sp", x1_d[rows, :], X1[:, :], X1.all(), [("x1_d", t)])
                op("dve", lambda e, X=X, X1=X1, b=b: e.tensor_tensor(X[:, :], X1[:, :], s2_bc[b][:, :], ALU.mult),
                   r=X1.all() + s2_bc[b].all(), w=X.all())
                op("dve", lambda e, X=X, U2=U2, b=b: e.tensor_tensor(U2[:, :], X[:, :], h2_bc[b][:, :], ALU.add),
                   r=X.all() + h2_bc[b].all(), w=U2.all())
                bkT = banks[4 + (t % 2)]
                pT = bkT.t[:, :].bitcast(BF16)
                with S.atomic():
                    for k in range(8):
                        op("pe", lambda e, k=k, U2=U2, pT=pT: e.transpose(pT[:, k * 128:(k + 1) * 128], U2[:, k * 128:(k + 1) * 128], ident_b[:, :]),
                           r=U2.all() + ident_b.all(), w=bkT.all())
                    op("act", lambda e, U2T=U2T, pT=pT: e.activation(U2T[:, :, :], pT.rearrange("p (k t) -> p k t", k=8), AF.Copy),
                       r=bkT.all(), w=U2T.all())
                bkR = banks[6 + (t % 2)]
                with S.atomic():
                    for k in range(8):
                        op("pe", lambda e, k=k, U2T=U2T, bkR=bkR: e.matmul(bkR[:, 0:NE], U2T[:, k, :], wr_b[:, k, :], start=(k == 0), stop=(k == 7)),
                           r=U2T.all() + wr_b.all(), w=bkR.all())
                    op("act", lambda e, SC=SC, bkR=bkR: e.activation(SC[:, :], bkR[:, 0:NE], AF.Sigmoid), r=bkR.all(), w=SC.all())
                op("dve", lambda e, SC=SC, CH=CH: e.tensor_tensor(CH[:, :], SC[:, :], rb_bc[:, :], ALU.add), r=SC.all() + rb_bc.all(), w=CH.all())
                ch3 = CH[:, :].rearrange("p (g j) -> p g j", g=8)
                tmp3 = TMP[:, :].rearrange("p (g j) -> p g j", g=8)
                m1, m2, gs, top4, keep, pen, top8, wsum, rw = (SM[:, 0:8], SM[:, 8:16], SM[:, 16:24], SM[:, 24:32], SM[:, 32:40],
                                                               SM[:, 40:48], SM[:, 48:56], SM[:, 56:57], SM[:, 57:58])
                op("dve", lambda e: e.tensor_reduce(m1, ch3, AX.X, ALU.max), r=CH.all(), w=SM.all())
                op("dve", lambda e: e.tensor_tensor(tmp3, ch3, m1.unsqueeze(2).to_broadcast([128, 8, 32]), ALU.is_equal),
                   r=CH.all() + SM.all(), w=TMP.all())
                op("dve", lambda e, TMP=TMP, CH=CH: e.scalar_tensor_tensor(TMP[:, :], TMP[:, :], -1e9, CH[:, :], ALU.mult, ALU.add),
                   r=TMP.all() + CH.all(), w=TMP.all())
                op("dve", lambda e: e.tensor_reduce(m2, tmp3, AX.X, ALU.max), r=TMP.all(), w=SM.all())
                op("dve", lambda e: e.tensor_tensor(gs, m1, m2, ALU.add), r=SM.all(), w=SM.all())
                op("dve", lambda e: e.max(top4, gs), r=SM.all(), w=SM.all())
                op("dve", lambda e: e.tensor_scalar(keep, gs, top4[:, 3:4], None, ALU.is_ge), r=SM.all(), w=SM.all())
                op("dve", lambda e: e.tensor_scalar(pen, keep, -1.0, 1e9, ALU.add, ALU.mult), r=SM.all(), w=SM.all())
                op("dve", lambda e: e.tensor_tensor(ch3, ch3, pen.unsqueeze(2).to_broadcast([128, 8, 32]), ALU.add),
                   r=CH.all() + SM.all(), w=CH.all())
                op("dve", lambda e, CH=CH: e.max(top8, CH[:, :]), r=CH.all(), w=SM.all())
                op("dve", lambda e, TMP=TMP, CH=CH: e.tensor_scalar(TMP[:, :], CH[:, :], top8[:, 7:8], None, ALU.is_ge),
                   r=CH.all() + SM.all(), w=TMP.all())
                op("pool", lambda e, TMP=TMP, SELB=SELB: e.tensor_copy(SELB[:, :], TMP[:, :]), r=TMP.all(), w=SELB.all())
                op("dve", lambda e, G=G, TMP=TMP, SC=SC: e.scalar_tensor_tensor(G[:, :], TMP[:, :], 1.0, SC[:, :], ALU.mult, ALU.mult, accum_out=wsum),
                   r=TMP.all() + SC.all(), w=G.all() + SM.all())
                op("dve", lambda e: e.reciprocal(rw, wsum), r=SM.all(), w=SM.all())
                op("dve", lambda e, G=G: e.tensor_scalar(G[:, :], G[:, :], rw, float(ROUTED_SCALE), ALU.mult, ALU.mult),
                   r=G.all() + SM.all(), w=G.all())
                S.cut()
                bkP = banks[2 + (t % 2)]
                op("pe", lambda e, bkP=bkP, SELB=SELB: e.matmul(bkP[:, 0:NE], tri_b[:, :], SELB[:, :], start=True, stop=True),
                   r=tri_b.all() + SELB.all(), w=bkP.all())
                op("pe", lambda e, bkP=bkP, SELB=SELB: e.matmul(bkP[:, NE:2 * NE], ones_b[:, :], SELB[:, :], start=False, stop=True),
                   r=ones_b.all() + SELB.all(), w=bkP.all())
                op("dve", lambda e, DD=DD, bkP=bkP: e.tensor_tensor(DD[:, :], bkP[:, 0:NE], carry[:, :], ALU.add),
                   r=bkP.all() + carry.all(), w=DD.all())
                op("dve", lambda e, bkP=bkP: e.tensor_tensor(carry[:, :], carry[:, :], bkP[:, NE:2 * NE], ALU.add),
                   r=bkP.all() + carry.all(), w=carry.all())
                S.cut()
                op("dve", lambda e, DD=DD: e.tensor_tensor(DD[:, :], DD[:, :], iotaC[:, :], ALU.add), r=DD.all() + iotaC.all(), w=DD.all())
                for k in range(8):
                    op("dve", lambda e, k=k, TMP=TMP, CH=CH, DD=DD, t=t: e.scalar_tensor_tensor(
                        TMP[:, :], CH[:, :], top8[:, k:k + 1], DD[:, :], ALU.is_equal, ALU.mult, accum_out=dest_f[:, t, k:k + 1]),
                       r=CH.all() + SM.all() + DD.all(), w=TMP.all() + [dest_f.k(t)])
                    op("dve", lambda e, k=k, TMP=TMP, CH=CH, G=G, t=t: e.scalar_tensor_tensor(
                        TMP[:, :], CH[:, :], top8[:, k:k + 1], G[:, :], ALU.is_equal, ALU.mult, accum_out=wk[:, t, k:k + 1]),
                       r=CH.all() + SM.all() + G.all(), w=TMP.all() + [wk.k(t)])
                DMA("sp", u2_d[rows, :], U2[:, :], U2.all(), [("u2_d", t)])

            for t0 in range(b * TPS, min((b + 1) * TPS, n_tiles), NB):
                grp = [t for t in range(t0, min(t0 + NB, n_tiles, (b + 1) * TPS))]
                parts = [S.record_parts(lambda t=t: phase1_tile(t)) for t in grp]
                S.merge_rr([p[0] for p in parts])
                S.merge_seq([p[1] for p in parts])
                S.merge_rr([p[2] for p in parts])
            self.pop()

        for b in range(BPC):
            if b * TPS >= n_tiles:
                break
            self.push()
            oaT = self.sb("oaT", [128, 4, SEQ], BF16, nreg=4)
            obT = self.sb("obT", [128, 4, SEQ], BF16, nreg=4)
            if do_mixer:
                dsa(b, oaT)
                nsa(b, obT)
            m3(b, oaT, obT)
            self.pop()

        if "dest" in dbg:
            DMA("sp", dbg["dest"], dest_f[:, :, :], dest_f.all(), [("dbg", 0)])
        if "wk" in dbg:
            DMA("sp", dbg["wk"], wk[:, :, :], wk.all(), [("dbg", 1)])

        self.push()
        nb_f = self.sb("nb_f", [128, NE], F32)
        nb_t = self.sb("nb_t", [128, NE], F32)
        nb_i = self.sb("nb_i", [128, NE], I32)
        nb_b = self.sb("nb_b", [128, NE], BF16)
        nbT = self.sb("nbT", [128, 2, 128], BF16)
        startr = self.sb("startr", [128, NE], F32)
        endb = self.sb("endb", [128, NE], F32)
        blkE = self.sb("blkE", [128, 4], F32)
        Rm = self.sb("Rm", [128, NBLK // 128, 128], BF16)
        widx_f = self.sb("widx_f", [128, NBLK], F32)
        widx_i = self.sb("widx_i", [128, NBLK], I32)
        TS("dve", nb_f[:, :], carry[:, :], float(BLK - 1), 1.0 / BLK, ALU.add, ALU.mult, carry.all(), nb_f.all())
        CP("dve", nb_i[:, :], nb_f[:, :], nb_f.all(), nb_i.all())
        CP("dve", nb_t[:, :], nb_i[:, :], nb_i.all(), nb_t.all())
        TT("dve", nb_f[:, :], nb_t[:, :], nb_f[:, :], ALU.is_gt, nb_t.all() + nb_f.all(), nb_f.all())
        TT("dve", nb_f[:, :], nb_t[:, :], nb_f[:, :], ALU.subtract, nb_t.all() + nb_f.all(), nb_f.all())
        CP("dve", nb_b[:, :], nb_f[:, :], nb_f.all(), nb_b.all())
        bk = banks[0]
        pT = bfv(bk)
        for a_ in range(2):
            TR(pT[:, a_ * 128:(a_ + 1) * 128], nb_b[:, a_ * 128:(a_ + 1) * 128], ident_b[:, :], nb_b.all() + ident_b.all(), bk.all())
        CP("act", nbT[:, :, :], pT[:, 0:256].rearrange("p (a t) -> p a t", a=2), bk.all(), nbT.all())
        bk = banks[1]
        for a_ in range(2):
            PE(bk[:, 0:NE], nbT[:, a_, :], Utri[:, a_, :], a_ == 0, a_ == 1, nbT.all() + Utri.all(), bk.all())
        TS("dve", startr[:, :], bk[:, 0:NE], float(BLK), None, ALU.mult, ALU.bypass, bk.all(), startr.all())
        TT("dve", endb[:, :], bk[:, 0:NE], nb_f[:, :], ALU.add, bk.all() + nb_f.all(), endb.all())
        for a_ in range(NBLK // 128):
            TS("dve", nb_t[:, :], endb[:, :], jcol[:, a_:a_ + 1], None, ALU.is_le, ALU.add, endb.all() + jcol.all(), nb_t.all() + blkE.all(),
               accum=blkE[:, a_:a_ + 1])
        for a_ in range(NBLK // 128):
            TS("dve", Rm[:, a_, :], ident_b[:, :], blkE[:, a_:a_ + 1], None, ALU.mult, ALU.bypass, ident_b.all() + blkE.all(), Rm.all())
        bk = banks[2]
        PE(bk[:, 0:NBLK], ones_b[:, :], Rm[:, :, :].rearrange("p a t -> p (a t)"), True, True, ones_b.all() + Rm.all(), bk.all())
        TS("dve", widx_f[:, :], bk[:, 0:NBLK], 128.0, pcol[:, 0:1], ALU.mult, ALU.add, bk.all() + pcol.all(), widx_f.all())
        CP("dve", widx_i[:, :], widx_f[:, :], widx_f.all(), widx_i.all())
        if "blkE" in dbg:
            DMA("sp", dbg["blkE"], blkE[:, :], blkE.all(), [("dbg", "blkE")])
            DMA("sp", dbg["startr"], startr[:, :], startr.all(), [("dbg", "startr")])

        self.push()
        U2s = [self.sb(f"p2_u{i}", [128, D], BF16) for i in range(4)]
        p2 = [self.sb(f"p2_s{i}", [128, 40], F32) for i in range(4)]
        p2i = [self.sb(f"p2_i{i}", [128, 8], I32) for i in range(4)]
        p2j = [self.sb(f"p2_j{i}", [128, NE], F32) for i in range(4)]
        def pass2_tile(t):
            U2 = U2s[t % 4]
            P2, P2I, P2J = p2[t % 4], p2i[t % 4], p2j[t % 4]
            ef, et, ps_, sl = P2[:, 0:8], P2[:, 8:16], P2[:, 16:24], P2[:, 24:32]
            rows = slice(t * 128, (t + 1) * 128)
            DMA("sp", U2[:, :], u2_d[rows, :], [("u2_d", t)], U2.all())
            TS("dve", ef, dest_f[:, t, :], 1.0 / 4096.0, None, ALU.mult, ALU.bypass, [dest_f.k(t)], P2.all())
            CP("dve", P2I[:, :], ef, P2.all(), P2I.all())
            CP("dve", et, P2I[:, :], P2I.all(), P2.all())
            TT("dve", ps_, et, ef, ALU.is_gt, P2.all(), P2.all())
            TT("dve", et, et, ps_, ALU.subtract, P2.all(), P2.all())
            STT(ps_, et, -4096.0, dest_f[:, t, :], ALU.mult, ALU.add, P2.all() + [dest_f.k(t)], P2.all())
            for k in range(8):
                STT(P2J[:, :], iotaE1[:, :], et[:, k:k + 1], startr[:, :], ALU.is_equal, ALU.mult, iotaE1.all() + P2.all() + startr.all(),
                    P2J.all() + P2.all(), accum=sl[:, k:k + 1])
            TT("dve", sl, sl, ps_, ALU.add, P2.all(), P2.all())
            CP("dve", dest_i[:, t, :], sl, P2.all(), [dest_i.k(t)])
            for k in range(8):
                op("pool", lambda e, k=k, t=t, U2=U2: e.indirect_dma_start(
                    out=xs_d[:, :], out_offset=IOA(ap=dest_i[:, t, k:k + 1], axis=0), in_=U2[:, :], in_offset=None,
                    bounds_check=bc_reg(e), oob_is_err=False),
                   r=U2.all() + [dest_i.k(t)], w=[("xs_d", "all")], dma=True)
        for t0 in range(0, n_tiles, 4):
            grp = list(range(t0, min(t0 + 4, n_tiles)))
            parts = [S.record_parts(lambda t=t: pass2_tile(t)) for t in grp]
            S.merge_rr([p[0] for p in parts])
        self.pop()
        if "desti" in dbg:
            self.push()
            dtmp = self.sb("dtmp", [128, NT, 8], F32)
            CP("dve", dtmp[:, :, :], dest_i[:, :, :], dest_i.all(), dtmp.all())
            DMA("sp", dbg["desti"], dtmp[:, :, :], dtmp.all(), [("dbg", "desti")])
            self.pop()

        n_blk = cfg.get("n_blk", NBLK)

        wgv = wg_d.rearrange("e (p k) n -> (e p) (k n)", k=8)
        wuv = wu_d.rearrange("e (p k) n -> (e p) (k n)", k=8)
        wdv = wd_d.rearrange("e (p h) n -> (e p) (h n)", h=2)

        def wreg(e):
            if "wb" not in _regs:
                _regs["wb"] = e.to_reg(NE * 128 - 1)
            return _regs["wb"]

        NWB = 3
        w_st = [self.sb(f"w_st{i}", [128, 6144], F32, nreg=3) for i in range(NWB)]
        w_bf = [self.sb(f"w_bf{i}", [128, 6144], BF16, nreg=3) for i in range(NWB)]
        hsel = [self.sb(f"hsel{i}", [128, D], BF16) for i in range(3)]
        hselT = [self.sb(f"hselT{i}", [128, 8, 128], BF16) for i in range(2)]
        sg = [self.sb(f"sg{i}", [128, 256], F32) for i in range(2)]
        actk = [self.sb(f"actk{i}", [128, 256], BF16) for i in range(2)]
        actT = [self.sb(f"actT{i}", [128, 2, 128], BF16) for i in range(2)]
        ysb = [self.sb(f"ysb{i}", [128, D], BF16, nreg=2) for i in range(3)]

        hselT3 = [self.sb(f"hselT3_{i}", [128, 8, 128], BF16) for i in range(3)]
        w_bf4 = w_bf + [self.sb("w_bf3", [128, 6144], BF16, nreg=3)]
        NWF = len(w_bf4)

        def stageA0(j):
            WS, HS = w_st[j % NWB], hsel[j % 3]
            for q, wv in enumerate((wgv, wuv, wdv)):
                op("pool", lambda e, q=q, wv=wv: e.indirect_dma_start(
                    out=WS[:, q * 2048:(q + 1) * 2048], out_offset=None, in_=wv, in_offset=IOA(ap=widx_i[:, j:j + 1], axis=0),
                    bounds_check=wreg(e), oob_is_err=False),
                   r=widx_i.all(), w=[WS.k(q)], dma=True)
            DMA("sp", HS[:, :], xs_d[j * BLK:(j + 1) * BLK, :], [("xs_d", "all")], HS.all())

        def stageA(j):
            WS, WB, HS, HT = w_st[j % NWB], w_bf4[j % NWF], hsel[j % 3], hselT3[j % 3]
            WBgu = WB[:, 0:4096].rearrange("p (k two n) -> p k two n", k=8, two=2)
            CP("act", WBgu[:, :, 0, :], WS[:, 0:2048].rearrange("p (k n) -> p k n", k=8), [WS.k(0)], [WB.k(0)])
            CP("dve", WBgu[:, :, 1, :], WS[:, 2048:4096].rearrange("p (k n) -> p k n", k=8), [WS.k(1)], [WB.k(1)])
            CP("dve", WB[:, 4096:6144], WS[:, 4096:6144], [WS.k(2)], [WB.k(2)])
            bkT = banks[j % 2]
            pT_ = bfv(bkT)
            with S.atomic():
                for k in range(8):
                    TR(pT_[:, k * 128:(k + 1) * 128], HS[:, k:D:8], ident_b[:, :], HS.all() + ident_b.all(), bkT.all())
                CP("act", HT[:, :, :], pT_.rearrange("p (k t) -> p k t", k=8), bkT.all(), HT.all())

        def stageB1(j):
            i = j % 2
            WB, HT, SG, AK = w_bf4[j % NWF], hselT3[j % 3], sg[i], actk[i]
            bk_ = banks[2 + j % 2]
            with S.atomic():
                for k in range(8):
                    PE(bk_[:, :], HT[:, k, :], WB[:, k * 512:(k + 1) * 512], k == 0, k == 7, HT.all() + [WB.k(0), WB.k(1)], bk_.all())
                ACT(SG[:, :], bk_[:, 0:256], AF.Silu, bk_.all(), SG.all())
                TT("dve", AK[:, :], SG[:, :], bk_[:, 256:512], ALU.mult, SG.all() + bk_.all(), AK.all())

        def stageB2(j):
            i = j % 2
            AK, AT = actk[i], actT[i]
            bk2 = banks[4 + j % 2]
            pT2 = bfv(bk2)
            with S.atomic():
                for h in range(2):
                    TR(pT2[:, h * 128:(h + 1) * 128], AK[:, h:256:2], ident_b[:, :], AK.all() + ident_b.all(), bk2.all())
                CP("act", AT[:, :, :], pT2[:, 0:256].rearrange("p (h t) -> p h t", h=2), bk2.all(), AT.all())

        def stageB3(j):
            i = j % 2
            WB, AT, YS = w_bf4[j % NWF], actT[i], ysb[j % 3]
            for hf in range(2):
                bkd = banks[6 + hf]
                with S.atomic():
                    for h in range(2):
                        col = 4096 + h * 1024 + hf * 512
                        PE(bkd[:, :], AT[:, h, :], WB[:, col:col + 512], h == 0, h == 1, AT.all() + [WB.k(2)], bkd.all())
                    CP("act" if hf == 0 else "dve", YS[:, hf * 512:(hf + 1) * 512], bkd[:, :], bkd.all(), [YS.k(hf)])
            DMA("sp", ys_d[j * BLK:(j + 1) * BLK, :], YS[:, :], YS.all(), [("ys_d", j)])

        stages = [(0, stageA0), (2, stageA), (3, stageB1), (4, stageB2), (5, stageB3)]
        for s_ in range(n_blk + 5):
            streams = []
            for off, st_fn in stages:
                j = s_ - off
                if 0 <= j < n_blk:
                    streams.append(S.record_parts(lambda j=j, st_fn=st_fn: st_fn(j))[0])
            S.merge_rr(list(reversed(streams)))
        ys_keys = [("ys_d", j) for j in range(n_blk)]
        self.pop()
        self.push()
        ln2g = self.sb("ln2g", [128, D], F32)
        ln2b = self.sb("ln2b", [128, D], F32)
        DMA("sp", ln2g[:, :], ln_d[2:3, :].to_broadcast([128, D]), [], ln2g.all())
        DMA("sp", ln2b[:, :], ln_d[3:4, :].to_broadcast([128, D]), [], ln2b.all())
        s2_bc, h2_bc, g2_bc = {}, {}, {}
        for b in range(BPC):
            g2_bc[b] = self.sb(f"f_g2{b}", [128, D], F32)
            load_mod(g2_bc[b], b, 5)
        GF = 4
        wsh = self.sb("wsh", [128, 6144], BF16, nreg=3)
        self.push()
        wsh_st = self.sb("wsh_st", [128, 6144], F32, nreg=3)
        DMA("sp", wsh_st[:, 0:2048].rearrange("p (k n) -> p k n", k=8), wsg_d.rearrange("(k p) n -> p k n", p=128), [], [wsh_st.k(0)])
        DMA("sp", wsh_st[:, 2048:4096].rearrange("p (k n) -> p k n", k=8), wsu_d.rearrange("(k p) n -> p k n", p=128), [], [wsh_st.k(1)])
        DMA("sp", wsh_st[:, 4096:6144].rearrange("p (k n) -> p k n", k=2), wsd_d.rearrange("(k p) n -> p k n", p=128), [], [wsh_st.k(2)])
        CP("act", wsh[:, 0:2048], wsh_st[:, 0:2048], [wsh_st.k(0)], [wsh.k(0)])
        CP("dve", wsh[:, 2048:4096], wsh_st[:, 2048:4096], [wsh_st.k(1)], [wsh.k(1)])
        CP("pool", wsh[:, 4096:6144], wsh_st[:, 4096:6144], [wsh_st.k(2)], [wsh.k(2)])
        self.pop()
        xt = [self.sb(f"fxt{i}", [128, D], F32) for i in range(GF)]
        x1t = [self.sb(f"fx1t{i}", [128, D], F32) for i in range(GF)]
        u2r = [self.sb(f"fu2r{i}", [128, D], BF16) for i in range(GF)]
        u2T = [self.sb(f"fu2T{i}", [128, 8, 128], BF16) for i in range(GF)]
        lnb = [(self.sb(f"fln_st{i}", [128, 2, 6], F32), self.sb(f"fln_mv{i}", [128, 2], F32),
                self.sb(f"fln_rs{i}", [128, 1], F32)) for i in range(GF)]
        yg = [self.sb(f"yg{i}", [128, 8, D], BF16, nreg=8) for i in range(GF)]
        acc = [self.sb(f"acc{i}", [128, D], F32) for i in range(GF)]
        sgs = [self.sb(f"sgs{i}", [128, 256], F32) for i in range(GF)]
        atk = [self.sb(f"atk{i}", [128, 256], BF16) for i in range(GF)]
        ats = [self.sb(f"ats{i}", [128, 2, 128], BF16) for i in range(GF)]

        def final_tile(t):
            b = t // TPS
            i = t % GF
            X, X1, U2, U2T, LB = xt[i], x1t[i], u2r[i], u2T[i], lnb[i]
            YG, ACC, SGS, ATK, ATS = yg[i], acc[i], sgs[i], atk[i], ats[i]
            rows = slice(t * 128, (t + 1) * 128)
            DMA("sp", X1[:, :], x1_d[rows, :], [("x1_d", t)], X1.all())
            DMA("sp", U2[:, :], u2_d[rows, :], [("u2_d", t)], U2.all())
            for k in range(8):
                op("pool", lambda e, k=k: e.indirect_dma_start(
                    out=YG[:, k, :], out_offset=None, in_=ys_d[:, :], in_offset=IOA(ap=dest_i[:, t, k:k + 1], axis=0),
                    bounds_check=bc_reg(e), oob_is_err=False),
                   r=ys_keys + [dest_i.k(t)], w=[YG.k(k)], dma=True)
            bkT = banks[t % 2]
            pT = bfv(bkT)
            with S.atomic():
                for k in range(8):
                    TR(pT[:, k * 128:(k + 1) * 128], U2[:, k * 128:(k + 1) * 128], ident_b[:, :], U2.all() + ident_b.all(), bkT.all())
                CP("act", U2T[:, :, :], pT.rearrange("p (k t) -> p k t", k=8), bkT.all(), U2T.all())
            bk = banks[2 + (t % 2)]
            with S.atomic():
                for k in range(8):
                    PE(bk[:, 0:256], U2T[:, k, :], wsh[:, k * 256:(k + 1) * 256], k == 0, False, U2T.all() + [wsh.k(0)], bk.all(), skip_group_check=True)
                for k in range(8):
                    PE(bk[:, 256:512], U2T[:, k, :], wsh[:, 2048 + k * 256:2048 + (k + 1) * 256], False, k == 7, U2T.all() + [wsh.k(1)], bk.all(), skip_group_check=True)
                ACT(SGS[:, :], bk[:, 0:256], AF.Silu, bk.all(), SGS.all())
                TT("dve", ATK[:, :], SGS[:, :], bk[:, 256:512], ALU.mult, SGS.all() + bk.all(), ATK.all())
            bk2 = banks[4 + (t % 2)]
            pT2 = bfv(bk2)
            with S.atomic():
                for h in range(2):
                    TR(pT2[:, h * 128:(h + 1) * 128], ATK[:, h * 128:(h + 1) * 128], ident_b[:, :], ATK.all() + ident_b.all(), bk2.all())
                CP("act", ATS[:, :, :], pT2[:, 0:256].rearrange("p (h t) -> p h t", h=2), bk2.all(), ATS.all())
            for hf in range(2):
                bkd = banks[6 + hf]
                with S.atomic():
                    for h in range(2):
                        col = 4096 + h * 1024 + hf * 512
                        PE(bkd[:, :], ATS[:, h, :], wsh[:, col:col + 512], h == 0, h == 1, ATS.all() + [wsh.k(2)], bkd.all())
                    CP("act", ACC[:, hf * 512:(hf + 1) * 512], bkd[:, :], bkd.all(), ACC.all())
            for k in range(8):
                STT(ACC[:, :], YG[:, k, :], wk[:, t, k:k + 1], ACC[:, :], ALU.mult, ALU.add, [YG.k(k), wk.k(t)] + ACC.all(), ACC.all())
            TT("dve", ACC[:, :], ACC[:, :], g2_bc[b][:, :], ALU.mult, ACC.all() + g2_bc[b].all(), ACC.all())
            STT(ACC[:, :], X1[:, :], float(DN_ALPHA), ACC[:, :], ALU.mult, ALU.add, ACC.all() + X1.all(), ACC.all())
            layernorm(ACC, X, ln2g, ln2b, LB, eng="dve")
            DMA("sp", out_d[rows, :], X[:, :], X.all(), [("out_d", t)])

        for t0 in range(0, n_tiles, GF):
            grp = list(range(t0, min(t0 + GF, n_tiles)))
            parts = [S.record_parts(lambda t=t: final_tile(t)) for t in grp]
            S.merge_rr([p[0] for p in parts])
        S.finalize_and_emit()
        return nc


_OFF = dict(q_a=0, k_a=512, v_a=640, q_idx=768, k_idx=1024, w_idx=1056, q_b=1064, k_cmp=1576, v_cmp=1704,
            k_sel=1832, v_sel=1960, k_win=2088, v_win=2216, g_nsa=2344, gate_a=2368, gate_b=3392)


def _sw64(d):
    return d + 8 if d < 8 else (d - 8 if d < 16 else d)


def _sw32(d):
    return d + 4 if d < 4 else (d - 4 if d < 8 else d)


def _chunks_A():
    O = _OFF
    ch = []
    for c in range(4):
        ch.append([O["q_a"] + c * 128 + p for p in range(128)])
    for c in range(4):
        ch.append([O["q_a"] + c * 128 + (p // 64) * 64 + _sw64(p % 64) for p in range(128)])
    for g in range(2):
        ch.append([O["k_a"] + g * 64 + (p % 64) for p in range(128)])
    for g in range(2):
        ch.append([O["k_a"] + g * 64 + _sw64(p % 64) for p in range(128)])
    for sw in (False, True):
        for c in range(3):
            cols = []
            for p in range(128):
                h = min(7, 3 * c + min(p, 95) // 32)
                d = p % 32
                cols.append(O["q_idx"] + h * 32 + (_sw32(d) if sw else d))
            ch.append(cols)
    ch.append([O["k_idx"] + (p % 32) for p in range(128)])
    ch.append([O["k_idx"] + _sw32(p % 32) for p in range(128)])
    return ch


def _chunks_B():
    O = _OFF
    ch = []
    for c in range(4):
        ch.append([O["q_b"] + c * 128 + p for p in range(128)])
    for c in range(4):
        ch.append([O["q_b"] + c * 128 + (p // 64) * 64 + _sw64(p % 64) for p in range(128)])
    ch.append([O["k_cmp"] + p for p in range(128)])
    ch.append([O["v_cmp"] + p for p in range(128)])
    for nm in ("k_sel", "k_win"):
        for g in range(2):
            ch.append([O[nm] + g * 64 + (p % 64) for p in range(128)])
        for g in range(2):
            ch.append([O[nm] + g * 64 + _sw64(p % 64) for p in range(128)])
    return ch


N_CH_A = 20
N_CH_B = 18


def _const_tables():
    theta = 500000.0
    cst = np.zeros((128, 8), np.float32)
    inv64 = (np.float32(theta) ** (-(np.arange(8, dtype=np.float32) * np.float32(2.0)) / np.float32(16))).astype(np.float32)
    inv32 = (np.float32(theta) ** (-(np.arange(4, dtype=np.float32) * np.float32(2.0)) / np.float32(8))).astype(np.float32)
    for p in range(128):
        d = p % 64
        if d < 16:
            cst[p, 0] = inv64[d % 8]
            cst[p, 1] = -1.0 if d < 8 else 1.0
        d2 = p % 32
        if p < 96 and d2 < 8:
            cst[p, 2] = inv32[d2 % 4]
            cst[p, 3] = -1.0 if d2 < 4 else 1.0
    selAB = np.zeros((128, 2, 16, 32), np.float32)
    for i in range(16):
        for tl in range(128):
            t = i * 128 + tl
            cur = t // 64
            for j in range(32):
                forced = (j == 0) or (0 <= cur - j < 2)
                causal = j * 64 <= t
                if forced:
                    selAB[tl, 0, i, j] = 0.0
                    selAB[tl, 1, i, j] = 1e9
                elif causal:
                    selAB[tl, 0, i, j] = 1.0
                else:
                    selAB[tl, 1, i, j] = -1e30
    expall = np.zeros((32, 16, 128), np.float32)
    for jj in range(16):
        for s in range(128):
            expall[2 * jj + s // 64, jj, s] = 1.0
    ov = np.zeros((128, 32), np.float32)
    for c in range(127):
        cs = 16 * c
        for j in range(32):
            ss = 64 * j
            if cs <= ss + 63 and cs + 31 >= ss:
                ov[c, j] = 1.0
    return cst, selAB, expall, ov


def _prep_inputs(inputs):
    f32 = lambda a: np.ascontiguousarray(np.asarray(a, dtype=np.float32))
    x = f32(inputs["x"])
    c = f32(inputs["c"])
    pos = np.ascontiguousarray(np.asarray(inputs["positions"], dtype=np.int32))
    w_in = f32(inputs["w_in"][0])
    w3 = w_in.reshape(8, 128, w_in.shape[1])

    def gather(chunks):
        return np.ascontiguousarray(np.stack([w3[:, :, cols].transpose(1, 0, 2) for cols in chunks]))

    O = _OFF
    tokA = list(range(O["v_a"], O["v_a"] + 128)) + list(range(O["w_idx"], O["w_idx"] + 8))
    tokB = list(range(O["v_sel"], O["v_sel"] + 128)) + list(range(O["v_win"], O["v_win"] + 128)) + list(range(O["g_nsa"], O["g_nsa"] + 24))
    gch = [[O["gate_a"] + cc * 128 + p for p in range(128)] for cc in range(8)] + \
          [[O["gate_b"] + cc * 128 + p for p in range(128)] for cc in range(8)]
    cst, selAB, expall, ov = _const_tables()
    cpos = np.stack([f32(inputs["cmp_pos_k"][0]).T, f32(inputs["cmp_pos_v"][0]).T], axis=1)
    cposT = np.ascontiguousarray(np.concatenate([cpos, cpos], axis=0))
    shared = {
        "w_ada": f32(inputs["w_ada"][0]),
        "b_ada": f32(inputs["b_ada"][0]).reshape(1, -1),
        "ln": f32(np.stack([inputs["ln1_g"][0], inputs["ln1_b"][0], inputs["ln2_g"][0], inputs["ln2_b"][0]])),
        "w_router": f32(inputs["w_router"][0]),
        "router_bias": f32(inputs["router_bias"][0]).reshape(1, -1),
        "w_exp_gate": f32(inputs["w_exp_gate"][0]),
        "w_exp_up": f32(inputs["w_exp_up"][0]),
        "w_exp_down": f32(inputs["w_exp_down"][0]),
        "w_sh_gate": f32(inputs["w_sh_gate"][0]),
        "w_sh_up": f32(inputs["w_sh_up"][0]),
        "w_sh_down": f32(inputs["w_sh_down"][0]),
        "wmA": gather(_chunks_A()),
        "wmB": gather(_chunks_B()),
        "wmG": gather(gch),
        "wtA": np.ascontiguousarray(w3[:, :, tokA].transpose(1, 0, 2)),
        "wtB": np.ascontiguousarray(w3[:, :, tokB].transpose(1, 0, 2)),
        "w_br_a": f32(inputs["w_br_a"][0]),
        "w_br_b": f32(inputs["w_br_b"][0]),
        "w_out": f32(inputs["w_out"][0]),
        "cmp_k_w1": f32(inputs["cmp_k_w1"][0]),
        "cmp_v_w1": f32(inputs["cmp_v_w1"][0]),
        "cmp_k_w2": f32(inputs["cmp_k_w2"][0]),
        "cmp_v_w2": f32(inputs["cmp_v_w2"][0]),
        "cmp_posT": cposT,
        "cst": cst,
        "selAB": selAB,
        "expall": expall,
        "ovl": ov,
    }
    in_maps = []
    for i in range(N_CORES):
        m = dict(shared)
        m["x"] = np.ascontiguousarray(x[BPC * i:BPC * (i + 1)].reshape(TOK, D))
        cc = c[BPC * i:BPC * (i + 1)]
        m["cT"] = np.ascontiguousarray(cc.reshape(BPC, 8, 128).transpose(2, 1, 0))
        m["positions"] = np.ascontiguousarray(pos[BPC * i:BPC * (i + 1)])
        in_maps.append(m)
    return in_maps


def kernel(**inputs):
    in_maps = _prep_inputs(inputs)
    prog = Prog()
    nc = prog.build()
    res = run_bass_kernel_spmd(nc, in_maps, core_ids=list(range(N_CORES)))
    outs = [np.asarray(r["out"], dtype=np.float32).reshape(BPC, SEQ, D) for r in res.results]
    return np.concatenate(outs, axis=0)
```

```python
import numpy as np
import concourse.bass as bass
import concourse.mybir as mybir
from concourse.bass_utils import run_bass_kernel_spmd

F32 = mybir.dt.float32
BF16 = mybir.dt.bfloat16
I32 = mybir.dt.int32
ALU = mybir.AluOpType
AF = mybir.ActivationFunctionType
AX = mybir.AxisListType
IOA = bass.IndirectOffsetOnAxis

N_CORES = 8
D = 1024
SEQ = 2048
BPC = 2
TOK = BPC * SEQ
NT = TOK // 128
NE = 256
BLK = 128
NBLK = TOK * 8 // BLK + NE
NSLOT = NBLK * BLK
CAP = BLK
DN_ALPHA = 2.0 ** 0.25
LN_EPS = 1e-5
ROUTED_SCALE = 2.5
EPOCH = 16000


class _Op:
    __slots__ = ("eng", "fn", "deps", "is_dma", "need_sig", "sig", "waits", "idx", "slot_prev", "r", "w", "is_barrier")


class Sched:
    ENGS = ("pe", "dve", "act", "pool", "sp")
    NSLOTS = {"sp": 12, "act": 4, "pool": 16}

    def __init__(self, nc):
        self.nc = nc
        self.cur = []
        self._atom = None
        self.ops = {e: [] for e in self.ENGS}
        self.allops = []
        self.lastw = {}
        self.readers = {}

    def op(self, eng, fn, r=(), w=(), dma=False):
        o = _Op()
        o.eng, o.fn, o.is_dma, o.deps, o.need_sig, o.sig, o.waits = eng, fn, dma, [], dma, None, []
        o.r, o.w, o.is_barrier, o.slot_prev = list(r), list(w), False, None
        if self._atom is not None:
            self._atom.append(o)
        else:
            self.cur.append([o])
        return o

    def barrier(self):
        o = _Op()
        o.eng, o.fn, o.is_dma, o.deps, o.need_sig, o.sig, o.waits = None, None, False, [], False, None, []
        o.r, o.w, o.is_barrier, o.slot_prev = [], [], True, None
        assert self._atom is None
        self.cur.append([o])

    class _Atomic:
        def __init__(self, s):
            self.s = s

        def __enter__(self):
            self.nested = self.s._atom is not None
            if not self.nested:
                self.s._atom = []

        def __exit__(self, *a):
            if not self.nested:
                seg, self.s._atom = self.s._atom, None
                if seg:
                    self.s.cur.append(seg)

    def atomic(self):
        return Sched._Atomic(self)

    def record_parts(self, body):
        old = self.cur
        self._parts = []
        self.cur = []
        body()
        self._parts.append(self.cur)
        parts, self._parts = self._parts, None
        self.cur = old
        return parts

    def cut(self):
        if getattr(self, "_parts", None) is not None:
            self._parts.append(self.cur)
            self.cur = []

    def merge_rr(self, streams):
        streams = [list(s) for s in streams]
        pos = [0] * len(streams)
        alive = True
        while alive:
            alive = False
            for i, s in enumerate(streams):
                if pos[i] < len(s):
                    self.cur.append(s[pos[i]])
                    pos[i] += 1
                    alive = True

    def merge_seq(self, streams):
        for s in streams:
            self.cur.extend(s)

    def _track(self, o):
        o.idx = len(self.allops)
        seen = set()

        def add(d):
            if d is not None and d.idx not in seen:
                seen.add(d.idx)
                o.deps.append(d)

        for k in o.r:
            add(self.lastw.get(k))
        for k in o.w:
            add(self.lastw.get(k))
            rd = self.readers.get(k)
            if rd:
                for d in rd[0].values():
                    add(d)
                for d in rd[1]:
                    add(d)
        for k in o.r:
            rd = self.readers.setdefault(k, ({}, []))
            if o.is_dma:
                rd[1].append(o)
            else:
                rd[0][o.eng] = o
        for k in o.w:
            self.lastw[k] = o
            self.readers[k] = ({}, [])
        self.ops[o.eng].append(o)
        self.allops.append(o)

    def _track_barrier(self):
        deps = []
        for e in self.ENGS:
            got_c = False
            nd = 0
            for o in reversed(self.ops[e]):
                if o.fn is None:
                    continue
                if o.is_dma:
                    if nd < self.NSLOTS.get(e, 0):
                        deps.append(o)
                        nd += 1
                elif not got_c:
                    deps.append(o)
                    got_c = True
                if got_c and nd >= self.NSLOTS.get(e, 0):
                    break
        for e in self.ENGS:
            o = _Op()
            o.eng, o.fn, o.is_dma, o.need_sig, o.sig, o.waits = e, None, False, False, None, []
            o.deps = list(deps)
            o.idx = len(self.allops)
            o.slot_prev = None
            o.r, o.w, o.is_barrier = [], [], False
            self.ops[e].append(o)
            self.allops.append(o)

    @staticmethod
    def _needs_sync(d, o):
        if d.is_dma:
            return True
        if d.eng == o.eng and d.eng == "pe" and not o.is_dma:
            return False
        if d.fn is None:
            return False
        return True

    def finalize_and_emit(self):
        nc = self.nc
        for seg in self.cur:
            for o in seg:
                if o.is_barrier:
                    self._track_barrier()
                else:
                    self._track(o)
        for o in self.allops:
            for d in o.deps:
                if self._needs_sync(d, o):
                    d.need_sig = True
        sems = {}

        def get_sem(name):
            if name not in sems:
                sems[name] = nc.alloc_semaphore(name)
            return sems[name]

        for e in self.ENGS:
            n = 0
            slot_cnt = {}
            nd = 0
            for o in self.ops[e]:
                if o.fn is None:
                    continue
                if o.is_dma:
                    ns = self.NSLOTS[e]
                    s = nd % ns
                    nd += 1
                    c = slot_cnt.get(s, 0)
                    sem = get_sem(f"d_{e}_{s}")
                    o.slot_prev = (sem, 16 * c) if c > 0 else None
                    slot_cnt[s] = c + 1
                    o.sig = (sem, 16 * (c + 1), 16)
                elif o.need_sig:
                    ep, v = divmod(n, EPOCH)
                    n += 1
                    o.sig = (get_sem(f"c_{e}_{ep}"), v + 1, 1)
            self._final_dma = getattr(self, "_final_dma", {})
            self._final_dma[e] = {s: c for s, c in slot_cnt.items()}
        for e in self.ENGS:
            waited = {}
            for o in self.ops[e]:
                need = {}
                for d in o.deps:
                    if d.sig is None:
                        continue
                    if not self._needs_sync(d, o):
                        continue
                    sem, val, _ = d.sig
                    if need.get(sem, (None, 0))[1] < val:
                        need[sem] = (sem, val)
                if o.is_dma and o.slot_prev is not None:
                    sem, val = o.slot_prev
                    if need.get(sem, (None, 0))[1] < val:
                        need[sem] = (sem, val)
                for sem, val in need.values():
                    if waited.get(sem, 0) < val:
                        waited[sem] = val
                        o.waits.append((sem, val))
        self.sems = sems
        engmap = {"pe": "tensor", "dve": "vector", "act": "scalar", "pool": "gpsimd", "sp": "sync"}
        with nc.Block() as block:
            for e in self.ENGS:
                def body(eng, e=e):
                    for o in self.ops[e]:
                        for sem, val in o.waits:
                            eng.wait_ge(sem, val)
                        if o.fn is None:
                            continue
                        ins = o.fn(eng)
                        if o.sig is not None:
                            ins.then_inc(o.sig[0], o.sig[2])
                    if e in self.NSLOTS:
                        for s, c in self._final_dma[e].items():
                            eng.wait_ge(sems[f"d_{e}_{s}"], 16 * c)
                getattr(block, engmap[e])(body)


class Buf:
    def __init__(self, t, name, nreg=1):
        self.t, self.name, self.nreg = t, name, nreg

    def k(self, i=0):
        return (self.name, i)

    def all(self):
        return [(self.name, i) for i in range(self.nreg)]

    def __getitem__(self, idx):
        return self.t[idx]


class Prog:
    def __init__(self, cfg=None):
        self.cfg = cfg or {}
        self.nc = bass.Bass("TRN2", target_bir_lowering=False)
        self.S = Sched(self.nc)
        self._uid = 0
        self._scopes = [[]]

    def sb(self, name, shape, dtype, nreg=1):
        self._uid += 1
        g = self.nc.sbuf_tensor(f"s{self._uid}_{name}", list(shape), dtype)
        t = g.__enter__()
        self._scopes[-1].append(g)
        return Buf(t, name, nreg)

    def push(self):
        self._scopes.append([])

    def pop(self):
        self.S.barrier()
        for g in reversed(self._scopes.pop()):
            g.__exit__(None, None, None)

    def dram(self, name, shape, dtype, kind):
        return self.nc.dram_tensor(name, list(shape), dtype, kind=kind).ap()

    def op(self, eng, fn, r=(), w=(), dma=False):
        return self.S.op(eng, fn, r, w, dma)

    def PE(self, out, lhsT, rhs, start, stop, r, w, **kw):
        return self.S.op("pe", lambda e: e.matmul(out, lhsT, rhs, start=start, stop=stop, **kw), r, w)

    def TR(self, out, in_, ident, r, w):
        return self.S.op("pe", lambda e: e.transpose(out, in_, ident), r, w)

    def ACT(self, out, in_, func, r, w, **kw):
        return self.S.op("act", lambda e: e.activation(out, in_, func, **kw), r, w)

    def TT(self, eng, out, a, b, alu, r, w):
        return self.S.op(eng, lambda e: e.tensor_tensor(out, a, b, alu), r, w)

    def TS(self, eng, out, a, s1, s2, op0, op1, r, w, accum=None):
        if accum is None:
            return self.S.op(eng, lambda e: e.tensor_scalar(out, a, s1, s2, op0, op1), r, w)
        return self.S.op(eng, lambda e: e.tensor_scalar(out, a, s1, s2, op0, op1, accum_out=accum), r, w)

    def STT(self, out, in0, scalar, in1, op0, op1, r, w, accum=None):
        if accum is None:
            return self.S.op("dve", lambda e: e.scalar_tensor_tensor(out, in0, scalar, in1, op0, op1), r, w)
        return self.S.op("dve", lambda e: e.scalar_tensor_tensor(out, in0, scalar, in1, op0, op1, accum_out=accum), r, w)

    def CP(self, eng, out, in_, r, w):
        if eng == "act":
            return self.S.op("act", lambda e: e.activation(out, in_, AF.Copy), r, w)
        return self.S.op(eng, lambda e: e.tensor_copy(out, in_), r, w)

    def DMA(self, q, out, in_, r, w):
        return self.S.op(q, lambda e: e.dma_start(out=out, in_=in_), r, w, dma=True)

    def build(self):
        nc, S = self.nc, self.S
        cfg = self.cfg
        op = self.op
        PE, TR, ACT, TT, TS, STT, CP, DMA = self.PE, self.TR, self.ACT, self.TT, self.TS, self.STT, self.CP, self.DMA
        do_mixer = cfg.get("mixer", True)
        x_d = self.dram("x", [TOK, D], F32, "ExternalInput")
        cT_d = self.dram("cT", [128, 8, BPC], F32, "ExternalInput")
        pos_d = self.dram("positions", [BPC, SEQ], I32, "ExternalInput")
        w_ada_d = self.dram("w_ada", [D, 6 * D], F32, "ExternalInput")
        b_ada_d = self.dram("b_ada", [1, 6 * D], F32, "ExternalInput")
        ln_d = self.dram("ln", [4, D], F32, "ExternalInput")
        w_router_d = self.dram("w_router", [D, NE], F32, "ExternalInput")
        rbias_d = self.dram("router_bias", [1, NE], F32, "ExternalInput")
        wg_d = self.dram("w_exp_gate", [NE, D, 256], F32, "ExternalInput")
        wu_d = self.dram("w_exp_up", [NE, D, 256], F32, "ExternalInput")
        wd_d = self.dram("w_exp_down", [NE, 256, D], F32, "ExternalInput")
        wsg_d = self.dram("w_sh_gate", [D, 256], F32, "ExternalInput")
        wsu_d = self.dram("w_sh_up", [D, 256], F32, "ExternalInput")
        wsd_d = self.dram("w_sh_down", [256, D], F32, "ExternalInput")
        wmA_d = self.dram("wmA", [N_CH_A, 128, 8, 128], F32, "ExternalInput")
        wmB_d = self.dram("wmB", [N_CH_B, 128, 8, 128], F32, "ExternalInput")
        wmG_d = self.dram("wmG", [16, 128, 8, 128], F32, "ExternalInput")
        wtA_d = self.dram("wtA", [128, 8, 136], F32, "ExternalInput")
        wtB_d = self.dram("wtB", [128, 8, 280], F32, "ExternalInput")
        wbrA_d = self.dram("w_br_a", [512, D], F32, "ExternalInput")
        wbrB_d = self.dram("w_br_b", [512, D], F32, "ExternalInput")
        wout_d = self.dram("w_out", [D, D], F32, "ExternalInput")
        cw1k_d = self.dram("cmp_k_w1", [2048, 256], F32, "ExternalInput")
        cw1v_d = self.dram("cmp_v_w1", [2048, 256], F32, "ExternalInput")
        cw2k_d = self.dram("cmp_k_w2", [256, 64], F32, "ExternalInput")
        cw2v_d = self.dram("cmp_v_w2", [256, 64], F32, "ExternalInput")
        cposT_d = self.dram("cmp_posT", [128, 2, 32], F32, "ExternalInput")
        cst_d = self.dram("cst", [128, 8], F32, "ExternalInput")
        selAB_d = self.dram("selAB", [128, 2, 16, 32], F32, "ExternalInput")
        expall_d = self.dram("expall", [32, 16, 128], F32, "ExternalInput")
        ov_d = self.dram("ovl", [128, 32], F32, "ExternalInput")
        out_d = self.dram("out", [TOK, D], F32, "ExternalOutput")
        x1_d = self.dram("x1_scr", [TOK, D], F32, "Internal")
        u2_d = self.dram("u2_scr", [TOK, D], BF16, "Internal")
        xs_d = self.dram("xs_scr", [NSLOT, D], BF16, "Internal")
        ys_d = self.dram("ys_scr", [NSLOT, D], BF16, "Internal")
        mod_d = self.dram("mod_scr", [BPC * 6, D], F32, "Internal")
        uT_d = self.dram("uT_scr", [BPC, 128, 8 * SEQ], BF16, "Internal")
        dbg = {}
        for name, shape in cfg.get("dbg", {}).items():
            dbg[name] = self.dram("dbg_" + name, shape, F32, "ExternalOutput")
        self.dbg = dbg

        _regs = {}

        def bc_reg(e):
            if "bc" not in _regs:
                _regs["bc"] = e.to_reg(NSLOT - 1)
            return _regs["bc"]

        banks = [Buf(nc.alloc_psum_tensor(f"pb{i}", [128, 512], F32), f"pb{i}") for i in range(8)]

        def bfv(bk):
            return bk.t[:, :].bitcast(BF16)

        ones_f = self.sb("ones_f", [128, 128], F32)
        ones_b = self.sb("ones_b", [128, 128], BF16)
        ident_b = self.sb("ident_b", [128, 128], BF16)
        tri_b = self.sb("tri_b", [128, 128], BF16)
        trib = self.sb("trib", [128, 128], BF16)
        edgeb = self.sb("edgeb", [128, 128], BF16)
        negb = self.sb("negb", [128, 128], F32)
        iotaC = self.sb("iotaC", [128, NE], F32)
        iotaE1 = self.sb("iotaE1", [128, NE], F32)
        jcol = self.sb("jcol", [128, NBLK // 128], F32)
        pcol = self.sb("pcol", [128, 1], F32)
        Utri = self.sb("Utri", [128, 2, NE], BF16)
        cst = self.sb("cst", [128, 8], F32)
        eps_t = self.sb("eps_t", [128, 1], F32)
        op("pool", lambda e: e.memset(ones_f[:, :], 1.0), w=ones_f.all())
        op("pool", lambda e: e.memset(ones_b[:, :], 1.0), w=ones_b.all())
        op("pool", lambda e: e.memset(negb[:, :], -30000.0), w=negb.all())
        op("pool", lambda e: e.memset(eps_t[:, :], LN_EPS), w=eps_t.all())
        op("pool", lambda e: e.affine_select(ident_b[:, :], ones_f[:, :], [[-1, 128]], ALU.is_equal, 0.0,
                                            base=0, channel_multiplier=1), r=ones_f.all(), w=ident_b.all())
        op("pool", lambda e: e.affine_select(tri_b[:, :], ones_f[:, :], [[1, 128]], ALU.is_gt, 0.0,
                                            base=0, channel_multiplier=-1), r=ones_f.all(), w=tri_b.all())
        op("pool", lambda e: e.affine_select(trib[:, :], negb[:, :], [[-1, 128]], ALU.is_gt, 0.0,
                                            base=0, channel_multiplier=1), r=negb.all(), w=trib.all())
        op("pool", lambda e: e.affine_select(edgeb[:, :], negb[:, :], [[1, 128]], ALU.is_ge, 0.0,
                                            base=0, channel_multiplier=-1), r=negb.all(), w=edgeb.all())
        op("pool", lambda e: e.iota(iotaC[:, :], [[4096, NE]], base=0, channel_multiplier=0,
                                    allow_small_or_imprecise_dtypes=True), w=iotaC.all())
        op("pool", lambda e: e.iota(jcol[:, :], [[128, NBLK // 128]], base=0, channel_multiplier=1,
                                    allow_small_or_imprecise_dtypes=True), w=jcol.all())
        for a_ in range(2):
            op("pool", lambda e, a_=a_: e.iota(iotaE1[:, :], [[1, NE]], base=-128 * a_, channel_multiplier=-1,
                                              allow_small_or_imprecise_dtypes=True), r=Utri.all(), w=iotaE1.all())
            TS("dve", Utri[:, a_, :], iotaE1[:, :], 0.0, None, ALU.is_gt, ALU.bypass, iotaE1.all(), Utri.all())
        op("pool", lambda e: e.iota(iotaE1[:, :], [[1, NE]], base=0, channel_multiplier=0,
                                    allow_small_or_imprecise_dtypes=True), r=Utri.all(), w=iotaE1.all())
        op("pool", lambda e: e.iota(pcol[:, :], [[0, 1]], base=0, channel_multiplier=1,
                                    allow_small_or_imprecise_dtypes=True), w=pcol.all())
        DMA("sp", cst[:, :], cst_d[:, :], [], cst.all())

        ln1g = self.sb("ln1g", [128, D], F32)
        ln1b = self.sb("ln1b", [128, D], F32)
        rb_bc = self.sb("rb_bc", [128, NE], F32)
        wr_b = self.sb("wr_b", [128, 8, NE], BF16)
        carry = self.sb("carry", [128, NE], F32)
        dest_f = self.sb("dest_f", [128, NT, 8], F32, nreg=NT)
        dest_i = self.sb("dest_i", [128, NT, 8], I32, nreg=NT)
        wk = self.sb("wk", [128, NT, 8], F32, nreg=NT)
        op("pool", lambda e: e.memset(carry[:, :], 0.0), w=carry.all())
        DMA("sp", ln1g[:, :], ln_d[0:1, :].to_broadcast([128, D]), [], ln1g.all())
        DMA("sp", ln1b[:, :], ln_d[1:2, :].to_broadcast([128, D]), [], ln1b.all())
        DMA("sp", rb_bc[:, :], rbias_d[0:1, :].to_broadcast([128, NE]), [], rb_bc.all())

        self.push()
        condT = self.sb("condT", [128, 8, BPC], F32)
        condS = self.sb("condS", [128, 8, BPC], F32)
        DMA("sp", condT[:, :, :], cT_d[:, :, :], [], condT.all())
        ACT(condS[:, :, :], condT[:, :, :], AF.Silu, condT.all(), condS.all())
        wst = [self.sb(f"wada_st{i}", [128, 8, 512], F32) for i in range(2)]
        mrow = [self.sb(f"mrow{i}", [1, D], F32) for i in range(4)]
        brow = [self.sb(f"brow{i}", [1, D], F32) for i in range(2)]
        w_ada_v = w_ada_d.rearrange("(k p) n -> p k n", p=128)
        nmm = 0
        for j in range(6):
            br = brow[j % 2]
            DMA("sp", br[:, :], b_ada_d[:, j * D:(j + 1) * D], [], br.all())
            rows_j = [mrow[(j * BPC + b) % 4] for b in range(BPC)]
            for half in range(2):
                cc = j * 2 + half
                st = wst[cc % 2]
                DMA("sp", st[:, :, :], w_ada_v[:, :, cc * 512:(cc + 1) * 512], [], st.all())
                for b in range(BPC):
                    bk = banks[nmm % 2]
                    nmm += 1
                    for k in range(8):
                        PE(bk[0:1, :], condS[:, k, b:b + 1], st[:, k, :], k == 0, k == 7, condS.all() + st.all(), bk.all())
                    add = 1.0 if j in (1, 4) else 0.0
                    STT(rows_j[b][0:1, half * 512:(half + 1) * 512], bk[0:1, :], add, br[0:1, half * 512:(half + 1) * 512],
                        ALU.add, ALU.add, bk.all() + br.all(), rows_j[b].all())
            for b in range(BPC):
                DMA("sp", mod_d[b * 6 + j:b * 6 + j + 1, :], rows_j[b][0:1, :], rows_j[b].all(), [("mod_d", b, j)])
        wr_st = self.sb("wr_st", [128, 8, NE], F32)
        DMA("sp", wr_st[:, :, :], w_router_d.rearrange("(k p) n -> p k n", p=128), [], wr_st.all())
        CP("pool", wr_b[:, :, :], wr_st[:, :, :], wr_st.all(), wr_b.all())
        self.pop()

        def load_mod(dst, b, j):
            DMA("sp", dst[:, :], mod_d[b * 6 + j:b * 6 + j + 1, :].to_broadcast([128, D]), [("mod_d", b, j)], dst.all())

        def layernorm(z, dst, g_bc, b_bc, tagbufs):
            stats, mv, rstd = tagbufs
            for h in range(2):
                op("dve", lambda e, h=h: e.bn_stats(stats[:, h, :], z[:, h * 512:(h + 1) * 512]), r=z.all(), w=stats.all())
            op("dve", lambda e: e.bn_aggr(mv[:, :], stats[:, :, :]), r=stats.all(), w=mv.all())
            ACT(rstd[:, :], mv[:, 1:2], AF.Sqrt, mv.all() + eps_t.all(), rstd.all(), bias=eps_t[:, 0:1], scale=1.0)
            op("dve", lambda e: e.reciprocal(rstd[:, :], rstd[:, :]), r=rstd.all(), w=rstd.all())
            TS("dve", z[:, :], z[:, :], mv[:, 0:1], rstd[:, 0:1], ALU.subtract, ALU.mult, z.all() + mv.all() + rstd.all(), z.all())
            TT("pool", z[:, :], z[:, :], g_bc[:, :], ALU.mult, z.all() + g_bc.all(), z.all())
            TT("pool", dst[:, :], z[:, :], b_bc[:, :], ALU.add, z.all() + b_bc.all(), dst.all())

        n_tiles = cfg.get("n_tiles", NT)
        TPS = SEQ // 128
        PI = float(np.pi)
        C1 = 6.28125
        C2 = float(2.0 * np.pi - 6.28125)
        NBIS = 18

        def build_uT(b, uT, first=False):
            if not first:
                DMA("sp", uT[:, :, :].rearrange("p k t -> p (k t)"), uT_d[b], [("uT_d", b)], uT.all())
                return
            self.push()
            s1 = self.sb("m0_s1", [128, D], F32)
            h1 = self.sb("m0_h1", [128, D], F32)
            load_mod(h1, b, 0)
            load_mod(s1, b, 1)
            xs = [self.sb(f"m0_x{i}", [128, D], F32) for i in range(2)]
            us = [self.sb(f"m0_u{i}", [128, D], BF16) for i in range(2)]
            for tt in range(TPS):
                X, U = xs[tt % 2], us[tt % 2]
                r0 = b * SEQ + tt * 128
                DMA("sp", X[:, :], x_d[r0:r0 + 128, :], [], X.all())
                TT("dve", X[:, :], X[:, :], s1[:, :], ALU.mult, X.all() + s1.all(), X.all())
                TT("pool", U[:, :], X[:, :], h1[:, :], ALU.add, X.all() + h1.all(), U.all())
                bk = banks[tt % 2]
                pT = bfv(bk)
                for k in range(8):
                    TR(pT[:, k * 128:(k + 1) * 128], U[:, k * 128:(k + 1) * 128], ident_b[:, :], U.all() + ident_b.all(), bk.all())
                CP("act", uT[:, :, tt * 128:(tt + 1) * 128], pT.rearrange("p (k t) -> p k t", k=8), bk.all(), [uT.k(tt // 4)])
            self.pop()
            DMA("sp", uT_d[b], uT[:, :, :].rearrange("p k t -> p (k t)"), uT.all(), [("uT_d", b)])

        def rope_tables(b, CS, col_inv, col_sgn, nrow):
            self.push()
            posi = self.sb("rp_posi", [1, SEQ], I32)
            posf = self.sb("rp_posf", [1, SEQ], F32)
            ang = self.sb("rp_ang", [128, SEQ], F32)
            a2 = self.sb("rp_a2", [128, SEQ], F32)
            qf = self.sb("rp_qf", [128, SEQ], F32)
            qi = self.sb("rp_qi", [128, SEQ], I32)
            DMA("sp", posi[:, :], pos_d[b:b + 1, :], [], posi.all())
            CP("dve", posf[:, :], posi[:, :], posi.all(), posf.all())
            for c4 in range(4):
                bk = banks[2 + c4 % 2]
                PE(bk[:, :], ones_f[0:1, :], posf[0:1, c4 * 512:(c4 + 1) * 512], True, True, ones_f.all() + posf.all(), bk.all())
                TS("dve", ang[0:nrow, c4 * 512:(c4 + 1) * 512], bk[0:nrow, :], cst[0:nrow, col_inv:col_inv + 1], None, ALU.mult, ALU.bypass,
                   bk.all() + cst.all(), ang.all())
            for which in range(2):
                src = ang
                if which == 0:
                    TS("dve", a2[0:nrow, :], ang[0:nrow, :], PI / 2.0, None, ALU.add, ALU.bypass, ang.all(), a2.all())
                    src = a2
                TS("dve", qf[0:nrow, :], src[0:nrow, :], 1.0 / (2.0 * PI), None, ALU.mult, ALU.bypass, src.all(), qf.all())
                CP("dve", qi[0:nrow, :], qf[0:nrow, :], qf.all(), qi.all())
                CP("dve", qf[0:nrow, :], qi[0:nrow, :], qi.all(), qf.all())
                STT(a2[0:nrow, :], qf[0:nrow, :], -C1, src[0:nrow, :], ALU.mult, ALU.add, qf.all() + src.all(), a2.all())
                STT(a2[0:nrow, :], qf[0:nrow, :], -C2, a2[0:nrow, :], ALU.mult, ALU.add, qf.all() + a2.all(), a2.all())
                TS("dve", qf[0:nrow, :], a2[0:nrow, :], PI, -2.0 * PI, ALU.is_gt, ALU.mult, a2.all(), qf.all())
                TT("dve", a2[0:nrow, :], a2[0:nrow, :], qf[0:nrow, :], ALU.add, a2.all() + qf.all(), a2.all())
                TS("dve", a2[0:nrow, :], a2[0:nrow, :], 3.14159, -3.14159, ALU.min, ALU.max, a2.all(), a2.all())
                if which == 0:
                    ACT(CS[0:nrow, 0, :], a2[0:nrow, :], AF.Sin, a2.all(), [CS.k(0)])
                else:
                    ACT(qf[0:nrow, :], a2[0:nrow, :], AF.Sin, a2.all(), qf.all())
                    TS("dve", CS[0:nrow, 1, :], qf[0:nrow, :], cst[0:nrow, col_sgn:col_sgn + 1], None, ALU.mult, ALU.bypass,
                       qf.all() + cst.all(), [CS.k(1)])
            self.pop()

        def project(wm_d, jobs, uT, wbufs):
            st, wb, t1, t2 = wbufs
            cnt = [0]

            def load(ch):
                i = cnt[0] % 3
                cnt[0] += 1
                DMA("sp", st[i][:, :, :], wm_d[ch], [], st[i].all())
                CP("pool", wb[i][:, :, :], st[i][:, :, :], st[i].all(), wb[i].all())
                return wb[i]

            nb = 0
            for (ch, chsw, nrow, dst_fn, CS) in jobs:
                wA = load(ch)
                wB = load(chsw) if chsw is not None else None
                for tc in range(4):
                    ts_ = slice(tc * 512, (tc + 1) * 512)
                    bA = banks[2 + nb % 4]
                    nb += 1
                    for k in range(8):
                        PE(bA[:, :], wA[:, k, :], uT[:, k, ts_], k == 0, k == 7, wA.all() + [uT.k(tc)], bA.all())
                    dst, dkeys = dst_fn(tc)
                    if wB is None:
                        CP("act", dst, bA[0:nrow, :], bA.all(), dkeys)
                    else:
                        bB = banks[2 + nb % 4]
                        nb += 1
                        for k in range(8):
                            PE(bB[:, :], wB[:, k, :], uT[:, k, ts_], k == 0, k == 7, wB.all() + [uT.k(tc)], bB.all())
                        T1, T2 = t1[tc % 2], t2[tc % 2]
                        TT("dve", T1[0:nrow, :], bA[0:nrow, :], CS[0:nrow, 0, ts_], ALU.mult, bA.all() + [CS.k(0)], T1.all())
                        TT("dve", T2[0:nrow, :], bB[0:nrow, :], CS[0:nrow, 1, ts_], ALU.mult, bB.all() + [CS.k(1)], T2.all())
                        TT("pool", dst, T1[0:nrow, :], T2[0:nrow, :], ALU.add, T1.all() + T2.all(), dkeys)

        def proj_bufs():
            st = [self.sb(f"pj_st{i}", [128, 8, 128], F32) for i in range(3)]
            wb = [self.sb(f"pj_wb{i}", [128, 8, 128], BF16) for i in range(3)]
            t1 = [self.sb(f"pj_t1{i}", [128, 512], F32) for i in range(2)]
            t2 = [self.sb(f"pj_t2{i}", [128, 512], F32) for i in range(2)]
            return st, wb, t1, t2

        def attn_out(accs, O, coef=None, first=True, tmpb=None):
            Ov = O[:, :].rearrange("p (g e q d) -> p g q e d", g=2, e=2, q=2)
            for g in range(2):
                a = accs[g]
                W = a.W
                av = a.buf[:, 0:4 * W].rearrange("p (q e w) -> p q e w", q=2, e=2)[:, :, :, 0:64]
                cf = coef[:, g * 4:(g + 1) * 4].rearrange("p (q e) -> p q e", q=2).unsqueeze(3).to_broadcast([128, 2, 2, 64])
                if first:
                    TT("dve", Ov[:, g], av, cf, ALU.mult, a.buf.all() + coef.all(), O.all())
                else:
                    tv = tmpb[:, :].rearrange("p (q e d) -> p q e d", q=2, e=2)
                    TT("dve", tv, av, cf, ALU.mult, a.buf.all() + coef.all(), tmpb.all())
                    TT("pool", Ov[:, g], Ov[:, g], tv, ALU.add, O.all() + tmpb.all(), O.all())

        class Acc:
            def __init__(self, buf, W):
                self.buf, self.W = buf, W

        def o_to_T(O, Ob, oT, i, bank):
            CP("act", Ob[:, :], O[:, :], O.all(), Ob.all())
            pT = bfv(bank)
            for c in range(4):
                TR(pT[:, c * 128:(c + 1) * 128], Ob[:, c * 128:(c + 1) * 128], ident_b[:, :], Ob.all() + ident_b.all(), bank.all())
            CP("dve", oT[:, :, i * 128:(i + 1) * 128], pT[:, 0:512].rearrange("p (c t) -> p c t", c=4), bank.all(), [oT.k(i // 4)])

        def dsa(b, oaT, CS64):
            self.push()
            QA = self.sb("QA", [128, 4, SEQ], BF16, nreg=4)
            KA = self.sb("KA", [128, 2, SEQ], BF16, nreg=4)
            QI = self.sb("QI", [96, 3, SEQ], BF16, nreg=4)
            KI = self.sb("KI", [96, SEQ], BF16, nreg=4)
            VA = self.sb("VA", [128, TPS, 2, 65], BF16)
            widx = self.sb("widx", [128, TPS, 8], F32)
            wabs = self.sb("wabs", [128, TPS, 8], F32)
            wsgn = self.sb("wsgn", [128, TPS, 8], F32)
            CS32 = self.sb("CS32", [128, 2, SEQ], BF16, nreg=2)
            rope_tables(b, CS32, 2, 3, 96)
            self.push()
            uT = self.sb("uT", [128, 8, SEQ], BF16, nreg=4)
            build_uT(b, uT, first=True)
            wbufs = proj_bufs()
            jobs = []
            for c in range(4):
                jobs.append((c, 4 + c, 128, (lambda tc, c=c: (QA[:, c, tc * 512:(tc + 1) * 512], [QA.k(tc)])), CS64))
            for g in range(2):
                jobs.append((8 + g, 10 + g, 128, (lambda tc, g=g: (KA[:, g, tc * 512:(tc + 1) * 512], [KA.k(tc)])), CS64))
            for c in range(3):
                jobs.append((12 + c, 15 + c, 96, (lambda tc, c=c: (QI[:, c, tc * 512:(tc + 1) * 512], [QI.k(tc)])), CS32))
            jobs.append((18, 19, 96, (lambda tc: (KI[:, tc * 512:(tc + 1) * 512], [KI.k(tc)])), CS32))
            project(wmA_d, jobs, uT, wbufs)
            wt_st = self.sb("wtA_st", [128, 8, 136], F32)
            wt_b = self.sb("wtA_b", [128, 8, 136], BF16)
            DMA("sp", wt_st[:, :, :], wtA_d[:, :, :], [], wt_st.all())
            CP("pool", wt_b[:, :, :], wt_st[:, :, :], wt_st.all(), wt_b.all())
            op("pool", lambda e: e.memset(VA[:, :, :, 64:65], 1.0), w=VA.all())
            for tt in range(TPS):
                bk = banks[6 + tt % 2]
                for k in range(8):
                    PE(bk[:, 0:136], uT[:, k, tt * 128:(tt + 1) * 128], wt_b[:, k, :], k == 0, k == 7, [uT.k(tt // 4)] + wt_b.all(), bk.all())
                CP("act", VA[:, tt, :, 0:64], bk[:, 0:128].rearrange("p (g d) -> p g d", g=2), bk.all(), VA.all())
                CP("dve", widx[:, tt, :], bk[:, 128:136], bk.all(), widx.all())
            c0 = float((32.0 ** -0.5) * (8.0 ** -0.5))
            ACT(wabs[:, :, :], widx[:, :, :], AF.Abs, widx.all(), wabs.all(), scale=c0)
            TS("dve", wsgn[:, :, :], widx[:, :, :], 0.0, 2.0, ALU.is_ge, ALU.mult, widx.all(), wsgn.all())
            TS("dve", wsgn[:, :, :], wsgn[:, :, :], -1.0, None, ALU.add, ALU.bypass, wsgn.all(), wsgn.all())
            self.pop()
            self.push()
            GQ = 4
            accs = [self.sb(f"ix_acc{i}", [128, SEQ], F32, nreg=4) for i in range(GQ)]
            junks = [self.sb(f"ix_junk{i}", [128, SEQ], BF16) for i in range(GQ)]
            bss = [self.sb(f"ix_bs{i}", [128, 8 + NBIS], F32) for i in range(GQ)]
            css = [self.sb(f"ix_cs{i}", [128, 1], F32) for i in range(GQ)]
            Mb = self.sb("ix_Mb", [128, SEQ], BF16)
            BT = self.sb("ix_BT", [128, TPS, 128], BF16, nreg=2)
            Rt = [self.sb(f"ix_R{i}", [128, 512], F32) for i in range(3)]
            pw2 = self.sb("ix_pw2", [128, NBIS], F32)
            thrneg = self.sb("ix_thrneg", [128, 1], F32)
            Et = [self.sb(f"at_E{i}", [128, 512], BF16) for i in range(4)]
            O = self.sb("at_O", [128, 512], F32)
            Ob = self.sb("at_Ob", [128, 512], BF16)
            RD = self.sb("at_RD", [128, 8], F32)
            for k in range(NBIS):
                op("pool", lambda e, k=k: e.memset(pw2[:, k:k + 1], 2.0 ** -(k + 1)), w=pw2.all())
            op("pool", lambda e: e.memset(thrneg[:, :], -1e30), w=thrneg.all())
            cnts = {"R": 0, "E": 0}
            nq = cfg.get("n_qtiles", TPS)

            def indexer(i):
                acc, bs = accs[i % GQ], bss[i % GQ]
                rmax, rmin = bs[:, 1:2], bs[:, 2:3]
                n = 128 * (i + 1)
                it = slice(i * 128, (i + 1) * 128)
                nch = (n + 511) // 512
                for h in range(8):
                    pb = 32 * (h % 3)
                    for kc in range(nch):
                        kw = min(512, n - kc * 512)
                        bk = banks[(h * nch + kc) % 2]
                        R = Rt[cnts["R"] % 3]
                        cnts["R"] += 1
                        with S.atomic():
                            PE(bk[:, 0:kw], QI[pb:pb + 32, h // 3, it], KI[pb:pb + 32, kc * 512:kc * 512 + kw], True, True,
                               [QI.k(i // 4), KI.k(kc)], bk.all())
                            ACT(R[:, 0:kw], bk[:, 0:kw], AF.Relu, bk.all() + wabs.all(), R.all(), scale=wabs[:, i, h:h + 1])
                        if h == 0:
                            TS("dve", acc[:, kc * 512:kc * 512 + kw], R[:, 0:kw], wsgn[:, i, 0:1], None, ALU.mult, ALU.bypass,
                               R.all() + wsgn.all(), [acc.k(kc)])
                        else:
                            STT(acc[:, kc * 512:kc * 512 + kw], R[:, 0:kw], wsgn[:, i, h:h + 1], acc[:, kc * 512:kc * 512 + kw],
                                ALU.mult, ALU.add, R.all() + wsgn.all() + [acc.k(kc)], [acc.k(kc)])
                akeys = [acc.k(kc) for kc in range(nch)]
                if i >= 2:
                    op("dve", lambda e: e.tensor_reduce(rmin, acc[:, 0:n], AX.X, ALU.min), r=akeys, w=bs.all())
                    op("dve", lambda e: e.tensor_reduce(rmax, acc[:, 0:n], AX.X, ALU.max), r=akeys, w=bs.all())
                op("pool", lambda e: e.affine_select(acc[:, it], acc[:, it], [[-1, 128]], ALU.is_ge, -3.0e38,
                                                    base=0, channel_multiplier=1), r=[acc.k(i // 4)], w=[acc.k(i // 4)])

            def bisect(i):
                acc, bs, junk = accs[i % GQ], bss[i % GQ], junks[i % GQ]
                lo, rmax, rmin, w0, mid, cnt, stp = (bs[:, 0:1], bs[:, 1:2], bs[:, 2:3], bs[:, 3:4], bs[:, 4:5], bs[:, 5:6], bs[:, 6:7])
                Wk = bs[:, 8:8 + NBIS]
                n = 128 * (i + 1)
                akeys = [acc.k(kc) for kc in range((n + 511) // 512)]
                TS("dve", lo, rmin, -1.0, None, ALU.add, ALU.bypass, bs.all(), bs.all())
                TT("dve", w0, rmax, lo, ALU.subtract, bs.all(), bs.all())
                TS("dve", Wk, pw2[:, :], w0, None, ALU.mult, ALU.bypass, bs.all() + pw2.all(), bs.all())
                cs_ = css[i % GQ]
                for k in range(NBIS):
                    if i % 2 == 0:
                        TS("dve", mid, lo, Wk[:, k:k + 1], -1.0, ALU.add, ALU.mult, bs.all(), bs.all())
                        ACT(junk[:, 0:n], acc[:, 0:n], AF.Sign, akeys + bs.all(), junk.all() + cs_.all(), bias=mid, scale=1.0, accum_out=cs_[:, 0:1])
                        TS("dve", stp, cs_[:, 0:1], float(511 - n), Wk[:, k:k + 1], ALU.is_ge, ALU.mult, bs.all() + cs_.all(), bs.all())
                    else:
                        TT("dve", mid, lo, Wk[:, k:k + 1], ALU.add, bs.all(), bs.all())
                        TS("dve", junk[:, 0:n], acc[:, 0:n], mid, None, ALU.is_ge, ALU.add, akeys + bs.all(), junk.all() + bs.all(), accum=cnt)
                        TS("dve", stp, cnt, 255.5, Wk[:, k:k + 1], ALU.is_ge, ALU.mult, bs.all(), bs.all())
                    TT("dve", lo, lo, stp, ALU.add, bs.all(), bs.all())

            def attend(i):
                acc, bs = accs[i % GQ], bss[i % GQ]
                n = 128 * (i + 1)
                it = slice(i * 128, (i + 1) * 128)
                akeys = [acc.k(kc) for kc in range((n + 511) // 512)]
                if i >= 2:
                    thr, thr_keys = bs[:, 0:1], bs.all()
                else:
                    thr, thr_keys = thrneg[:, 0:1], thrneg.all()
                TS("dve", Mb[:, 0:n], acc[:, 0:n], thr, -30000.0, ALU.is_lt, ALU.mult, akeys + thr_keys, Mb.all())
                for j0 in range(0, i + 1, 8):
                    j1 = min(i + 1, j0 + 8)
                    bk = banks[2]
                    pT = bfv(bk)
                    for j in range(j0, j1):
                        TR(pT[:, (j - j0) * 128:(j - j0 + 1) * 128], Mb[:, j * 128:(j + 1) * 128], ident_b[:, :], Mb.all() + ident_b.all(), bk.all())
                    CP("act", BT[:, j0:j1, :], pT[:, 0:(j1 - j0) * 128].rearrange("p (j t) -> p j t", t=128), bk.all(), [BT.k(j0 // 8)])
                accb = [banks[5], banks[6]]
                def scores(j):
                    jt = slice(j * 128, (j + 1) * 128)
                    sA, sB = (banks[3], banks[4]) if j % 2 == 0 else (banks[7], banks[2])
                    for par, sb_ in ((0, sA), (1, sB)):
                        ps = slice(par * 64, par * 64 + 64)
                        for g in range(2):
                            PE(sb_[:, g * 256:(g + 1) * 256], KA[ps, g, jt], QA[ps, 2 * g:2 * g + 2, it], g == 0, False,
                               [KA.k(j // 4), QA.k(i // 4)], sb_.all(), skip_group_check=True)
                        PE(sb_[:, :], ident_b[:, :], BT[:, j, :].unsqueeze(1).to_broadcast([128, 4, 128]), False, True,
                           ident_b.all() + [BT.k(j // 8)], sb_.all(), skip_group_check=True)

                def exp_pv(j):
                    sA, sB = (banks[3], banks[4]) if j % 2 == 0 else (banks[7], banks[2])
                    EA, EB = Et[cnts["E"] % 4], Et[(cnts["E"] + 1) % 4]
                    cnts["E"] += 2
                    ACT(EA[:, :], sA[:, :], AF.Exp, sA.all(), EA.all(), scale=0.125)
                    ACT(EB[:, :], sB[:, :], AF.Exp, sB.all(), EB.all(), scale=0.125)
                    for g in range(2):
                        for par, E_ in ((0, EA), (1, EB)):
                            for e_ in range(2):
                                r_ = par * 2 + e_
                                blk = (g * 2 + e_) * 128
                                PE(accb[g][:, r_ * 65:(r_ + 1) * 65], E_[:, blk:blk + 128], VA[:, j, g, :],
                                   (j == 0 and r_ == 0), (j == i and r_ == 3), E_.all() + VA.all(), accb[g].all(), skip_group_check=True)

                scores(0)
                for j in range(i + 1):
                    if j + 1 <= i:
                        scores(j + 1)
                    exp_pv(j)
                for g in range(2):
                    op("dve", lambda e, g=g: e.reciprocal(RD[:, g * 4:(g + 1) * 4], accb[g][:, 0:260].rearrange("p (r w) -> p r w", w=65)[:, :, 64]),
                       r=accb[g].all(), w=RD.all())
                attn_out([Acc(accb[0], 65), Acc(accb[1], 65)], O, coef=RD, first=True)
                o_to_T(O, Ob, oaT, i, banks[7])

            for i0 in range(0, nq, GQ):
                grp = list(range(i0, min(i0 + GQ, nq)))
                for i in grp:
                    indexer(i)
                parts = [S.record_parts(lambda i=i: bisect(i)) for i in grp if i >= 2]
                if parts:
                    S.merge_rr([p[0] for p in parts])
                for i in grp:
                    attend(i)
            self.pop()
            self.pop()
        def nsa(b, obT, CS64):
            self.push()
            QBr = self.sb("QBr", [128, 4, SEQ], BF16, nreg=4)
            QB = self.sb("QB", [128, 4, SEQ], BF16, nreg=4)
            KCf = self.sb("KCf", [128, SEQ], BF16, nreg=4)
            VCf = self.sb("VCf", [128, SEQ], BF16, nreg=4)
            KS = self.sb("KS", [128, 2, SEQ], BF16, nreg=4)
            KW = self.sb("KW", [128, 2, SEQ], BF16, nreg=4)
            VS = self.sb("VS", [128, TPS, 2, 65], BF16)
            VW = self.sb("VW", [128, TPS, 2, 65], BF16)
            gns = self.sb("gns", [128, TPS, 24], F32)
            self.push()
            uT = self.sb("uTn", [128, 8, SEQ], BF16, nreg=4)
            build_uT(b, uT)
            wbufs = proj_bufs()
            jobs = []
            for c in range(4):
                jobs.append((c, None, 128, (lambda tc, c=c: (QBr[:, c, tc * 512:(tc + 1) * 512], [QBr.k(tc)])), None))
            for c in range(4):
                jobs.append((c, 4 + c, 128, (lambda tc, c=c: (QB[:, c, tc * 512:(tc + 1) * 512], [QB.k(tc)])), CS64))
            jobs.append((8, None, 128, (lambda tc: (KCf[:, tc * 512:(tc + 1) * 512], [KCf.k(tc)])), None))
            jobs.append((9, None, 128, (lambda tc: (VCf[:, tc * 512:(tc + 1) * 512], [VCf.k(tc)])), None))
            for g in range(2):
                jobs.append((10 + g, 12 + g, 128, (lambda tc, g=g: (KS[:, g, tc * 512:(tc + 1) * 512], [KS.k(tc)])), CS64))
            for g in range(2):
                jobs.append((14 + g, 16 + g, 128, (lambda tc, g=g: (KW[:, g, tc * 512:(tc + 1) * 512], [KW.k(tc)])), CS64))
            project(wmB_d, jobs, uT, wbufs)
            wt_st = self.sb("wtB_st", [128, 8, 280], F32)
            wt_b = self.sb("wtB_b", [128, 8, 280], BF16)
            DMA("sp", wt_st[:, :, :], wtB_d[:, :, :], [], wt_st.all())
            CP("pool", wt_b[:, :, :], wt_st[:, :, :], wt_st.all(), wt_b.all())
            op("pool", lambda e: e.memset(VS[:, :, :, 64:65], 1.0), w=VS.all())
            op("pool", lambda e: e.memset(VW[:, :, :, 64:65], 1.0), w=VW.all())
            for tt in range(TPS):
                bk = banks[6 + tt % 2]
                for k in range(8):
                    PE(bk[:, 0:280], uT[:, k, tt * 128:(tt + 1) * 128], wt_b[:, k, :], k == 0, k == 7, [uT.k(tt // 4)] + wt_b.all(), bk.all())
                CP("act", VS[:, tt, :, 0:64], bk[:, 0:128].rearrange("p (g d) -> p g d", g=2), bk.all(), VS.all())
                CP("act", VW[:, tt, :, 0:64], bk[:, 128:256].rearrange("p (g d) -> p g d", g=2), bk.all(), VW.all())
                ACT(gns[:, tt, :], bk[:, 256:280], AF.Sigmoid, bk.all(), gns.all())
            self.pop()
            self.push()
            KCc = self.sb("KCc", [128, 2, 128], BF16)
            VX = self.sb("VX", [128, 2, 97], BF16)
            ovst = self.sb("ovst", [128, 32], F32)
            DMA("sp", ovst[:, :], ov_d[:, :], [], ovst.all())
            op("pool", lambda e: e.memset(VX[:, :, 64:65], 1.0), w=VX.all())
            for g in range(2):
                CP("pool", VX[:, g, 65:97], ovst[:, :], ovst.all(), VX.all())
            self.push()
            W1s = self.sb("W1s", [128, 8, 256], F32)
            W1 = self.sb("W1", [128, 32, 256], BF16, nreg=4)
            w2s = self.sb("w2s", [128, 2, 64], F32)
            w2 = self.sb("w2", [128, 2, 128], BF16)
            posT = self.sb("posT", [128, 2, 32], F32)
            posTb = self.sb("posTb", [128, 2, 32], BF16)
            pbias = self.sb("pbias", [128, 2], F32)
            hx = self.sb("hx", [128, 128], F32)
            hx2 = self.sb("hx2", [128, 128], F32)
            hT = self.sb("hT", [128, 2, 2, 128], BF16)
            DMA("sp", posT[:, :, :], cposT_d[:, :, :], [], posT.all())
            CP("pool", posTb[:, :, :], posT[:, :, :], posT.all(), posTb.all())
            for kind, (w1_d, w2_d, SRC) in enumerate(((cw1k_d, cw2k_d, KCf), (cw1v_d, cw2v_d, VCf))):
                w1v = w1_d.rearrange("(l d) n -> d l n", d=64)
                for l4 in range(4):
                    for hh in range(2):
                        DMA("sp", W1s[hh * 64:(hh + 1) * 64, :, :], w1v[:, l4 * 8:(l4 + 1) * 8, :], [], W1s.all())
                    CP("pool", W1[:, l4 * 8:(l4 + 1) * 8, :], W1s[:, :, :], W1s.all(), [W1.k(l4)])
                DMA("sp", w2s[:, :, :], w2_d.rearrange("(h p) d -> p h d", p=128), [], w2s.all())
                for dd in range(2):
                    CP("pool", w2[:, :, dd * 64:(dd + 1) * 64], w2s[:, :, :], w2s.all(), w2.all())
                for hc in range(2):
                    bk = banks[0]
                    for l in range(32):
                        PE(bk[:, hc:hc + 1], W1[0:64, l, hc * 128:(hc + 1) * 128], posTb[0:64, kind, l:l + 1], (l == 0 and hc == 0), (l == 31 and hc == 1),
                           W1.all() + posTb.all(), bk.all(), skip_group_check=True)
                CP("dve", pbias[:, :], banks[0][:, 0:2], banks[0].all(), pbias.all())
                for g in range(2):
                    ps = slice(g * 64, (g + 1) * 64)
                    for hc in range(2):
                        bk = banks[1 + g]
                        for l in range(32):
                            PE(bk[:, hc * 128:hc * 128 + 127], W1[ps, l, hc * 128:(hc + 1) * 128], SRC[ps, l:l + 16 * 126 + 1:16], (l == 0 and hc == 0), (l == 31 and hc == 1),
                               W1.all() + SRC.all(), bk.all(), skip_group_check=True)
                    for hc in range(2):
                        bk = banks[1 + g]
                        ACT(hx[:, 0:127], bk[:, hc * 128:hc * 128 + 127], AF.Identity, bk.all() + pbias.all(), hx.all(), bias=pbias[:, hc:hc + 1], scale=1.0)
                        TT("dve", hx2[:, 0:127], hx[:, 0:127], hx[:, 0:127], ALU.mult, hx.all(), hx2.all())
                        TS("dve", hx2[:, 0:127], hx2[:, 0:127], 0.044715, 1.0, ALU.mult, ALU.add, hx2.all(), hx2.all())
                        TT("dve", hx2[:, 0:127], hx2[:, 0:127], hx[:, 0:127], ALU.mult, hx.all() + hx2.all(), hx2.all())
                        ACT(hx2[:, 0:127], hx2[:, 0:127], AF.Sigmoid, hx2.all(), hx2.all(), scale=1.5957691216057308)
                        TT("dve", hT[:, g, hc, 0:127], hx2[:, 0:127], hx[:, 0:127], ALU.mult, hx.all() + hx2.all(), hT.all())
                    bk = banks[3 + g]
                    if kind == 0:
                        for hc in range(2):
                            PE(bk[:, 0:127], w2[:, hc, :], hT[:, g, hc, 0:127], hc == 0, hc == 1, w2.all() + hT.all(), bk.all())
                        CP("act", KCc[:, g, 0:127], bk[:, 0:127], bk.all(), KCc.all())
                    else:
                        for hc in range(2):
                            PE(bk[0:127, 0:64], hT[:, g, hc, 0:127], w2[:, hc, 0:64], hc == 0, hc == 1, w2.all() + hT.all(), bk.all())
                        CP("act", VX[0:127, g, 0:64], bk[0:127, 0:64], bk.all(), VX.all())
            self.pop()
            CM = self.sb("CM", [128, SEQ], BF16)
            onesb2 = self.sb("onesb2", [128, SEQ], BF16)
            op("pool", lambda e: e.memset(onesb2[:, :], 1.0), w=onesb2.all())
            op("pool", lambda e: e.affine_select(CM[:, :], onesb2[:, :], [[1, SEQ]], ALU.is_ge, 0.0, base=-31, channel_multiplier=-16),
               r=onesb2.all(), w=CM.all())
            selAB = self.sb("selAB", [128, 2, 16, 32], F32)
            DMA("sp", selAB[:, :, :, :], selAB_d[:, :, :, :], [], selAB.all())
            expst = self.sb("expst", [32, 16, 128], F32)
            expall = self.sb("expall", [32, 16, 128], BF16)
            DMA("sp", expst[:, :, :], expall_d[:, :, :], [], expst.all())
            CP("pool", expall[:, :, :], expst[:, :, :], expst.all(), expall.all())
            Et = [self.sb(f"nt_E{i}", [128, 512], BF16) for i in range(4)]
            O = self.sb("nt_O", [128, 512], F32)
            Ob = self.sb("nt_Ob", [128, 512], BF16)
            otmp = self.sb("nt_otmp", [128, 256], F32)
            RD = self.sb("nt_RD", [128, 8], F32)
            coef = self.sb("nt_coef", [128, 8], F32)
            imp = self.sb("nt_imp", [128, 2, 32], F32)
            imt = self.sb("nt_imt", [128, 4, 32], F32)
            top8 = self.sb("nt_top8", [128, 2, 8], F32)
            imw = self.sb("nt_imw", [128, 32], F32)
            selb = self.sb("nt_selb", [128, 2, 32], BF16)
            selT = self.sb("nt_selT", [32, 2, 128], BF16)
            nE = 0
            cntE = [0]
            nq = cfg.get("n_qtiles", TPS)
            for i in range(nq):
                it = slice(i * 128, (i + 1) * 128)
                gv = gns[:, i, :].rearrange("p (g e q k) -> p g q e k", g=2, e=2, q=2)

                def mk_coef(kbr, accs, W):
                    for g in range(2):
                        dcol = accs[g][:, 0:4 * W].rearrange("p (r w) -> p r w", w=W)[:, :, 64]
                        TS("dve", RD[:, g * 4:(g + 1) * 4], dcol, 1e-30, None, ALU.max, ALU.bypass, accs[g].all(), RD.all())
                    op("dve", lambda e: e.reciprocal(RD[:, :], RD[:, :]), r=RD.all(), w=RD.all())
                    TT("dve", coef[:, :].rearrange("p (g q e) -> p g q e", g=2, q=2), RD[:, :].rearrange("p (g q e) -> p g q e", g=2, q=2),
                       gv[:, :, :, :, kbr], ALU.mult, RD.all() + gns.all(), coef.all())

                ncv = min(127, 8 * i + 7)
                sA, sB = banks[3], banks[4]
                for par, sb_ in ((0, sA), (1, sB)):
                    ps = slice(par * 64, par * 64 + 64)
                    for g in range(2):
                        PE(sb_[0:ncv, g * 256:(g + 1) * 256], KCc[ps, g, 0:ncv], QBr[ps, 2 * g:2 * g + 2, it], g == 0, g == 1,
                           KCc.all() + [QBr.k(i // 4)], sb_.all(), skip_group_check=True)
                EA, EB = Et[nE % 4], Et[(nE + 1) % 4]
                nE += 2
                for sb_, E_ in ((sA, EA), (sB, EB)):
                    ACT(E_[0:ncv, :], sb_[0:ncv, :], AF.Exp, sb_.all(), E_.all(), scale=0.125)
                    TT("pool", E_[0:ncv, :].rearrange("p (r t) -> p r t", r=4), E_[0:ncv, :].rearrange("p (r t) -> p r t", r=4),
                       CM[0:ncv, it].unsqueeze(1).to_broadcast([ncv, 4, 128]), ALU.mult, E_.all() + CM.all(), E_.all())
                accc = [banks[5], banks[6]]
                for g in range(2):
                    for par, E_ in ((0, EA), (1, EB)):
                        for e_ in range(2):
                            r_ = par * 2 + e_
                            blk = (g * 2 + e_) * 128
                            PE(accc[g][:, r_ * 97:(r_ + 1) * 97], E_[0:ncv, blk:blk + 128], VX[0:ncv, g, :], r_ == 0, r_ == 3,
                               E_.all() + VX.all(), accc[g].all(), skip_group_check=True)
                mk_coef(0, accc, 97)
                attn_out([Acc(accc[0], 97), Acc(accc[1], 97)], O, coef=coef, first=True)
                need_sel = i >= 8
                if need_sel:
                    for g in range(2):
                        TT("dve", imt[:, :, :], accc[g][:, 0:388].rearrange("p (r w) -> p r w", w=97)[:, :, 65:97],
                           RD[:, g * 4:(g + 1) * 4].unsqueeze(2).to_broadcast([128, 4, 32]), ALU.mult, accc[g].all() + RD.all(), imt.all())
                        op("dve", lambda e, g=g: e.tensor_reduce(imp[:, g, :], imt[:, :, :].rearrange("p r j -> p j r"), AX.X, ALU.add),
                           r=imt.all(), w=imp.all())
                        TT("dve", imp[:, g, :], imp[:, g, :], selAB[:, 0, i, :], ALU.mult, imp.all() + selAB.all(), imp.all())
                        TT("dve", imp[:, g, :], imp[:, g, :], selAB[:, 1, i, :], ALU.add, imp.all() + selAB.all(), imp.all())
                        op("dve", lambda e, g=g: e.max(top8[:, g, :], imp[:, g, :]), r=imp.all(), w=top8.all())
                        op("dve", lambda e, g=g: e.match_replace(imw[:, :], top8[:, g, :], imp[:, g, :], -3.0e38), r=imp.all() + top8.all(), w=imw.all())
                        op("dve", lambda e, g=g: e.max(top8[:, g, :], imw[:, :]), r=imw.all(), w=top8.all())
                        TS("dve", selb[:, g, :], imp[:, g, :], top8[:, g, 7:8], -30000.0, ALU.is_lt, ALU.mult, imp.all() + top8.all(), selb.all())
                    bk = banks[2]
                    pT = bfv(bk)
                    for g in range(2):
                        TR(pT[0:32, g * 128:(g + 1) * 128], selb[:, g, :], ident_b[:, :], selb.all() + ident_b.all(), bk.all())
                    CP("act", selT[:, :, :], pT[0:32, 0:256].rearrange("p (g t) -> p g t", g=2), bk.all(), selT.all())

                for kbr, (KK, VV) in ((2, (KW, VW)), (1, (KS, VS))):
                    j_lo = 0 if kbr == 1 else max(0, i - 4)
                    accb = [banks[7], banks[0]] if kbr == 2 else [banks[5], banks[6]]
                    def scores(j, kbr=kbr, KK=KK):
                        jt = slice(j * 128, (j + 1) * 128)
                        sA, sB = (banks[3], banks[4]) if (j % 2 == 0) else (banks[1], banks[2])
                        for par, sb_ in ((0, sA), (1, sB)):
                            ps = slice(par * 64, par * 64 + 64)
                            bias = None
                            if j == i:
                                bias = trib
                            elif kbr == 2 and j == i - 4:
                                bias = edgeb
                            selbias = (kbr == 1 and need_sel and j < i)
                            last_plain = (bias is None and not selbias)
                            for g in range(2):
                                PE(sb_[:, g * 256:(g + 1) * 256], KK[ps, g, jt], QB[ps, 2 * g:2 * g + 2, it], g == 0, (g == 1 and last_plain),
                                   [KK.k(j // 4), QB.k(i // 4)], sb_.all(), skip_group_check=True)
                            if bias is not None:
                                PE(sb_[:, :], ident_b[:, :], bias[:, :].unsqueeze(1).to_broadcast([128, 4, 128]), False, True,
                                   ident_b.all() + bias.all(), sb_.all(), skip_group_check=True)
                            elif selbias:
                                for g in range(2):
                                    PE(sb_[:, g * 256:(g + 1) * 256], expall[:, j, :], selT[:, g, :].unsqueeze(1).to_broadcast([32, 2, 128]), False, g == 1,
                                       expall.all() + selT.all(), sb_.all(), skip_group_check=True)

                    def exp_pv(j, VV=VV, accb=accb, j_lo=j_lo):
                        sA, sB = (banks[3], banks[4]) if (j % 2 == 0) else (banks[1], banks[2])
                        EA, EB = Et[cntE[0] % 4], Et[(cntE[0] + 1) % 4]
                        cntE[0] += 2
                        ACT(EA[:, :], sA[:, :], AF.Exp, sA.all(), EA.all(), scale=0.125)
                        ACT(EB[:, :], sB[:, :], AF.Exp, sB.all(), EB.all(), scale=0.125)
                        for g in range(2):
                            for par, E_ in ((0, EA), (1, EB)):
                                for e_ in range(2):
                                    r_ = par * 2 + e_
                                    blk = (g * 2 + e_) * 128
                                    PE(accb[g][:, r_ * 65:(r_ + 1) * 65], E_[:, blk:blk + 128], VV[:, j, g, :],
                                       (j == j_lo and r_ == 0), (j == i and r_ == 3), E_.all() + VV.all(), accb[g].all(), skip_group_check=True)

                    scores(j_lo)
                    for j in range(j_lo, i + 1):
                        if j + 1 <= i:
                            scores(j + 1)
                        exp_pv(j)
                    mk_coef(kbr, accb, 65)
                    attn_out([Acc(accb[0], 65), Acc(accb[1], 65)], O, coef=coef, first=False, tmpb=otmp)
                o_to_T(O, Ob, obT, i, banks[2])
            self.pop()
            self.pop()

        NB = 4

        def m3(b, oaT, obT):
            self.push()
            mergedT = self.sb("mergedT", [128, 8, SEQ], BF16, nreg=4)
            wout = self.sb("wout", [128, 8, D], BF16, nreg=2)
            g1t = self.sb("g1t", [128, D], F32)
            s2t = self.sb("s2t", [128, D], F32)
            h2t = self.sb("h2t", [128, D], F32)
            load_mod(g1t, b, 2)
            load_mod(h2t, b, 3)
            load_mod(s2t, b, 4)
            if do_mixer:
                self.push()
                uT = self.sb("uTm", [128, 8, SEQ], BF16, nreg=4)
                wbra = self.sb("wbra", [128, 4, D], BF16)
                wbrb = self.sb("wbrb", [128, 4, D], BF16)
                build_uT(b, uT)
                self.push()
                wst_ = self.sb("m3_wst", [128, 4, D], F32)
                DMA("sp", wst_[:, :, :], wbrA_d.rearrange("(k p) n -> p k n", p=128), [], wst_.all())
                CP("pool", wbra[:, :, :], wst_[:, :, :], wst_.all(), wbra.all())
                DMA("sp", wst_[:, :, :], wbrB_d.rearrange("(k p) n -> p k n", p=128), [], wst_.all())
                CP("pool", wbrb[:, :, :], wst_[:, :, :], wst_.all(), wbrb.all())
                wov = wout_d.rearrange("(k p) n -> p k n", p=128)
                for hh in range(2):
                    DMA("sp", wst_[:, :, :], wov[:, hh * 4:(hh + 1) * 4, :], [], wst_.all())
                    CP("pool", wout[:, hh * 4:(hh + 1) * 4, :], wst_[:, :, :], wst_.all(), [wout.k(hh)])
                self.pop()
                gst = [self.sb(f"m3_gst{i}", [128, 8, 128], F32) for i in range(2)]
                gwb = [self.sb(f"m3_gwb{i}", [128, 8, 128], BF16) for i in range(4)]
                sga = [self.sb(f"m3_sga{i}", [128, 512], F32) for i in range(2)]
                sgb = [self.sb(f"m3_sgb{i}", [128, 512], F32) for i in range(2)]
                nl = 0
                for fc in range(8):
                    gws = []
                    for which in range(2):
                        st_, wb_ = gst[nl % 2], gwb[nl % 4]
                        nl += 1
                        DMA("sp", st_[:, :, :], wmG_d[which * 8 + fc], [], st_.all())
                        CP("pool", wb_[:, :, :], st_[:, :, :], st_.all(), wb_.all())
                        gws.append(wb_)
                    fs = slice(fc * 128, (fc + 1) * 128)
                    for tc in range(4):
                        ts_ = slice(tc * 512, (tc + 1) * 512)
                        bya, byb, bga, bgb = banks[0], banks[1], banks[2], banks[3]
                        if tc % 2 == 1:
                            bya, byb, bga, bgb = banks[4], banks[5], banks[6], banks[7]
                        for k in range(4):
                            PE(bya[:, :], wbra[:, k, fs], oaT[:, k, ts_], k == 0, k == 3, wbra.all() + [oaT.k(tc)], bya.all())
                        for k in range(4):
                            PE(byb[:, :], wbrb[:, k, fs], obT[:, k, ts_], k == 0, k == 3, wbrb.all() + [obT.k(tc)], byb.all())
                        for k in range(8):
                            PE(bga[:, :], gws[0][:, k, :], uT[:, k, ts_], k == 0, k == 7, gws[0].all() + [uT.k(tc)], bga.all())
                        for k in range(8):
                            PE(bgb[:, :], gws[1][:, k, :], uT[:, k, ts_], k == 0, k == 7, gws[1].all() + [uT.k(tc)], bgb.all())
                        SA, SB = sga[tc % 2], sgb[tc % 2]
                        ACT(SA[:, :], bga[:, :], AF.Sigmoid, bga.all(), SA.all())
                        ACT(SB[:, :], bgb[:, :], AF.Sigmoid, bgb.all(), SB.all())
                        TT("dve", SA[:, :], SA[:, :], bya[:, :], ALU.mult, SA.all() + bya.all(), SA.all())
                        TT("dve", SB[:, :], SB[:, :], byb[:, :], ALU.mult, SB.all() + byb.all(), SB.all())
                        TT("pool", mergedT[:, fc, ts_], SA[:, :], SB[:, :], ALU.add, SA.all() + SB.all(), [mergedT.k(tc)])
                self.pop()
            if "mergedT" in dbg and b == 0:
                self.push()
                stg = [self.sb(f"dbg_stg{i}", [128, SEQ], F32) for i in range(2)]
                nd = 0
                for nm, srcb, nk in (("mergedT", mergedT, 8), ("oaT", oaT, 4), ("obT", obT, 4)):
                    for k in range(nk):
                        sg_ = stg[nd % 2]
                        nd += 1
                        CP("dve", sg_[:, :], srcb[:, k, :], srcb.all(), sg_.all())
                        DMA("sp", dbg[nm][:, k, :], sg_[:, :], sg_.all(), [("dbg", nm, k)])
                self.pop()
            s2_bc = {b: s2t}
            h2_bc = {b: h2t}
            xt = [self.sb(f"xt{i}", [128, D], F32) for i in range(NB)]
            x1t = [self.sb(f"x1t{i}", [128, D], F32) for i in range(NB)]
            u2r = [self.sb(f"u2r{i}", [128, D], BF16) for i in range(NB)]
            u2T = [self.sb(f"u2T{i}", [128, 8, 128], BF16) for i in range(NB)]
            lnb = [(self.sb(f"ln_st{i}", [128, 2, 6], F32), self.sb(f"ln_mv{i}", [128, 2], F32),
                    self.sb(f"ln_rs{i}", [128, 1], F32)) for i in range(NB)]
            sc = [self.sb(f"r_sc{i}", [128, NE], F32) for i in range(NB)]
            ch = [self.sb(f"r_ch{i}", [128, NE], F32) for i in range(NB)]
            tmp = [self.sb(f"r_tmp{i}", [128, NE], F32) for i in range(NB)]
            selb = [self.sb(f"r_selb{i}", [128, NE], BF16) for i in range(NB)]
            Gm = [self.sb(f"r_G{i}", [128, NE], F32) for i in range(NB)]
            Dm = [self.sb(f"r_D{i}", [128, NE], F32) for i in range(NB)]
            sm = [self.sb(f"r_sm{i}", [128, 64], F32) for i in range(NB)]
            mixs = [self.sb(f"mixs{i}", [128, D], F32) for i in range(NB)]

            def phase1_tile(t):
                i = t % NB
                tt = t % TPS
                X, X1, U2, U2T, LB = xt[i], x1t[i], u2r[i], u2T[i], lnb[i]
                SC, CH, TMP, SELB, G, DD, SM = sc[i], ch[i], tmp[i], selb[i], Gm[i], Dm[i], sm[i]
                MX = mixs[i]
                rows = slice(t * 128, (t + 1) * 128)
                DMA("sp", X[:, :], x_d[rows, :], [], X.all())
                if do_mixer:
                    for hf in range(2):
                        bk = banks[hf]
                        with S.atomic():
                            for k in range(8):
                                PE(bk[:, :], mergedT[:, k, tt * 128:(tt + 1) * 128], wout[:, k, hf * 512:(hf + 1) * 512], k == 0, k == 7,
                                   [mergedT.k(tt // 4)] + wout.all(), bk.all())
                            TT("dve", MX[:, hf * 512:(hf + 1) * 512], bk[:, :], g1t[:, hf * 512:(hf + 1) * 512], ALU.mult, bk.all() + g1t.all(), MX.all())
                    if "mix" in dbg and b == 0:
                        DMA("sp", dbg["mix"][tt * 128:(tt + 1) * 128, :], MX[:, :], MX.all(), [("dbg", "mix", tt)])
                    STT(X[:, :], X[:, :], float(DN_ALPHA), MX[:, :], ALU.mult, ALU.add, X.all() + MX.all(), X.all())
                else:
                    ACT(X[:, :], X[:, :], AF.Copy, X.all(), X.all(), scale=float(DN_ALPHA))
                layernorm(X, X1, ln1g, ln1b, LB)
                DMA("sp", x1_d[rows, :], X1[:, :], X1.all(), [("x1_d", t)])
                op("dve", lambda e, X=X, X1=X1, b=b: e.tensor_tensor(X[:, :], X1[:, :], s2_bc[b][:, :], ALU.mult),
                   r=X1.all() + s2_bc[b].all(), w=X.all())
                op("dve", lambda e, X=X, U2=U2, b=b: e.tensor_tensor(U2[:, :], X[:, :], h2_bc[b][:, :], ALU.add),
                   r=X.all() + h2_bc[b].all(), w=U2.all())
                bkT = banks[4 + (t % 2)]
                pT = bkT.t[:, :].bitcast(BF16)
                with S.atomic():
                    for k in range(8):
                        op("pe", lambda e, k=k, U2=U2, pT=pT: e.transpose(pT[:, k * 128:(k + 1) * 128], U2[:, k * 128:(k + 1) * 128], ident_b[:, :]),
                           r=U2.all() + ident_b.all(), w=bkT.all())
                    op("act", lambda e, U2T=U2T, pT=pT: e.activation(U2T[:, :, :], pT.rearrange("p (k t) -> p k t", k=8), AF.Copy),
                       r=bkT.all(), w=U2T.all())
                bkR = banks[6 + (t % 2)]
                with S.atomic():
                    for k in range(8):
                        op("pe", lambda e, k=k, U2T=U2T, bkR=bkR: e.matmul(bkR[:, 0:NE], U2T[:, k, :], wr_b[:, k, :], start=(k == 0), stop=(k == 7)),
                           r=U2T.all() + wr_b.all(), w=bkR.all())
                    op("act", lambda e, SC=SC, bkR=bkR: e.activation(SC[:, :], bkR[:, 0:NE], AF.Sigmoid), r=bkR.all(), w=SC.all())
                op("dve", lambda e, SC=SC, CH=CH: e.tensor_tensor(CH[:, :], SC[:, :], rb_bc[:, :], ALU.add), r=SC.all() + rb_bc.all(), w=CH.all())
                ch3 = CH[:, :].rearrange("p (g j) -> p g j", g=8)
                tmp3 = TMP[:, :].rearrange("p (g j) -> p g j", g=8)
                m1, m2, gs, top4, keep, pen, top8, wsum, rw = (SM[:, 0:8], SM[:, 8:16], SM[:, 16:24], SM[:, 24:32], SM[:, 32:40],
                                                               SM[:, 40:48], SM[:, 48:56], SM[:, 56:57], SM[:, 57:58])
                op("dve", lambda e: e.tensor_reduce(m1, ch3, AX.X, ALU.max), r=CH.all(), w=SM.all())
                op("dve", lambda e: e.tensor_tensor(tmp3, ch3, m1.unsqueeze(2).to_broadcast([128, 8, 32]), ALU.is_equal),
                   r=CH.all() + SM.all(), w=TMP.all())
                op("dve", lambda e, TMP=TMP, CH=CH: e.scalar_tensor_tensor(TMP[:, :], TMP[:, :], -1e9, CH[:, :], ALU.mult, ALU.add),
                   r=TMP.all() + CH.all(), w=TMP.all())
                op("dve", lambda e: e.tensor_reduce(m2, tmp3, AX.X, ALU.max), r=TMP.all(), w=SM.all())
                op("dve", lambda e: e.tensor_tensor(gs, m1, m2, ALU.add), r=SM.all(), w=SM.all())
                op("dve", lambda e: e.max(top4, gs), r=SM.all(), w=SM.all())
                op("dve", lambda e: e.tensor_scalar(keep, gs, top4[:, 3:4], None, ALU.is_ge), r=SM.all(), w=SM.all())
                op("dve", lambda e: e.tensor_scalar(pen, keep, -1.0, 1e9, ALU.add, ALU.mult), r=SM.all(), w=SM.all())
                op("dve", lambda e: e.tensor_tensor(ch3, ch3, pen.unsqueeze(2).to_broadcast([128, 8, 32]), ALU.add),
                   r=CH.all() + SM.all(), w=CH.all())
                op("dve", lambda e, CH=CH: e.max(top8, CH[:, :]), r=CH.all(), w=SM.all())
                op("dve", lambda e, TMP=TMP, CH=CH: e.tensor_scalar(TMP[:, :], CH[:, :], top8[:, 7:8], None, ALU.is_ge),
                   r=CH.all() + SM.all(), w=TMP.all())
                op("pool", lambda e, TMP=TMP, SELB=SELB: e.tensor_copy(SELB[:, :], TMP[:, :]), r=TMP.all(), w=SELB.all())
                op("dve", lambda e, G=G, TMP=TMP, SC=SC: e.scalar_tensor_tensor(G[:, :], TMP[:, :], 1.0, SC[:, :], ALU.mult, ALU.mult, accum_out=wsum),
                   r=TMP.all() + SC.all(), w=G.all() + SM.all())
                op("dve", lambda e: e.reciprocal(rw, wsum), r=SM.all(), w=SM.all())
                op("dve", lambda e, G=G: e.tensor_scalar(G[:, :], G[:, :], rw, float(ROUTED_SCALE), ALU.mult, ALU.mult),
                   r=G.all() + SM.all(), w=G.all())
                S.cut()
                bkP = banks[2 + (t % 2)]
                op("pe", lambda e, bkP=bkP, SELB=SELB: e.matmul(bkP[:, 0:NE], tri_b[:, :], SELB[:, :], start=True, stop=True),
                   r=tri_b.all() + SELB.all(), w=bkP.all())
                op("pe", lambda e, bkP=bkP, SELB=SELB: e.matmul(bkP[:, NE:2 * NE], ones_b[:, :], SELB[:, :], start=False, stop=True),
                   r=ones_b.all() + SELB.all(), w=bkP.all())
                op("dve", lambda e, DD=DD, bkP=bkP: e.tensor_tensor(DD[:, :], bkP[:, 0:NE], carry[:, :], ALU.add),
                   r=bkP.all() + carry.all(), w=DD.all())
                op("dve", lambda e, bkP=bkP: e.tensor_tensor(carry[:, :], carry[:, :], bkP[:, NE:2 * NE], ALU.add),
                   r=bkP.all() + carry.all(), w=carry.all())
                S.cut()
                op("dve", lambda e, DD=DD: e.tensor_tensor(DD[:, :], DD[:, :], iotaC[:, :], ALU.add), r=DD.all() + iotaC.all(), w=DD.all())
                for k in range(8):
                    op("dve", lambda e, k=k, TMP=TMP, CH=CH, DD=DD, t=t: e.scalar_tensor_tensor(
                        TMP[:, :], CH[:, :], top8[:, k:k + 1], DD[:, :], ALU.is_equal, ALU.mult, accum_out=dest_f[:, t, k:k + 1]),
                       r=CH.all() + SM.all() + DD.all(), w=TMP.all() + [dest_f.k(t)])
                    op("dve", lambda e, k=k, TMP=TMP, CH=CH, G=G, t=t: e.scalar_tensor_tensor(
                        TMP[:, :], CH[:, :], top8[:, k:k + 1], G[:, :], ALU.is_equal, ALU.mult, accum_out=wk[:, t, k:k + 1]),
                       r=CH.all() + SM.all() + G.all(), w=TMP.all() + [wk.k(t)])
                DMA("sp", u2_d[rows, :], U2[:, :], U2.all(), [("u2_d", t)])

            for t0 in range(b * TPS, min((b + 1) * TPS, n_tiles), NB):
                grp = [t for t in range(t0, min(t0 + NB, n_tiles, (b + 1) * TPS))]
                parts = [S.record_parts(lambda t=t: phase1_tile(t)) for t in grp]
                S.merge_rr([p[0] for p in parts])
                S.merge_seq([p[1] for p in parts])
                S.merge_rr([p[2] for p in parts])
            self.pop()

        for b in range(BPC):
            if b * TPS >= n_tiles:
                break
            self.push()
            oaT = self.sb("oaT", [128, 4, SEQ], BF16, nreg=4)
            obT = self.sb("obT", [128, 4, SEQ], BF16, nreg=4)
            if do_mixer:
                self.push()
                CS64 = self.sb("CS64", [128, 2, SEQ], BF16, nreg=2)
                rope_tables(b, CS64, 0, 1, 128)
                dsa(b, oaT, CS64)
                nsa(b, obT, CS64)
                self.pop()
            m3(b, oaT, obT)
            self.pop()

        if "dest" in dbg:
            DMA("sp", dbg["dest"], dest_f[:, :, :], dest_f.all(), [("dbg", 0)])
        if "wk" in dbg:
            DMA("sp", dbg["wk"], wk[:, :, :], wk.all(), [("dbg", 1)])

        self.push()
        nb_f = self.sb("nb_f", [128, NE], F32)
        nb_t = self.sb("nb_t", [128, NE], F32)
        nb_i = self.sb("nb_i", [128, NE], I32)
        nb_b = self.sb("nb_b", [128, NE], BF16)
        nbT = self.sb("nbT", [128, 2, 128], BF16)
        startr = self.sb("startr", [128, NE], F32)
        endb = self.sb("endb", [128, NE], F32)
        blkE = self.sb("blkE", [128, 4], F32)
        Rm = self.sb("Rm", [128, NBLK // 128, 128], BF16)
        widx_f = self.sb("widx_f", [128, NBLK], F32)
        widx_i = self.sb("widx_i", [128, NBLK], I32)
        TS("dve", nb_f[:, :], carry[:, :], float(BLK - 1), 1.0 / BLK, ALU.add, ALU.mult, carry.all(), nb_f.all())
        CP("dve", nb_i[:, :], nb_f[:, :], nb_f.all(), nb_i.all())
        CP("dve", nb_t[:, :], nb_i[:, :], nb_i.all(), nb_t.all())
        TT("dve", nb_f[:, :], nb_t[:, :], nb_f[:, :], ALU.is_gt, nb_t.all() + nb_f.all(), nb_f.all())
        TT("dve", nb_f[:, :], nb_t[:, :], nb_f[:, :], ALU.subtract, nb_t.all() + nb_f.all(), nb_f.all())
        CP("dve", nb_b[:, :], nb_f[:, :], nb_f.all(), nb_b.all())
        bk = banks[0]
        pT = bfv(bk)
        for a_ in range(2):
            TR(pT[:, a_ * 128:(a_ + 1) * 128], nb_b[:, a_ * 128:(a_ + 1) * 128], ident_b[:, :], nb_b.all() + ident_b.all(), bk.all())
        CP("act", nbT[:, :, :], pT[:, 0:256].rearrange("p (a t) -> p a t", a=2), bk.all(), nbT.all())
        bk = banks[1]
        for a_ in range(2):
            PE(bk[:, 0:NE], nbT[:, a_, :], Utri[:, a_, :], a_ == 0, a_ == 1, nbT.all() + Utri.all(), bk.all())
        TS("dve", startr[:, :], bk[:, 0:NE], float(BLK), None, ALU.mult, ALU.bypass, bk.all(), startr.all())
        TT("dve", endb[:, :], bk[:, 0:NE], nb_f[:, :], ALU.add, bk.all() + nb_f.all(), endb.all())
        for a_ in range(NBLK // 128):
            TS("dve", nb_t[:, :], endb[:, :], jcol[:, a_:a_ + 1], None, ALU.is_le, ALU.add, endb.all() + jcol.all(), nb_t.all() + blkE.all(),
               accum=blkE[:, a_:a_ + 1])
        for a_ in range(NBLK // 128):
            TS("dve", Rm[:, a_, :], ident_b[:, :], blkE[:, a_:a_ + 1], None, ALU.mult, ALU.bypass, ident_b.all() + blkE.all(), Rm.all())
        bk = banks[2]
        PE(bk[:, 0:NBLK], ones_b[:, :], Rm[:, :, :].rearrange("p a t -> p (a t)"), True, True, ones_b.all() + Rm.all(), bk.all())
        TS("dve", widx_f[:, :], bk[:, 0:NBLK], 128.0, pcol[:, 0:1], ALU.mult, ALU.add, bk.all() + pcol.all(), widx_f.all())
        CP("dve", widx_i[:, :], widx_f[:, :], widx_f.all(), widx_i.all())
        if "blkE" in dbg:
            DMA("sp", dbg["blkE"], blkE[:, :], blkE.all(), [("dbg", "blkE")])
            DMA("sp", dbg["startr"], startr[:, :], startr.all(), [("dbg", "startr")])

        self.push()
        U2s = [self.sb(f"p2_u{i}", [128, D], BF16) for i in range(4)]
        p2 = [self.sb(f"p2_s{i}", [128, 40], F32) for i in range(4)]
        p2i = [self.sb(f"p2_i{i}", [128, 8], I32) for i in range(4)]
        p2j = [self.sb(f"p2_j{i}", [128, NE], F32) for i in range(4)]
        def pass2_tile(t):
            U2 = U2s[t % 4]
            P2, P2I, P2J = p2[t % 4], p2i[t % 4], p2j[t % 4]
            ef, et, ps_, sl = P2[:, 0:8], P2[:, 8:16], P2[:, 16:24], P2[:, 24:32]
            rows = slice(t * 128, (t + 1) * 128)
            DMA("sp", U2[:, :], u2_d[rows, :], [("u2_d", t)], U2.all())
            TS("dve", ef, dest_f[:, t, :], 1.0 / 4096.0, None, ALU.mult, ALU.bypass, [dest_f.k(t)], P2.all())
            CP("dve", P2I[:, :], ef, P2.all(), P2I.all())
            CP("dve", et, P2I[:, :], P2I.all(), P2.all())
            TT("dve", ps_, et, ef, ALU.is_gt, P2.all(), P2.all())
            TT("dve", et, et, ps_, ALU.subtract, P2.all(), P2.all())
            STT(ps_, et, -4096.0, dest_f[:, t, :], ALU.mult, ALU.add, P2.all() + [dest_f.k(t)], P2.all())
            for k in range(8):
                STT(P2J[:, :], iotaE1[:, :], et[:, k:k + 1], startr[:, :], ALU.is_equal, ALU.mult, iotaE1.all() + P2.all() + startr.all(),
                    P2J.all() + P2.all(), accum=sl[:, k:k + 1])
            TT("dve", sl, sl, ps_, ALU.add, P2.all(), P2.all())
            CP("dve", dest_i[:, t, :], sl, P2.all(), [dest_i.k(t)])
            for k in range(8):
                op("pool", lambda e, k=k, t=t, U2=U2: e.indirect_dma_start(
                    out=xs_d[:, :], out_offset=IOA(ap=dest_i[:, t, k:k + 1], axis=0), in_=U2[:, :], in_offset=None,
                    bounds_check=bc_reg(e), oob_is_err=False),
                   r=U2.all() + [dest_i.k(t)], w=[("xs_d", "all")], dma=True)
        for t0 in range(0, n_tiles, 4):
            grp = list(range(t0, min(t0 + 4, n_tiles)))
            parts = [S.record_parts(lambda t=t: pass2_tile(t)) for t in grp]
            S.merge_rr([p[0] for p in parts])
        self.pop()
        if "desti" in dbg:
            self.push()
            dtmp = self.sb("dtmp", [128, NT, 8], F32)
            CP("dve", dtmp[:, :, :], dest_i[:, :, :], dest_i.all(), dtmp.all())
            DMA("sp", dbg["desti"], dtmp[:, :, :], dtmp.all(), [("dbg", "desti")])
            self.pop()

        n_blk = cfg.get("n_blk", NBLK)

        wgv = wg_d.rearrange("e (p k) n -> (e p) (k n)", k=8)
        wuv = wu_d.rearrange("e (p k) n -> (e p) (k n)", k=8)
        wdv = wd_d.rearrange("e (p h) n -> (e p) (h n)", h=2)

        def wreg(e):
            if "wb" not in _regs:
                _regs["wb"] = e.to_reg(NE * 128 - 1)
            return _regs["wb"]

        NWB = 3
        w_st = [self.sb(f"w_st{i}", [128, 6144], F32, nreg=3) for i in range(NWB)]
        w_bf = [self.sb(f"w_bf{i}", [128, 6144], BF16, nreg=3) for i in range(NWB)]
        hsel = [self.sb(f"hsel{i}", [128, D], BF16) for i in range(3)]
        hselT = [self.sb(f"hselT{i}", [128, 8, 128], BF16) for i in range(2)]
        sg = [self.sb(f"sg{i}", [128, 256], F32) for i in range(2)]
        actk = [self.sb(f"actk{i}", [128, 256], BF16) for i in range(2)]
        actT = [self.sb(f"actT{i}", [128, 2, 128], BF16) for i in range(2)]
        ysb = [self.sb(f"ysb{i}", [128, D], BF16, nreg=2) for i in range(3)]

        hselT3 = [self.sb(f"hselT3_{i}", [128, 8, 128], BF16) for i in range(3)]
        w_bf4 = w_bf + [self.sb("w_bf3", [128, 6144], BF16, nreg=3)]
        NWF = len(w_bf4)

        def stageA0(j):
            WS, HS = w_st[j % NWB], hsel[j % 3]
            for q, wv in enumerate((wgv, wuv, wdv)):
                op("pool", lambda e, q=q, wv=wv: e.indirect_dma_start(
                    out=WS[:, q * 2048:(q + 1) * 2048], out_offset=None, in_=wv, in_offset=IOA(ap=widx_i[:, j:j + 1], axis=0),
                    bounds_check=wreg(e), oob_is_err=False),
                   r=widx_i.all(), w=[WS.k(q)], dma=True)
            DMA("sp", HS[:, :], xs_d[j * BLK:(j + 1) * BLK, :], [("xs_d", "all")], HS.all())

        def stageA(j):
            WS, WB, HS, HT = w_st[j % NWB], w_bf4[j % NWF], hsel[j % 3], hselT3[j % 3]
            WBgu = WB[:, 0:4096].rearrange("p (k two n) -> p k two n", k=8, two=2)
            CP("act", WBgu[:, :, 0, :], WS[:, 0:2048].rearrange("p (k n) -> p k n", k=8), [WS.k(0)], [WB.k(0)])
            CP("dve", WBgu[:, :, 1, :], WS[:, 2048:4096].rearrange("p (k n) -> p k n", k=8), [WS.k(1)], [WB.k(1)])
            CP("dve", WB[:, 4096:6144], WS[:, 4096:6144], [WS.k(2)], [WB.k(2)])
            bkT = banks[j % 2]
            pT_ = bfv(bkT)
            with S.atomic():
                for k in range(8):
                    TR(pT_[:, k * 128:(k + 1) * 128], HS[:, k:D:8], ident_b[:, :], HS.all() + ident_b.all(), bkT.all())
                CP("act", HT[:, :, :], pT_.rearrange("p (k t) -> p k t", k=8), bkT.all(), HT.all())

        def stageB1(j):
            i = j % 2
            WB, HT, SG, AK = w_bf4[j % NWF], hselT3[j % 3], sg[i], actk[i]
            bk_ = banks[2 + j % 2]
            with S.atomic():
                for k in range(8):
                    PE(bk_[:, :], HT[:, k, :], WB[:, k * 512:(k + 1) * 512], k == 0, k == 7, HT.all() + [WB.k(0), WB.k(1)], bk_.all())
                ACT(SG[:, :], bk_[:, 0:256], AF.Silu, bk_.all(), SG.all())
                TT("dve", AK[:, :], SG[:, :], bk_[:, 256:512], ALU.mult, SG.all() + bk_.all(), AK.all())

        def stageB2(j):
            i = j % 2
            AK, AT = actk[i], actT[i]
            bk2 = banks[4 + j % 2]
            pT2 = bfv(bk2)
            with S.atomic():
                for h in range(2):
                    TR(pT2[:, h * 128:(h + 1) * 128], AK[:, h:256:2], ident_b[:, :], AK.all() + ident_b.all(), bk2.all())
                CP("act", AT[:, :, :], pT2[:, 0:256].rearrange("p (h t) -> p h t", h=2), bk2.all(), AT.all())

        def stageB3(j):
            i = j % 2
            WB, AT, YS = w_bf4[j % NWF], actT[i], ysb[j % 3]
            for hf in range(2):
                bkd = banks[6 + hf]
                with S.atomic():
                    for h in range(2):
                        col = 4096 + h * 1024 + hf * 512
                        PE(bkd[:, :], AT[:, h, :], WB[:, col:col + 512], h == 0, h == 1, AT.all() + [WB.k(2)], bkd.all())
                    CP("act" if hf == 0 else "dve", YS[:, hf * 512:(hf + 1) * 512], bkd[:, :], bkd.all(), [YS.k(hf)])
            DMA("sp", ys_d[j * BLK:(j + 1) * BLK, :], YS[:, :], YS.all(), [("ys_d", j)])

        stages = [(0, stageA0), (2, stageA), (3, stageB1), (4, stageB2), (5, stageB3)]
        for s_ in range(n_blk + 5):
            streams = []
            for off, st_fn in stages:
                j = s_ - off
                if 0 <= j < n_blk:
                    streams.append(S.record_parts(lambda j=j, st_fn=st_fn: st_fn(j))[0])
            S.merge_rr(list(reversed(streams)))
        ys_keys = [("ys_d", j) for j in range(n_blk)]
        self.pop()
        self.push()
        ln2g = self.sb("ln2g", [128, D], F32)
        ln2b = self.sb("ln2b", [128, D], F32)
        DMA("sp", ln2g[:, :], ln_d[2:3, :].to_broadcast([128, D]), [], ln2g.all())
        DMA("sp", ln2b[:, :], ln_d[3:4, :].to_broadcast([128, D]), [], ln2b.all())
        s2_bc, h2_bc, g2_bc = {}, {}, {}
        for b in range(BPC):
            g2_bc[b] = self.sb(f"f_g2{b}", [128, D], F32)
            load_mod(g2_bc[b], b, 5)
        GF = 4
        wsh = self.sb("wsh", [128, 6144], BF16, nreg=3)
        self.push()
        wsh_st = self.sb("wsh_st", [128, 6144], F32, nreg=3)
        DMA("sp", wsh_st[:, 0:2048].rearrange("p (k n) -> p k n", k=8), wsg_d.rearrange("(k p) n -> p k n", p=128), [], [wsh_st.k(0)])
        DMA("sp", wsh_st[:, 2048:4096].rearrange("p (k n) -> p k n", k=8), wsu_d.rearrange("(k p) n -> p k n", p=128), [], [wsh_st.k(1)])
        DMA("sp", wsh_st[:, 4096:6144].rearrange("p (k n) -> p k n", k=2), wsd_d.rearrange("(k p) n -> p k n", p=128), [], [wsh_st.k(2)])
        CP("act", wsh[:, 0:2048], wsh_st[:, 0:2048], [wsh_st.k(0)], [wsh.k(0)])
        CP("dve", wsh[:, 2048:4096], wsh_st[:, 2048:4096], [wsh_st.k(1)], [wsh.k(1)])
        CP("pool", wsh[:, 4096:6144], wsh_st[:, 4096:6144], [wsh_st.k(2)], [wsh.k(2)])
        self.pop()
        xt = [self.sb(f"fxt{i}", [128, D], F32) for i in range(GF)]
        x1t = [self.sb(f"fx1t{i}", [128, D], F32) for i in range(GF)]
        u2r = [self.sb(f"fu2r{i}", [128, D], BF16) for i in range(GF)]
        u2T = [self.sb(f"fu2T{i}", [128, 8, 128], BF16) for i in range(GF)]
        lnb = [(self.sb(f"fln_st{i}", [128, 2, 6], F32), self.sb(f"fln_mv{i}", [128, 2], F32),
                self.sb(f"fln_rs{i}", [128, 1], F32)) for i in range(GF)]
        yg = [self.sb(f"yg{i}", [128, 8, D], BF16, nreg=8) for i in range(GF)]
        acc = [self.sb(f"acc{i}", [128, D], F32) for i in range(GF)]
        sgs = [self.sb(f"sgs{i}", [128, 256], F32) for i in range(GF)]
        atk = [self.sb(f"atk{i}", [128, 256], BF16) for i in range(GF)]
        ats = [self.sb(f"ats{i}", [128, 2, 128], BF16) for i in range(GF)]

        def final_tile(t):
            b = t // TPS
            i = t % GF
            X, X1, U2, U2T, LB = xt[i], x1t[i], u2r[i], u2T[i], lnb[i]
            YG, ACC, SGS, ATK, ATS = yg[i], acc[i], sgs[i], atk[i], ats[i]
            rows = slice(t * 128, (t + 1) * 128)
            DMA("sp", X1[:, :], x1_d[rows, :], [("x1_d", t)], X1.all())
            DMA("sp", U2[:, :], u2_d[rows, :], [("u2_d", t)], U2.all())
            for k in range(8):
                op("pool", lambda e, k=k: e.indirect_dma_start(
                    out=YG[:, k, :], out_offset=None, in_=ys_d[:, :], in_offset=IOA(ap=dest_i[:, t, k:k + 1], axis=0),
                    bounds_check=bc_reg(e), oob_is_err=False),
                   r=ys_keys + [dest_i.k(t)], w=[YG.k(k)], dma=True)
            bkT = banks[t % 2]
            pT = bfv(bkT)
            with S.atomic():
                for k in range(8):
                    TR(pT[:, k * 128:(k + 1) * 128], U2[:, k * 128:(k + 1) * 128], ident_b[:, :], U2.all() + ident_b.all(), bkT.all())
                CP("act", U2T[:, :, :], pT.rearrange("p (k t) -> p k t", k=8), bkT.all(), U2T.all())
            bk = banks[2 + (t % 2)]
            with S.atomic():
                for k in range(8):
                    PE(bk[:, 0:256], U2T[:, k, :], wsh[:, k * 256:(k + 1) * 256], k == 0, False, U2T.all() + [wsh.k(0)], bk.all(), skip_group_check=True)
                for k in range(8):
                    PE(bk[:, 256:512], U2T[:, k, :], wsh[:, 2048 + k * 256:2048 + (k + 1) * 256], False, k == 7, U2T.all() + [wsh.k(1)], bk.all(), skip_group_check=True)
                ACT(SGS[:, :], bk[:, 0:256], AF.Silu, bk.all(), SGS.all())
                TT("dve", ATK[:, :], SGS[:, :], bk[:, 256:512], ALU.mult, SGS.all() + bk.all(), ATK.all())
            bk2 = banks[4 + (t % 2)]
            pT2 = bfv(bk2)
            with S.atomic():
                for h in range(2):
                    TR(pT2[:, h * 128:(h + 1) * 128], ATK[:, h * 128:(h + 1) * 128], ident_b[:, :], ATK.all() + ident_b.all(), bk2.all())
                CP("act", ATS[:, :, :], pT2[:, 0:256].rearrange("p (h t) -> p h t", h=2), bk2.all(), ATS.all())
            for hf in range(2):
                bkd = banks[6 + hf]
                with S.atomic():
                    for h in range(2):
                        col = 4096 + h * 1024 + hf * 512
                        PE(bkd[:, :], ATS[:, h, :], wsh[:, col:col + 512], h == 0, h == 1, ATS.all() + [wsh.k(2)], bkd.all())
                    CP("act", ACC[:, hf * 512:(hf + 1) * 512], bkd[:, :], bkd.all(), ACC.all())
            for k in range(8):
                STT(ACC[:, :], YG[:, k, :], wk[:, t, k:k + 1], ACC[:, :], ALU.mult, ALU.add, [YG.k(k), wk.k(t)] + ACC.all(), ACC.all())
            TT("pool", ACC[:, :], ACC[:, :], g2_bc[b][:, :], ALU.mult, ACC.all() + g2_bc[b].all(), ACC.all())
            STT(ACC[:, :], X1[:, :], float(DN_ALPHA), ACC[:, :], ALU.mult, ALU.add, ACC.all() + X1.all(), ACC.all())
            layernorm(ACC, X, ln2g, ln2b, LB)
            DMA("sp", out_d[rows, :], X[:, :], X.all(), [("out_d", t)])

        for t0 in range(0, n_tiles, GF):
            grp = list(range(t0, min(t0 + GF, n_tiles)))
            parts = [S.record_parts(lambda t=t: final_tile(t)) for t in grp]
            S.merge_rr([p[0] for p in parts])
        S.finalize_and_emit()
        return nc


_OFF = dict(q_a=0, k_a=512, v_a=640, q_idx=768, k_idx=1024, w_idx=1056, q_b=1064, k_cmp=1576, v_cmp=1704,
            k_sel=1832, v_sel=1960, k_win=2088, v_win=2216, g_nsa=2344, gate_a=2368, gate_b=3392)


def _sw64(d):
    return d + 8 if d < 8 else (d - 8 if d < 16 else d)


def _sw32(d):
    return d + 4 if d < 4 else (d - 4 if d < 8 else d)


def _chunks_A():
    O = _OFF
    ch = []
    for c in range(4):
        ch.append([O["q_a"] + c * 128 + p for p in range(128)])
    for c in range(4):
        ch.append([O["q_a"] + c * 128 + (p // 64) * 64 + _sw64(p % 64) for p in range(128)])
    for g in range(2):
        ch.append([O["k_a"] + g * 64 + (p % 64) for p in range(128)])
    for g in range(2):
        ch.append([O["k_a"] + g * 64 + _sw64(p % 64) for p in range(128)])
    for sw in (False, True):
        for c in range(3):
            cols = []
            for p in range(128):
                h = min(7, 3 * c + min(p, 95) // 32)
                d = p % 32
                cols.append(O["q_idx"] + h * 32 + (_sw32(d) if sw else d))
            ch.append(cols)
    ch.append([O["k_idx"] + (p % 32) for p in range(128)])
    ch.append([O["k_idx"] + _sw32(p % 32) for p in range(128)])
    return ch


def _chunks_B():
    O = _OFF
    ch = []
    for c in range(4):
        ch.append([O["q_b"] + c * 128 + p for p in range(128)])
    for c in range(4):
        ch.append([O["q_b"] + c * 128 + (p // 64) * 64 + _sw64(p % 64) for p in range(128)])
    ch.append([O["k_cmp"] + p for p in range(128)])
    ch.append([O["v_cmp"] + p for p in range(128)])
    for nm in ("k_sel", "k_win"):
        for g in range(2):
            ch.append([O[nm] + g * 64 + (p % 64) for p in range(128)])
        for g in range(2):
            ch.append([O[nm] + g * 64 + _sw64(p % 64) for p in range(128)])
    return ch


N_CH_A = 20
N_CH_B = 18


def _const_tables():
    theta = 500000.0
    cst = np.zeros((128, 8), np.float32)
    inv64 = (np.float32(theta) ** (-(np.arange(8, dtype=np.float32) * np.float32(2.0)) / np.float32(16))).astype(np.float32)
    inv32 = (np.float32(theta) ** (-(np.arange(4, dtype=np.float32) * np.float32(2.0)) / np.float32(8))).astype(np.float32)
    for p in range(128):
        d = p % 64
        if d < 16:
            cst[p, 0] = inv64[d % 8]
            cst[p, 1] = -1.0 if d < 8 else 1.0
        d2 = p % 32
        if p < 96 and d2 < 8:
            cst[p, 2] = inv32[d2 % 4]
            cst[p, 3] = -1.0 if d2 < 4 else 1.0
    selAB = np.zeros((128, 2, 16, 32), np.float32)
    for i in range(16):
        for tl in range(128):
            t = i * 128 + tl
            cur = t // 64
            for j in range(32):
                forced = (j == 0) or (0 <= cur - j < 2)
                causal = j * 64 <= t
                if forced:
                    selAB[tl, 0, i, j] = 0.0
                    selAB[tl, 1, i, j] = 1e9
                elif causal:
                    selAB[tl, 0, i, j] = 1.0
                else:
                    selAB[tl, 1, i, j] = -1e30
    expall = np.zeros((32, 16, 128), np.float32)
    for jj in range(16):
        for s in range(128):
            expall[2 * jj + s // 64, jj, s] = 1.0
    ov = np.zeros((128, 32), np.float32)
    for c in range(127):
        cs = 16 * c
        for j in range(32):
            ss = 64 * j
            if cs <= ss + 63 and cs + 31 >= ss:
                ov[c, j] = 1.0
    return cst, selAB, expall, ov


def _prep_inputs(inputs):
    f32 = lambda a: np.ascontiguousarray(np.asarray(a, dtype=np.float32))
    x = f32(inputs["x"])
    c = f32(inputs["c"])
    pos = np.ascontiguousarray(np.asarray(inputs["positions"], dtype=np.int32))
    w_in = f32(inputs["w_in"][0])
    w3 = w_in.reshape(8, 128, w_in.shape[1])

    def gather(chunks):
        return np.ascontiguousarray(np.stack([w3[:, :, cols].transpose(1, 0, 2) for cols in chunks]))

    O = _OFF
    tokA = list(range(O["v_a"], O["v_a"] + 128)) + list(range(O["w_idx"], O["w_idx"] + 8))
    tokB = list(range(O["v_sel"], O["v_sel"] + 128)) + list(range(O["v_win"], O["v_win"] + 128)) + list(range(O["g_nsa"], O["g_nsa"] + 24))
    gch = [[O["gate_a"] + cc * 128 + p for p in range(128)] for cc in range(8)] + \
          [[O["gate_b"] + cc * 128 + p for p in range(128)] for cc in range(8)]
    cst, selAB, expall, ov = _const_tables()
    cpos = np.stack([f32(inputs["cmp_pos_k"][0]).T, f32(inputs["cmp_pos_v"][0]).T], axis=1)
    cposT = np.ascontiguousarray(np.concatenate([cpos, cpos], axis=0))
    shared = {
        "w_ada": f32(inputs["w_ada"][0]),
        "b_ada": f32(inputs["b_ada"][0]).reshape(1, -1),
        "ln": f32(np.stack([inputs["ln1_g"][0], inputs["ln1_b"][0], inputs["ln2_g"][0], inputs["ln2_b"][0]])),
        "w_router": f32(inputs["w_router"][0]),
        "router_bias": f32(inputs["router_bias"][0]).reshape(1, -1),
        "w_exp_gate": f32(inputs["w_exp_gate"][0]),
        "w_exp_up": f32(inputs["w_exp_up"][0]),
        "w_exp_down": f32(inputs["w_exp_down"][0]),
        "w_sh_gate": f32(inputs["w_sh_gate"][0]),
        "w_sh_up": f32(inputs["w_sh_up"][0]),
        "w_sh_down": f32(inputs["w_sh_down"][0]),
        "wmA": gather(_chunks_A()),
        "wmB": gather(_chunks_B()),
        "wmG": gather(gch),
        "wtA": np.ascontiguousarray(w3[:, :, tokA].transpose(1, 0, 2)),
        "wtB": np.ascontiguousarray(w3[:, :, tokB].transpose(1, 0, 2)),
        "w_br_a": f32(inputs["w_br_a"][0]),
        "w_br_b": f32(inputs["w_br_b"][0]),
        "w_out": f32(inputs["w_out"][0]),
        "cmp_k_w1": f32(inputs["cmp_k_w1"][0]),
        "cmp_v_w1": f32(inputs["cmp_v_w1"][0]),
        "cmp_k_w2": f32(inputs["cmp_k_w2"][0]),
        "cmp_v_w2": f32(inputs["cmp_v_w2"][0]),
        "cmp_posT": cposT,
        "cst": cst,
        "selAB": selAB,
        "expall": expall,
        "ovl": ov,
    }
    in_maps = []
    for i in range(N_CORES):
        m = dict(shared)
        m["x"] = np.ascontiguousarray(x[BPC * i:BPC * (i + 1)].reshape(TOK, D))
        cc = c[BPC * i:BPC * (i + 1)]
        m["cT"] = np.ascontiguousarray(cc.reshape(BPC, 8, 128).transpose(2, 1, 0))
        m["positions"] = np.ascontiguousarray(pos[BPC * i:BPC * (i + 1)])
        in_maps.append(m)
    return in_maps


def kernel(**inputs):
    in_maps = _prep_inputs(inputs)
    prog = Prog()
    nc = prog.build()
    res = run_bass_kernel_spmd(nc, in_maps, core_ids=list(range(N_CORES)))
    outs = [np.asarray(r["out"], dtype=np.float32).reshape(BPC, SEQ, D) for r in res.results]
    return np.concatenate(outs, axis=0)
```

```python
import numpy as np
import concourse.bass as bass
import concourse.mybir as mybir
from concourse.bass_utils import run_bass_kernel_spmd

F32 = mybir.dt.float32
BF16 = mybir.dt.bfloat16
I32 = mybir.dt.int32
ALU = mybir.AluOpType
AF = mybir.ActivationFunctionType
AX = mybir.AxisListType
IOA = bass.IndirectOffsetOnAxis

N_CORES = 8
D = 1024
SEQ = 2048
BPC = 2
TOK = BPC * SEQ
NT = TOK // 128
NE = 256
BLK = 128
NBLK = TOK * 8 // BLK + NE
NSLOT = NBLK * BLK
CAP = BLK
DN_ALPHA = 2.0 ** 0.25
LN_EPS = 1e-5
ROUTED_SCALE = 2.5
EPOCH = 16000


class _Op:
    __slots__ = ("eng", "fn", "deps", "is_dma", "need_sig", "sig", "waits", "idx", "slot_prev", "r", "w", "is_barrier")


class Sched:
    ENGS = ("pe", "dve", "act", "pool", "sp")
    NSLOTS = {"sp": 12, "act": 4, "pool": 16}

    def __init__(self, nc):
        self.nc = nc
        self.cur = []
        self._atom = None
        self.ops = {e: [] for e in self.ENGS}
        self.allops = []
        self.lastw = {}
        self.readers = {}

    def op(self, eng, fn, r=(), w=(), dma=False):
        o = _Op()
        o.eng, o.fn, o.is_dma, o.deps, o.need_sig, o.sig, o.waits = eng, fn, dma, [], dma, None, []
        o.r, o.w, o.is_barrier, o.slot_prev = list(r), list(w), False, None
        if self._atom is not None:
            self._atom.append(o)
        else:
            self.cur.append([o])
        return o

    def barrier(self):
        o = _Op()
        o.eng, o.fn, o.is_dma, o.deps, o.need_sig, o.sig, o.waits = None, None, False, [], False, None, []
        o.r, o.w, o.is_barrier, o.slot_prev = [], [], True, None
        assert self._atom is None
        self.cur.append([o])

    class _Atomic:
        def __init__(self, s):
            self.s = s

        def __enter__(self):
            self.nested = self.s._atom is not None
            if not self.nested:
                self.s._atom = []

        def __exit__(self, *a):
            if not self.nested:
                seg, self.s._atom = self.s._atom, None
                if seg:
                    self.s.cur.append(seg)

    def atomic(self):
        return Sched._Atomic(self)

    def record_parts(self, body):
        old = self.cur
        self._parts = []
        self.cur = []
        body()
        self._parts.append(self.cur)
        parts, self._parts = self._parts, None
        self.cur = old
        return parts

    def cut(self):
        if getattr(self, "_parts", None) is not None:
            self._parts.append(self.cur)
            self.cur = []

    def merge_rr(self, streams):
        streams = [list(s) for s in streams]
        pos = [0] * len(streams)
        alive = True
        while alive:
            alive = False
            for i, s in enumerate(streams):
                if pos[i] < len(s):
                    self.cur.append(s[pos[i]])
                    pos[i] += 1
                    alive = True

    def merge_seq(self, streams):
        for s in streams:
            self.cur.extend(s)

    def _track(self, o):
        o.idx = len(self.allops)
        seen = set()

        def add(d):
            if d is not None and d.idx not in seen:
                seen.add(d.idx)
                o.deps.append(d)

        for k in o.r:
            add(self.lastw.get(k))
        for k in o.w:
            add(self.lastw.get(k))
            rd = self.readers.get(k)
            if rd:
                for d in rd[0].values():
                    add(d)
                for d in rd[1]:
                    add(d)
        for k in o.r:
            rd = self.readers.setdefault(k, ({}, []))
            if o.is_dma:
                rd[1].append(o)
            else:
                rd[0][o.eng] = o
        for k in o.w:
            self.lastw[k] = o
            self.readers[k] = ({}, [])
        self.ops[o.eng].append(o)
        self.allops.append(o)

    def _track_barrier(self):
        deps = []
        for e in self.ENGS:
            got_c = False
            nd = 0
            for o in reversed(self.ops[e]):
                if o.fn is None:
                    continue
                if o.is_dma:
                    if nd < self.NSLOTS.get(e, 0):
                        deps.append(o)
                        nd += 1
                elif not got_c:
                    deps.append(o)
                    got_c = True
                if got_c and nd >= self.NSLOTS.get(e, 0):
                    break
        for e in self.ENGS:
            o = _Op()
            o.eng, o.fn, o.is_dma, o.need_sig, o.sig, o.waits = e, None, False, False, None, []
            o.deps = list(deps)
            o.idx = len(self.allops)
            o.slot_prev = None
            o.r, o.w, o.is_barrier = [], [], False
            self.ops[e].append(o)
            self.allops.append(o)

    @staticmethod
    def _needs_sync(d, o):
        if d.is_dma:
            return True
        if d.eng == o.eng and d.eng == "pe" and not o.is_dma:
            return False
        if d.fn is None:
            return False
        return True

    def finalize_and_emit(self):
        nc = self.nc
        for seg in self.cur:
            for o in seg:
                if o.is_barrier:
                    self._track_barrier()
                else:
                    self._track(o)
        for o in self.allops:
            for d in o.deps:
                if self._needs_sync(d, o):
                    d.need_sig = True
        sems = {}

        def get_sem(name):
            if name not in sems:
                sems[name] = nc.alloc_semaphore(name)
            return sems[name]

        for e in self.ENGS:
            n = 0
            slot_cnt = {}
            nd = 0
            for o in self.ops[e]:
                if o.fn is None:
                    continue
                if o.is_dma:
                    ns = self.NSLOTS[e]
                    s = nd % ns
                    nd += 1
                    c = slot_cnt.get(s, 0)
                    sem = get_sem(f"d_{e}_{s}")
                    o.slot_prev = (sem, 16 * c) if c > 0 else None
                    slot_cnt[s] = c + 1
                    o.sig = (sem, 16 * (c + 1), 16)
                elif o.need_sig:
                    ep, v = divmod(n, EPOCH)
                    n += 1
                    o.sig = (get_sem(f"c_{e}_{ep}"), v + 1, 1)
            self._final_dma = getattr(self, "_final_dma", {})
            self._final_dma[e] = {s: c for s, c in slot_cnt.items()}
        for e in self.ENGS:
            waited = {}
            for o in self.ops[e]:
                need = {}
                for d in o.deps:
                    if d.sig is None:
                        continue
                    if not self._needs_sync(d, o):
                        continue
                    sem, val, _ = d.sig
                    if need.get(sem, (None, 0))[1] < val:
                        need[sem] = (sem, val)
                if o.is_dma and o.slot_prev is not None:
                    sem, val = o.slot_prev
                    if need.get(sem, (None, 0))[1] < val:
                        need[sem] = (sem, val)
                for sem, val in need.values():
                    if waited.get(sem, 0) < val:
                        waited[sem] = val
                        o.waits.append((sem, val))
        self.sems = sems
        engmap = {"pe": "tensor", "dve": "vector", "act": "scalar", "pool": "gpsimd", "sp": "sync"}
        with nc.Block() as block:
            for e in self.ENGS:
                def body(eng, e=e):
                    for o in self.ops[e]:
                        for sem, val in o.waits:
                            eng.wait_ge(sem, val)
                        if o.fn is None:
                            continue
                        ins = o.fn(eng)
                        if o.sig is not None:
                            ins.then_inc(o.sig[0], o.sig[2])
                    if e in self.NSLOTS:
                        for s, c in self._final_dma[e].items():
                            eng.wait_ge(sems[f"d_{e}_{s}"], 16 * c)
                getattr(block, engmap[e])(body)


class Buf:
    def __init__(self, t, name, nreg=1):
        self.t, self.name, self.nreg = t, name, nreg

    def k(self, i=0):
        return (self.name, i)

    def all(self):
        return [(self.name, i) for i in range(self.nreg)]

    def __getitem__(self, idx):
        return self.t[idx]


class Prog:
    def __init__(self, cfg=None):
        self.cfg = cfg or {}
        self.nc = bass.Bass("TRN2", target_bir_lowering=False)
        self.S = Sched(self.nc)
        self._uid = 0
        self._scopes = [[]]

    def sb(self, name, shape, dtype, nreg=1):
        self._uid += 1
        g = self.nc.sbuf_tensor(f"s{self._uid}_{name}", list(shape), dtype)
        t = g.__enter__()
        self._scopes[-1].append(g)
        return Buf(t, name, nreg)

    def push(self):
        self._scopes.append([])

    def pop(self):
        self.S.barrier()
        for g in reversed(self._scopes.pop()):
            g.__exit__(None, None, None)

    def dram(self, name, shape, dtype, kind):
        return self.nc.dram_tensor(name, list(shape), dtype, kind=kind).ap()

    def op(self, eng, fn, r=(), w=(), dma=False):
        return self.S.op(eng, fn, r, w, dma)

    def PE(self, out, lhsT, rhs, start, stop, r, w, **kw):
        return self.S.op("pe", lambda e: e.matmul(out, lhsT, rhs, start=start, stop=stop, **kw), r, w)

    def TR(self, out, in_, ident, r, w):
        return self.S.op("pe", lambda e: e.transpose(out, in_, ident), r, w)

    def ACT(self, out, in_, func, r, w, **kw):
        return self.S.op("act", lambda e: e.activation(out, in_, func, **kw), r, w)

    def TT(self, eng, out, a, b, alu, r, w):
        return self.S.op(eng, lambda e: e.tensor_tensor(out, a, b, alu), r, w)

    def TS(self, eng, out, a, s1, s2, op0, op1, r, w, accum=None):
        if accum is None:
            return self.S.op(eng, lambda e: e.tensor_scalar(out, a, s1, s2, op0, op1), r, w)
        return self.S.op(eng, lambda e: e.tensor_scalar(out, a, s1, s2, op0, op1, accum_out=accum), r, w)

    def STT(self, out, in0, scalar, in1, op0, op1, r, w, accum=None):
        if accum is None:
            return self.S.op("dve", lambda e: e.scalar_tensor_tensor(out, in0, scalar, in1, op0, op1), r, w)
        return self.S.op("dve", lambda e: e.scalar_tensor_tensor(out, in0, scalar, in1, op0, op1, accum_out=accum), r, w)

    def CP(self, eng, out, in_, r, w):
        if eng == "act":
            return self.S.op("act", lambda e: e.activation(out, in_, AF.Copy), r, w)
        return self.S.op(eng, lambda e: e.tensor_copy(out, in_), r, w)

    def DMA(self, q, out, in_, r, w):
        return self.S.op(q, lambda e: e.dma_start(out=out, in_=in_), r, w, dma=True)

    def build(self):
        nc, S = self.nc, self.S
        cfg = self.cfg
        op = self.op
        PE, TR, ACT, TT, TS, STT, CP, DMA = self.PE, self.TR, self.ACT, self.TT, self.TS, self.STT, self.CP, self.DMA
        do_mixer = cfg.get("mixer", True)
        x_d = self.dram("x", [TOK, D], F32, "ExternalInput")
        cT_d = self.dram("cT", [128, 8, BPC], F32, "ExternalInput")
        pos_d = self.dram("positions", [BPC, SEQ], I32, "ExternalInput")
        w_ada_d = self.dram("w_ada", [D, 6 * D], F32, "ExternalInput")
        b_ada_d = self.dram("b_ada", [1, 6 * D], F32, "ExternalInput")
        ln_d = self.dram("ln", [4, D], F32, "ExternalInput")
        w_router_d = self.dram("w_router", [D, NE], F32, "ExternalInput")
        rbias_d = self.dram("router_bias", [1, NE], F32, "ExternalInput")
        wg_d = self.dram("w_exp_gate", [NE, D, 256], F32, "ExternalInput")
        wu_d = self.dram("w_exp_up", [NE, D, 256], F32, "ExternalInput")
        wd_d = self.dram("w_exp_down", [NE, 256, D], F32, "ExternalInput")
        wsg_d = self.dram("w_sh_gate", [D, 256], F32, "ExternalInput")
        wsu_d = self.dram("w_sh_up", [D, 256], F32, "ExternalInput")
        wsd_d = self.dram("w_sh_down", [256, D], F32, "ExternalInput")
        wmA_d = self.dram("wmA", [N_CH_A, 128, 8, 128], F32, "ExternalInput")
        wmB_d = self.dram("wmB", [N_CH_B, 128, 8, 128], F32, "ExternalInput")
        wmG_d = self.dram("wmG", [16, 128, 8, 128], F32, "ExternalInput")
        wtA_d = self.dram("wtA", [128, 8, 136], F32, "ExternalInput")
        wtB_d = self.dram("wtB", [128, 8, 280], F32, "ExternalInput")
        wbrA_d = self.dram("w_br_a", [512, D], F32, "ExternalInput")
        wbrB_d = self.dram("w_br_b", [512, D], F32, "ExternalInput")
        wout_d = self.dram("w_out", [D, D], F32, "ExternalInput")
        cw1k_d = self.dram("cmp_k_w1", [2048, 256], F32, "ExternalInput")
        cw1v_d = self.dram("cmp_v_w1", [2048, 256], F32, "ExternalInput")
        cw2k_d = self.dram("cmp_k_w2", [256, 64], F32, "ExternalInput")
        cw2v_d = self.dram("cmp_v_w2", [256, 64], F32, "ExternalInput")
        cposT_d = self.dram("cmp_posT", [128, 2, 32], F32, "ExternalInput")
        cst_d = self.dram("cst", [128, 8], F32, "ExternalInput")
        selAB_d = self.dram("selAB", [128, 2, 16, 32], F32, "ExternalInput")
        expall_d = self.dram("expall", [32, 16, 128], F32, "ExternalInput")
        ov_d = self.dram("ovl", [128, 32], F32, "ExternalInput")
        out_d = self.dram("out", [TOK, D], F32, "ExternalOutput")
        x1_d = self.dram("x1_scr", [TOK, D], F32, "Internal")
        u2_d = self.dram("u2_scr", [TOK, D], BF16, "Internal")
        xs_d = self.dram("xs_scr", [NSLOT, D], BF16, "Internal")
        ys_d = self.dram("ys_scr", [NSLOT, D], BF16, "Internal")
        mod_d = self.dram("mod_scr", [BPC * 6, D], F32, "Internal")
        uT_d = self.dram("uT_scr", [BPC, 128, 8 * SEQ], BF16, "Internal")
        wbf_ds = [self.dram(f"wbf_scr{q_}", [NE * 128, 2048], BF16, "Internal") for q_ in range(3)]
        _bg = []
        for e_ in range(NE):
            for q_, (src_d, pat, kw_) in enumerate(((wg_d, "(p k) n -> p (k n)", dict(k=8)), (wu_d, "(p k) n -> p (k n)", dict(k=8)),
                                                     (wd_d, "(p h) n -> p (h n)", dict(h=2)))):
                _bg.append((e_, q_, src_d[e_].rearrange(pat, **kw_), wbf_ds[q_][e_ * 128:(e_ + 1) * 128, :]))
        _bgpos = [0]

        def bg_issue(n):
            for _ in range(n):
                if _bgpos[0] >= len(_bg):
                    return
                e_, q_, src_, dst_ = _bg[_bgpos[0]]
                _bgpos[0] += 1
                self.S.op("pool", lambda e, src_=src_, dst_=dst_: e.dma_start(out=dst_, in_=src_), [], [("wbf", e_, q_)], dma=True)
        wbf_keys = [("wbf", e_, q_) for e_ in range(NE) for q_ in range(3)]
        dbg = {}
        for name, shape in cfg.get("dbg", {}).items():
            dbg[name] = self.dram("dbg_" + name, shape, F32, "ExternalOutput")
        self.dbg = dbg

        _regs = {}

        def bc_reg(e):
            if "bc" not in _regs:
                _regs["bc"] = e.to_reg(NSLOT - 1)
            return _regs["bc"]

        banks = [Buf(nc.alloc_psum_tensor(f"pb{i}", [128, 512], F32), f"pb{i}") for i in range(8)]

        def bfv(bk):
            return bk.t[:, :].bitcast(BF16)

        ones_f = self.sb("ones_f", [128, 128], F32)
        ones_b = self.sb("ones_b", [128, 128], BF16)
        ident_b = self.sb("ident_b", [128, 128], BF16)
        tri_b = self.sb("tri_b", [128, 128], BF16)
        trib = self.sb("trib", [128, 128], BF16)
        edgeb = self.sb("edgeb", [128, 128], BF16)
        negb = self.sb("negb", [128, 128], F32)
        iotaC = self.sb("iotaC", [128, NE], F32)
        iotaE1 = self.sb("iotaE1", [128, NE], F32)
        jcol = self.sb("jcol", [128, NBLK // 128], F32)
        pcol = self.sb("pcol", [128, 1], F32)
        Utri = self.sb("Utri", [128, 2, NE], BF16)
        cst = self.sb("cst", [128, 8], F32)
        eps_t = self.sb("eps_t", [128, 1], F32)
        op("pool", lambda e: e.memset(ones_f[:, :], 1.0), w=ones_f.all())
        op("pool", lambda e: e.memset(ones_b[:, :], 1.0), w=ones_b.all())
        op("pool", lambda e: e.memset(negb[:, :], -30000.0), w=negb.all())
        op("pool", lambda e: e.memset(eps_t[:, :], LN_EPS), w=eps_t.all())
        op("pool", lambda e: e.affine_select(ident_b[:, :], ones_f[:, :], [[-1, 128]], ALU.is_equal, 0.0,
                                            base=0, channel_multiplier=1), r=ones_f.all(), w=ident_b.all())
        op("pool", lambda e: e.affine_select(tri_b[:, :], ones_f[:, :], [[1, 128]], ALU.is_gt, 0.0,
                                            base=0, channel_multiplier=-1), r=ones_f.all(), w=tri_b.all())
        op("pool", lambda e: e.affine_select(trib[:, :], negb[:, :], [[-1, 128]], ALU.is_gt, 0.0,
                                            base=0, channel_multiplier=1), r=negb.all(), w=trib.all())
        op("pool", lambda e: e.affine_select(edgeb[:, :], negb[:, :], [[1, 128]], ALU.is_ge, 0.0,
                                            base=0, channel_multiplier=-1), r=negb.all(), w=edgeb.all())
        op("pool", lambda e: e.iota(iotaC[:, :], [[4096, NE]], base=0, channel_multiplier=0,
                                    allow_small_or_imprecise_dtypes=True), w=iotaC.all())
        op("pool", lambda e: e.iota(jcol[:, :], [[128, NBLK // 128]], base=0, channel_multiplier=1,
                                    allow_small_or_imprecise_dtypes=True), w=jcol.all())
        for a_ in range(2):
            op("pool", lambda e, a_=a_: e.iota(iotaE1[:, :], [[1, NE]], base=-128 * a_, channel_multiplier=-1,
                                              allow_small_or_imprecise_dtypes=True), r=Utri.all(), w=iotaE1.all())
            TS("dve", Utri[:, a_, :], iotaE1[:, :], 0.0, None, ALU.is_gt, ALU.bypass, iotaE1.all(), Utri.all())
        op("pool", lambda e: e.iota(iotaE1[:, :], [[1, NE]], base=0, channel_multiplier=0,
                                    allow_small_or_imprecise_dtypes=True), r=Utri.all(), w=iotaE1.all())
        op("pool", lambda e: e.iota(pcol[:, :], [[0, 1]], base=0, channel_multiplier=1,
                                    allow_small_or_imprecise_dtypes=True), w=pcol.all())
        DMA("sp", cst[:, :], cst_d[:, :], [], cst.all())

        ln1g = self.sb("ln1g", [128, D], F32)
        ln1b = self.sb("ln1b", [128, D], F32)
        rb_bc = self.sb("rb_bc", [128, NE], F32)
        wr_b = self.sb("wr_b", [128, 8, NE], BF16)
        carry = self.sb("carry", [128, NE], F32)
        dest_f = self.sb("dest_f", [128, NT, 8], F32, nreg=NT)
        dest_i = self.sb("dest_i", [128, NT, 8], I32, nreg=NT)
        wk = self.sb("wk", [128, NT, 8], F32, nreg=NT)
        op("pool", lambda e: e.memset(carry[:, :], 0.0), w=carry.all())
        DMA("sp", ln1g[:, :], ln_d[0:1, :].to_broadcast([128, D]), [], ln1g.all())
        DMA("sp", ln1b[:, :], ln_d[1:2, :].to_broadcast([128, D]), [], ln1b.all())
        DMA("sp", rb_bc[:, :], rbias_d[0:1, :].to_broadcast([128, NE]), [], rb_bc.all())

        self.push()
        condT = self.sb("condT", [128, 8, BPC], F32)
        condS = self.sb("condS", [128, 8, BPC], F32)
        DMA("sp", condT[:, :, :], cT_d[:, :, :], [], condT.all())
        ACT(condS[:, :, :], condT[:, :, :], AF.Silu, condT.all(), condS.all())
        wst = [self.sb(f"wada_st{i}", [128, 8, 512], F32) for i in range(2)]
        mrow = [self.sb(f"mrow{i}", [1, D], F32) for i in range(4)]
        brow = [self.sb(f"brow{i}", [1, D], F32) for i in range(2)]
        w_ada_v = w_ada_d.rearrange("(k p) n -> p k n", p=128)
        nmm = 0
        for j in range(6):
            br = brow[j % 2]
            DMA("sp", br[:, :], b_ada_d[:, j * D:(j + 1) * D], [], br.all())
            rows_j = [mrow[(j * BPC + b) % 4] for b in range(BPC)]
            for half in range(2):
                cc = j * 2 + half
                st = wst[cc % 2]
                DMA("sp", st[:, :, :], w_ada_v[:, :, cc * 512:(cc + 1) * 512], [], st.all())
                for b in range(BPC):
                    bk = banks[nmm % 2]
                    nmm += 1
                    for k in range(8):
                        PE(bk[0:1, :], condS[:, k, b:b + 1], st[:, k, :], k == 0, k == 7, condS.all() + st.all(), bk.all())
                    add = 1.0 if j in (1, 4) else 0.0
                    STT(rows_j[b][0:1, half * 512:(half + 1) * 512], bk[0:1, :], add, br[0:1, half * 512:(half + 1) * 512],
                        ALU.add, ALU.add, bk.all() + br.all(), rows_j[b].all())
            for b in range(BPC):
                DMA("sp", mod_d[b * 6 + j:b * 6 + j + 1, :], rows_j[b][0:1, :], rows_j[b].all(), [("mod_d", b, j)])
        wr_st = self.sb("wr_st", [128, 8, NE], F32)
        DMA("sp", wr_st[:, :, :], w_router_d.rearrange("(k p) n -> p k n", p=128), [], wr_st.all())
        CP("pool", wr_b[:, :, :], wr_st[:, :, :], wr_st.all(), wr_b.all())
        self.pop()

        def load_mod(dst, b, j):
            DMA("sp", dst[:, :], mod_d[b * 6 + j:b * 6 + j + 1, :].to_broadcast([128, D]), [("mod_d", b, j)], dst.all())

        def layernorm(z, dst, g_bc, b_bc, tagbufs):
            stats, mv, rstd = tagbufs
            for h in range(2):
                op("dve", lambda e, h=h: e.bn_stats(stats[:, h, :], z[:, h * 512:(h + 1) * 512]), r=z.all(), w=stats.all())
            op("dve", lambda e: e.bn_aggr(mv[:, :], stats[:, :, :]), r=stats.all(), w=mv.all())
            ACT(rstd[:, :], mv[:, 1:2], AF.Sqrt, mv.all() + eps_t.all(), rstd.all(), bias=eps_t[:, 0:1], scale=1.0)
            op("dve", lambda e: e.reciprocal(rstd[:, :], rstd[:, :]), r=rstd.all(), w=rstd.all())
            TS("dve", z[:, :], z[:, :], mv[:, 0:1], rstd[:, 0:1], ALU.subtract, ALU.mult, z.all() + mv.all() + rstd.all(), z.all())
            TT("pool", z[:, :], z[:, :], g_bc[:, :], ALU.mult, z.all() + g_bc.all(), z.all())
            TT("pool", dst[:, :], z[:, :], b_bc[:, :], ALU.add, z.all() + b_bc.all(), dst.all())

        n_tiles = cfg.get("n_tiles", NT)
        TPS = SEQ // 128
        PI = float(np.pi)
        C1 = 6.28125
        C2 = float(2.0 * np.pi - 6.28125)
        NBIS = 18

        def build_uT(b, uT, first=False):
            if not first:
                DMA("sp", uT[:, :, :].rearrange("p k t -> p (k t)"), uT_d[b], [("uT_d", b)], uT.all())
                return
            self.push()
            s1 = self.sb("m0_s1", [128, D], F32)
            h1 = self.sb("m0_h1", [128, D], F32)
            load_mod(h1, b, 0)
            load_mod(s1, b, 1)
            xs = [self.sb(f"m0_x{i}", [128, D], F32) for i in range(2)]
            us = [self.sb(f"m0_u{i}", [128, D], BF16) for i in range(2)]
            for tt in range(TPS):
                X, U = xs[tt % 2], us[tt % 2]
                r0 = b * SEQ + tt * 128
                DMA("sp", X[:, :], x_d[r0:r0 + 128, :], [], X.all())
                TT("dve", X[:, :], X[:, :], s1[:, :], ALU.mult, X.all() + s1.all(), X.all())
                TT("pool", U[:, :], X[:, :], h1[:, :], ALU.add, X.all() + h1.all(), U.all())
                bk = banks[tt % 2]
                pT = bfv(bk)
                for k in range(8):
                    TR(pT[:, k * 128:(k + 1) * 128], U[:, k * 128:(k + 1) * 128], ident_b[:, :], U.all() + ident_b.all(), bk.all())
                CP("act", uT[:, :, tt * 128:(tt + 1) * 128], pT.rearrange("p (k t) -> p k t", k=8), bk.all(), [uT.k(tt // 4)])
            self.pop()
            DMA("sp", uT_d[b], uT[:, :, :].rearrange("p k t -> p (k t)"), uT.all(), [("uT_d", b)])

        def rope_tables(b, CS, col_inv, col_sgn, nrow):
            self.push()
            posi = self.sb("rp_posi", [1, SEQ], I32)
            posf = self.sb("rp_posf", [1, SEQ], F32)
            ang = self.sb("rp_ang", [128, SEQ], F32)
            a2 = self.sb("rp_a2", [128, SEQ], F32)
            qf = self.sb("rp_qf", [128, SEQ], F32)
            qi = self.sb("rp_qi", [128, SEQ], I32)
            DMA("sp", posi[:, :], pos_d[b:b + 1, :], [], posi.all())
            CP("dve", posf[:, :], posi[:, :], posi.all(), posf.all())
            for c4 in range(4):
                bk = banks[2 + c4 % 2]
                PE(bk[:, :], ones_f[0:1, :], posf[0:1, c4 * 512:(c4 + 1) * 512], True, True, ones_f.all() + posf.all(), bk.all())
                TS("dve", ang[0:nrow, c4 * 512:(c4 + 1) * 512], bk[0:nrow, :], cst[0:nrow, col_inv:col_inv + 1], None, ALU.mult, ALU.bypass,
                   bk.all() + cst.all(), ang.all())
            for which in range(2):
                src = ang
                if which == 0:
                    TS("dve", a2[0:nrow, :], ang[0:nrow, :], PI / 2.0, None, ALU.add, ALU.bypass, ang.all(), a2.all())
                    src = a2
                TS("dve", qf[0:nrow, :], src[0:nrow, :], 1.0 / (2.0 * PI), None, ALU.mult, ALU.bypass, src.all(), qf.all())
                CP("dve", qi[0:nrow, :], qf[0:nrow, :], qf.all(), qi.all())
                CP("dve", qf[0:nrow, :], qi[0:nrow, :], qi.all(), qf.all())
                STT(a2[0:nrow, :], qf[0:nrow, :], -C1, src[0:nrow, :], ALU.mult, ALU.add, qf.all() + src.all(), a2.all())
                STT(a2[0:nrow, :], qf[0:nrow, :], -C2, a2[0:nrow, :], ALU.mult, ALU.add, qf.all() + a2.all(), a2.all())
                TS("dve", qf[0:nrow, :], a2[0:nrow, :], PI, -2.0 * PI, ALU.is_gt, ALU.mult, a2.all(), qf.all())
                TT("dve", a2[0:nrow, :], a2[0:nrow, :], qf[0:nrow, :], ALU.add, a2.all() + qf.all(), a2.all())
                TS("dve", a2[0:nrow, :], a2[0:nrow, :], 3.14159, -3.14159, ALU.min, ALU.max, a2.all(), a2.all())
                if which == 0:
                    ACT(CS[0:nrow, 0, :], a2[0:nrow, :], AF.Sin, a2.all(), [CS.k(0)])
                else:
                    ACT(qf[0:nrow, :], a2[0:nrow, :], AF.Sin, a2.all(), qf.all())
                    TS("dve", CS[0:nrow, 1, :], qf[0:nrow, :], cst[0:nrow, col_sgn:col_sgn + 1], None, ALU.mult, ALU.bypass,
                       qf.all() + cst.all(), [CS.k(1)])
            self.pop()

        def project(wm_d, jobs, uT, wbufs):
            st, wb, t1, t2 = wbufs
            cnt = [0]

            def load(ch):
                i = cnt[0] % 3
                cnt[0] += 1
                DMA("sp", st[i][:, :, :], wm_d[ch], [], st[i].all())
                CP("pool", wb[i][:, :, :], st[i][:, :, :], st[i].all(), wb[i].all())
                return wb[i]

            nb = 0
            for (ch, chsw, nrow, dst_fn, CS) in jobs:
                bg_issue(4)
                wA = load(ch)
                wB = load(chsw) if chsw is not None else None
                for tc in range(4):
                    ts_ = slice(tc * 512, (tc + 1) * 512)
                    bA = banks[2 + nb % 4]
                    nb += 1
                    for k in range(8):
                        PE(bA[:, :], wA[:, k, :], uT[:, k, ts_], k == 0, k == 7, wA.all() + [uT.k(tc)], bA.all())
                    dst, dkeys = dst_fn(tc)
                    if wB is None:
                        CP("act", dst, bA[0:nrow, :], bA.all(), dkeys)
                    else:
                        bB = banks[2 + nb % 4]
                        nb += 1
                        for k in range(8):
                            PE(bB[:, :], wB[:, k, :], uT[:, k, ts_], k == 0, k == 7, wB.all() + [uT.k(tc)], bB.all())
                        T1, T2 = t1[tc % 2], t2[tc % 2]
                        TT("dve", T1[0:nrow, :], bA[0:nrow, :], CS[0:nrow, 0, ts_], ALU.mult, bA.all() + [CS.k(0)], T1.all())
                        TT("dve", T2[0:nrow, :], bB[0:nrow, :], CS[0:nrow, 1, ts_], ALU.mult, bB.all() + [CS.k(1)], T2.all())
                        TT("pool", dst, T1[0:nrow, :], T2[0:nrow, :], ALU.add, T1.all() + T2.all(), dkeys)

        def proj_bufs():
            st = [self.sb(f"pj_st{i}", [128, 8, 128], F32) for i in range(3)]
            wb = [self.sb(f"pj_wb{i}", [128, 8, 128], BF16) for i in range(3)]
            t1 = [self.sb(f"pj_t1{i}", [128, 512], F32) for i in range(2)]
            t2 = [self.sb(f"pj_t2{i}", [128, 512], F32) for i in range(2)]
            return st, wb, t1, t2

        def attn_out(accs, O, coef=None, first=True, tmpb=None):
            Ov = O[:, :].rearrange("p (g e q d) -> p g q e d", g=2, e=2, q=2)
            for g in range(2):
                a = accs[g]
                W = a.W
                av = a.buf[:, 0:4 * W].rearrange("p (q e w) -> p q e w", q=2, e=2)[:, :, :, 0:64]
                cf = coef[:, g * 4:(g + 1) * 4].rearrange("p (q e) -> p q e", q=2).unsqueeze(3).to_broadcast([128, 2, 2, 64])
                if first:
                    TT("dve", Ov[:, g], av, cf, ALU.mult, a.buf.all() + coef.all(), O.all())
                else:
                    tv = tmpb[:, :].rearrange("p (q e d) -> p q e d", q=2, e=2)
                    TT("dve", tv, av, cf, ALU.mult, a.buf.all() + coef.all(), tmpb.all())
                    TT("pool", Ov[:, g], Ov[:, g], tv, ALU.add, O.all() + tmpb.all(), O.all())

        class Acc:
            def __init__(self, buf, W):
                self.buf, self.W = buf, W

        def o_to_T(O, Ob, oT, i, bank):
            CP("act", Ob[:, :], O[:, :], O.all(), Ob.all())
            pT = bfv(bank)
            for c in range(4):
                TR(pT[:, c * 128:(c + 1) * 128], Ob[:, c * 128:(c + 1) * 128], ident_b[:, :], Ob.all() + ident_b.all(), bank.all())
            CP("dve", oT[:, :, i * 128:(i + 1) * 128], pT[:, 0:512].rearrange("p (c t) -> p c t", c=4), bank.all(), [oT.k(i // 4)])

        def dsa(b, oaT, CS64):
            self.push()
            QA = self.sb("QA", [128, 4, SEQ], BF16, nreg=4)
            KA = self.sb("KA", [128, 2, SEQ], BF16, nreg=4)
            QI = self.sb("QI", [96, 3, SEQ], BF16, nreg=4)
            KI = self.sb("KI", [96, SEQ], BF16, nreg=4)
            VA = self.sb("VA", [128, TPS, 2, 65], BF16)
            widx = self.sb("widx", [128, TPS, 8], F32)
            wabs = self.sb("wabs", [128, TPS, 8], F32)
            wsgn = self.sb("wsgn", [128, TPS, 8], F32)
            CS32 = self.sb("CS32", [128, 2, SEQ], BF16, nreg=2)
            rope_tables(b, CS32, 2, 3, 96)
            self.push()
            uT = self.sb("uT", [128, 8, SEQ], BF16, nreg=4)
            build_uT(b, uT, first=True)
            wbufs = proj_bufs()
            jobs = []
            for c in range(4):
                jobs.append((c, 4 + c, 128, (lambda tc, c=c: (QA[:, c, tc * 512:(tc + 1) * 512], [QA.k(tc)])), CS64))
            for g in range(2):
                jobs.append((8 + g, 10 + g, 128, (lambda tc, g=g: (KA[:, g, tc * 512:(tc + 1) * 512], [KA.k(tc)])), CS64))
            for c in range(3):
                jobs.append((12 + c, 15 + c, 96, (lambda tc, c=c: (QI[:, c, tc * 512:(tc + 1) * 512], [QI.k(tc)])), CS32))
            jobs.append((18, 19, 96, (lambda tc: (KI[:, tc * 512:(tc + 1) * 512], [KI.k(tc)])), CS32))
            project(wmA_d, jobs, uT, wbufs)
            wt_st = self.sb("wtA_st", [128, 8, 136], F32)
            wt_b = self.sb("wtA_b", [128, 8, 136], BF16)
            DMA("sp", wt_st[:, :, :], wtA_d[:, :, :], [], wt_st.all())
            CP("pool", wt_b[:, :, :], wt_st[:, :, :], wt_st.all(), wt_b.all())
            op("pool", lambda e: e.memset(VA[:, :, :, 64:65], 1.0), w=VA.all())
            for tt in range(TPS):
                bk = banks[6 + tt % 2]
                for k in range(8):
                    PE(bk[:, 0:136], uT[:, k, tt * 128:(tt + 1) * 128], wt_b[:, k, :], k == 0, k == 7, [uT.k(tt // 4)] + wt_b.all(), bk.all())
                CP("act", VA[:, tt, :, 0:64], bk[:, 0:128].rearrange("p (g d) -> p g d", g=2), bk.all(), VA.all())
                CP("dve", widx[:, tt, :], bk[:, 128:136], bk.all(), widx.all())
            c0 = float((32.0 ** -0.5) * (8.0 ** -0.5))
            ACT(wabs[:, :, :], widx[:, :, :], AF.Abs, widx.all(), wabs.all(), scale=c0)
            TS("dve", wsgn[:, :, :], widx[:, :, :], 0.0, 2.0, ALU.is_ge, ALU.mult, widx.all(), wsgn.all())
            TS("dve", wsgn[:, :, :], wsgn[:, :, :], -1.0, None, ALU.add, ALU.bypass, wsgn.all(), wsgn.all())
            self.pop()
            self.push()
            GQ = 4
            accs = [self.sb(f"ix_acc{i}", [128, SEQ], F32, nreg=4) for i in range(GQ)]
            junks = [self.sb(f"ix_junk{i}", [128, SEQ], BF16) for i in range(GQ)]
            bss = [self.sb(f"ix_bs{i}", [128, 8 + NBIS], F32) for i in range(GQ)]
            css = [self.sb(f"ix_cs{i}", [128, 1], F32) for i in range(GQ)]
            Mb = self.sb("ix_Mb", [128, SEQ], BF16)
            BT = self.sb("ix_BT", [128, TPS, 128], BF16, nreg=2)
            Rt = [self.sb(f"ix_R{i}", [128, 512], F32) for i in range(3)]
            pw2 = self.sb("ix_pw2", [128, NBIS], F32)
            thrneg = self.sb("ix_thrneg", [128, 1], F32)
            Et = [self.sb(f"at_E{i}", [128, 512], BF16) for i in range(4)]
            O = self.sb("at_O", [128, 512], F32)
            Ob = self.sb("at_Ob", [128, 512], BF16)
            RD = self.sb("at_RD", [128, 8], F32)
            for k in range(NBIS):
                op("pool", lambda e, k=k: e.memset(pw2[:, k:k + 1], 2.0 ** -(k + 1)), w=pw2.all())
            op("pool", lambda e: e.memset(thrneg[:, :], -1e30), w=thrneg.all())
            cnts = {"R": 0, "E": 0}
            nq = cfg.get("n_qtiles", TPS)

            def indexer(i):
                acc, bs = accs[i % GQ], bss[i % GQ]
                rmax, rmin = bs[:, 1:2], bs[:, 2:3]
                n = 128 * (i + 1)
                it = slice(i * 128, (i + 1) * 128)
                nch = (n + 511) // 512
                for h in range(8):
                    pb = 32 * (h % 3)
                    for kc in range(nch):
                        kw = min(512, n - kc * 512)
                        bk = banks[(h * nch + kc) % 2]
                        R = Rt[cnts["R"] % 3]
                        cnts["R"] += 1
                        with S.atomic():
                            PE(bk[:, 0:kw], QI[pb:pb + 32, h // 3, it], KI[pb:pb + 32, kc * 512:kc * 512 + kw], True, True,
                               [QI.k(i // 4), KI.k(kc)], bk.all())
                            ACT(R[:, 0:kw], bk[:, 0:kw], AF.Relu, bk.all() + wabs.all(), R.all(), scale=wabs[:, i, h:h + 1])
                        if h == 0:
                            TS("dve", acc[:, kc * 512:kc * 512 + kw], R[:, 0:kw], wsgn[:, i, 0:1], None, ALU.mult, ALU.bypass,
                               R.all() + wsgn.all(), [acc.k(kc)])
                        else:
                            STT(acc[:, kc * 512:kc * 512 + kw], R[:, 0:kw], wsgn[:, i, h:h + 1], acc[:, kc * 512:kc * 512 + kw],
                                ALU.mult, ALU.add, R.all() + wsgn.all() + [acc.k(kc)], [acc.k(kc)])
                akeys = [acc.k(kc) for kc in range(nch)]
                if i >= 2:
                    op("dve", lambda e: e.tensor_reduce(rmin, acc[:, 0:n], AX.X, ALU.min), r=akeys, w=bs.all())
                    op("dve", lambda e: e.tensor_reduce(rmax, acc[:, 0:n], AX.X, ALU.max), r=akeys, w=bs.all())
                op("pool", lambda e: e.affine_select(acc[:, it], acc[:, it], [[-1, 128]], ALU.is_ge, -3.0e38,
                                                    base=0, channel_multiplier=1), r=[acc.k(i // 4)], w=[acc.k(i // 4)])

            def bisect(i):
                acc, bs, junk = accs[i % GQ], bss[i % GQ], junks[i % GQ]
                lo, rmax, rmin, w0, mid, cnt, stp = (bs[:, 0:1], bs[:, 1:2], bs[:, 2:3], bs[:, 3:4], bs[:, 4:5], bs[:, 5:6], bs[:, 6:7])
                Wk = bs[:, 8:8 + NBIS]
                n = 128 * (i + 1)
                akeys = [acc.k(kc) for kc in range((n + 511) // 512)]
                TS("dve", lo, rmin, -1.0, None, ALU.add, ALU.bypass, bs.all(), bs.all())
                TT("dve", w0, rmax, lo, ALU.subtract, bs.all(), bs.all())
                TS("dve", Wk, pw2[:, :], w0, None, ALU.mult, ALU.bypass, bs.all() + pw2.all(), bs.all())
                cs_ = css[i % GQ]
                for k in range(NBIS):
                    if i % 2 == 0:
                        TS("dve", mid, lo, Wk[:, k:k + 1], -1.0, ALU.add, ALU.mult, bs.all(), bs.all())
                        ACT(junk[:, 0:n], acc[:, 0:n], AF.Sign, akeys + bs.all(), junk.all() + cs_.all(), bias=mid, scale=1.0, accum_out=cs_[:, 0:1])
                        TS("dve", stp, cs_[:, 0:1], float(511 - n), Wk[:, k:k + 1], ALU.is_ge, ALU.mult, bs.all() + cs_.all(), bs.all())
                    else:
                        TT("dve", mid, lo, Wk[:, k:k + 1], ALU.add, bs.all(), bs.all())
                        TS("dve", junk[:, 0:n], acc[:, 0:n], mid, None, ALU.is_ge, ALU.add, akeys + bs.all(), junk.all() + bs.all(), accum=cnt)
                        TS("dve", stp, cnt, 255.5, Wk[:, k:k + 1], ALU.is_ge, ALU.mult, bs.all(), bs.all())
                    TT("dve", lo, lo, stp, ALU.add, bs.all(), bs.all())

            def attend(i):
                bg_issue(7)
                acc, bs = accs[i % GQ], bss[i % GQ]
                n = 128 * (i + 1)
                it = slice(i * 128, (i + 1) * 128)
                akeys = [acc.k(kc) for kc in range((n + 511) // 512)]
                if i >= 2:
                    thr, thr_keys = bs[:, 0:1], bs.all()
                else:
                    thr, thr_keys = thrneg[:, 0:1], thrneg.all()
                TS("dve", Mb[:, 0:n], acc[:, 0:n], thr, -30000.0, ALU.is_lt, ALU.mult, akeys + thr_keys, Mb.all())
                for j0 in range(0, i + 1, 8):
                    j1 = min(i + 1, j0 + 8)
                    bk = banks[2]
                    pT = bfv(bk)
                    for j in range(j0, j1):
                        TR(pT[:, (j - j0) * 128:(j - j0 + 1) * 128], Mb[:, j * 128:(j + 1) * 128], ident_b[:, :], Mb.all() + ident_b.all(), bk.all())
                    CP("act", BT[:, j0:j1, :], pT[:, 0:(j1 - j0) * 128].rearrange("p (j t) -> p j t", t=128), bk.all(), [BT.k(j0 // 8)])
                accb = [banks[5], banks[6]]
                def scores(j):
                    jt = slice(j * 128, (j + 1) * 128)
                    sA, sB = (banks[3], banks[4]) if j % 2 == 0 else (banks[7], banks[2])
                    for par, sb_ in ((0, sA), (1, sB)):
                        ps = slice(par * 64, par * 64 + 64)
                        for g in range(2):
                            PE(sb_[:, g * 256:(g + 1) * 256], KA[ps, g, jt], QA[ps, 2 * g:2 * g + 2, it], g == 0, False,
                               [KA.k(j // 4), QA.k(i // 4)], sb_.all(), skip_group_check=True)
                        PE(sb_[:, :], ident_b[:, :], BT[:, j, :].unsqueeze(1).to_broadcast([128, 4, 128]), False, True,
                           ident_b.all() + [BT.k(j // 8)], sb_.all(), skip_group_check=True)

                def exp_pv(j):
                    sA, sB = (banks[3], banks[4]) if j % 2 == 0 else (banks[7], banks[2])
                    EA, EB = Et[cnts["E"] % 4], Et[(cnts["E"] + 1) % 4]
                    cnts["E"] += 2
                    ACT(EA[:, :], sA[:, :], AF.Exp, sA.all(), EA.all(), scale=0.125)
                    ACT(EB[:, :], sB[:, :], AF.Exp, sB.all(), EB.all(), scale=0.125)
                    for g in range(2):
                        for par, E_ in ((0, EA), (1, EB)):
                            for e_ in range(2):
                                r_ = par * 2 + e_
                                blk = (g * 2 + e_) * 128
                                PE(accb[g][:, r_ * 65:(r_ + 1) * 65], E_[:, blk:blk + 128], VA[:, j, g, :],
                                   (j == 0 and r_ == 0), (j == i and r_ == 3), E_.all() + VA.all(), accb[g].all(), skip_group_check=True)

                scores(0)
                for j in range(i + 1):
                    if j + 1 <= i:
                        scores(j + 1)
                    exp_pv(j)
                for g in range(2):
                    op("dve", lambda e, g=g: e.reciprocal(RD[:, g * 4:(g + 1) * 4], accb[g][:, 0:260].rearrange("p (r w) -> p r w", w=65)[:, :, 64]),
                       r=accb[g].all(), w=RD.all())
                attn_out([Acc(accb[0], 65), Acc(accb[1], 65)], O, coef=RD, first=True)
                o_to_T(O, Ob, oaT, i, banks[7])

            for i0 in range(0, nq, GQ):
                grp = list(range(i0, min(i0 + GQ, nq)))
                for i in grp:
                    indexer(i)
                parts = [S.record_parts(lambda i=i: bisect(i)) for i in grp if i >= 2]
                if parts:
                    S.merge_rr([p[0] for p in parts])
                for i in grp:
                    attend(i)
            self.pop()
            self.pop()
        def nsa(b, obT, CS64):
            self.push()
            QBr = self.sb("QBr", [128, 4, SEQ], BF16, nreg=4)
            QB = self.sb("QB", [128, 4, SEQ], BF16, nreg=4)
            KCf = self.sb("KCf", [128, SEQ], BF16, nreg=4)
            VCf = self.sb("VCf", [128, SEQ], BF16, nreg=4)
            KS = self.sb("KS", [128, 2, SEQ], BF16, nreg=4)
            KW = self.sb("KW", [128, 2, SEQ], BF16, nreg=4)
            VS = self.sb("VS", [128, TPS, 2, 65], BF16)
            VW = self.sb("VW", [128, TPS, 2, 65], BF16)
            gns = self.sb("gns", [128, TPS, 24], F32)
            self.push()
            uT = self.sb("uTn", [128, 8, SEQ], BF16, nreg=4)
            build_uT(b, uT)
            wbufs = proj_bufs()
            jobs = []
            for c in range(4):
                jobs.append((c, None, 128, (lambda tc, c=c: (QBr[:, c, tc * 512:(tc + 1) * 512], [QBr.k(tc)])), None))
            for c in range(4):
                jobs.append((c, 4 + c, 128, (lambda tc, c=c: (QB[:, c, tc * 512:(tc + 1) * 512], [QB.k(tc)])), CS64))
            jobs.append((8, None, 128, (lambda tc: (KCf[:, tc * 512:(tc + 1) * 512], [KCf.k(tc)])), None))
            jobs.append((9, None, 128, (lambda tc: (VCf[:, tc * 512:(tc + 1) * 512], [VCf.k(tc)])), None))
            for g in range(2):
                jobs.append((10 + g, 12 + g, 128, (lambda tc, g=g: (KS[:, g, tc * 512:(tc + 1) * 512], [KS.k(tc)])), CS64))
            for g in range(2):
                jobs.append((14 + g, 16 + g, 128, (lambda tc, g=g: (KW[:, g, tc * 512:(tc + 1) * 512], [KW.k(tc)])), CS64))
            project(wmB_d, jobs, uT, wbufs)
            wt_st = self.sb("wtB_st", [128, 8, 280], F32)
            wt_b = self.sb("wtB_b", [128, 8, 280], BF16)
            DMA("sp", wt_st[:, :, :], wtB_d[:, :, :], [], wt_st.all())
            CP("pool", wt_b[:, :, :], wt_st[:, :, :], wt_st.all(), wt_b.all())
            op("pool", lambda e: e.memset(VS[:, :, :, 64:65], 1.0), w=VS.all())
            op("pool", lambda e: e.memset(VW[:, :, :, 64:65], 1.0), w=VW.all())
            for tt in range(TPS):
                bk = banks[6 + tt % 2]
                for k in range(8):
                    PE(bk[:, 0:280], uT[:, k, tt * 128:(tt + 1) * 128], wt_b[:, k, :], k == 0, k == 7, [uT.k(tt // 4)] + wt_b.all(), bk.all())
                CP("act", VS[:, tt, :, 0:64], bk[:, 0:128].rearrange("p (g d) -> p g d", g=2), bk.all(), VS.all())
                CP("act", VW[:, tt, :, 0:64], bk[:, 128:256].rearrange("p (g d) -> p g d", g=2), bk.all(), VW.all())
                ACT(gns[:, tt, :], bk[:, 256:280], AF.Sigmoid, bk.all(), gns.all())
            self.pop()
            self.push()
            KCc = self.sb("KCc", [128, 2, 128], BF16)
            VX = self.sb("VX", [128, 2, 97], BF16)
            ovst = self.sb("ovst", [128, 32], F32)
            DMA("sp", ovst[:, :], ov_d[:, :], [], ovst.all())
            op("pool", lambda e: e.memset(VX[:, :, 64:65], 1.0), w=VX.all())
            for g in range(2):
                CP("pool", VX[:, g, 65:97], ovst[:, :], ovst.all(), VX.all())
            self.push()
            W1s = self.sb("W1s", [128, 8, 256], F32)
            W1 = self.sb("W1", [128, 32, 256], BF16, nreg=4)
            w2s = self.sb("w2s", [128, 2, 64], F32)
            w2 = self.sb("w2", [128, 2, 128], BF16)
            posT = self.sb("posT", [128, 2, 32], F32)
            posTb = self.sb("posTb", [128, 2, 32], BF16)
            pbias = self.sb("pbias", [128, 2], F32)
            hx = self.sb("hx", [128, 128], F32)
            hx2 = self.sb("hx2", [128, 128], F32)
            hT = self.sb("hT", [128, 2, 2, 128], BF16)
            DMA("sp", posT[:, :, :], cposT_d[:, :, :], [], posT.all())
            CP("pool", posTb[:, :, :], posT[:, :, :], posT.all(), posTb.all())
            for kind, (w1_d, w2_d, SRC) in enumerate(((cw1k_d, cw2k_d, KCf), (cw1v_d, cw2v_d, VCf))):
                w1v = w1_d.rearrange("(l d) n -> d l n", d=64)
                for l4 in range(4):
                    for hh in range(2):
                        DMA("sp", W1s[hh * 64:(hh + 1) * 64, :, :], w1v[:, l4 * 8:(l4 + 1) * 8, :], [], W1s.all())
                    CP("pool", W1[:, l4 * 8:(l4 + 1) * 8, :], W1s[:, :, :], W1s.all(), [W1.k(l4)])
                DMA("sp", w2s[:, :, :], w2_d.rearrange("(h p) d -> p h d", p=128), [], w2s.all())
                for dd in range(2):
                    CP("pool", w2[:, :, dd * 64:(dd + 1) * 64], w2s[:, :, :], w2s.all(), w2.all())
                for hc in range(2):
                    bk = banks[0]
                    for l in range(32):
                        PE(bk[:, hc:hc + 1], W1[0:64, l, hc * 128:(hc + 1) * 128], posTb[0:64, kind, l:l + 1], (l == 0 and hc == 0), (l == 31 and hc == 1),
                           W1.all() + posTb.all(), bk.all(), skip_group_check=True)
                CP("dve", pbias[:, :], banks[0][:, 0:2], banks[0].all(), pbias.all())
                for g in range(2):
                    ps = slice(g * 64, (g + 1) * 64)
                    for hc in range(2):
                        bk = banks[1 + g]
                        for l in range(32):
                            PE(bk[:, hc * 128:hc * 128 + 127], W1[ps, l, hc * 128:(hc + 1) * 128], SRC[ps, l:l + 16 * 126 + 1:16], (l == 0 and hc == 0), (l == 31 and hc == 1),
                               W1.all() + SRC.all(), bk.all(), skip_group_check=True)
                    for hc in range(2):
                        bk = banks[1 + g]
                        ACT(hx[:, 0:127], bk[:, hc * 128:hc * 128 + 127], AF.Identity, bk.all() + pbias.all(), hx.all(), bias=pbias[:, hc:hc + 1], scale=1.0)
                        TT("dve", hx2[:, 0:127], hx[:, 0:127], hx[:, 0:127], ALU.mult, hx.all(), hx2.all())
                        TS("dve", hx2[:, 0:127], hx2[:, 0:127], 0.044715, 1.0, ALU.mult, ALU.add, hx2.all(), hx2.all())
                        TT("dve", hx2[:, 0:127], hx2[:, 0:127], hx[:, 0:127], ALU.mult, hx.all() + hx2.all(), hx2.all())
                        ACT(hx2[:, 0:127], hx2[:, 0:127], AF.Sigmoid, hx2.all(), hx2.all(), scale=1.5957691216057308)
                        TT("dve", hT[:, g, hc, 0:127], hx2[:, 0:127], hx[:, 0:127], ALU.mult, hx.all() + hx2.all(), hT.all())
                    bk = banks[3 + g]
                    if kind == 0:
                        for hc in range(2):
                            PE(bk[:, 0:127], w2[:, hc, :], hT[:, g, hc, 0:127], hc == 0, hc == 1, w2.all() + hT.all(), bk.all())
                        CP("act", KCc[:, g, 0:127], bk[:, 0:127], bk.all(), KCc.all())
                    else:
                        for hc in range(2):
                            PE(bk[0:127, 0:64], hT[:, g, hc, 0:127], w2[:, hc, 0:64], hc == 0, hc == 1, w2.all() + hT.all(), bk.all())
                        CP("act", VX[0:127, g, 0:64], bk[0:127, 0:64], bk.all(), VX.all())
            self.pop()
            CM = self.sb("CM", [128, SEQ], BF16)
            onesb2 = self.sb("onesb2", [128, SEQ], BF16)
            op("pool", lambda e: e.memset(onesb2[:, :], 1.0), w=onesb2.all())
            op("pool", lambda e: e.affine_select(CM[:, :], onesb2[:, :], [[1, SEQ]], ALU.is_ge, 0.0, base=-31, channel_multiplier=-16),
               r=onesb2.all(), w=CM.all())
            selAB = self.sb("selAB", [128, 2, 16, 32], F32)
            DMA("sp", selAB[:, :, :, :], selAB_d[:, :, :, :], [], selAB.all())
            expst = self.sb("expst", [32, 16, 128], F32)
            expall = self.sb("expall", [32, 16, 128], BF16)
            DMA("sp", expst[:, :, :], expall_d[:, :, :], [], expst.all())
            CP("pool", expall[:, :, :], expst[:, :, :], expst.all(), expall.all())
            Et = [self.sb(f"nt_E{i}", [128, 512], BF16) for i in range(4)]
            O = self.sb("nt_O", [128, 512], F32)
            Ob = self.sb("nt_Ob", [128, 512], BF16)
            otmp = self.sb("nt_otmp", [128, 256], F32)
            RD = self.sb("nt_RD", [128, 8], F32)
            coef = self.sb("nt_coef", [128, 8], F32)
            imp = self.sb("nt_imp", [128, 2, 32], F32)
            imt = self.sb("nt_imt", [128, 4, 32], F32)
            top8 = self.sb("nt_top8", [128, 2, 8], F32)
            imw = self.sb("nt_imw", [128, 32], F32)
            selb = self.sb("nt_selb", [128, 2, 32], BF16)
            selT = self.sb("nt_selT", [32, 2, 128], BF16)
            nE = 0
            cntE = [0]
            nq = cfg.get("n_qtiles", TPS)
            for i in range(nq):
                bg_issue(8)
                it = slice(i * 128, (i + 1) * 128)
                gv = gns[:, i, :].rearrange("p (g e q k) -> p g q e k", g=2, e=2, q=2)

                def mk_coef(kbr, accs, W):
                    for g in range(2):
                        dcol = accs[g][:, 0:4 * W].rearrange("p (r w) -> p r w", w=W)[:, :, 64]
                        TS("dve", RD[:, g * 4:(g + 1) * 4], dcol, 1e-30, None, ALU.max, ALU.bypass, accs[g].all(), RD.all())
                    op("dve", lambda e: e.reciprocal(RD[:, :], RD[:, :]), r=RD.all(), w=RD.all())
                    TT("dve", coef[:, :].rearrange("p (g q e) -> p g q e", g=2, q=2), RD[:, :].rearrange("p (g q e) -> p g q e", g=2, q=2),
                       gv[:, :, :, :, kbr], ALU.mult, RD.all() + gns.all(), coef.all())

                ncv = min(127, 8 * i + 7)
                sA, sB = banks[3], banks[4]
                for par, sb_ in ((0, sA), (1, sB)):
                    ps = slice(par * 64, par * 64 + 64)
                    for g in range(2):
                        PE(sb_[0:ncv, g * 256:(g + 1) * 256], KCc[ps, g, 0:ncv], QBr[ps, 2 * g:2 * g + 2, it], g == 0, g == 1,
                           KCc.all() + [QBr.k(i // 4)], sb_.all(), skip_group_check=True)
                EA, EB = Et[nE % 4], Et[(nE + 1) % 4]
                nE += 2
                for sb_, E_ in ((sA, EA), (sB, EB)):
                    ACT(E_[0:ncv, :], sb_[0:ncv, :], AF.Exp, sb_.all(), E_.all(), scale=0.125)
                    TT("pool", E_[0:ncv, :].rearrange("p (r t) -> p r t", r=4), E_[0:ncv, :].rearrange("p (r t) -> p r t", r=4),
                       CM[0:ncv, it].unsqueeze(1).to_broadcast([ncv, 4, 128]), ALU.mult, E_.all() + CM.all(), E_.all())
                accc = [banks[5], banks[6]]
                for g in range(2):
                    for par, E_ in ((0, EA), (1, EB)):
                        for e_ in range(2):
                            r_ = par * 2 + e_
                            blk = (g * 2 + e_) * 128
                            PE(accc[g][:, r_ * 97:(r_ + 1) * 97], E_[0:ncv, blk:blk + 128], VX[0:ncv, g, :], r_ == 0, r_ == 3,
                               E_.all() + VX.all(), accc[g].all(), skip_group_check=True)
                mk_coef(0, accc, 97)
                attn_out([Acc(accc[0], 97), Acc(accc[1], 97)], O, coef=coef, first=True)
                need_sel = i >= 8
                if need_sel:
                    for g in range(2):
                        TT("dve", imt[:, :, :], accc[g][:, 0:388].rearrange("p (r w) -> p r w", w=97)[:, :, 65:97],
                           RD[:, g * 4:(g + 1) * 4].unsqueeze(2).to_broadcast([128, 4, 32]), ALU.mult, accc[g].all() + RD.all(), imt.all())
                        op("dve", lambda e, g=g: e.tensor_reduce(imp[:, g, :], imt[:, :, :].rearrange("p r j -> p j r"), AX.X, ALU.add),
                           r=imt.all(), w=imp.all())
                        TT("dve", imp[:, g, :], imp[:, g, :], selAB[:, 0, i, :], ALU.mult, imp.all() + selAB.all(), imp.all())
                        TT("dve", imp[:, g, :], imp[:, g, :], selAB[:, 1, i, :], ALU.add, imp.all() + selAB.all(), imp.all())
                        op("dve", lambda e, g=g: e.max(top8[:, g, :], imp[:, g, :]), r=imp.all(), w=top8.all())
                        op("dve", lambda e, g=g: e.match_replace(imw[:, :], top8[:, g, :], imp[:, g, :], -3.0e38), r=imp.all() + top8.all(), w=imw.all())
                        op("dve", lambda e, g=g: e.max(top8[:, g, :], imw[:, :]), r=imw.all(), w=top8.all())
                        TS("dve", selb[:, g, :], imp[:, g, :], top8[:, g, 7:8], -30000.0, ALU.is_lt, ALU.mult, imp.all() + top8.all(), selb.all())
                    bk = banks[2]
                    pT = bfv(bk)
                    for g in range(2):
                        TR(pT[0:32, g * 128:(g + 1) * 128], selb[:, g, :], ident_b[:, :], selb.all() + ident_b.all(), bk.all())
                    CP("act", selT[:, :, :], pT[0:32, 0:256].rearrange("p (g t) -> p g t", g=2), bk.all(), selT.all())

                for kbr, (KK, VV) in ((2, (KW, VW)), (1, (KS, VS))):
                    j_lo = 0 if kbr == 1 else max(0, i - 4)
                    accb = [banks[7], banks[0]] if kbr == 2 else [banks[5], banks[6]]
                    def scores(j, kbr=kbr, KK=KK):
                        jt = slice(j * 128, (j + 1) * 128)
                        sA, sB = (banks[3], banks[4]) if (j % 2 == 0) else (banks[1], banks[2])
                        for par, sb_ in ((0, sA), (1, sB)):
                            ps = slice(par * 64, par * 64 + 64)
                            bias = None
                            if j == i:
                                bias = trib
                            elif kbr == 2 and j == i - 4:
                                bias = edgeb
                            selbias = (kbr == 1 and need_sel and j < i)
                            last_plain = (bias is None and not selbias)
                            for g in range(2):
                                PE(sb_[:, g * 256:(g + 1) * 256], KK[ps, g, jt], QB[ps, 2 * g:2 * g + 2, it], g == 0, (g == 1 and last_plain),
                                   [KK.k(j // 4), QB.k(i // 4)], sb_.all(), skip_group_check=True)
                            if bias is not None:
                                PE(sb_[:, :], ident_b[:, :], bias[:, :].unsqueeze(1).to_broadcast([128, 4, 128]), False, True,
                                   ident_b.all() + bias.all(), sb_.all(), skip_group_check=True)
                            elif selbias:
                                for g in range(2):
                                    PE(sb_[:, g * 256:(g + 1) * 256], expall[:, j, :], selT[:, g, :].unsqueeze(1).to_broadcast([32, 2, 128]), False, g == 1,
                                       expall.all() + selT.all(), sb_.all(), skip_group_check=True)

                    def exp_pv(j, VV=VV, accb=accb, j_lo=j_lo):
                        sA, sB = (banks[3], banks[4]) if (j % 2 == 0) else (banks[1], banks[2])
                        EA, EB = Et[cntE[0] % 4], Et[(cntE[0] + 1) % 4]
                        cntE[0] += 2
                        ACT(EA[:, :], sA[:, :], AF.Exp, sA.all(), EA.all(), scale=0.125)
                        ACT(EB[:, :], sB[:, :], AF.Exp, sB.all(), EB.all(), scale=0.125)
                        for g in range(2):
                            for par, E_ in ((0, EA), (1, EB)):
                                for e_ in range(2):
                                    r_ = par * 2 + e_
                                    blk = (g * 2 + e_) * 128
                                    PE(accb[g][:, r_ * 65:(r_ + 1) * 65], E_[:, blk:blk + 128], VV[:, j, g, :],
                                       (j == j_lo and r_ == 0), (j == i and r_ == 3), E_.all() + VV.all(), accb[g].all(), skip_group_check=True)

                    scores(j_lo)
                    for j in range(j_lo, i + 1):
                        if j + 1 <= i:
                            scores(j + 1)
                        exp_pv(j)
                    mk_coef(kbr, accb, 65)
                    attn_out([Acc(accb[0], 65), Acc(accb[1], 65)], O, coef=coef, first=False, tmpb=otmp)
                o_to_T(O, Ob, obT, i, banks[2])
            self.pop()
            self.pop()

        NB = 4

        def m3(b, oaT, obT):
            self.push()
            mergedT = self.sb("mergedT", [128, 8, SEQ], BF16, nreg=4)
            wout = self.sb("wout", [128, 8, D], BF16, nreg=2)
            g1t = self.sb("g1t", [128, D], F32)
            s2t = self.sb("s2t", [128, D], F32)
            h2t = self.sb("h2t", [128, D], F32)
            load_mod(g1t, b, 2)
            load_mod(h2t, b, 3)
            load_mod(s2t, b, 4)
            if do_mixer:
                self.push()
                uT = self.sb("uTm", [128, 8, SEQ], BF16, nreg=4)
                wbra = self.sb("wbra", [128, 4, D], BF16)
                wbrb = self.sb("wbrb", [128, 4, D], BF16)
                build_uT(b, uT)
                self.push()
                wst_ = self.sb("m3_wst", [128, 4, D], F32)
                DMA("sp", wst_[:, :, :], wbrA_d.rearrange("(k p) n -> p k n", p=128), [], wst_.all())
                CP("pool", wbra[:, :, :], wst_[:, :, :], wst_.all(), wbra.all())
                DMA("sp", wst_[:, :, :], wbrB_d.rearrange("(k p) n -> p k n", p=128), [], wst_.all())
                CP("pool", wbrb[:, :, :], wst_[:, :, :], wst_.all(), wbrb.all())
                wov = wout_d.rearrange("(k p) n -> p k n", p=128)
                for hh in range(2):
                    DMA("sp", wst_[:, :, :], wov[:, hh * 4:(hh + 1) * 4, :], [], wst_.all())
                    CP("pool", wout[:, hh * 4:(hh + 1) * 4, :], wst_[:, :, :], wst_.all(), [wout.k(hh)])
                self.pop()
                gst = [self.sb(f"m3_gst{i}", [128, 8, 128], F32) for i in range(2)]
                gwb = [self.sb(f"m3_gwb{i}", [128, 8, 128], BF16) for i in range(4)]
                sga = [self.sb(f"m3_sga{i}", [128, 512], F32) for i in range(2)]
                sgb = [self.sb(f"m3_sgb{i}", [128, 512], F32) for i in range(2)]
                nl = 0
                for fc in range(8):
                    gws = []
                    for which in range(2):
                        st_, wb_ = gst[nl % 2], gwb[nl % 4]
                        nl += 1
                        DMA("sp", st_[:, :, :], wmG_d[which * 8 + fc], [], st_.all())
                        CP("pool", wb_[:, :, :], st_[:, :, :], st_.all(), wb_.all())
                        gws.append(wb_)
                    fs = slice(fc * 128, (fc + 1) * 128)
                    for tc in range(4):
                        ts_ = slice(tc * 512, (tc + 1) * 512)
                        bya, byb, bga, bgb = banks[0], banks[1], banks[2], banks[3]
                        if tc % 2 == 1:
                            bya, byb, bga, bgb = banks[4], banks[5], banks[6], banks[7]
                        for k in range(4):
                            PE(bya[:, :], wbra[:, k, fs], oaT[:, k, ts_], k == 0, k == 3, wbra.all() + [oaT.k(tc)], bya.all())
                        for k in range(4):
                            PE(byb[:, :], wbrb[:, k, fs], obT[:, k, ts_], k == 0, k == 3, wbrb.all() + [obT.k(tc)], byb.all())
                        for k in range(8):
                            PE(bga[:, :], gws[0][:, k, :], uT[:, k, ts_], k == 0, k == 7, gws[0].all() + [uT.k(tc)], bga.all())
                        for k in range(8):
                            PE(bgb[:, :], gws[1][:, k, :], uT[:, k, ts_], k == 0, k == 7, gws[1].all() + [uT.k(tc)], bgb.all())
                        SA, SB = sga[tc % 2], sgb[tc % 2]
                        ACT(SA[:, :], bga[:, :], AF.Sigmoid, bga.all(), SA.all())
                        ACT(SB[:, :], bgb[:, :], AF.Sigmoid, bgb.all(), SB.all())
                        TT("dve", SA[:, :], SA[:, :], bya[:, :], ALU.mult, SA.all() + bya.all(), SA.all())
                        TT("dve", SB[:, :], SB[:, :], byb[:, :], ALU.mult, SB.all() + byb.all(), SB.all())
                        TT("pool", mergedT[:, fc, ts_], SA[:, :], SB[:, :], ALU.add, SA.all() + SB.all(), [mergedT.k(tc)])
                self.pop()
            if "mergedT" in dbg and b == 0:
                self.push()
                stg = [self.sb(f"dbg_stg{i}", [128, SEQ], F32) for i in range(2)]
                nd = 0
                for nm, srcb, nk in (("mergedT", mergedT, 8), ("oaT", oaT, 4), ("obT", obT, 4)):
                    for k in range(nk):
                        sg_ = stg[nd % 2]
                        nd += 1
                        CP("dve", sg_[:, :], srcb[:, k, :], srcb.all(), sg_.all())
                        DMA("sp", dbg[nm][:, k, :], sg_[:, :], sg_.all(), [("dbg", nm, k)])
                self.pop()
            s2_bc = {b: s2t}
            h2_bc = {b: h2t}
            xt = [self.sb(f"xt{i}", [128, D], F32) for i in range(NB)]
            x1t = [self.sb(f"x1t{i}", [128, D], F32) for i in range(NB)]
            u2r = [self.sb(f"u2r{i}", [128, D], BF16) for i in range(NB)]
            u2T = [self.sb(f"u2T{i}", [128, 8, 128], BF16) for i in range(NB)]
            lnb = [(self.sb(f"ln_st{i}", [128, 2, 6], F32), self.sb(f"ln_mv{i}", [128, 2], F32),
                    self.sb(f"ln_rs{i}", [128, 1], F32)) for i in range(NB)]
            sc = [self.sb(f"r_sc{i}", [128, NE], F32) for i in range(NB)]
            ch = [self.sb(f"r_ch{i}", [128, NE], F32) for i in range(NB)]
            tmp = [self.sb(f"r_tmp{i}", [128, NE], F32) for i in range(NB)]
            selb = [self.sb(f"r_selb{i}", [128, NE], BF16) for i in range(NB)]
            Gm = [self.sb(f"r_G{i}", [128, NE], F32) for i in range(NB)]
            Dm = [self.sb(f"r_D{i}", [128, NE], F32) for i in range(NB)]
            sm = [self.sb(f"r_sm{i}", [128, 64], F32) for i in range(NB)]
            mixs = [self.sb(f"mixs{i}", [128, D], F32) for i in range(NB)]

            def phase1_tile(t):
                i = t % NB
                tt = t % TPS
                X, X1, U2, U2T, LB = xt[i], x1t[i], u2r[i], u2T[i], lnb[i]
                SC, CH, TMP, SELB, G, DD, SM = sc[i], ch[i], tmp[i], selb[i], Gm[i], Dm[i], sm[i]
                MX = mixs[i]
                rows = slice(t * 128, (t + 1) * 128)
                DMA("sp", X[:, :], x_d[rows, :], [], X.all())
                if do_mixer:
                    for hf in range(2):
                        bk = banks[hf]
                        with S.atomic():
                            for k in range(8):
                                PE(bk[:, :], mergedT[:, k, tt * 128:(tt + 1) * 128], wout[:, k, hf * 512:(hf + 1) * 512], k == 0, k == 7,
                                   [mergedT.k(tt // 4)] + wout.all(), bk.all())
                            TT("dve", MX[:, hf * 512:(hf + 1) * 512], bk[:, :], g1t[:, hf * 512:(hf + 1) * 512], ALU.mult, bk.all() + g1t.all(), MX.all())
                    if "mix" in dbg and b == 0:
                        DMA("sp", dbg["mix"][tt * 128:(tt + 1) * 128, :], MX[:, :], MX.all(), [("dbg", "mix", tt)])
                    STT(X[:, :], X[:, :], float(DN_ALPHA), MX[:, :], ALU.mult, ALU.add, X.all() + MX.all(), X.all())
                else:
                    ACT(X[:, :], X[:, :], AF.Copy, X.all(), X.all(), scale=float(DN_ALPHA))
                layernorm(X, X1, ln1g, ln1b, LB)
                DMA("sp", x1_d[rows, :], X1[:, :], X1.all(), [("x1_d", t)])
                op("dve", lambda e, X=X, X1=X1, b=b: e.tensor_tensor(X[:, :], X1[:, :], s2_bc[b][:, :], ALU.mult),
                   r=X1.all() + s2_bc[b].all(), w=X.all())
                op("dve", lambda e, X=X, U2=U2, b=b: e.tensor_tensor(U2[:, :], X[:, :], h2_bc[b][:, :], ALU.add),
                   r=X.all() + h2_bc[b].all(), w=U2.all())
                bkT = banks[4 + (t % 2)]
                pT = bkT.t[:, :].bitcast(BF16)
                with S.atomic():
                    for k in range(8):
                        op("pe", lambda e, k=k, U2=U2, pT=pT: e.transpose(pT[:, k * 128:(k + 1) * 128], U2[:, k * 128:(k + 1) * 128], ident_b[:, :]),
                           r=U2.all() + ident_b.all(), w=bkT.all())
                    op("act", lambda e, U2T=U2T, pT=pT: e.activation(U2T[:, :, :], pT.rearrange("p (k t) -> p k t", k=8), AF.Copy),
                       r=bkT.all(), w=U2T.all())
                bkR = banks[6 + (t % 2)]
                with S.atomic():
                    for k in range(8):
                        op("pe", lambda e, k=k, U2T=U2T, bkR=bkR: e.matmul(bkR[:, 0:NE], U2T[:, k, :], wr_b[:, k, :], start=(k == 0), stop=(k == 7)),
                           r=U2T.all() + wr_b.all(), w=bkR.all())
                    op("act", lambda e, SC=SC, bkR=bkR: e.activation(SC[:, :], bkR[:, 0:NE], AF.Sigmoid), r=bkR.all(), w=SC.all())
                op("dve", lambda e, SC=SC, CH=CH: e.tensor_tensor(CH[:, :], SC[:, :], rb_bc[:, :], ALU.add), r=SC.all() + rb_bc.all(), w=CH.all())
                ch3 = CH[:, :].rearrange("p (g j) -> p g j", g=8)
                tmp3 = TMP[:, :].rearrange("p (g j) -> p g j", g=8)
                m1, m2, gs, top4, keep, pen, top8, wsum, rw = (SM[:, 0:8], SM[:, 8:16], SM[:, 16:24], SM[:, 24:32], SM[:, 32:40],
                                                               SM[:, 40:48], SM[:, 48:56], SM[:, 56:57], SM[:, 57:58])
                op("dve", lambda e: e.tensor_reduce(m1, ch3, AX.X, ALU.max), r=CH.all(), w=SM.all())
                op("dve", lambda e: e.tensor_tensor(tmp3, ch3, m1.unsqueeze(2).to_broadcast([128, 8, 32]), ALU.is_equal),
                   r=CH.all() + SM.all(), w=TMP.all())
                op("dve", lambda e, TMP=TMP, CH=CH: e.scalar_tensor_tensor(TMP[:, :], TMP[:, :], -1e9, CH[:, :], ALU.mult, ALU.add),
                   r=TMP.all() + CH.all(), w=TMP.all())
                op("dve", lambda e: e.tensor_reduce(m2, tmp3, AX.X, ALU.max), r=TMP.all(), w=SM.all())
                op("dve", lambda e: e.tensor_tensor(gs, m1, m2, ALU.add), r=SM.all(), w=SM.all())
                op("dve", lambda e: e.max(top4, gs), r=SM.all(), w=SM.all())
                op("dve", lambda e: e.tensor_scalar(keep, gs, top4[:, 3:4], None, ALU.is_ge), r=SM.all(), w=SM.all())
                op("dve", lambda e: e.tensor_scalar(pen, keep, -1.0, 1e9, ALU.add, ALU.mult), r=SM.all(), w=SM.all())
                op("dve", lambda e: e.tensor_tensor(ch3, ch3, pen.unsqueeze(2).to_broadcast([128, 8, 32]), ALU.add),
                   r=CH.all() + SM.all(), w=CH.all())
                op("dve", lambda e, CH=CH: e.max(top8, CH[:, :]), r=CH.all(), w=SM.all())
                op("dve", lambda e, TMP=TMP, CH=CH: e.tensor_scalar(TMP[:, :], CH[:, :], top8[:, 7:8], None, ALU.is_ge),
                   r=CH.all() + SM.all(), w=TMP.all())
                op("pool", lambda e, TMP=TMP, SELB=SELB: e.tensor_copy(SELB[:, :], TMP[:, :]), r=TMP.all(), w=SELB.all())
                op("dve", lambda e, G=G, TMP=TMP, SC=SC: e.scalar_tensor_tensor(G[:, :], TMP[:, :], 1.0, SC[:, :], ALU.mult, ALU.mult, accum_out=wsum),
                   r=TMP.all() + SC.all(), w=G.all() + SM.all())
                op("dve", lambda e: e.reciprocal(rw, wsum), r=SM.all(), w=SM.all())
                op("dve", lambda e, G=G: e.tensor_scalar(G[:, :], G[:, :], rw, float(ROUTED_SCALE), ALU.mult, ALU.mult),
                   r=G.all() + SM.all(), w=G.all())
                S.cut()
                bkP = banks[2 + (t % 2)]
                op("pe", lambda e, bkP=bkP, SELB=SELB: e.matmul(bkP[:, 0:NE], tri_b[:, :], SELB[:, :], start=True, stop=True),
                   r=tri_b.all() + SELB.all(), w=bkP.all())
                op("pe", lambda e, bkP=bkP, SELB=SELB: e.matmul(bkP[:, NE:2 * NE], ones_b[:, :], SELB[:, :], start=False, stop=True),
                   r=ones_b.all() + SELB.all(), w=bkP.all())
                op("dve", lambda e, DD=DD, bkP=bkP: e.tensor_tensor(DD[:, :], bkP[:, 0:NE], carry[:, :], ALU.add),
                   r=bkP.all() + carry.all(), w=DD.all())
                op("dve", lambda e, bkP=bkP: e.tensor_tensor(carry[:, :], carry[:, :], bkP[:, NE:2 * NE], ALU.add),
                   r=bkP.all() + carry.all(), w=carry.all())
                S.cut()
                op("dve", lambda e, DD=DD: e.tensor_tensor(DD[:, :], DD[:, :], iotaC[:, :], ALU.add), r=DD.all() + iotaC.all(), w=DD.all())
                for k in range(8):
                    op("dve", lambda e, k=k, TMP=TMP, CH=CH, DD=DD, t=t: e.scalar_tensor_tensor(
                        TMP[:, :], CH[:, :], top8[:, k:k + 1], DD[:, :], ALU.is_equal, ALU.mult, accum_out=dest_f[:, t, k:k + 1]),
                       r=CH.all() + SM.all() + DD.all(), w=TMP.all() + [dest_f.k(t)])
                    op("dve", lambda e, k=k, TMP=TMP, CH=CH, G=G, t=t: e.scalar_tensor_tensor(
                        TMP[:, :], CH[:, :], top8[:, k:k + 1], G[:, :], ALU.is_equal, ALU.mult, accum_out=wk[:, t, k:k + 1]),
                       r=CH.all() + SM.all() + G.all(), w=TMP.all() + [wk.k(t)])
                DMA("sp", u2_d[rows, :], U2[:, :], U2.all(), [("u2_d", t)])

            for t0 in range(b * TPS, min((b + 1) * TPS, n_tiles), NB):
                grp = [t for t in range(t0, min(t0 + NB, n_tiles, (b + 1) * TPS))]
                parts = [S.record_parts(lambda t=t: phase1_tile(t)) for t in grp]
                S.merge_rr([p[0] for p in parts])
                S.merge_seq([p[1] for p in parts])
                S.merge_rr([p[2] for p in parts])
            self.pop()

        for b in range(BPC):
            if b * TPS >= n_tiles:
                break
            self.push()
            oaT = self.sb("oaT", [128, 4, SEQ], BF16, nreg=4)
            obT = self.sb("obT", [128, 4, SEQ], BF16, nreg=4)
            if do_mixer:
                self.push()
                CS64 = self.sb("CS64", [128, 2, SEQ], BF16, nreg=2)
                rope_tables(b, CS64, 0, 1, 128)
                dsa(b, oaT, CS64)
                nsa(b, obT, CS64)
                self.pop()
            m3(b, oaT, obT)
            self.pop()

        if "dest" in dbg:
            DMA("sp", dbg["dest"], dest_f[:, :, :], dest_f.all(), [("dbg", 0)])
        if "wk" in dbg:
            DMA("sp", dbg["wk"], wk[:, :, :], wk.all(), [("dbg", 1)])

        bg_issue(len(_bg))
        self.push()
        nb_f = self.sb("nb_f", [128, NE], F32)
        nb_t = self.sb("nb_t", [128, NE], F32)
        nb_i = self.sb("nb_i", [128, NE], I32)
        nb_b = self.sb("nb_b", [128, NE], BF16)
        nbT = self.sb("nbT", [128, 2, 128], BF16)
        startr = self.sb("startr", [128, NE], F32)
        endb = self.sb("endb", [128, NE], F32)
        blkE = self.sb("blkE", [128, 4], F32)
        Rm = self.sb("Rm", [128, NBLK // 128, 128], BF16)
        widx_f = self.sb("widx_f", [128, NBLK], F32)
        widx_i = self.sb("widx_i", [128, NBLK], I32)
        TS("dve", nb_f[:, :], carry[:, :], float(BLK - 1), 1.0 / BLK, ALU.add, ALU.mult, carry.all(), nb_f.all())
        CP("dve", nb_i[:, :], nb_f[:, :], nb_f.all(), nb_i.all())
        CP("dve", nb_t[:, :], nb_i[:, :], nb_i.all(), nb_t.all())
        TT("dve", nb_f[:, :], nb_t[:, :], nb_f[:, :], ALU.is_gt, nb_t.all() + nb_f.all(), nb_f.all())
        TT("dve", nb_f[:, :], nb_t[:, :], nb_f[:, :], ALU.subtract, nb_t.all() + nb_f.all(), nb_f.all())
        CP("dve", nb_b[:, :], nb_f[:, :], nb_f.all(), nb_b.all())
        bk = banks[0]
        pT = bfv(bk)
        for a_ in range(2):
            TR(pT[:, a_ * 128:(a_ + 1) * 128], nb_b[:, a_ * 128:(a_ + 1) * 128], ident_b[:, :], nb_b.all() + ident_b.all(), bk.all())
        CP("act", nbT[:, :, :], pT[:, 0:256].rearrange("p (a t) -> p a t", a=2), bk.all(), nbT.all())
        bk = banks[1]
        for a_ in range(2):
            PE(bk[:, 0:NE], nbT[:, a_, :], Utri[:, a_, :], a_ == 0, a_ == 1, nbT.all() + Utri.all(), bk.all())
        TS("dve", startr[:, :], bk[:, 0:NE], float(BLK), None, ALU.mult, ALU.bypass, bk.all(), startr.all())
        TT("dve", endb[:, :], bk[:, 0:NE], nb_f[:, :], ALU.add, bk.all() + nb_f.all(), endb.all())
        for a_ in range(NBLK // 128):
            TS("dve", nb_t[:, :], endb[:, :], jcol[:, a_:a_ + 1], None, ALU.is_le, ALU.add, endb.all() + jcol.all(), nb_t.all() + blkE.all(),
               accum=blkE[:, a_:a_ + 1])
        for a_ in range(NBLK // 128):
            TS("dve", Rm[:, a_, :], ident_b[:, :], blkE[:, a_:a_ + 1], None, ALU.mult, ALU.bypass, ident_b.all() + blkE.all(), Rm.all())
        bk = banks[2]
        PE(bk[:, 0:NBLK], ones_b[:, :], Rm[:, :, :].rearrange("p a t -> p (a t)"), True, True, ones_b.all() + Rm.all(), bk.all())
        TS("dve", widx_f[:, :], bk[:, 0:NBLK], 128.0, pcol[:, 0:1], ALU.mult, ALU.add, bk.all() + pcol.all(), widx_f.all())
        CP("dve", widx_i[:, :], widx_f[:, :], widx_f.all(), widx_i.all())
        if "blkE" in dbg:
            DMA("sp", dbg["blkE"], blkE[:, :], blkE.all(), [("dbg", "blkE")])
            DMA("sp", dbg["startr"], startr[:, :], startr.all(), [("dbg", "startr")])

        self.push()
        U2s = [self.sb(f"p2_u{i}", [128, D], BF16) for i in range(4)]
        p2 = [self.sb(f"p2_s{i}", [128, 40], F32) for i in range(4)]
        p2i = [self.sb(f"p2_i{i}", [128, 8], I32) for i in range(4)]
        p2j = [self.sb(f"p2_j{i}", [128, NE], F32) for i in range(4)]
        def pass2_tile(t):
            U2 = U2s[t % 4]
            P2, P2I, P2J = p2[t % 4], p2i[t % 4], p2j[t % 4]
            ef, et, ps_, sl = P2[:, 0:8], P2[:, 8:16], P2[:, 16:24], P2[:, 24:32]
            rows = slice(t * 128, (t + 1) * 128)
            DMA("sp", U2[:, :], u2_d[rows, :], [("u2_d", t)], U2.all())
            TS("dve", ef, dest_f[:, t, :], 1.0 / 4096.0, None, ALU.mult, ALU.bypass, [dest_f.k(t)], P2.all())
            CP("dve", P2I[:, :], ef, P2.all(), P2I.all())
            CP("dve", et, P2I[:, :], P2I.all(), P2.all())
            TT("dve", ps_, et, ef, ALU.is_gt, P2.all(), P2.all())
            TT("dve", et, et, ps_, ALU.subtract, P2.all(), P2.all())
            STT(ps_, et, -4096.0, dest_f[:, t, :], ALU.mult, ALU.add, P2.all() + [dest_f.k(t)], P2.all())
            for k in range(8):
                STT(P2J[:, :], iotaE1[:, :], et[:, k:k + 1], startr[:, :], ALU.is_equal, ALU.mult, iotaE1.all() + P2.all() + startr.all(),
                    P2J.all() + P2.all(), accum=sl[:, k:k + 1])
            TT("dve", sl, sl, ps_, ALU.add, P2.all(), P2.all())
            CP("dve", dest_i[:, t, :], sl, P2.all(), [dest_i.k(t)])
            for k in range(8):
                op("pool", lambda e, k=k, t=t, U2=U2: e.indirect_dma_start(
                    out=xs_d[:, :], out_offset=IOA(ap=dest_i[:, t, k:k + 1], axis=0), in_=U2[:, :], in_offset=None,
                    bounds_check=bc_reg(e), oob_is_err=False),
                   r=U2.all() + [dest_i.k(t)], w=[("xs_d", "all")], dma=True)
        for t0 in range(0, n_tiles, 4):
            grp = list(range(t0, min(t0 + 4, n_tiles)))
            parts = [S.record_parts(lambda t=t: pass2_tile(t)) for t in grp]
            S.merge_rr([p[0] for p in parts])
        self.pop()
        if "desti" in dbg:
            self.push()
            dtmp = self.sb("dtmp", [128, NT, 8], F32)
            CP("dve", dtmp[:, :, :], dest_i[:, :, :], dest_i.all(), dtmp.all())
            DMA("sp", dbg["desti"], dtmp[:, :, :], dtmp.all(), [("dbg", "desti")])
            self.pop()

        n_blk = cfg.get("n_blk", NBLK)

        wgv = wg_d.rearrange("e (p k) n -> (e p) (k n)", k=8)
        wuv = wu_d.rearrange("e (p k) n -> (e p) (k n)", k=8)
        wdv = wd_d.rearrange("e (p h) n -> (e p) (h n)", h=2)

        def wreg(e):
            if "wb" not in _regs:
                _regs["wb"] = e.to_reg(NE * 128 - 1)
            return _regs["wb"]

        NWF = 6
        w_bf4 = [self.sb(f"w_bf{i}", [128, 6144], BF16, nreg=3) for i in range(NWF)]
        wbv = [wbf_ds[q_][:, :] for q_ in range(3)]
        hsel = [self.sb(f"hsel{i}", [128, D], BF16) for i in range(3)]
        hselT = [self.sb(f"hselT{i}", [128, 8, 128], BF16) for i in range(2)]
        sg = [self.sb(f"sg{i}", [128, 256], F32) for i in range(2)]
        actk = [self.sb(f"actk{i}", [128, 256], BF16) for i in range(2)]
        actT = [self.sb(f"actT{i}", [128, 2, 128], BF16) for i in range(2)]
        ysb = [self.sb(f"ysb{i}", [128, D], BF16, nreg=2) for i in range(3)]

        hselT3 = [self.sb(f"hselT3_{i}", [128, 8, 128], BF16) for i in range(3)]
        def stageA0(j):
            WB, HS = w_bf4[j % NWF], hsel[j % 3]
            for q in range(3):
                op("pool", lambda e, q=q: e.indirect_dma_start(
                    out=WB[:, q * 2048:(q + 1) * 2048], out_offset=None, in_=wbv[q], in_offset=IOA(ap=widx_i[:, j:j + 1], axis=0),
                    bounds_check=wreg(e), oob_is_err=False),
                   r=widx_i.all() + wbf_keys, w=[WB.k(q)], dma=True)
            DMA("sp", HS[:, :], xs_d[j * BLK:(j + 1) * BLK, :], [("xs_d", "all")], HS.all())

        def stageA(j):
            HS, HT = hsel[j % 3], hselT3[j % 3]
            bkT = banks[j % 2]
            pT_ = bfv(bkT)
            with S.atomic():
                for k in range(8):
                    TR(pT_[:, k * 128:(k + 1) * 128], HS[:, k:D:8], ident_b[:, :], HS.all() + ident_b.all(), bkT.all())
                CP("act", HT[:, :, :], pT_.rearrange("p (k t) -> p k t", k=8), bkT.all(), HT.all())

        def stageB1(j):
            i = j % 2
            WB, HT, SG, AK = w_bf4[j % NWF], hselT3[j % 3], sg[i], actk[i]
            bk_ = banks[2 + j % 2]
            with S.atomic():
                for k in range(8):
                    PE(bk_[:, :], HT[:, k, :], WB[:, 0:4096].rearrange("p (two k n) -> p two k n", two=2, k=8)[:, :, k, :], k == 0, k == 7,
                       HT.all() + [WB.k(0), WB.k(1)], bk_.all())
                ACT(SG[:, :], bk_[:, 0:256], AF.Silu, bk_.all(), SG.all())
                TT("dve", AK[:, :], SG[:, :], bk_[:, 256:512], ALU.mult, SG.all() + bk_.all(), AK.all())

        def stageB2(j):
            i = j % 2
            AK, AT = actk[i], actT[i]
            bk2 = banks[4 + j % 2]
            pT2 = bfv(bk2)
            with S.atomic():
                for h in range(2):
                    TR(pT2[:, h * 128:(h + 1) * 128], AK[:, h:256:2], ident_b[:, :], AK.all() + ident_b.all(), bk2.all())
                CP("act", AT[:, :, :], pT2[:, 0:256].rearrange("p (h t) -> p h t", h=2), bk2.all(), AT.all())

        def stageB3(j):
            i = j % 2
            WB, AT, YS = w_bf4[j % NWF], actT[i], ysb[j % 3]
            for hf in range(2):
                bkd = banks[6 + hf]
                with S.atomic():
                    for h in range(2):
                        col = 4096 + h * 1024 + hf * 512
                        PE(bkd[:, :], AT[:, h, :], WB[:, col:col + 512], h == 0, h == 1, AT.all() + [WB.k(2)], bkd.all())
                    CP("act" if hf == 0 else "dve", YS[:, hf * 512:(hf + 1) * 512], bkd[:, :], bkd.all(), [YS.k(hf)])
            DMA("sp", ys_d[j * BLK:(j + 1) * BLK, :], YS[:, :], YS.all(), [("ys_d", j)])

        stages = [(0, stageA0), (2, stageA), (3, stageB1), (4, stageB2), (5, stageB3)]
        for s_ in range(n_blk + 5):
            streams = []
            for off, st_fn in stages:
                j = s_ - off
                if 0 <= j < n_blk:
                    streams.append(S.record_parts(lambda j=j, st_fn=st_fn: st_fn(j))[0])
            S.merge_rr(list(reversed(streams)))
        ys_keys = [("ys_d", j) for j in range(n_blk)]
        self.pop()
        self.push()
        ln2g = self.sb("ln2g", [128, D], F32)
        ln2b = self.sb("ln2b", [128, D], F32)
        DMA("sp", ln2g[:, :], ln_d[2:3, :].to_broadcast([128, D]), [], ln2g.all())
        DMA("sp", ln2b[:, :], ln_d[3:4, :].to_broadcast([128, D]), [], ln2b.all())
        s2_bc, h2_bc, g2_bc = {}, {}, {}
        for b in range(BPC):
            g2_bc[b] = self.sb(f"f_g2{b}", [128, D], F32)
            load_mod(g2_bc[b], b, 5)
        GF = 4
        wsh = self.sb("wsh", [128, 6144], BF16, nreg=3)
        self.push()
        wsh_st = self.sb("wsh_st", [128, 6144], F32, nreg=3)
        DMA("sp", wsh_st[:, 0:2048].rearrange("p (k n) -> p k n", k=8), wsg_d.rearrange("(k p) n -> p k n", p=128), [], [wsh_st.k(0)])
        DMA("sp", wsh_st[:, 2048:4096].rearrange("p (k n) -> p k n", k=8), wsu_d.rearrange("(k p) n -> p k n", p=128), [], [wsh_st.k(1)])
        DMA("sp", wsh_st[:, 4096:6144].rearrange("p (k n) -> p k n", k=2), wsd_d.rearrange("(k p) n -> p k n", p=128), [], [wsh_st.k(2)])
        CP("act", wsh[:, 0:2048], wsh_st[:, 0:2048], [wsh_st.k(0)], [wsh.k(0)])
        CP("dve", wsh[:, 2048:4096], wsh_st[:, 2048:4096], [wsh_st.k(1)], [wsh.k(1)])
        CP("pool", wsh[:, 4096:6144], wsh_st[:, 4096:6144], [wsh_st.k(2)], [wsh.k(2)])
        self.pop()
        xt = [self.sb(f"fxt{i}", [128, D], F32) for i in range(GF)]
        x1t = [self.sb(f"fx1t{i}", [128, D], F32) for i in range(GF)]
        u2r = [self.sb(f"fu2r{i}", [128, D], BF16) for i in range(GF)]
        u2T = [self.sb(f"fu2T{i}", [128, 8, 128], BF16) for i in range(GF)]
        lnb = [(self.sb(f"fln_st{i}", [128, 2, 6], F32), self.sb(f"fln_mv{i}", [128, 2], F32),
                self.sb(f"fln_rs{i}", [128, 1], F32)) for i in range(GF)]
        yg = [self.sb(f"yg{i}", [128, 8, D], BF16, nreg=8) for i in range(GF)]
        acc = [self.sb(f"acc{i}", [128, D], F32) for i in range(GF)]
        sgs = [self.sb(f"sgs{i}", [128, 256], F32) for i in range(GF)]
        atk = [self.sb(f"atk{i}", [128, 256], BF16) for i in range(GF)]
        ats = [self.sb(f"ats{i}", [128, 2, 128], BF16) for i in range(GF)]

        def final_tile(t):
            b = t // TPS
            i = t % GF
            X, X1, U2, U2T, LB = xt[i], x1t[i], u2r[i], u2T[i], lnb[i]
            YG, ACC, SGS, ATK, ATS = yg[i], acc[i], sgs[i], atk[i], ats[i]
            rows = slice(t * 128, (t + 1) * 128)
            DMA("sp", X1[:, :], x1_d[rows, :], [("x1_d", t)], X1.all())
            DMA("sp", U2[:, :], u2_d[rows, :], [("u2_d", t)], U2.all())
            for k in range(8):
                op("pool", lambda e, k=k: e.indirect_dma_start(
                    out=YG[:, k, :], out_offset=None, in_=ys_d[:, :], in_offset=IOA(ap=dest_i[:, t, k:k + 1], axis=0),
                    bounds_check=bc_reg(e), oob_is_err=False),
                   r=ys_keys + [dest_i.k(t)], w=[YG.k(k)], dma=True)
            bkT = banks[t % 2]
            pT = bfv(bkT)
            with S.atomic():
                for k in range(8):
                    TR(pT[:, k * 128:(k + 1) * 128], U2[:, k * 128:(k + 1) * 128], ident_b[:, :], U2.all() + ident_b.all(), bkT.all())
                CP("act", U2T[:, :, :], pT.rearrange("p (k t) -> p k t", k=8), bkT.all(), U2T.all())
            bk = banks[2 + (t % 2)]
            with S.atomic():
                for k in range(8):
                    PE(bk[:, 0:256], U2T[:, k, :], wsh[:, k * 256:(k + 1) * 256], k == 0, False, U2T.all() + [wsh.k(0)], bk.all(), skip_group_check=True)
                for k in range(8):
                    PE(bk[:, 256:512], U2T[:, k, :], wsh[:, 2048 + k * 256:2048 + (k + 1) * 256], False, k == 7, U2T.all() + [wsh.k(1)], bk.all(), skip_group_check=True)
                ACT(SGS[:, :], bk[:, 0:256], AF.Silu, bk.all(), SGS.all())
                TT("dve", ATK[:, :], SGS[:, :], bk[:, 256:512], ALU.mult, SGS.all() + bk.all(), ATK.all())
            bk2 = banks[4 + (t % 2)]
            pT2 = bfv(bk2)
            with S.atomic():
                for h in range(2):
                    TR(pT2[:, h * 128:(h + 1) * 128], ATK[:, h * 128:(h + 1) * 128], ident_b[:, :], ATK.all() + ident_b.all(), bk2.all())
                CP("act", ATS[:, :, :], pT2[:, 0:256].rearrange("p (h t) -> p h t", h=2), bk2.all(), ATS.all())
            for hf in range(2):
                bkd = banks[6 + hf]
                with S.atomic():
                    for h in range(2):
                        col = 4096 + h * 1024 + hf * 512
                        PE(bkd[:, :], ATS[:, h, :], wsh[:, col:col + 512], h == 0, h == 1, ATS.all() + [wsh.k(2)], bkd.all())
                    CP("act", ACC[:, hf * 512:(hf + 1) * 512], bkd[:, :], bkd.all(), ACC.all())
            for k in range(8):
                STT(ACC[:, :], YG[:, k, :], wk[:, t, k:k + 1], ACC[:, :], ALU.mult, ALU.add, [YG.k(k), wk.k(t)] + ACC.all(), ACC.all())
            TT("pool", ACC[:, :], ACC[:, :], g2_bc[b][:, :], ALU.mult, ACC.all() + g2_bc[b].all(), ACC.all())
            STT(ACC[:, :], X1[:, :], float(DN_ALPHA), ACC[:, :], ALU.mult, ALU.add, ACC.all() + X1.all(), ACC.all())
            layernorm(ACC, X, ln2g, ln2b, LB)
            DMA("sp", out_d[rows, :], X[:, :], X.all(), [("out_d", t)])

        for t0 in range(0, n_tiles, GF):
            grp = list(range(t0, min(t0 + GF, n_tiles)))
            parts = [S.record_parts(lambda t=t: final_tile(t)) for t in grp]
            S.merge_rr([p[0] for p in parts])
        S.finalize_and_emit()
        return nc


_OFF = dict(q_a=0, k_a=512, v_a=640, q_idx=768, k_idx=1024, w_idx=1056, q_b=1064, k_cmp=1576, v_cmp=1704,
            k_sel=1832, v_sel=1960, k_win=2088, v_win=2216, g_nsa=2344, gate_a=2368, gate_b=3392)


def _sw64(d):
    return d + 8 if d < 8 else (d - 8 if d < 16 else d)


def _sw32(d):
    return d + 4 if d < 4 else (d - 4 if d < 8 else d)


def _chunks_A():
    O = _OFF
    ch = []
    for c in range(4):
        ch.append([O["q_a"] + c * 128 + p for p in range(128)])
    for c in range(4):
        ch.append([O["q_a"] + c * 128 + (p // 64) * 64 + _sw64(p % 64) for p in range(128)])
    for g in range(2):
        ch.append([O["k_a"] + g * 64 + (p % 64) for p in range(128)])
    for g in range(2):
        ch.append([O["k_a"] + g * 64 + _sw64(p % 64) for p in range(128)])
    for sw in (False, True):
        for c in range(3):
            cols = []
            for p in range(128):
                h = min(7, 3 * c + min(p, 95) // 32)
                d = p % 32
                cols.append(O["q_idx"] + h * 32 + (_sw32(d) if sw else d))
            ch.append(cols)
    ch.append([O["k_idx"] + (p % 32) for p in range(128)])
    ch.append([O["k_idx"] + _sw32(p % 32) for p in range(128)])
    return ch


def _chunks_B():
    O = _OFF
    ch = []
    for c in range(4):
        ch.append([O["q_b"] + c * 128 + p for p in range(128)])
    for c in range(4):
        ch.append([O["q_b"] + c * 128 + (p // 64) * 64 + _sw64(p % 64) for p in range(128)])
    ch.append([O["k_cmp"] + p for p in range(128)])
    ch.append([O["v_cmp"] + p for p in range(128)])
    for nm in ("k_sel", "k_win"):
        for g in range(2):
            ch.append([O[nm] + g * 64 + (p % 64) for p in range(128)])
        for g in range(2):
            ch.append([O[nm] + g * 64 + _sw64(p % 64) for p in range(128)])
    return ch


N_CH_A = 20
N_CH_B = 18


def _const_tables():
    theta = 500000.0
    cst = np.zeros((128, 8), np.float32)
    inv64 = (np.float32(theta) ** (-(np.arange(8, dtype=np.float32) * np.float32(2.0)) / np.float32(16))).astype(np.float32)
    inv32 = (np.float32(theta) ** (-(np.arange(4, dtype=np.float32) * np.float32(2.0)) / np.float32(8))).astype(np.float32)
    for p in range(128):
        d = p % 64
        if d < 16:
            cst[p, 0] = inv64[d % 8]
            cst[p, 1] = -1.0 if d < 8 else 1.0
        d2 = p % 32
        if p < 96 and d2 < 8:
            cst[p, 2] = inv32[d2 % 4]
            cst[p, 3] = -1.0 if d2 < 4 else 1.0
    selAB = np.zeros((128, 2, 16, 32), np.float32)
    for i in range(16):
        for tl in range(128):
            t = i * 128 + tl
            cur = t // 64
            for j in range(32):
                forced = (j == 0) or (0 <= cur - j < 2)
                causal = j * 64 <= t
                if forced:
                    selAB[tl, 0, i, j] = 0.0
                    selAB[tl, 1, i, j] = 1e9
                elif causal:
                    selAB[tl, 0, i, j] = 1.0
                else:
                    selAB[tl, 1, i, j] = -1e30
    expall = np.zeros((32, 16, 128), np.float32)
    for jj in range(16):
        for s in range(128):
            expall[2 * jj + s // 64, jj, s] = 1.0
    ov = np.zeros((128, 32), np.float32)
    for c in range(127):
        cs = 16 * c
        for j in range(32):
            ss = 64 * j
            if cs <= ss + 63 and cs + 31 >= ss:
                ov[c, j] = 1.0
    return cst, selAB, expall, ov


def _prep_inputs(inputs):
    f32 = lambda a: np.ascontiguousarray(np.asarray(a, dtype=np.float32))
    x = f32(inputs["x"])
    c = f32(inputs["c"])
    pos = np.ascontiguousarray(np.asarray(inputs["positions"], dtype=np.int32))
    w_in = f32(inputs["w_in"][0])
    w3 = w_in.reshape(8, 128, w_in.shape[1])

    def gather(chunks):
        return np.ascontiguousarray(np.stack([w3[:, :, cols].transpose(1, 0, 2) for cols in chunks]))

    O = _OFF
    tokA = list(range(O["v_a"], O["v_a"] + 128)) + list(range(O["w_idx"], O["w_idx"] + 8))
    tokB = list(range(O["v_sel"], O["v_sel"] + 128)) + list(range(O["v_win"], O["v_win"] + 128)) + list(range(O["g_nsa"], O["g_nsa"] + 24))
    gch = [[O["gate_a"] + cc * 128 + p for p in range(128)] for cc in range(8)] + \
          [[O["gate_b"] + cc * 128 + p for p in range(128)] for cc in range(8)]
    cst, selAB, expall, ov = _const_tables()
    cpos = np.stack([f32(inputs["cmp_pos_k"][0]).T, f32(inputs["cmp_pos_v"][0]).T], axis=1)
    cposT = np.ascontiguousarray(np.concatenate([cpos, cpos], axis=0))
    shared = {
        "w_ada": f32(inputs["w_ada"][0]),
        "b_ada": f32(inputs["b_ada"][0]).reshape(1, -1),
        "ln": f32(np.stack([inputs["ln1_g"][0], inputs["ln1_b"][0], inputs["ln2_g"][0], inputs["ln2_b"][0]])),
        "w_router": f32(inputs["w_router"][0]),
        "router_bias": f32(inputs["router_bias"][0]).reshape(1, -1),
        "w_exp_gate": f32(inputs["w_exp_gate"][0]),
        "w_exp_up": f32(inputs["w_exp_up"][0]),
        "w_exp_down": f32(inputs["w_exp_down"][0]),
        "w_sh_gate": f32(inputs["w_sh_gate"][0]),
        "w_sh_up": f32(inputs["w_sh_up"][0]),
        "w_sh_down": f32(inputs["w_sh_down"][0]),
        "wmA": gather(_chunks_A()),
        "wmB": gather(_chunks_B()),
        "wmG": gather(gch),
        "wtA": np.ascontiguousarray(w3[:, :, tokA].transpose(1, 0, 2)),
        "wtB": np.ascontiguousarray(w3[:, :, tokB].transpose(1, 0, 2)),
        "w_br_a": f32(inputs["w_br_a"][0]),
        "w_br_b": f32(inputs["w_br_b"][0]),
        "w_out": f32(inputs["w_out"][0]),
        "cmp_k_w1": f32(inputs["cmp_k_w1"][0]),
        "cmp_v_w1": f32(inputs["cmp_v_w1"][0]),
        "cmp_k_w2": f32(inputs["cmp_k_w2"][0]),
        "cmp_v_w2": f32(inputs["cmp_v_w2"][0]),
        "cmp_posT": cposT,
        "cst": cst,
        "selAB": selAB,
        "expall": expall,
        "ovl": ov,
    }
    in_maps = []
    for i in range(N_CORES):
        m = dict(shared)
        m["x"] = np.ascontiguousarray(x[BPC * i:BPC * (i + 1)].reshape(TOK, D))
        cc = c[BPC * i:BPC * (i + 1)]
        m["cT"] = np.ascontiguousarray(cc.reshape(BPC, 8, 128).transpose(2, 1, 0))
        m["positions"] = np.ascontiguousarray(pos[BPC * i:BPC * (i + 1)])
        in_maps.append(m)
    return in_maps


def kernel(**inputs):
    in_maps = _prep_inputs(inputs)
    prog = Prog()
    nc = prog.build()
    res = run_bass_kernel_spmd(nc, in_maps, core_ids=list(range(N_CORES)))
    outs = [np.asarray(r["out"], dtype=np.float32).reshape(BPC, SEQ, D) for r in res.results]
    return np.concatenate(outs, axis=0)
```
